# Optimizing a Trainium2 kernel written in Bass

```python
import math
import jax, jax.numpy as jnp
from jax import lax
import numpy as np

D_MODEL = 1024
BATCH = 32
SEQ = 2048
DEPTH = 1

CTX_LEN = 256
GRID_W = 64
ATTN_HEADS = 4
ATTN_HEAD_DIM = 64
ATTN_V_DIM = 2 * ATTN_HEAD_DIM
ATTN_QK_W = ATTN_HEADS * 2 * ATTN_HEAD_DIM
ATTN_V_W = ATTN_HEADS * ATTN_V_DIM
CONV_W = 512
CONV_K = 3
MIX_W = ATTN_V_W + CONV_W
PROJ_W = 2 * ATTN_QK_W + ATTN_V_W + 3 * CONV_W
Q_BLOCK = 128
ROPE_BASE = 10000.0
ROT_AXIS = ATTN_HEAD_DIM // 2
N_EXPERTS = 16
CAP_FACTOR = 2
D_EXPERT = 1024
EPS = 1e-6

kernel_name = "hybrid_diffattn_shortconv_ecmoe_dit"


def rmsnorm(x, g):
    xf = x.astype(jnp.float32)
    y = xf * lax.rsqrt(jnp.mean(xf * xf, axis=-1, keepdims=True) + EPS)
    return (y * g.astype(jnp.float32)).astype(x.dtype)


def modulate(x, shift, scale):
    return x * (1 + scale) + shift


def axial_rope_tables(n_tok):
    rows = n_tok // GRID_W
    row = jnp.repeat(jnp.arange(rows, dtype=jnp.float32), GRID_W)
    col = jnp.tile(jnp.arange(GRID_W, dtype=jnp.float32), rows)
    inv = ROPE_BASE ** (-jnp.arange(0, ROT_AXIS, 2, dtype=jnp.float32) / ROT_AXIS)
    ang_r = row[:, None] * inv[None, :]
    ang_c = col[:, None] * inv[None, :]
    ang = jnp.concatenate([ang_r, ang_r, ang_c, ang_c], axis=-1)
    return jnp.cos(ang), jnp.sin(ang)


def apply_rope(x, cos, sin):
    q = ROT_AXIS // 2
    a, b, c2, d2 = x[..., :q], x[..., q:2 * q], x[..., 2 * q:3 * q], x[..., 3 * q:]
    rot = jnp.concatenate([-b, a, -d2, c2], axis=-1)
    cs = cos[None, :, None, None, :].astype(x.dtype)
    sn = sin[None, :, None, None, :].astype(x.dtype)
    return x * cs + rot * sn


def diff_attend(q, k, v, lam):
    s = jnp.einsum('bqhmd,bkhmd->bhmqk', q, k, preferred_element_type=jnp.float32)
    a = jax.nn.softmax(s * (1.0 / math.sqrt(ATTN_HEAD_DIM)), axis=-1)
    a = a[:, :, 0] - lam * a[:, :, 1]
    return jnp.einsum('bhqk,bkhe->bqhe', a.astype(v.dtype), v)


def latent_diff_attention(q, k_all, v_all, lam):
    b, t, h, _, d = q.shape
    nblk = t // Q_BLOCK
    qb = q.reshape(b, nblk, Q_BLOCK, h, 2, d).transpose(1, 0, 2, 3, 4, 5)
    o = lax.map(lambda qblk: diff_attend(qblk, k_all, v_all, lam), qb)
    return o.transpose(1, 0, 2, 3, 4).reshape(b, t, h, ATTN_V_DIM)


def split_proj(p):
    b, t, _ = p.shape
    i0 = ATTN_QK_W
    i1 = i0 + ATTN_QK_W
    i2 = i1 + ATTN_V_W
    i3 = i2 + CONV_W
    i4 = i3 + CONV_W
    q = p[..., :i0].reshape(b, t, ATTN_HEADS, 2, ATTN_HEAD_DIM)
    k = p[..., i0:i1].reshape(b, t, ATTN_HEADS, 2, ATTN_HEAD_DIM)
    v = p[..., i1:i2].reshape(b, t, ATTN_HEADS, ATTN_V_DIM)
    return q, k, v, p[..., i2:i3], p[..., i3:i4], p[..., i4:]


def short_conv(gb, gc, xc, conv_w):
    u = gc * xc
    y = lax.conv_general_dilated(u, conv_w[:, None, :].astype(u.dtype), window_strides=(1,),
                                 padding=((CONV_K // 2, CONV_K // 2),),
                                 dimension_numbers=('NWC', 'WIO', 'NWC'),
                                 feature_group_count=CONV_W)
    return gb * y


def head_out(o, subln_g, lam_init):
    b, t = o.shape[:2]
    return (rmsnorm(o, subln_g) * (1.0 - lam_init)).reshape(b, t, ATTN_V_W)


def hybrid_mixer(xn, xn_c, w_in, conv_w, lq1, lk1, lq2, lk2, subln_g, w_out, layer, need_ctx):
    lam_init = 0.8 - 0.6 * math.exp(-0.3 * layer)
    lam = (jnp.exp(jnp.sum(lq1.astype(jnp.float32) * lk1.astype(jnp.float32)))
           - jnp.exp(jnp.sum(lq2.astype(jnp.float32) * lk2.astype(jnp.float32))) + lam_init)
    t = xn.shape[1]
    cos, sin = axial_rope_tables(t)
    q, k, v, gb, gc, xc = split_proj(jnp.einsum('btd,dn->btn', xn, w_in))
    qc, kc, vc, gbc, gcc, xcc = split_proj(jnp.einsum('btd,dn->btn', xn_c, w_in))
    q = apply_rope(q, cos, sin)
    k = apply_rope(k, cos, sin)
    k_all = jnp.concatenate([kc, k], axis=1)
    v_all = jnp.concatenate([vc, v], axis=1)
    attn = head_out(latent_diff_attention(q, k_all, v_all, lam), subln_g, lam_init)
    conv = short_conv(gb, gc, xc, conv_w)
    out = jnp.einsum('btn,nd->btd', jnp.concatenate([attn, conv], axis=-1), w_out)
    out_c = None
    if need_ctx:
        attn_c = head_out(diff_attend(qc, kc, vc, lam), subln_g, lam_init)
        conv_c = short_conv(gbc, gcc, xcc, conv_w)
        out_c = jnp.einsum('btn,nd->btd', jnp.concatenate([attn_c, conv_c], axis=-1), w_out)
    return out, out_c


def ec_moe(x_tok, w_router, w_gate, w_up, w_down):
    t = x_tok.shape[1]
    cap = CAP_FACTOR * t // N_EXPERTS
    logits = jnp.einsum('btd,de->bte', x_tok, w_router, preferred_element_type=jnp.float32)
    aff = jax.nn.softmax(logits, axis=-1)
    g, idx = lax.top_k(jnp.swapaxes(aff, 1, 2), cap)
    xs = jax.vmap(lambda xt, ix: xt[ix])(x_tok, idx)
    hg = jnp.einsum('becd,edf->becf', xs, w_gate)
    hu = jnp.einsum('becd,edf->becf', xs, w_up)
    y = jnp.einsum('becf,efd->becd', jax.nn.silu(hg) * hu, w_down) * g[..., None].astype(xs.dtype)
    return jax.vmap(lambda ix, val: jnp.zeros((t, D_MODEL), val.dtype)
                    .at[ix.reshape(-1)].add(val.reshape(-1, D_MODEL)))(idx, y)


def setup_inputs(seed: int = 0) -> dict:
    key = jax.random.key(seed)
    ks = jax.random.split(key, 24)
    f32 = jnp.float32
    nrm = lambda k, shape, s: jax.random.normal(k, shape, f32) * s
    gain = lambda k: 1.0 + 0.05 * jax.random.normal(k, (DEPTH, D_MODEL), f32)
    return {
        "x": nrm(ks[0], (BATCH, SEQ, D_MODEL), 1.0),
        "c": nrm(ks[1], (BATCH, D_MODEL), 1.0),
        "ctx": nrm(ks[2], (BATCH, CTX_LEN, D_MODEL), 1.0),
        "c_ctx": nrm(ks[3], (D_MODEL,), 1.0),
        "w_ada": nrm(ks[4], (DEPTH, D_MODEL, 6 * D_MODEL), 0.5 * D_MODEL ** -0.5),
        "b_ada": nrm(ks[5], (DEPTH, 6 * D_MODEL), 0.01),
        "norm_pre_mix": gain(ks[6]),
        "norm_post_mix": gain(ks[7]),
        "norm_pre_ffn": gain(ks[8]),
        "norm_post_ffn": gain(ks[9]),
        "w_in": nrm(ks[10], (DEPTH, D_MODEL, PROJ_W), D_MODEL ** -0.5),
        "conv_w": nrm(ks[11], (DEPTH, CONV_K, CONV_W), CONV_K ** -0.5),
        "lambda_q1": nrm(ks[12], (DEPTH, ATTN_HEAD_DIM), 0.1),
        "lambda_k1": nrm(ks[13], (DEPTH, ATTN_HEAD_DIM), 0.1),
        "lambda_q2": nrm(ks[14], (DEPTH, ATTN_HEAD_DIM), 0.1),
        "lambda_k2": nrm(ks[15], (DEPTH, ATTN_HEAD_DIM), 0.1),
        "subln_g": 1.0 + 0.05 * jax.random.normal(ks[16], (DEPTH, ATTN_V_DIM), f32),
        "w_out": nrm(ks[17], (DEPTH, MIX_W, D_MODEL), MIX_W ** -0.5),
        "w_router": nrm(ks[18], (DEPTH, D_MODEL, N_EXPERTS), D_MODEL ** -0.5),
        "w_gate": nrm(ks[19], (DEPTH, N_EXPERTS, D_MODEL, D_EXPERT), D_MODEL ** -0.5),
        "w_up": nrm(ks[20], (DEPTH, N_EXPERTS, D_MODEL, D_EXPERT), D_MODEL ** -0.5),
        "w_down": nrm(ks[21], (DEPTH, N_EXPERTS, D_EXPERT, D_MODEL), D_EXPERT ** -0.5),
    }


def reference(x, c, ctx, c_ctx, w_ada, b_ada, norm_pre_mix, norm_post_mix, norm_pre_ffn,
              norm_post_ffn, w_in, conv_w, lambda_q1, lambda_k1, lambda_q2, lambda_k2,
              subln_g, w_out, w_router, w_gate, w_up, w_down):
    h = x
    hc = ctx
    for layer in range(DEPTH):
        need_ctx = layer < DEPTH - 1
        mod = jnp.einsum('bd,dn->bn', jax.nn.silu(c), w_ada[layer]) + b_ada[layer]
        mod_c = jnp.einsum('d,dn->n', jax.nn.silu(c_ctx), w_ada[layer]) + b_ada[layer]
        sh1, sc1, g1, sh2, sc2, g2 = [m[:, None, :] for m in jnp.split(mod, 6, axis=-1)]
        csh1, csc1, cg1, csh2, csc2, cg2 = jnp.split(mod_c, 6, axis=-1)
        xn = modulate(rmsnorm(h, norm_pre_mix[layer]), sh1, sc1)
        xn_c = modulate(rmsnorm(hc, norm_pre_mix[layer]), csh1, csc1)
        mix, mix_c = hybrid_mixer(xn, xn_c, w_in[layer], conv_w[layer], lambda_q1[layer],
                                  lambda_k1[layer], lambda_q2[layer], lambda_k2[layer],
                                  subln_g[layer], w_out[layer], layer, need_ctx)
        h = h + g1 * rmsnorm(mix, norm_post_mix[layer])
        if need_ctx:
            hc = hc + cg1 * rmsnorm(mix_c, norm_post_mix[layer])
        xn = modulate(rmsnorm(h, norm_pre_ffn[layer]), sh2, sc2)
        ff = ec_moe(xn, w_router[layer], w_gate[layer], w_up[layer], w_down[layer])
        h = h + g2 * rmsnorm(ff, norm_post_ffn[layer])
        if need_ctx:
            xn_c = modulate(rmsnorm(hc, norm_pre_ffn[layer]), csh2, csc2)
            ff_c = ec_moe(xn_c, w_router[layer], w_gate[layer], w_up[layer], w_down[layer])
            hc = hc + cg2 * rmsnorm(ff_c, norm_post_ffn[layer])
    return h
```

```python
import math
from contextlib import ExitStack
import numpy as np
import concourse.bass as bass
import concourse.mybir as mybir
from concourse.bass_utils import run_bass_kernel_spmd

F32 = mybir.dt.float32
BF16 = mybir.dt.bfloat16
U32 = mybir.dt.uint32
ACT = mybir.ActivationFunctionType
ALU = mybir.AluOpType
AX = mybir.AxisListType

T = 2048
C = 256
D = 1024
TK = T + C
NE = 16
CAP = 256
EPS = 1e-6
NCORES = 8


class Sched:
    ENGS = ("pe", "act", "dve", "pool", "sp")

    def __init__(self, nc, stack):
        self.nc = nc
        self.stack = stack
        self.sems = {}
        self.eng = {}
        for n in self.ENGS:
            self.sems["s_" + n] = stack.enter_context(nc.semaphore("s_" + n))
            self.eng[n] = dict(ops=[], cnt=0, waited={})
        self.lastw = {}
        self.readers = {}
        self.dcum = {}

    def _deps(self, reads, writes):
        d = []
        for r in reads:
            if r in self.lastw:
                d.append(self.lastw[r])
        for w in writes:
            if w in self.lastw:
                d.append(self.lastw[w])
            d.extend(self.readers.get(w, ()))
        return d

    def _waits(self, en, deps):
        E = self.eng[en]
        need = {}
        for (s, v) in deps:
            if en == "pe" and s == "s_pe":
                continue
            if E["waited"].get(s, 0) >= v:
                continue
            if need.get(s, 0) < v:
                need[s] = v
        for s, v in need.items():
            E["waited"][s] = v
        return list(need.items())

    def _record(self, ev, reads, writes):
        for r in reads:
            self.readers.setdefault(r, []).append(ev)
        for w in writes:
            self.lastw[w] = ev
            self.readers[w] = []

    def op(self, en, fn, reads=(), writes=()):
        excl = [r for r in reads if r.startswith("ps") and r not in writes]
        if excl:
            reads = [r for r in reads if r not in excl]
            writes = list(writes) + excl
        deps = self._deps(reads, writes)
        waits = self._waits(en, deps)
        E = self.eng[en]
        E["cnt"] += 1
        ev = ("s_" + en, E["cnt"])
        self._record(ev, reads, writes)
        sems = self.sems

        def run(e, fn=fn, waits=waits, sem=sems["s_" + en]):
            for (s, v) in waits:
                e.wait_ge(sems[s], v)
            fn(e).then_inc(sem, 1)

        E["ops"].append(run)

    def dma(self, q, mk, reads, writes, key):
        deps = self._deps(reads, writes)
        waits = self._waits(q, deps)
        if key not in self.sems:
            self.sems[key] = self.stack.enter_context(self.nc.semaphore(key))
            self.dcum[key] = 0
        self.dcum[key] += 16
        ev = (key, self.dcum[key])
        self._record(ev, reads, writes)
        sems = self.sems

        def run(e, mk=mk, waits=waits, sem=sems[key]):
            for (s, v) in waits:
                e.wait_ge(sems[s], v)
            mk(e).then_inc(sem, 16)

        self.eng[q]["ops"].append(run)

    def _all_events(self):
        ev = [("s_" + n, self.eng[n]["cnt"]) for n in self.ENGS if self.eng[n]["cnt"] > 0]
        ev += [(k, v) for k, v in self.dcum.items() if v > 0]
        return ev

    def barrier(self, engines=None):
        allev = self._all_events()
        for n in (engines or self.ENGS):
            waits = self._waits(n, [ev for ev in allev if ev[0] != "s_" + n])
            sems = self.sems

            def run(e, waits=waits):
                for (s, v) in waits:
                    e.wait_ge(sems[s], v)

            self.eng[n]["ops"].append(run)

    def emit(self):
        nc = self.nc
        with nc.Block() as block:
            @block.tensor
            def _(e):
                for f in self.eng["pe"]["ops"]:
                    f(e)

            @block.scalar
            def _(e):
                for f in self.eng["act"]["ops"]:
                    f(e)

            @block.vector
            def _(e):
                for f in self.eng["dve"]["ops"]:
                    f(e)

            @block.gpsimd
            def _(e):
                for f in self.eng["pool"]["ops"]:
                    f(e)

            @block.sync
            def _(e):
                for f in self.eng["sp"]["ops"]:
                    f(e)


DT_BYTES = {F32: 4, BF16: 2, U32: 4}


class Arena:
    def __init__(self, t, nbytes):
        self.t = t
        self.nbytes = nbytes
        self.off = 0

    def reset(self):
        self.off = 0

    def take(self, free, dtype):
        n = 1
        for s in free:
            n *= s
        sz = n * DT_BYTES[dtype]
        a = self.off
        self.off += (sz + 63) // 64 * 64
        assert self.off <= self.nbytes, (self.off, self.nbytes)
        ap = self.t[:, a // 4:(a + sz) // 4]
        if dtype != F32:
            ap = ap.bitcast(dtype)
        if len(free) == 2:
            ap = ap.rearrange("p (a b) -> p a b", a=free[0])
        elif len(free) == 3:
            ap = ap.rearrange("p (a b c) -> p a b c", a=free[0], b=free[1])
        return ap


def build(NB=4, stage="full"):
    nc = bass.Bass("TRN2", target_bir_lowering=False)

    def din(n, s, d=F32):
        return nc.dram_tensor(n, s, d, kind="ExternalInput").ap()

    x = din("x", [NB, T, D])
    ctx = din("ctx", [NB, C, D])
    ccT = din("ccT", [128, 8, 5])
    w_ada = din("w_ada", [D, 6 * D])
    b_ada = din("b_ada", [1, 6 * D])
    gains = din("gains", [4, D])
    w_in_r = din("w_in_r", [24, 128, 8, 128])
    w_v_r = din("w_v_r", [128, 8, 512])
    convw = din("convw", [128, 4, 3])
    lqk = din("lqk", [1, 256])
    subln = din("subln", [128, 1])
    w_out = din("w_out", [D, D])
    w_router = din("w_router", [128, 8, 16])
    NE_decl = NE if stage == "full" else 1
    w_gate = din("w_gate", [NE_decl, D, D])
    w_up = din("w_up", [NE_decl, D, D])
    w_down = din("w_down", [NE_decl, D, D])
    ident = din("ident", [128, 128])
    perm = din("perm", [128, 128])
    cosT_d = din("cosT", [128, T])
    sinT_d = din("sinT", [128, T])
    out = nc.dram_tensor("out", [NB, T, D], F32, kind="ExternalOutput").ap()
    mod_d = nc.dram_tensor("mod_d", [5, 6 * D], F32).ap()
    h_d = nc.dram_tensor("h_d", [NB, T, D], F32).ap()
    aff_d = nc.dram_tensor("aff_d", [128, T], F32).ap()
    xn2_d = [nc.dram_tensor(f"xn2_d{b}", [T, D], BF16).ap() for b in range(NB)]
    ff_d = [nc.dram_tensor(f"ff_d{b}", [T, D], F32).ap() for b in range(NB)]

    with ExitStack() as st:
        S = Sched(nc, st)

        def sb(n, s, d):
            return st.enter_context(nc.sbuf_tensor(n, s, d))

        idf = sb("idf", [128, 128], F32)
        idb = sb("idb", [128, 128], BF16)
        permf = sb("permf", [128, 128], F32)
        permb = sb("permb", [128, 128], BF16)
        ones_b = sb("ones_b", [128, 128], BF16)
        ones_f = sb("ones_f", [128, 128], F32)
        neghalf = sb("neghalf", [128, 256], F32)
        cw = sb("cw", [128, 4, 3], F32)
        sgs = sb("sgs", [128, 1], F32)
        neglam = sb("neglam", [128, 2], F32)
        wr_sb = sb("wr_sb", [128, 8, 16], F32)
        stat = sb("stat", [128, 96], F32)
        lq_sb = sb("lq_sb", [1, 256], F32)
        lq2 = sb("lq2", [1, 128], F32)
        lq3 = sb("lq3", [1, 8], F32)
        ARENA_BYTES = 200 * 1024
        arena_t = sb("arena", [128, ARENA_BYTES // 4], F32)
        AR = Arena(arena_t, ARENA_BYTES)
        psS = [st.enter_context(nc.psum_tensor(f"psS{i}", [128, 512], F32)) for i in range(8)]
        bank = [p[:] for p in psS]
        PSN = [f"ps{i}" for i in range(8)]

        stat_ctr = [0]

        def stat_slot(n=1):
            c = stat_ctr[0]
            if c + n > 96:
                c = 0
            stat_ctr[0] = c + n
            return c

        def rstd_of(src, nfree, src_reads, junk, inv_n):
            c = stat_slot(3)
            r0, r1, r2 = f"st{c}", f"st{c+1}", f"st{c+2}"
            S.op("act", lambda e: e.activation(out=junk, in_=src, func=ACT.Square, accum_out=stat[:, c:c + 1]),
                 src_reads, [r0, "junk"])
            S.op("dve", lambda e: e.tensor_scalar(stat[:, c + 1:c + 2], stat[:, c:c + 1], inv_n, EPS, op0=ALU.mult, op1=ALU.add),
                 [r0], [r1])
            S.op("pool", lambda e: e.tensor_tensor(stat[:, c + 2:c + 3], stat[:, c + 1:c + 2], neghalf[:, 0:1], op=ALU.pow),
                 [r1, "neghalf"], [r2])
            return stat[:, c + 2:c + 3], r2

        def rstd_of2(src0, src1, src_reads, junk, inv_n):
            c = stat_slot(5)
            ra, rb, r0, r1, r2 = (f"st{c + i}" for i in range(5))
            S.op("act", lambda e: e.activation(out=junk[:, 0:512], in_=src0, func=ACT.Square, accum_out=stat[:, c:c + 1]),
                 src_reads, [ra, "junk"])
            S.op("act", lambda e: e.activation(out=junk[:, 512:1024], in_=src1, func=ACT.Square, accum_out=stat[:, c + 1:c + 2]),
                 src_reads, [rb, "junk"])
            S.op("dve", lambda e: e.tensor_tensor(stat[:, c + 2:c + 3], stat[:, c:c + 1], stat[:, c + 1:c + 2], op=ALU.add), [ra, rb], [r0])
            S.op("dve", lambda e: e.tensor_scalar(stat[:, c + 3:c + 4], stat[:, c + 2:c + 3], inv_n, EPS, op0=ALU.mult, op1=ALU.add), [r0], [r1])
            S.op("pool", lambda e: e.tensor_tensor(stat[:, c + 4:c + 5], stat[:, c + 3:c + 4], neghalf[:, 0:1], op=ALU.pow), [r1, "neghalf"], [r2])
            return stat[:, c + 4:c + 5], r2

        S.dma("sp", lambda e: e.dma_start(out=idf[:], in_=ident), [], ["idf"], "d_c1")
        S.dma("sp", lambda e: e.dma_start(out=permf[:], in_=perm), [], ["permf"], "d_c2")
        S.dma("sp", lambda e: e.dma_start(out=cw[:], in_=convw), [], ["cw"], "d_c3")
        S.dma("sp", lambda e: e.dma_start(out=sgs[:], in_=subln), [], ["sgs"], "d_c4")
        S.dma("sp", lambda e: e.dma_start(out=wr_sb[:], in_=w_router), [], ["wr_sb"], "d_c5")
        S.dma("sp", lambda e: e.dma_start(out=lq_sb[:], in_=lqk), [], ["lq_sb"], "d_c6")
        S.op("dve", lambda e: e.tensor_copy(idb[:], idf[:]), ["idf"], ["idb"])
        S.op("dve", lambda e: e.tensor_copy(permb[:], permf[:]), ["permf"], ["permb"])
        S.op("dve", lambda e: e.memset(ones_b[:], 1.0), [], ["ones_b"])
        S.op("dve", lambda e: e.memset(ones_f[:], 1.0), [], ["ones_f"])
        S.op("dve", lambda e: e.memset(neghalf[:], -0.5), [], ["neghalf"])
        S.op("dve", lambda e: e.tensor_scalar(sgs[:], sgs[:], 0.8, None, op0=ALU.mult), ["sgs"], ["sgs"])
        lqv = lq_sb[0:1, :].rearrange("p (a b c) -> p a b c", a=2, b=2)
        S.op("dve", lambda e: e.tensor_tensor(lq2[0:1, :].rearrange("p (a c) -> p a c", a=2), lqv[:, :, 0, :], lqv[:, :, 1, :], op=ALU.mult),
             ["lq_sb"], ["lq2"])
        S.op("dve", lambda e: e.reduce_sum(lq3[0:1, 0:2], lq2[0:1, :].rearrange("p (a c) -> p a c", a=2), axis=AX.X), ["lq2"], ["lq3a"])
        S.op("act", lambda e: e.activation(out=lq3[0:1, 2:4], in_=lq3[0:1, 0:2], func=ACT.Exp), ["lq3a"], ["lq3b"])
        S.op("dve", lambda e: e.tensor_tensor(lq3[0:1, 4:5], lq3[0:1, 3:4], lq3[0:1, 2:3], op=ALU.subtract), ["lq3b"], ["lq3c"])
        S.op("dve", lambda e: e.tensor_scalar(lq3[0:1, 6:7], lq3[0:1, 4:5], -0.2, None, op0=ALU.add), ["lq3c"], ["lq3d"])
        S.op("dve", lambda e: e.tensor_copy(lq3[0:1, 7:8], lq3[0:1, 6:7]), ["lq3d"], ["lq3e"])
        S.op("pe", lambda e: e.matmul(bank[7][:, 0:2], ones_f[0:1, :], lq3[0:1, 6:8], start=True, stop=True),
             ["ones_f", "lq3d", "lq3e"], [PSN[7]])
        S.op("dve", lambda e: e.tensor_copy(neglam[:], bank[7][:, 0:2]), [PSN[7]], ["neglam"])

        AR.reset()
        ccT_sb = AR.take([8, 5], F32)
        siluT = AR.take([8, 5], F32)
        bada_sb = AR.take([6 * D], F32)
        wa = [AR.take([8, 512], F32) for _ in range(2)]
        mstage = [AR.take([512], F32) for _ in range(2)]
        zero_t = AR.take([2048], F32)
        S.dma("sp", lambda e: e.dma_start(out=ccT_sb, in_=ccT), [], ["ccT"], "d_c7")
        S.dma("sp", lambda e: e.dma_start(out=bada_sb[0:1, :], in_=b_ada), [], ["bada"], "d_c8")
        S.op("act", lambda e: e.activation(out=siluT, in_=ccT_sb, func=ACT.Silu), ["ccT"], ["siluT"])
        S.op("dve", lambda e: e.memset(zero_t, 0.0), [], ["zero_t"])
        for b in range(NB):
            ffv = ff_d[b].rearrange("(p r) d -> p (r d)", p=128)
            for j in range(8):
                S.dma("sp", lambda e, ffv=ffv, j=j: e.dma_start(out=ffv[:, j * 2048:(j + 1) * 2048], in_=zero_t),
                      ["zero_t"], [f"ff{b}"], "d_z")
        S.dma("sp", lambda e: e.dma_start(out=aff_d, in_=zero_t), ["zero_t"], ["aff_d"], "d_z")
        for j in range(12):
            s = j % 2
            S.dma("sp", lambda e, j=j, s=s: e.dma_start(out=wa[s], in_=w_ada[:, j * 512:(j + 1) * 512].rearrange("(k p) n -> p k n", p=128)),
                  [], [f"wa{s}"], f"d_wa{s}")

            def mm0(e, j=j, s=s):
                for k in range(8):
                    e.matmul(bank[4][0:5, :], siluT[:, k, :], wa[s][:, k, :], start=(k == 0), stop=False)
                return e.matmul(bank[4][0:5, :], ones_f[0:1, 0:5], bada_sb[0:1, j * 512:(j + 1) * 512], start=False, stop=True)
            S.op("pe", mm0, ["siluT", f"wa{s}", "bada", "ones_f"], [PSN[4]])
            S.op("act", lambda e, s=s: e.activation(out=mstage[s][0:5, :], in_=bank[4][0:5, :], func=ACT.Copy), [PSN[4]], [f"ms{s}"])
            S.dma("sp", lambda e, j=j, s=s: e.dma_start(out=mod_d[:, j * 512:(j + 1) * 512], in_=mstage[s][0:5, :]),
                  [f"ms{s}"], ["mod_d"], f"d_ms{s}")
        S.barrier()

        AR.reset()
        cosT = AR.take([T], F32)
        sinT = AR.take([T], F32)
        xnT = AR.take([8, TK], BF16)
        R1 = xnT.rearrange("p a b -> p (a b)")
        attnT = R1[:, 0:4 * T].rearrange("p (a b) -> p a b", a=4)
        w_out_sb = R1[:, 4 * T:4 * T + 8 * D].rearrange("p (a b) -> p a b", a=8)
        kT = AR.take([4, TK], BF16)
        v_sb = AR.take([18, 512], BF16)
        KVf = arena_t[:, 0:0]
        qc_off = AR.off
        qT = AR.take([4, T], BF16)
        AR.off = qc_off
        u_t = AR.take([2064], F32)
        gb_t = AR.take([T], F32)
        y_t = AR.take([1024], F32)
        qc_end = max(AR.off, qc_off + 4 * T * 2)
        AR.off = qc_off
        A2 = AR.take([D], F32)
        B2 = AR.take([D], F32)
        G1 = AR.take([D], F32)
        gtmp2 = AR.take([D], F32)
        AR.off = qc_end
        convT = AR.take([4, T], BF16)
        A1 = AR.take([D], F32)
        B1 = AR.take([D], F32)
        A1c = AR.take([D], F32)
        B1c = AR.take([D], F32)
        ta_off = AR.off
        XT = [AR.take([D], F32) for _ in range(2)]
        xb = [AR.take([D], BF16) for _ in range(2)]
        tmpA = AR.take([D], F32)
        junk = AR.take([D], BF16)
        ta_end = AR.off
        AR.off = ta_off
        at_t = [AR.take([512], BF16) for _ in range(3)]
        rden = AR.take([512], F32)
        tO = AR.take([512], F32)
        o_t = [AR.take([256], F32) for _ in range(2)]
        osq = [AR.take([256], F32) for _ in range(2)]
        ms_t = [AR.take([256], F32) for _ in range(2)]
        rs_t = [AR.take([256], F32) for _ in range(2)]
        on_t = [AR.take([256], F32) for _ in range(2)]
        assert AR.off <= ta_end
        AR.off = ta_end
        pb = [AR.take([512], BF16) for _ in range(2)]
        t1 = [AR.take([512], F32) for _ in range(2)]
        t2 = [AR.take([512], F32) for _ in range(2)]
        xc_sb = [AR.take([512], F32) for _ in range(2)]
        wslot = [AR.take([8, 128], BF16) for _ in range(6)]
        wv_sb = AR.take([8, 512], BF16)
        mix_end = AR.off
        kv_off = (kT.offset if hasattr(kT, "offset") else None)
        AR.off = 2 * T * 4 + 8 * TK * 2
        xt2 = [AR.take([D], F32) for _ in range(2)]
        tD = AR.take([D], F32)
        h_sb = [AR.take([D], F32) for _ in range(2)]
        t2D = AR.take([D], F32)
        xn2 = [AR.take([D], F32) for _ in range(2)]
        xn2T = AR.take([8, 128], F32)
        assert AR.off <= 2 * T * 4 + 8 * TK * 2 + 4 * TK * 2 + 18 * 512 * 2 + 64
        AR.off = mix_end
        lg_sb = AR.take([16], F32)
        ex_sb = AR.take([16], F32)
        aff_sb = [AR.take([16], F32) for _ in range(2)]
        affT_sb = [AR.take([128], F32) for _ in range(2)]
        print("mixer arena bytes", AR.off)

        S.dma("sp", lambda e: e.dma_start(out=cosT, in_=cosT_d), [], ["cosT"], "d_c9")
        S.dma("sp", lambda e: e.dma_start(out=sinT, in_=sinT_d), [], ["sinT"], "d_c10")
        S.op("dve", lambda e: e.memset(u_t, 0.0), [], ["u_t"])

        def bc_load(dst, row_ap, region, key, guard=()):
            S.dma("sp", lambda e: e.dma_start(out=dst, in_=row_ap.partition_broadcast(128)), list(guard), [region], key)

        def mod_tile(dst, region, b, seg, gain_idx, tmp, tmp_region, plus_one, guard=()):
            bc_load(dst, mod_d[b:b + 1, seg * D:(seg + 1) * D], region, "d_bc_" + region, guard)
            if gain_idx is not None:
                bc_load(tmp, gains[gain_idx:gain_idx + 1, :], tmp_region, "d_bc_" + tmp_region, guard)
                if plus_one:
                    S.op("dve", lambda e: e.scalar_tensor_tensor(dst, dst, 1.0, tmp, op0=ALU.add, op1=ALU.mult),
                         [region, tmp_region], [region])
                else:
                    S.op("dve", lambda e: e.tensor_tensor(dst, dst, tmp, op=ALU.mult), [region, tmp_region], [region])

        mod_tile(A1c, "A1c", 4, 1, 0, tmpA, "tmpA", True)
        mod_tile(B1c, "B1c", 4, 0, None, None, None, False)

        grp_ctr = [0]

        def load_group(g):
            s = grp_ctr[0] % 6
            grp_ctr[0] += 1
            S.dma("pool", lambda e: e.dma_start(out=wslot[s], in_=w_in_r[g]), [], [f"ws{s}"], f"d_ws{s}")
            return s

        pbank_ctr = [0]

        def next_pbank():
            i = pbank_ctr[0] % 4
            pbank_ctr[0] += 1
            return i

        rope_ctr = [0]

        def do_batch(b):
            mod_tile(A1, "A1", b, 1, 0, tmpA, "tmpA", True)
            mod_tile(B1, "B1", b, 0, None, None, None, False)
            S.dma("pool", lambda e: e.dma_start(out=wv_sb, in_=w_v_r), [], ["wv"], "d_wv")
            order = [("k", h, 4 + h) for h in range(4)]
            for j in range(4):
                order += [("gb", j, 12 + j), ("gc", j, 16 + j), ("xc", j, 20 + j)]
            order += [("q", h, h) for h in range(4)]
            slots = {}
            nload = [0]

            def ensure_loaded(upto):
                while nload[0] <= min(upto, len(order) - 1):
                    slots[nload[0]] = load_group(order[nload[0]][2])
                    nload[0] += 1
            ensure_loaded(4)

            for tt in range(18):
                s = tt % 2
                src = ctx[b, tt * 128:(tt + 1) * 128, :] if tt < 2 else x[b, (tt - 2) * 128:(tt - 1) * 128, :]
                Ab, Bb, An, Bn = (A1c, B1c, "A1c", "B1c") if tt < 2 else (A1, B1, "A1", "B1")
                S.dma("sp", lambda e, s=s, src=src: e.dma_start(out=XT[s], in_=src), [], [f"XT{s}"], f"d_XT{s}")
                rs, rsn = rstd_of(XT[s], D, [f"XT{s}"], junk, 1.0 / D)
                S.op("dve", lambda e, s=s, rs=rs, Ab=Ab: e.scalar_tensor_tensor(tmpA, XT[s], rs, Ab, op0=ALU.mult, op1=ALU.mult),
                     [f"XT{s}", rsn, An], ["tmpA"])
                S.op("dve", lambda e, s=s, Bb=Bb: e.tensor_tensor(xb[s], tmpA, Bb, op=ALU.add), ["tmpA", Bn], [f"xb{s}"])
                pbk = 6 + (tt % 2)
                pv = bank[pbk].bitcast(BF16)

                def tpA(e, s=s, pv=pv):
                    for k in range(8):
                        ins = e.transpose(pv[:, k * 128:(k + 1) * 128], xb[s][:, k * 128:(k + 1) * 128], idb[:])
                    return ins
                S.op("pe", tpA, [f"xb{s}", "idb"], [PSN[pbk]])
                S.op("act", lambda e, tt=tt, pv=pv: e.activation(out=xnT[:, :, tt * 128:(tt + 1) * 128],
                                                                   in_=pv.rearrange("p (k t) -> p k t", k=8), func=ACT.Copy),
                     [PSN[pbk]], [f"xn{tt}"])

            if stage == "A":
                return
            pending = []

            def flush(keep=0):
                while len(pending) > keep:
                    pending.pop(0)()

            def proj(slot, tok0, ntok):
                pbk = next_pbank()
                tiles = sorted(set(range(tok0 // 128, (tok0 + ntok + 127) // 128)))

                def mm(e):
                    for k in range(8):
                        ins = e.matmul(bank[pbk][:, 0:ntok], wslot[slot][:, k, :], xnT[:, k, tok0:tok0 + ntok], start=(k == 0), stop=(k == 7))
                    return ins
                S.op("pe", mm, [f"ws{slot}"] + [f"xn{t}" for t in tiles], [PSN[pbk]])
                return pbk

            def rope(pbk, dst, dst_region, tb, extra_reads=()):
                i = rope_ctr[0] % 2
                rope_ctr[0] += 1
                qb_ = 4 + i
                S.op("act", lambda e: e.activation(out=pb[i], in_=bank[pbk], func=ACT.Copy), [PSN[pbk]], [f"pb{i}"])
                S.op("dve", lambda e: e.tensor_tensor(t2[i], bank[pbk], cosT[:, tb * 512:(tb + 1) * 512], op=ALU.mult),
                     [PSN[pbk], "cosT"], [f"t2{i}"])

                def part2():
                    S.op("pe", lambda e: e.matmul(bank[qb_], permb[:], pb[i], start=True, stop=True), ["permb", f"pb{i}"], [PSN[qb_]])
                    S.op("dve", lambda e: e.tensor_tensor(t1[i], bank[qb_], sinT[:, tb * 512:(tb + 1) * 512], op=ALU.mult),
                         [PSN[qb_], "sinT"], [f"t1{i}"])
                    S.op("dve", lambda e: e.tensor_tensor(dst, t1[i], t2[i], op=ALU.add),
                         [f"t1{i}", f"t2{i}"] + list(extra_reads), [dst_region])
                pending.append(part2)

            gi = 0
            for h in range(4):
                ensure_loaded(gi + 4)
                sl = slots[gi]
                gi += 1
                pbk = proj(sl, 0, C)
                flush(0)
                S.op("act", lambda e, h=h, pbk=pbk: e.activation(out=kT[:, h, 0:C], in_=bank[pbk][:, 0:C], func=ACT.Copy),
                     [PSN[pbk]], [f"kT{h}"])
                if stage == "B0a":
                    return
                for tb in range(4):
                    pbk = proj(sl, C + tb * 512, 512)
                    flush(0)
                    if stage == "B0c":
                        S.op("act", lambda e, h=h, pbk=pbk, tb=tb: e.activation(out=kT[:, h, C + tb * 512:C + (tb + 1) * 512], in_=bank[pbk], func=ACT.Copy),
                             [PSN[pbk]], [f"kT{h}"])
                        return
                    if stage == "B0d":
                        S.op("dve", lambda e, pbk=pbk, tb=tb: e.tensor_tensor(t2[0], bank[pbk], cosT[:, tb * 512:(tb + 1) * 512], op=ALU.mult),
                             [PSN[pbk], "cosT"], ["t20"])
                        return
                    rope(pbk, kT[:, h, C + tb * 512:C + (tb + 1) * 512], f"kT{h}", tb)
                    if stage == "B0b":
                        flush(0)
                        return
            flush(0)
            if stage == "B1":
                return
            for tt in range(18):
                pbk = next_pbank()

                def mmv(e, tt=tt, pbk=pbk):
                    for k in range(8):
                        ins = e.matmul(bank[pbk], xnT[:, k, tt * 128:(tt + 1) * 128], wv_sb[:, k, :], start=(k == 0), stop=(k == 7))
                    return ins
                S.op("pe", mmv, ["wv", f"xn{tt}"], [PSN[pbk]])
                S.op("act", lambda e, tt=tt, pbk=pbk: e.activation(out=v_sb[:, tt, :], in_=bank[pbk], func=ACT.Copy), [PSN[pbk]], ["v_sb"])
            if stage == "B2":
                return
            S.op("dve", lambda e: e.memset(u_t[:, 0:1], 0.0), ["qdead", "u_t"], ["u_t"])
            S.op("dve", lambda e: e.memset(u_t[:, 2049:2050], 0.0), ["qdead", "u_t"], ["u_t"])
            for j in range(4):
                ensure_loaded(gi + 5)
                sgb, sgc, sxc = slots[gi], slots[gi + 1], slots[gi + 2]
                gi += 3
                for tb in range(4):
                    i = (j * 4 + tb) % 2
                    p_xc = proj(sxc, C + tb * 512, 512)
                    p_gc = proj(sgc, C + tb * 512, 512)
                    p_gb = proj(sgb, C + tb * 512, 512)
                    S.op("act", lambda e, i=i, p_xc=p_xc: e.activation(out=xc_sb[i], in_=bank[p_xc], func=ACT.Copy), [PSN[p_xc]], [f"xc{i}"])
                    S.op("dve", lambda e, i=i, p_gc=p_gc, tb=tb: e.tensor_tensor(u_t[:, 1 + tb * 512:1 + (tb + 1) * 512], bank[p_gc], xc_sb[i], op=ALU.mult),
                         [PSN[p_gc], f"xc{i}", "qdead"], ["u_t"])
                    S.op("act", lambda e, p_gb=p_gb, tb=tb: e.activation(out=gb_t[:, tb * 512:(tb + 1) * 512], in_=bank[p_gb], func=ACT.Copy),
                         [PSN[p_gb], "qdead"], ["gb_t"])
                for hf in range(2):
                    o0 = hf * 1024
                    S.op("act", lambda e, j=j, o0=o0: e.activation(out=y_t, in_=u_t[:, 1 + o0:1 + o0 + 1024], func=ACT.Identity, scale=cw[:, j, 1:2]),
                         ["u_t", "cw", "qdead"], ["y_t"])
                    S.op("dve", lambda e, j=j, o0=o0: e.scalar_tensor_tensor(y_t, u_t[:, o0:o0 + 1024], cw[:, j, 0:1], y_t, op0=ALU.mult, op1=ALU.add),
                         ["u_t", "cw", "y_t"], ["y_t"])
                    S.op("dve", lambda e, j=j, o0=o0: e.scalar_tensor_tensor(y_t, u_t[:, 2 + o0:2 + o0 + 1024], cw[:, j, 2:3], y_t, op0=ALU.mult, op1=ALU.add),
                         ["u_t", "cw", "y_t"], ["y_t"])
                    S.op("dve", lambda e, j=j, o0=o0: e.tensor_tensor(convT[:, j, o0:o0 + 1024], y_t, gb_t[:, o0:o0 + 1024], op=ALU.mult),
                         ["y_t", "gb_t"], ["convT", "convdead"])
            if stage == "B3":
                return
            for h in range(4):
                ensure_loaded(gi + 4)
                sl = slots[gi]
                gi += 1
                for tb in range(4):
                    pbk = proj(sl, C + tb * 512, 512)
                    flush(0)
                    rope(pbk, qT[:, h, tb * 512:(tb + 1) * 512], f"qT{h}", tb, extra_reads=("convdead",))
            flush(0)
            S.op("pe", lambda e: e.matmul(bank[7][:, 0:2], ones_f[0:1, :], lq3[0:1, 6:8], start=True, stop=True),
                 [f"xn{t}" for t in range(18)] + ["ones_f"], [PSN[7], "xndead"])
            for k in range(8):
                S.dma("pool", lambda e, k=k: e.dma_start(out=w_out_sb[:, k, :], in_=w_out[k * 128:(k + 1) * 128, :]),
                      ["xndead"], ["w_out_sb"], "d_wout")

            if stage == "B":
                return
            steps = [(h, qb, kt) for h in range(4) for qb in range(8) for kt in range(18)]
            deferred = {}

            def qk(si):
                h, qb, kt = steps[si]
                sb0 = (si % 2) * 2

                def mm(e):
                    for m in range(2):
                        ins = e.matmul(bank[sb0 + m][:, 0:256], kT[64 * m:64 * (m + 1), h, kt * 128:(kt + 1) * 128],
                                       qT[64 * m:64 * (m + 1), h, qb * 256:(qb + 1) * 256], start=True, stop=True)
                    return ins
                S.op("pe", mm, [f"kT{h}", f"qT{h}"], [PSN[sb0], PSN[sb0 + 1]])

            def fin1(h, qb, fi):
                ob = 4
                db = 5
                i = fi % 2
                S.op("dve", lambda e: e.reciprocal(rden, bank[db]), [PSN[db], "xndead"], ["rden"])
                S.op("dve", lambda e: e.tensor_tensor(tO, bank[ob], rden, op=ALU.mult), [PSN[ob], "rden"], ["tO"])
                S.op("dve", lambda e: e.scalar_tensor_tensor(o_t[i], tO[:, 256:512], neglam[:, 0:1], tO[:, 0:256], op0=ALU.mult, op1=ALU.add),
                     ["tO", "neglam"], [f"o{i}"])
                S.op("dve", lambda e: e.tensor_tensor(osq[i], o_t[i], o_t[i], op=ALU.mult), [f"o{i}"], [f"osq{i}"])

            def fin2(h, qb, fi):
                i = fi % 2
                S.op("pe", lambda e: e.matmul(bank[6][:, 0:256], ones_f[:], osq[i], start=True, stop=True), ["ones_f", f"osq{i}"], [PSN[6]])
                S.op("dve", lambda e: e.tensor_scalar(ms_t[i], bank[6][:, 0:256], 1.0 / 128, EPS, op0=ALU.mult, op1=ALU.add), [PSN[6]], [f"ms{i}"])
                S.op("pool", lambda e: e.tensor_tensor(rs_t[i], ms_t[i], neghalf[:, 0:256], op=ALU.pow), [f"ms{i}", "neghalf"], [f"rs{i}"])
                S.op("dve", lambda e: e.tensor_tensor(on_t[i], o_t[i], rs_t[i], op=ALU.mult), [f"o{i}", f"rs{i}"], [f"on{i}"])
                S.op("act", lambda e: e.activation(out=attnT[:, h, qb * 256:(qb + 1) * 256], in_=on_t[i], func=ACT.Identity, scale=sgs[:, 0:1]),
                     [f"on{i}", "sgs", "xndead"], ["attnT"])

            qk(0)
            fi = 0
            for si, (h, qb, kt) in enumerate(steps):
                if si + 1 < len(steps):
                    qk(si + 1)
                sb0 = (si % 2) * 2
                ai = si % 3
                for m in range(2):
                    S.op("act", lambda e, sb0=sb0, ai=ai, m=m: e.activation(out=at_t[ai][:, m * 256:(m + 1) * 256], in_=bank[sb0 + m][:, 0:256], func=ACT.Exp, scale=0.125),
                         [PSN[sb0 + m], "xndead"], [f"at{ai}"])
                ob = 4
                db = 5

                def av(e, h=h, kt=kt, ai=ai, ob=ob, db=db):
                    e.matmul(bank[ob], v_sb[:, kt, h * 128:(h + 1) * 128], at_t[ai], start=(kt == 0), stop=(kt == 17))
                    return e.matmul(bank[db], ones_b[:], at_t[ai], start=(kt == 0), stop=(kt == 17))
                S.op("pe", av, ["v_sb", f"at{ai}", "ones_b"], [PSN[ob], PSN[db]])
                if si in deferred:
                    deferred.pop(si)()
                if kt == 17:
                    fin1(h, qb, fi)
                    deferred[min(si + 4, len(steps) - 1) if si + 4 < len(steps) else -1] = (lambda h=h, qb=qb, fi=fi: fin2(h, qb, fi))
                    fi += 1
            for k_ in sorted(deferred):
                deferred[k_]()
            S.op("pe", lambda e: e.matmul(bank[7][:, 0:2], ones_f[0:1, :], lq3[0:1, 6:8], start=True, stop=True),
                 ["ones_f", "v_sb"] + [f"kT{h}" for h in range(4)] + [f"qT{h}" for h in range(4)], [PSN[7], "kvdead", "qdead"])

            if stage == "C":
                return
            mod_tile(G1, "G1", b, 2, 1, gtmp2, "gtmp2", False, guard=("qdead",))
            mod_tile(A2, "A2", b, 4, 2, gtmp2, "gtmp2", True, guard=("qdead",))
            mod_tile(B2, "B2", b, 3, None, None, None, False, guard=("qdead",))
            mixin = [attnT[:, h, :] for h in range(4)] + [convT[:, j, :] for j in range(4)]

            def d1(tt):
                s = tt % 2
                S.dma("sp", lambda e: e.dma_start(out=xt2[s], in_=x[b, tt * 128:(tt + 1) * 128, :]), ["kvdead"], [f"xt2{s}"], f"d_xt2{s}")
                for hf in range(2):
                    def mm(e, hf=hf):
                        for k in range(8):
                            ins = e.matmul(bank[hf], mixin[k][:, tt * 128:(tt + 1) * 128], w_out_sb[:, k, hf * 512:(hf + 1) * 512], start=(k == 0), stop=(k == 7))
                        return ins
                    S.op("pe", mm, ["attnT", "convT", "w_out_sb"], [PSN[hf]])
                rs, rsn = rstd_of2(bank[0], bank[1], [PSN[0], PSN[1]], junk, 1.0 / D)
                for hf in range(2):
                    S.op("dve", lambda e, hf=hf: e.scalar_tensor_tensor(tD[:, hf * 512:(hf + 1) * 512], bank[hf], rs, G1[:, hf * 512:(hf + 1) * 512], op0=ALU.mult, op1=ALU.mult),
                         [PSN[hf], rsn, "G1", "kvdead"], ["tD"])
                S.op("dve", lambda e: e.tensor_tensor(h_sb[s], tD, xt2[s], op=ALU.add), ["tD", f"xt2{s}", "kvdead"], [f"h{s}"])
                S.dma("sp", lambda e: e.dma_start(out=h_d[b, tt * 128:(tt + 1) * 128, :], in_=h_sb[s]), [f"h{s}"], [f"h_d{b}_{tt}"], f"d_h{s}")
                rs2, rsn2 = rstd_of(h_sb[s], D, [f"h{s}"], junk, 1.0 / D)
                S.op("dve", lambda e: e.scalar_tensor_tensor(t2D, h_sb[s], rs2, A2, op0=ALU.mult, op1=ALU.mult),
                     [f"h{s}", rsn2, "A2", "kvdead"], ["t2D"])
                S.op("dve", lambda e: e.tensor_tensor(xn2[s], t2D, B2, op=ALU.add), ["t2D", "B2"], [f"xn2{s}"])
                S.dma("pool", lambda e: e.dma_start(out=xn2_d[b][tt * 128:(tt + 1) * 128, :], in_=xn2[s]), [f"xn2{s}"], [f"xn2d{b}"], f"d_x2{s}")

            def d2(tt):
                s = tt % 2

                def tp(e):
                    for k in range(8):
                        ins = e.transpose(bank[2 + k // 4][:, (k % 4) * 128:(k % 4 + 1) * 128], xn2[s][:, k * 128:(k + 1) * 128], idf[:])
                    return ins
                S.op("pe", tp, [f"xn2{s}", "idf"], [PSN[2], PSN[3]])
                for hb in range(2):
                    S.op("act", lambda e, hb=hb: e.activation(out=xn2T[:, hb * 4:(hb + 1) * 4, :], in_=bank[2 + hb].rearrange("p (k t) -> p k t", k=4), func=ACT.Copy),
                         [PSN[2 + hb], "kvdead"], ["xn2T"])

            def d3(tt):
                s = tt % 2

                def mm(e):
                    for k in range(8):
                        ins = e.matmul(bank[4][:, 0:16], xn2T[:, k, :], wr_sb[:, k, :], start=(k == 0), stop=(k == 7))
                    return ins
                S.op("pe", mm, ["xn2T", "wr_sb"], [PSN[4]])
                c = stat_slot(4)
                S.op("dve", lambda e: e.reduce_max(stat[:, c:c + 1], bank[4][:, 0:16], axis=AX.X), [PSN[4]], [f"st{c}"])
                S.op("dve", lambda e: e.tensor_scalar(stat[:, c + 1:c + 2], stat[:, c:c + 1], -1.0, None, op0=ALU.mult), [f"st{c}"], [f"st{c+1}"])
                S.op("act", lambda e: e.activation(out=ex_sb, in_=bank[4][:, 0:16], func=ACT.Exp, bias=stat[:, c + 1:c + 2], accum_out=stat[:, c + 2:c + 3]),
                     [PSN[4], f"st{c+1}"], ["ex_sb", f"st{c+2}"])
                S.op("dve", lambda e: e.reciprocal(stat[:, c + 3:c + 4], stat[:, c + 2:c + 3]), [f"st{c+2}"], [f"st{c+3}"])
                S.op("dve", lambda e: e.tensor_scalar(aff_sb[s], ex_sb, stat[:, c + 3:c + 4], None, op0=ALU.mult), ["ex_sb", f"st{c+3}"], [f"aff{s}"])

            def d4(tt):
                s = tt % 2
                S.op("pe", lambda e: e.transpose(bank[5][0:16, 0:128], aff_sb[s], idf[:]), [f"aff{s}", "idf"], [PSN[5]])
                S.op("act", lambda e: e.activation(out=affT_sb[s][0:16, :], in_=bank[5][0:16, 0:128], func=ACT.Copy), [PSN[5]], [f"affT{s}"])
                S.dma("sp", lambda e: e.dma_start(out=aff_d[32 * b:32 * b + 16, tt * 128:(tt + 1) * 128], in_=affT_sb[s][0:16, :]),
                      [f"affT{s}"], ["aff_d"], f"d_af{s}")

            for i in range(16 + 3):
                if 0 <= i - 3 < 16:
                    d4(i - 3)
                if 0 <= i - 2 < 16:
                    d3(i - 2)
                if 0 <= i - 1 < 16:
                    d2(i - 1)
                if i < 16:
                    d1(i)
            S.barrier()

        for b_ in range(NB if stage != "phase0" else 0):
            do_batch(b_)
        if stage in ("A", "B", "C", "B1", "B2", "B3", "B0a", "B0b", "B0c", "B0d"):
            S.barrier()

        if stage in ("mixer", "phase0", "A", "B", "C", "B1", "B2", "B3", "B0a", "B0b", "B0c", "B0d"):
            AR.reset()
            cp = [AR.take([D], F32) for _ in range(2)]
            for b in range(NB):
                for tt in range(16):
                    s = tt % 2
                    S.dma("sp", lambda e, b=b, tt=tt, s=s: e.dma_start(out=cp[s], in_=h_d[b, tt * 128:(tt + 1) * 128, :]), [], [f"cp{s}"], f"d_cp{s}")
                    S.dma("sp", lambda e, b=b, tt=tt, s=s: e.dma_start(out=out[b, tt * 128:(tt + 1) * 128, :], in_=cp[s]), [f"cp{s}"], ["out"], f"d_co{s}")
            S.barrier(["sp"])
            S.emit()
            return nc

        AR.reset()
        wexp = [[AR.take([8, D], BF16) for _ in range(3)] for _ in range(2)]
        xs = [AR.take([D], BF16) for _ in range(4)]
        xsT = [AR.take([8, 512], BF16) for _ in range(2)]
        actT = [AR.take([8, 512], BF16) for _ in range(2)]
        sgt = [AR.take([512], F32) for _ in range(2)]
        y_sb = [AR.take([D], F32) for _ in range(3)]
        moe_end = AR.off
        work = AR.take([T], F32)
        vals = AR.take([CAP], F32)
        idx = AR.take([CAP], U32)
        idxf = AR.take([CAP], F32)
        idxT = AR.take([2, 128], U32)
        gT = AR.take([2, 128], F32)
        rt_end = AR.off
        AR.off = 0
        ffl = [AR.take([D], F32) for _ in range(2)]
        hl = [AR.take([D], F32) for _ in range(2)]
        G2 = AR.take([D], F32)
        gtmp3 = AR.take([D], F32)
        tF = AR.take([D], F32)
        ob_t = [AR.take([D], F32) for _ in range(2)]
        junk2 = AR.take([D], BF16)
        AR.off = rt_end
        print("moe arena bytes", AR.off)

        def load_expert(e_):
            s = e_ % 2
            for wi, wsrc in enumerate((w_gate, w_up, w_down)):
                for k in range(8):
                    S.dma("pool", lambda e, s=s, wi=wi, wsrc=wsrc, k=k: e.dma_start(out=wexp[s][wi][:, k, :], in_=wsrc[e_, k * 128:(k + 1) * 128, :]),
                          [], [f"we{s}_{wi}"], f"d_we{s}_{wi}")

        load_expert(0)
        S.dma("sp", lambda e: e.dma_start(out=work, in_=aff_d), ["aff_d"], ["work"], "d_work")
        for r in range(CAP // 8):
            S.op("dve", lambda e, r=r: e.max(out=vals[:, r * 8:(r + 1) * 8], in_=work), ["work"], ["vals"])
            S.op("dve", lambda e, r=r: e.max_index(out=idx[:, r * 8:(r + 1) * 8], in_max=vals[:, r * 8:(r + 1) * 8], in_values=work), ["work", "vals"], ["idx"])
            S.op("dve", lambda e, r=r: e.match_replace(out=work, in_to_replace=vals[:, r * 8:(r + 1) * 8], in_values=work, imm_value=-1.0), ["vals", "idx"], ["work"])
        S.op("dve", lambda e: e.tensor_copy(idxf, idx), ["idx"], ["idxf"])
        for ct in range(2):
            S.op("pe", lambda e, ct=ct: e.transpose(bank[0][:, 0:128], idxf[:, ct * 128:(ct + 1) * 128], idf[:]), ["idxf", "idf"], [PSN[0]])
            S.op("dve", lambda e, ct=ct: e.tensor_copy(idxT[:, ct, :], bank[0][:, 0:128]), [PSN[0]], ["idxT"])
            S.op("pe", lambda e, ct=ct: e.transpose(bank[1][:, 0:128], vals[:, ct * 128:(ct + 1) * 128], idf[:]), ["vals", "idf"], [PSN[1]])
            S.op("dve", lambda e, ct=ct: e.tensor_copy(gT[:, ct, :], bank[1][:, 0:128]), [PSN[1]], ["gT"])

        gctr = [0]
        yctr = [0]
        NP = (NB + 1) // 2

        def do_pair(e_, pr):
            ws = e_ % 2
            wg_, wu_, wd_ = wexp[ws]
            bl = [bb for bb in (2 * pr, 2 * pr + 1) if bb < NB]
            ncol = len(bl) * 256
            ps_ = (e_ * NP + pr) % 2
            for bi, bb in enumerate(bl):
                row = 32 * bb + e_
                for ct in range(2):
                    gs = gctr[0] % 4
                    gctr[0] += 1
                    S.dma("pool", lambda e, gs=gs, bb=bb, ct=ct, row=row: e.indirect_dma_start(
                        out=xs[gs], out_offset=None, in_=xn2_d[bb],
                        in_offset=bass.IndirectOffsetOnAxis(ap=idxT[:, ct, row:row + 1], axis=0)),
                        [f"xn2d{bb}", "idxT"], [f"xs{gs}"], f"d_xs{gs}")
                    pbk = 6 + gs % 2
                    pv = bank[pbk].bitcast(BF16)

                    def tpx(e, gs=gs, pv=pv):
                        for k in range(8):
                            ins = e.transpose(pv[:, k * 128:(k + 1) * 128], xs[gs][:, k * 128:(k + 1) * 128], idb[:])
                        return ins
                    S.op("pe", tpx, [f"xs{gs}", "idb"], [PSN[pbk]])
                    c0 = (bi * 2 + ct) * 128
                    S.op("act", lambda e, ps_=ps_, c0=c0, pv=pv: e.activation(out=xsT[ps_][:, :, c0:c0 + 128], in_=pv.rearrange("p (k t) -> p k t", k=8), func=ACT.Copy),
                         [PSN[pbk]], [f"xsT{ps_}"])
            for fc in range(8):
                bg = 0 + 2 * (fc % 2)
                bu = 1 + 2 * (fc % 2)
                si_ = fc % 2

                def mmg(e, fc=fc, bg=bg):
                    for k in range(8):
                        ins = e.matmul(bank[bg][:, 0:ncol], wg_[:, k, fc * 128:(fc + 1) * 128], xsT[ps_][:, k, 0:ncol], start=(k == 0), stop=(k == 7))
                    return ins

                def mmu(e, fc=fc, bu=bu):
                    for k in range(8):
                        ins = e.matmul(bank[bu][:, 0:ncol], wu_[:, k, fc * 128:(fc + 1) * 128], xsT[ps_][:, k, 0:ncol], start=(k == 0), stop=(k == 7))
                    return ins
                S.op("pe", mmg, [f"we{ws}_0", f"xsT{ps_}"], [PSN[bg]])
                S.op("pe", mmu, [f"we{ws}_1", f"xsT{ps_}"], [PSN[bu]])
                S.op("act", lambda e, bg=bg, si_=si_: e.activation(out=sgt[si_][:, 0:ncol], in_=bank[bg][:, 0:ncol], func=ACT.Silu), [PSN[bg]], [f"sgt{si_}"])
                S.op("dve", lambda e, fc=fc, bu=bu, si_=si_: e.tensor_tensor(actT[ps_][:, fc, 0:ncol], sgt[si_][:, 0:ncol], bank[bu][:, 0:ncol], op=ALU.mult),
                     [PSN[bu], f"sgt{si_}"], [f"actT{ps_}"])
            for bi, bb in enumerate(bl):
                row = 32 * bb + e_
                for ct in range(2):
                    c0 = (bi * 2 + ct) * 128
                    ys = yctr[0] % 3
                    yctr[0] += 1
                    for hf in range(2):
                        yb = 4 + hf

                        def mmd(e, c0=c0, hf=hf, yb=yb):
                            for k in range(8):
                                ins = e.matmul(bank[yb], actT[ps_][:, k, c0:c0 + 128], wd_[:, k, hf * 512:(hf + 1) * 512], start=(k == 0), stop=(k == 7))
                            return ins
                        S.op("pe", mmd, [f"actT{ps_}", f"we{ws}_2"], [PSN[yb]])
                        if hf == 0:
                            S.op("act", lambda e, ys=ys, yb=yb, ct=ct, row=row: e.activation(out=y_sb[ys][:, 0:512], in_=bank[yb], func=ACT.Identity, scale=gT[:, ct, row:row + 1]),
                                 [PSN[yb], "gT"], [f"y{ys}a"])
                        else:
                            S.op("dve", lambda e, ys=ys, yb=yb, ct=ct, row=row: e.tensor_scalar(y_sb[ys][:, 512:1024], bank[yb], gT[:, ct, row:row + 1], None, op0=ALU.mult),
                                 [PSN[yb], "gT"], [f"y{ys}b"])
                    S.dma("pool", lambda e, ys=ys, bb=bb, ct=ct, row=row: e.indirect_dma_start(
                        out=ff_d[bb], out_offset=bass.IndirectOffsetOnAxis(ap=idxT[:, ct, row:row + 1], axis=0),
                        in_=y_sb[ys], in_offset=None, compute_op=ALU.add),
                        [f"y{ys}a", f"y{ys}b", "idxT"], [f"ff{bb}"], f"d_y{ys}")

        for e_ in range(NE):
            if e_ + 1 < NE:
                load_expert(e_ + 1)
            for pr in range(NP):
                do_pair(e_, pr)
        S.barrier()

        for b in range(NB):
            bc_load(G2, mod_d[b:b + 1, 5 * D:6 * D], "G2", "d_bc_G2")
            bc_load(gtmp3, gains[3:4, :], "gtmp3", "d_bc_gtmp3")
            S.op("dve", lambda e: e.tensor_tensor(G2, G2, gtmp3, op=ALU.mult), ["G2", "gtmp3"], ["G2"])
            for tt in range(16):
                s = tt % 2
                S.dma("sp", lambda e, b=b, tt=tt, s=s: e.dma_start(out=ffl[s], in_=ff_d[b][tt * 128:(tt + 1) * 128, :]), [f"ff{b}"], [f"ffl{s}"], f"d_ffl{s}")
                S.dma("sp", lambda e, b=b, tt=tt, s=s: e.dma_start(out=hl[s], in_=h_d[b, tt * 128:(tt + 1) * 128, :]), [f"h_d{b}_{tt}"], [f"hl{s}"], f"d_hl{s}")
                rs, rsn = rstd_of(ffl[s], D, [f"ffl{s}"], junk2, 1.0 / D)
                S.op("dve", lambda e, s=s, rs=rs: e.scalar_tensor_tensor(tF, ffl[s], rs, G2, op0=ALU.mult, op1=ALU.mult), [f"ffl{s}", rsn, "G2"], ["tF"])
                S.op("dve", lambda e, s=s: e.tensor_tensor(ob_t[s], tF, hl[s], op=ALU.add), ["tF", f"hl{s}"], [f"ob{s}"])
                S.dma("sp", lambda e, b=b, tt=tt, s=s: e.dma_start(out=out[b, tt * 128:(tt + 1) * 128, :], in_=ob_t[s]), [f"ob{s}"], ["out"], f"d_out{s}")
        S.barrier(["sp"])
        S.emit()
    return nc


def _rope_tables():
    t = np.arange(T)
    row = (t // 64).astype(np.float32)
    col = (t % 64).astype(np.float32)
    inv = (10000.0 ** (-np.arange(0, 32, 2, dtype=np.float32) / 32)).astype(np.float32)
    ang_r = row[:, None] * inv[None, :]
    ang_c = col[:, None] * inv[None, :]
    ang = np.concatenate([ang_r, ang_r, ang_c, ang_c], axis=-1)
    cos = np.cos(ang).astype(np.float32).T
    sin = np.sin(ang).astype(np.float32).T
    sgn = np.concatenate([-np.ones(16), np.ones(16), -np.ones(16), np.ones(16)]).astype(np.float32)[:, None]
    sin = sin * sgn
    cosT = np.ascontiguousarray(np.concatenate([cos, cos], 0))
    sinT = np.ascontiguousarray(np.concatenate([sin, sin], 0))
    perm = np.zeros((128, 128), np.float32)
    for i in range(128):
        perm[i ^ 16, i] = 1.0
    return cosT, sinT, perm


def make_in_maps(inputs, NB=4, ncores=NCORES, ne=NE):
    f = lambda a: np.ascontiguousarray(np.asarray(a, dtype=np.float32))
    x = f(inputs["x"]); c = f(inputs["c"]); ctx = f(inputs["ctx"]); c_ctx = f(inputs["c_ctx"])
    w_in = f(inputs["w_in"])[0]
    cosT, sinT, perm = _rope_tables()
    w_in_r = np.ascontiguousarray(w_in.reshape(8, 128, 24, 128).transpose(2, 1, 0, 3))
    w_v_r = np.ascontiguousarray(w_in[:, 1024:1536].reshape(8, 128, 512).transpose(1, 0, 2))
    shared = dict(
        w_ada=f(inputs["w_ada"])[0], b_ada=f(inputs["b_ada"]),
        gains=np.ascontiguousarray(np.concatenate([f(inputs["norm_pre_mix"]), f(inputs["norm_post_mix"]),
                                                   f(inputs["norm_pre_ffn"]), f(inputs["norm_post_ffn"])], 0)),
        w_in_r=w_in_r, w_v_r=w_v_r,
        convw=np.ascontiguousarray(f(inputs["conv_w"])[0].T.reshape(4, 128, 3).transpose(1, 0, 2)),
        lqk=np.ascontiguousarray(np.concatenate([f(inputs["lambda_q1"]), f(inputs["lambda_k1"]),
                                                 f(inputs["lambda_q2"]), f(inputs["lambda_k2"])], 1)),
        subln=np.ascontiguousarray(f(inputs["subln_g"]).reshape(128, 1)),
        w_out=f(inputs["w_out"])[0],
        w_router=np.ascontiguousarray(f(inputs["w_router"])[0].reshape(8, 128, 16).transpose(1, 0, 2)),
        w_gate=f(inputs["w_gate"])[0][:ne], w_up=f(inputs["w_up"])[0][:ne], w_down=f(inputs["w_down"])[0][:ne],
        ident=np.eye(128, dtype=np.float32), perm=perm, cosT=cosT, sinT=sinT,
    )
    maps = []
    for i in range(ncores):
        sl = slice(i * NB, (i + 1) * NB)
        cc = np.concatenate([c[sl], c_ctx[None, :]], 0)
        if cc.shape[0] < 5:
            cc = np.concatenate([cc[:-1], np.zeros((5 - cc.shape[0], D), np.float32), cc[-1:]], 0)
        ccT = np.ascontiguousarray(cc.T.reshape(8, 128, 5).transpose(1, 0, 2))
        m = dict(shared)
        m.update(x=np.ascontiguousarray(x[sl]), ctx=np.ascontiguousarray(ctx[sl]), ccT=ccT)
        maps.append(m)
    return maps


def kernel(**inputs):
    NB = 4
    nc = build(NB=NB, stage="full")
    maps = make_in_maps(inputs, NB=NB, ncores=NCORES)
    res = run_bass_kernel_spmd(nc, maps, core_ids=list(range(NCORES)))
    return np.concatenate([np.asarray(r["out"]) for r in res.results], axis=0).astype(np.float32)
```

```python
import math
from contextlib import ExitStack
import numpy as np
import concourse.bass as bass
import concourse.mybir as mybir
from concourse.bass_utils import run_bass_kernel_spmd

F32 = mybir.dt.float32
BF16 = mybir.dt.bfloat16
U32 = mybir.dt.uint32
ACT = mybir.ActivationFunctionType
ALU = mybir.AluOpType
AX = mybir.AxisListType

T = 2048
C = 256
D = 1024
TK = T + C
NE = 16
CAP = 256
EPS = 1e-6
NCORES = 8


class Sched:
    ENGS = ("pe", "act", "dve", "pool", "sp")

    def __init__(self, nc, stack):
        self.nc = nc
        self.stack = stack
        self.sems = {}
        self.eng = {}
        for n in self.ENGS:
            self.sems["s_" + n] = stack.enter_context(nc.semaphore("s_" + n))
            self.eng[n] = dict(ops=[], cnt=0, waited={})
        self.lastw = {}
        self.readers = {}
        self.dcum = {}

    def _deps(self, reads, writes):
        d = []
        for r in reads:
            if r in self.lastw:
                d.append(self.lastw[r])
        for w in writes:
            if w in self.lastw:
                d.append(self.lastw[w])
            d.extend(self.readers.get(w, ()))
        return d

    def _waits(self, en, deps):
        E = self.eng[en]
        need = {}
        for (s, v) in deps:
            if en == "pe" and s == "s_pe":
                continue
            if E["waited"].get(s, 0) >= v:
                continue
            if need.get(s, 0) < v:
                need[s] = v
        for s, v in need.items():
            E["waited"][s] = v
        return list(need.items())

    def _record(self, ev, reads, writes):
        for r in reads:
            self.readers.setdefault(r, []).append(ev)
        for w in writes:
            self.lastw[w] = ev
            self.readers[w] = []

    def op(self, en, fn, reads=(), writes=()):
        excl = [r for r in reads if r.startswith("ps") and r not in writes]
        if excl:
            reads = [r for r in reads if r not in excl]
            writes = list(writes) + excl
        deps = self._deps(reads, writes)
        waits = self._waits(en, deps)
        E = self.eng[en]
        E["cnt"] += 1
        ev = ("s_" + en, E["cnt"])
        self._record(ev, reads, writes)
        sems = self.sems

        def run(e, fn=fn, waits=waits, sem=sems["s_" + en]):
            for (s, v) in waits:
                e.wait_ge(sems[s], v)
            fn(e).then_inc(sem, 1)

        E["ops"].append(run)

    def dma(self, q, mk, reads, writes, key):
        deps = self._deps(reads, writes)
        waits = self._waits(q, deps)
        if key not in self.sems:
            self.sems[key] = self.stack.enter_context(self.nc.semaphore(key))
            self.dcum[key] = 0
        self.dcum[key] += 16
        ev = (key, self.dcum[key])
        self._record(ev, reads, writes)
        sems = self.sems

        def run(e, mk=mk, waits=waits, sem=sems[key]):
            for (s, v) in waits:
                e.wait_ge(sems[s], v)
            mk(e).then_inc(sem, 16)

        self.eng[q]["ops"].append(run)

    def _all_events(self):
        ev = [("s_" + n, self.eng[n]["cnt"]) for n in self.ENGS if self.eng[n]["cnt"] > 0]
        ev += [(k, v) for k, v in self.dcum.items() if v > 0]
        return ev

    def barrier(self, engines=None):
        allev = self._all_events()
        for n in (engines or self.ENGS):
            waits = self._waits(n, [ev for ev in allev if ev[0] != "s_" + n])
            sems = self.sems

            def run(e, waits=waits):
                for (s, v) in waits:
                    e.wait_ge(sems[s], v)

            self.eng[n]["ops"].append(run)

    def emit(self):
        nc = self.nc
        with nc.Block() as block:
            @block.tensor
            def _(e):
                for f in self.eng["pe"]["ops"]:
                    f(e)

            @block.scalar
            def _(e):
                for f in self.eng["act"]["ops"]:
                    f(e)

            @block.vector
            def _(e):
                for f in self.eng["dve"]["ops"]:
                    f(e)

            @block.gpsimd
            def _(e):
                for f in self.eng["pool"]["ops"]:
                    f(e)

            @block.sync
            def _(e):
                for f in self.eng["sp"]["ops"]:
                    f(e)


DT_BYTES = {F32: 4, BF16: 2, U32: 4}


class Arena:
    def __init__(self, t, nbytes):
        self.t = t
        self.nbytes = nbytes
        self.off = 0

    def reset(self):
        self.off = 0

    def take(self, free, dtype):
        n = 1
        for s in free:
            n *= s
        sz = n * DT_BYTES[dtype]
        a = self.off
        self.off += (sz + 63) // 64 * 64
        assert self.off <= self.nbytes, (self.off, self.nbytes)
        ap = self.t[:, a // 4:(a + sz) // 4]
        if dtype != F32:
            ap = ap.bitcast(dtype)
        if len(free) == 2:
            ap = ap.rearrange("p (a b) -> p a b", a=free[0])
        elif len(free) == 3:
            ap = ap.rearrange("p (a b c) -> p a b c", a=free[0], b=free[1])
        return ap


def build(NB=4, stage="full"):
    nc = bass.Bass("TRN2", target_bir_lowering=False)

    def din(n, s, d=F32):
        return nc.dram_tensor(n, s, d, kind="ExternalInput").ap()

    x = din("x", [NB, T, D])
    ctx = din("ctx", [NB, C, D])
    ccT = din("ccT", [128, 8, 5])
    w_ada = din("w_ada", [D, 6 * D])
    b_ada = din("b_ada", [1, 6 * D])
    gains = din("gains", [4, D])
    w_in_r = din("w_in_r", [24, 128, 8, 128])
    w_v_r = din("w_v_r", [128, 8, 512])
    convw = din("convw", [128, 4, 3])
    lqk = din("lqk", [1, 256])
    subln = din("subln", [128, 1])
    w_out = din("w_out", [D, D])
    w_router = din("w_router", [128, 8, 16])
    NE_decl = NE if stage == "full" else 1
    w_gate = din("w_gate", [NE_decl, D, D])
    w_up = din("w_up", [NE_decl, D, D])
    w_down = din("w_down", [NE_decl, D, D])
    ident = din("ident", [128, 128])
    perm = din("perm", [128, 128])
    cosT_d = din("cosT", [128, T])
    sinT_d = din("sinT", [128, T])
    out = nc.dram_tensor("out", [NB, T, D], F32, kind="ExternalOutput").ap()
    mod_d = nc.dram_tensor("mod_d", [5, 6 * D], F32).ap()
    h_d = nc.dram_tensor("h_d", [NB, T, D], F32).ap()
    aff_d = nc.dram_tensor("aff_d", [128, T], F32).ap()
    xn2_d = [nc.dram_tensor(f"xn2_d{b}", [T, D], BF16).ap() for b in range(NB)]
    ff_d = [nc.dram_tensor(f"ff_d{b}", [T, D], F32).ap() for b in range(NB)]

    with ExitStack() as st:
        S = Sched(nc, st)

        def sb(n, s, d):
            return st.enter_context(nc.sbuf_tensor(n, s, d))

        idf = sb("idf", [128, 128], F32)
        idb = sb("idb", [128, 128], BF16)
        permf = sb("permf", [128, 128], F32)
        permb = sb("permb", [128, 128], BF16)
        ones_b = sb("ones_b", [128, 128], BF16)
        ones_f = sb("ones_f", [128, 128], F32)
        neghalf = sb("neghalf", [128, 256], F32)
        eps_t = sb("eps_t", [128, 1], F32)
        cw = sb("cw", [128, 4, 3], F32)
        sgs = sb("sgs", [128, 1], F32)
        neglam = sb("neglam", [128, 2], F32)
        wr_sb = sb("wr_sb", [128, 8, 16], F32)
        stat = sb("stat", [128, 96], F32)
        lq_sb = sb("lq_sb", [1, 256], F32)
        lq2 = sb("lq2", [1, 128], F32)
        lq3 = sb("lq3", [1, 8], F32)
        ARENA_BYTES = 200 * 1024
        arena_t = sb("arena", [128, ARENA_BYTES // 4], F32)
        AR = Arena(arena_t, ARENA_BYTES)
        psS = [st.enter_context(nc.psum_tensor(f"psS{i}", [128, 512], F32)) for i in range(8)]
        bank = [p[:] for p in psS]
        PSN = [f"ps{i}" for i in range(8)]

        stat_ctr = [0]

        def stat_slot(n=1):
            c = stat_ctr[0]
            if c + n > 96:
                c = 0
            stat_ctr[0] = c + n
            return c

        def rstd_of(src, nfree, src_reads, junk, inv_n):
            c = stat_slot(3)
            r0, r1, r2 = f"st{c}", f"st{c+1}", f"st{c+2}"
            S.op("act", lambda e: e.activation(out=junk, in_=src, func=ACT.Square, accum_out=stat[:, c:c + 1]),
                 src_reads, [r0, "junk"])
            S.op("dve", lambda e: e.tensor_scalar(stat[:, c + 1:c + 2], stat[:, c:c + 1], inv_n, EPS, op0=ALU.mult, op1=ALU.add),
                 [r0], [r1])
            S.op("pool", lambda e: e.tensor_tensor(stat[:, c + 2:c + 3], stat[:, c + 1:c + 2], neghalf[:, 0:1], op=ALU.pow),
                 [r1, "neghalf"], [r2])
            return stat[:, c + 2:c + 3], r2

        def rstd_of2(src0, src1, src_reads, junk, inv_n):
            c = stat_slot(5)
            ra, rb, r0, r1, r2 = (f"st{c + i}" for i in range(5))
            S.op("act", lambda e: e.activation(out=junk[:, 0:512], in_=src0, func=ACT.Square, accum_out=stat[:, c:c + 1]),
                 src_reads, [ra, "junk"])
            S.op("act", lambda e: e.activation(out=junk[:, 512:1024], in_=src1, func=ACT.Square, accum_out=stat[:, c + 1:c + 2]),
                 src_reads, [rb, "junk"])
            S.op("dve", lambda e: e.tensor_tensor(stat[:, c + 2:c + 3], stat[:, c:c + 1], stat[:, c + 1:c + 2], op=ALU.add), [ra, rb], [r0])
            S.op("dve", lambda e: e.tensor_scalar(stat[:, c + 3:c + 4], stat[:, c + 2:c + 3], inv_n, EPS, op0=ALU.mult, op1=ALU.add), [r0], [r1])
            S.op("pool", lambda e: e.tensor_tensor(stat[:, c + 4:c + 5], stat[:, c + 3:c + 4], neghalf[:, 0:1], op=ALU.pow), [r1, "neghalf"], [r2])
            return stat[:, c + 4:c + 5], r2

        S.dma("sp", lambda e: e.dma_start(out=idf[:], in_=ident), [], ["idf"], "d_c1")
        S.dma("sp", lambda e: e.dma_start(out=permf[:], in_=perm), [], ["permf"], "d_c2")
        S.dma("sp", lambda e: e.dma_start(out=cw[:], in_=convw), [], ["cw"], "d_c3")
        S.dma("sp", lambda e: e.dma_start(out=sgs[:], in_=subln), [], ["sgs"], "d_c4")
        S.dma("sp", lambda e: e.dma_start(out=wr_sb[:], in_=w_router), [], ["wr_sb"], "d_c5")
        S.dma("sp", lambda e: e.dma_start(out=lq_sb[:], in_=lqk), [], ["lq_sb"], "d_c6")
        S.op("dve", lambda e: e.tensor_copy(idb[:], idf[:]), ["idf"], ["idb"])
        S.op("dve", lambda e: e.tensor_copy(permb[:], permf[:]), ["permf"], ["permb"])
        S.op("dve", lambda e: e.memset(ones_b[:], 1.0), [], ["ones_b"])
        S.op("dve", lambda e: e.memset(ones_f[:], 1.0), [], ["ones_f"])
        S.op("dve", lambda e: e.memset(neghalf[:], -0.5), [], ["neghalf"])
        S.op("dve", lambda e: e.memset(eps_t[:], EPS), [], ["eps_t"])
        S.op("dve", lambda e: e.tensor_scalar(sgs[:], sgs[:], 0.8, None, op0=ALU.mult), ["sgs"], ["sgs"])
        lqv = lq_sb[0:1, :].rearrange("p (a b c) -> p a b c", a=2, b=2)
        S.op("dve", lambda e: e.tensor_tensor(lq2[0:1, :].rearrange("p (a c) -> p a c", a=2), lqv[:, :, 0, :], lqv[:, :, 1, :], op=ALU.mult),
             ["lq_sb"], ["lq2"])
        S.op("dve", lambda e: e.reduce_sum(lq3[0:1, 0:2], lq2[0:1, :].rearrange("p (a c) -> p a c", a=2), axis=AX.X), ["lq2"], ["lq3a"])
        S.op("act", lambda e: e.activation(out=lq3[0:1, 2:4], in_=lq3[0:1, 0:2], func=ACT.Exp), ["lq3a"], ["lq3b"])
        S.op("dve", lambda e: e.tensor_tensor(lq3[0:1, 4:5], lq3[0:1, 3:4], lq3[0:1, 2:3], op=ALU.subtract), ["lq3b"], ["lq3c"])
        S.op("dve", lambda e: e.tensor_scalar(lq3[0:1, 6:7], lq3[0:1, 4:5], -0.2, None, op0=ALU.add), ["lq3c"], ["lq3d"])
        S.op("dve", lambda e: e.tensor_copy(lq3[0:1, 7:8], lq3[0:1, 6:7]), ["lq3d"], ["lq3e"])
        S.op("pe", lambda e: e.matmul(bank[7][:, 0:2], ones_f[0:1, :], lq3[0:1, 6:8], start=True, stop=True),
             ["ones_f", "lq3d", "lq3e"], [PSN[7]])
        S.op("dve", lambda e: e.tensor_copy(neglam[:], bank[7][:, 0:2]), [PSN[7]], ["neglam"])

        AR.reset()
        ccT_sb = AR.take([8, 5], F32)
        siluT = AR.take([8, 5], F32)
        bada_sb = AR.take([6 * D], F32)
        wa = [AR.take([8, 512], F32) for _ in range(2)]
        mstage = [AR.take([512], F32) for _ in range(2)]
        zero_t = AR.take([2048], F32)
        S.dma("sp", lambda e: e.dma_start(out=ccT_sb, in_=ccT), [], ["ccT"], "d_c7")
        S.dma("sp", lambda e: e.dma_start(out=bada_sb[0:1, :], in_=b_ada), [], ["bada"], "d_c8")
        S.op("act", lambda e: e.activation(out=siluT, in_=ccT_sb, func=ACT.Silu), ["ccT"], ["siluT"])
        S.op("dve", lambda e: e.memset(zero_t, 0.0), [], ["zero_t"])
        for b in range(NB):
            ffv = ff_d[b].rearrange("(p r) d -> p (r d)", p=128)
            for j in range(8):
                S.dma("sp", lambda e, ffv=ffv, j=j: e.dma_start(out=ffv[:, j * 2048:(j + 1) * 2048], in_=zero_t),
                      ["zero_t"], [f"ff{b}"], "d_z")
        S.dma("sp", lambda e: e.dma_start(out=aff_d, in_=zero_t), ["zero_t"], ["aff_d"], "d_z")
        for j in range(12):
            s = j % 2
            S.dma("sp", lambda e, j=j, s=s: e.dma_start(out=wa[s], in_=w_ada[:, j * 512:(j + 1) * 512].rearrange("(k p) n -> p k n", p=128)),
                  [], [f"wa{s}"], f"d_wa{s}")

            def mm0(e, j=j, s=s):
                for k in range(8):
                    e.matmul(bank[4][0:5, :], siluT[:, k, :], wa[s][:, k, :], start=(k == 0), stop=False)
                return e.matmul(bank[4][0:5, :], ones_f[0:1, 0:5], bada_sb[0:1, j * 512:(j + 1) * 512], start=False, stop=True)
            S.op("pe", mm0, ["siluT", f"wa{s}", "bada", "ones_f"], [PSN[4]])
            S.op("act", lambda e, s=s: e.activation(out=mstage[s][0:5, :], in_=bank[4][0:5, :], func=ACT.Copy), [PSN[4]], [f"ms{s}"])
            S.dma("sp", lambda e, j=j, s=s: e.dma_start(out=mod_d[:, j * 512:(j + 1) * 512], in_=mstage[s][0:5, :]),
                  [f"ms{s}"], ["mod_d"], f"d_ms{s}")
        S.barrier()

        AR.reset()
        cosT = AR.take([T], F32)
        sinT = AR.take([T], F32)
        xnT = AR.take([8, TK], BF16)
        R1 = xnT.rearrange("p a b -> p (a b)")
        attnT = R1[:, 0:4 * T].rearrange("p (a b) -> p a b", a=4)
        w_out_sb = R1[:, 4 * T:4 * T + 8 * D].rearrange("p (a b) -> p a b", a=8)
        kT = AR.take([4, TK], BF16)
        v_sb = AR.take([18, 512], BF16)
        KVf = arena_t[:, 0:0]
        qc_off = AR.off
        qT = AR.take([4, T], BF16)
        AR.off = qc_off
        u_t = AR.take([2064], F32)
        gb_t = AR.take([T], F32)
        y_t = AR.take([1024], F32)
        qc_end = max(AR.off, qc_off + 4 * T * 2)
        AR.off = qc_off
        A2 = AR.take([D], F32)
        B2 = AR.take([D], F32)
        G1 = AR.take([D], F32)
        gtmp2 = AR.take([D], F32)
        AR.off = qc_end
        convT = AR.take([4, T], BF16)
        A1 = AR.take([D], F32)
        B1 = AR.take([D], F32)
        A1c = AR.take([D], F32)
        B1c = AR.take([D], F32)
        ta_off = AR.off
        XT = [AR.take([D], F32) for _ in range(2)]
        xb = [AR.take([D], BF16) for _ in range(2)]
        tmpA = AR.take([D], F32)
        junk = AR.take([D], BF16)
        ta_end = AR.off
        AR.off = ta_off
        at_t = [AR.take([512], BF16) for _ in range(3)]
        tO = AR.take([512], F32)
        o_t = [AR.take([256], F32) for _ in range(2)]
        osq = [AR.take([256], F32) for _ in range(2)]
        ln_t = [AR.take([256], F32) for _ in range(2)]
        rs_t = [AR.take([256], F32) for _ in range(2)]
        Ocp = AR.take([512], F32)
        dcp = AR.take([512], F32)
        assert AR.off <= ta_end
        AR.off = ta_end
        pb = [AR.take([512], BF16) for _ in range(2)]
        t1 = [AR.take([512], F32) for _ in range(2)]
        t2 = [AR.take([512], F32) for _ in range(2)]
        xc_sb = [AR.take([512], F32) for _ in range(2)]
        wslot = [AR.take([8, 128], BF16) for _ in range(6)]
        wv_sb = AR.take([8, 512], BF16)
        mix_end = AR.off
        kv_off = (kT.offset if hasattr(kT, "offset") else None)
        AR.off = 2 * T * 4 + 8 * TK * 2
        xt2 = [AR.take([D], F32) for _ in range(2)]
        tD = AR.take([D], F32)
        h_sb = [AR.take([D], F32) for _ in range(2)]
        t2D = AR.take([D], F32)
        xn2 = [AR.take([D], F32) for _ in range(2)]
        xn2T = AR.take([8, 128], F32)
        assert AR.off <= 2 * T * 4 + 8 * TK * 2 + 4 * TK * 2 + 18 * 512 * 2 + 64
        AR.off = mix_end
        lg_sb = AR.take([16], F32)
        ex_sb = AR.take([16], F32)
        aff_sb = [AR.take([16], F32) for _ in range(2)]
        affT_sb = [AR.take([128], F32) for _ in range(2)]
        print("mixer arena bytes", AR.off)

        S.dma("sp", lambda e: e.dma_start(out=cosT, in_=cosT_d), [], ["cosT"], "d_c9")
        S.dma("sp", lambda e: e.dma_start(out=sinT, in_=sinT_d), [], ["sinT"], "d_c10")
        S.op("dve", lambda e: e.memset(u_t, 0.0), [], ["u_t"])

        def bc_load(dst, row_ap, region, key, guard=()):
            S.dma("sp", lambda e: e.dma_start(out=dst, in_=row_ap.partition_broadcast(128)), list(guard), [region], key)

        def mod_tile(dst, region, b, seg, gain_idx, tmp, tmp_region, plus_one, guard=()):
            bc_load(dst, mod_d[b:b + 1, seg * D:(seg + 1) * D], region, "d_bc_" + region, guard)
            if gain_idx is not None:
                bc_load(tmp, gains[gain_idx:gain_idx + 1, :], tmp_region, "d_bc_" + tmp_region, guard)
                if plus_one:
                    S.op("dve", lambda e: e.scalar_tensor_tensor(dst, dst, 1.0, tmp, op0=ALU.add, op1=ALU.mult),
                         [region, tmp_region], [region])
                else:
                    S.op("dve", lambda e: e.tensor_tensor(dst, dst, tmp, op=ALU.mult), [region, tmp_region], [region])

        mod_tile(A1c, "A1c", 4, 1, 0, tmpA, "tmpA", True)
        mod_tile(B1c, "B1c", 4, 0, None, None, None, False)

        grp_ctr = [0]

        def load_group(g):
            s = grp_ctr[0] % 6
            grp_ctr[0] += 1
            S.dma("pool", lambda e: e.dma_start(out=wslot[s], in_=w_in_r[g]), [], [f"ws{s}"], f"d_ws{s}")
            return s

        pbank_ctr = [0]

        def next_pbank():
            i = pbank_ctr[0] % 4
            pbank_ctr[0] += 1
            return i

        rope_ctr = [0]

        def do_batch(b):
            mod_tile(A1, "A1", b, 1, 0, tmpA, "tmpA", True)
            mod_tile(B1, "B1", b, 0, None, None, None, False)
            S.dma("pool", lambda e: e.dma_start(out=wv_sb, in_=w_v_r), [], ["wv"], "d_wv")
            order = [("k", h, 4 + h) for h in range(4)]
            for j in range(4):
                order += [("gb", j, 12 + j), ("gc", j, 16 + j), ("xc", j, 20 + j)]
            order += [("q", h, h) for h in range(4)]
            slots = {}
            nload = [0]

            def ensure_loaded(upto):
                while nload[0] <= min(upto, len(order) - 1):
                    slots[nload[0]] = load_group(order[nload[0]][2])
                    nload[0] += 1
            ensure_loaded(4)

            for tt in range(18):
                s = tt % 2
                src = ctx[b, tt * 128:(tt + 1) * 128, :] if tt < 2 else x[b, (tt - 2) * 128:(tt - 1) * 128, :]
                Ab, Bb, An, Bn = (A1c, B1c, "A1c", "B1c") if tt < 2 else (A1, B1, "A1", "B1")
                S.dma("sp", lambda e, s=s, src=src: e.dma_start(out=XT[s], in_=src), [], [f"XT{s}"], f"d_XT{s}")
                rs, rsn = rstd_of(XT[s], D, [f"XT{s}"], junk, 1.0 / D)
                S.op("dve", lambda e, s=s, rs=rs, Ab=Ab: e.scalar_tensor_tensor(tmpA, XT[s], rs, Ab, op0=ALU.mult, op1=ALU.mult),
                     [f"XT{s}", rsn, An], ["tmpA"])
                S.op("dve", lambda e, s=s, Bb=Bb: e.tensor_tensor(xb[s], tmpA, Bb, op=ALU.add), ["tmpA", Bn], [f"xb{s}"])
                pbk = 6 + (tt % 2)
                pv = bank[pbk].bitcast(BF16)

                def tpA(e, s=s, pv=pv):
                    for k in range(8):
                        ins = e.transpose(pv[:, k * 128:(k + 1) * 128], xb[s][:, k * 128:(k + 1) * 128], idb[:])
                    return ins
                S.op("pe", tpA, [f"xb{s}", "idb"], [PSN[pbk]])
                S.op("act", lambda e, tt=tt, pv=pv: e.activation(out=xnT[:, :, tt * 128:(tt + 1) * 128],
                                                                   in_=pv.rearrange("p (k t) -> p k t", k=8), func=ACT.Copy),
                     [PSN[pbk]], [f"xn{tt}"])

            if stage == "A":
                return
            pending = []

            def flush(keep=0):
                while len(pending) > keep:
                    pending.pop(0)()

            def proj(slot, tok0, ntok):
                pbk = next_pbank()
                tiles = sorted(set(range(tok0 // 128, (tok0 + ntok + 127) // 128)))

                def mm(e):
                    for k in range(8):
                        ins = e.matmul(bank[pbk][:, 0:ntok], wslot[slot][:, k, :], xnT[:, k, tok0:tok0 + ntok], start=(k == 0), stop=(k == 7))
                    return ins
                S.op("pe", mm, [f"ws{slot}"] + [f"xn{t}" for t in tiles], [PSN[pbk]])
                return pbk

            def rope(pbk, dst, dst_region, tb, extra_reads=()):
                i = rope_ctr[0] % 2
                rope_ctr[0] += 1
                qb_ = 4 + i
                S.op("act", lambda e: e.activation(out=pb[i], in_=bank[pbk], func=ACT.Copy), [PSN[pbk]], [f"pb{i}"])
                S.op("dve", lambda e: e.tensor_tensor(t2[i], bank[pbk], cosT[:, tb * 512:(tb + 1) * 512], op=ALU.mult),
                     [PSN[pbk], "cosT"], [f"t2{i}"])

                def part2():
                    S.op("pe", lambda e: e.matmul(bank[qb_], permb[:], pb[i], start=True, stop=True), ["permb", f"pb{i}"], [PSN[qb_]])
                    S.op("dve", lambda e: e.tensor_tensor(t1[i], bank[qb_], sinT[:, tb * 512:(tb + 1) * 512], op=ALU.mult),
                         [PSN[qb_], "sinT"], [f"t1{i}"])
                    S.op("dve", lambda e: e.tensor_tensor(dst, t1[i], t2[i], op=ALU.add),
                         [f"t1{i}", f"t2{i}"] + list(extra_reads), [dst_region])
                pending.append(part2)

            gi = 0
            for h in range(4):
                ensure_loaded(gi + 4)
                sl = slots[gi]
                gi += 1
                pbk = proj(sl, 0, C)
                flush(0)
                S.op("act", lambda e, h=h, pbk=pbk: e.activation(out=kT[:, h, 0:C], in_=bank[pbk][:, 0:C], func=ACT.Copy),
                     [PSN[pbk]], [f"kT{h}"])
                if stage == "B0a":
                    return
                for tb in range(4):
                    pbk = proj(sl, C + tb * 512, 512)
                    flush(0)
                    if stage == "B0c":
                        S.op("act", lambda e, h=h, pbk=pbk, tb=tb: e.activation(out=kT[:, h, C + tb * 512:C + (tb + 1) * 512], in_=bank[pbk], func=ACT.Copy),
                             [PSN[pbk]], [f"kT{h}"])
                        return
                    if stage == "B0d":
                        S.op("dve", lambda e, pbk=pbk, tb=tb: e.tensor_tensor(t2[0], bank[pbk], cosT[:, tb * 512:(tb + 1) * 512], op=ALU.mult),
                             [PSN[pbk], "cosT"], ["t20"])
                        return
                    rope(pbk, kT[:, h, C + tb * 512:C + (tb + 1) * 512], f"kT{h}", tb)
                    if stage == "B0b":
                        flush(0)
                        return
            flush(0)
            if stage == "B1":
                return
            for tt in range(18):
                pbk = next_pbank()

                def mmv(e, tt=tt, pbk=pbk):
                    for k in range(8):
                        ins = e.matmul(bank[pbk], xnT[:, k, tt * 128:(tt + 1) * 128], wv_sb[:, k, :], start=(k == 0), stop=(k == 7))
                    return ins
                S.op("pe", mmv, ["wv", f"xn{tt}"], [PSN[pbk]])
                S.op("act", lambda e, tt=tt, pbk=pbk: e.activation(out=v_sb[:, tt, :], in_=bank[pbk], func=ACT.Copy), [PSN[pbk]], ["v_sb"])
            if stage == "B2":
                return
            S.op("dve", lambda e: e.memset(u_t[:, 0:1], 0.0), ["qdead", "u_t"], ["u_t"])
            S.op("dve", lambda e: e.memset(u_t[:, 2049:2050], 0.0), ["qdead", "u_t"], ["u_t"])
            for j in range(4):
                ensure_loaded(gi + 5)
                sgb, sgc, sxc = slots[gi], slots[gi + 1], slots[gi + 2]
                gi += 3
                for tb in range(4):
                    i = (j * 4 + tb) % 2
                    p_xc = proj(sxc, C + tb * 512, 512)
                    p_gc = proj(sgc, C + tb * 512, 512)
                    p_gb = proj(sgb, C + tb * 512, 512)
                    S.op("act", lambda e, i=i, p_xc=p_xc: e.activation(out=xc_sb[i], in_=bank[p_xc], func=ACT.Copy), [PSN[p_xc]], [f"xc{i}"])
                    S.op("dve", lambda e, i=i, p_gc=p_gc, tb=tb: e.tensor_tensor(u_t[:, 1 + tb * 512:1 + (tb + 1) * 512], bank[p_gc], xc_sb[i], op=ALU.mult),
                         [PSN[p_gc], f"xc{i}", "qdead"], ["u_t"])
                    S.op("act", lambda e, p_gb=p_gb, tb=tb: e.activation(out=gb_t[:, tb * 512:(tb + 1) * 512], in_=bank[p_gb], func=ACT.Copy),
                         [PSN[p_gb], "qdead"], ["gb_t"])
                for hf in range(2):
                    o0 = hf * 1024
                    S.op("act", lambda e, j=j, o0=o0: e.activation(out=y_t, in_=u_t[:, 1 + o0:1 + o0 + 1024], func=ACT.Identity, scale=cw[:, j, 1:2]),
                         ["u_t", "cw", "qdead"], ["y_t"])
                    S.op("dve", lambda e, j=j, o0=o0: e.scalar_tensor_tensor(y_t, u_t[:, o0:o0 + 1024], cw[:, j, 0:1], y_t, op0=ALU.mult, op1=ALU.add),
                         ["u_t", "cw", "y_t"], ["y_t"])
                    S.op("dve", lambda e, j=j, o0=o0: e.scalar_tensor_tensor(y_t, u_t[:, 2 + o0:2 + o0 + 1024], cw[:, j, 2:3], y_t, op0=ALU.mult, op1=ALU.add),
                         ["u_t", "cw", "y_t"], ["y_t"])
                    S.op("dve", lambda e, j=j, o0=o0: e.tensor_tensor(convT[:, j, o0:o0 + 1024], y_t, gb_t[:, o0:o0 + 1024], op=ALU.mult),
                         ["y_t", "gb_t"], ["convT", "convdead"])
            if stage == "B3":
                return
            for h in range(4):
                ensure_loaded(gi + 4)
                sl = slots[gi]
                gi += 1
                for tb in range(4):
                    pbk = proj(sl, C + tb * 512, 512)
                    flush(0)
                    rope(pbk, qT[:, h, tb * 512:(tb + 1) * 512], f"qT{h}", tb, extra_reads=("convdead",))
            flush(0)
            S.op("pe", lambda e: e.matmul(bank[7][:, 0:2], ones_f[0:1, :], lq3[0:1, 6:8], start=True, stop=True),
                 [f"xn{t}" for t in range(18)] + ["ones_f"], [PSN[7], "xndead"])
            for k in range(8):
                S.dma("pool", lambda e, k=k: e.dma_start(out=w_out_sb[:, k, :], in_=w_out[k * 128:(k + 1) * 128, :]),
                      ["xndead"], ["w_out_sb"], "d_wout")

            if stage == "B":
                return
            steps = [(h, qb, kt) for h in range(4) for qb in range(8) for kt in range(18)]
            deferred = {}

            def qk(si):
                h, qb, kt = steps[si]
                sb0 = (si % 2) * 2

                def mm(e):
                    for m in range(2):
                        ins = e.matmul(bank[sb0 + m][:, 0:256], kT[64 * m:64 * (m + 1), h, kt * 128:(kt + 1) * 128],
                                       qT[64 * m:64 * (m + 1), h, qb * 256:(qb + 1) * 256], start=True, stop=True)
                    return ins
                S.op("pe", mm, [f"kT{h}", f"qT{h}"], [PSN[sb0], PSN[sb0 + 1]])

            def fin1(h, qb, fi):
                ob = 4
                db = 5
                i = fi % 2
                S.op("act", lambda e: e.activation(out=dcp, in_=bank[db], func=ACT.Copy), [PSN[db], "xndead"], ["dcp"])
                S.op("act", lambda e: e.activation(out=Ocp, in_=bank[ob], func=ACT.Copy), [PSN[ob], "xndead"], ["Ocp"])
                S.op("dve", lambda e: e.reciprocal(dcp, dcp), ["dcp", "xndead"], ["dcp"])
                S.op("dve", lambda e: e.tensor_tensor(tO, Ocp, dcp, op=ALU.mult), ["Ocp", "dcp"], ["tO"])
                S.op("dve", lambda e: e.scalar_tensor_tensor(o_t[i], tO[:, 256:512], neglam[:, 0:1], tO[:, 0:256], op0=ALU.mult, op1=ALU.add),
                     ["tO", "neglam"], [f"o{i}"])
                S.op("dve", lambda e: e.tensor_tensor(osq[i], o_t[i], o_t[i], op=ALU.mult), [f"o{i}"], [f"osq{i}"])

            def fin2(h, qb, fi):
                i = fi % 2
                S.op("pe", lambda e: e.matmul(bank[6][:, 0:256], ones_f[:], osq[i], start=True, stop=True), ["ones_f", f"osq{i}"], [PSN[6]])
                S.op("act", lambda e: e.activation(out=ln_t[i], in_=bank[6][:, 0:256], func=ACT.Ln, scale=1.0 / 128, bias=eps_t[:, 0:1]),
                     [PSN[6], "eps_t"], [f"ln{i}"])
                S.op("act", lambda e: e.activation(out=rs_t[i], in_=ln_t[i], func=ACT.Exp, scale=-0.5), [f"ln{i}"], [f"rs{i}"])
                S.op("dve", lambda e: e.scalar_tensor_tensor(attnT[:, h, qb * 256:(qb + 1) * 256], o_t[i], sgs[:, 0:1], rs_t[i], op0=ALU.mult, op1=ALU.mult),
                     [f"o{i}", f"rs{i}", "sgs", "xndead"], ["attnT"])

            qk(0)
            fi = 0
            for si, (h, qb, kt) in enumerate(steps):
                if si + 1 < len(steps):
                    qk(si + 1)
                sb0 = (si % 2) * 2
                ai = si % 3
                for m in range(2):
                    S.op("act", lambda e, sb0=sb0, ai=ai, m=m: e.activation(out=at_t[ai][:, m * 256:(m + 1) * 256], in_=bank[sb0 + m][:, 0:256], func=ACT.Exp, scale=0.125),
                         [PSN[sb0 + m], "xndead"], [f"at{ai}"])
                ob = 4
                db = 5

                def av(e, h=h, kt=kt, ai=ai, ob=ob, db=db):
                    e.matmul(bank[ob], v_sb[:, kt, h * 128:(h + 1) * 128], at_t[ai], start=(kt == 0), stop=(kt == 17))
                    return e.matmul(bank[db], ones_b[:], at_t[ai], start=(kt == 0), stop=(kt == 17))
                S.op("pe", av, ["v_sb", f"at{ai}", "ones_b"], [PSN[ob], PSN[db]])
                if si in deferred:
                    deferred.pop(si)()
                if kt == 17:
                    fin1(h, qb, fi)
                    deferred[min(si + 4, len(steps) - 1) if si + 4 < len(steps) else -1] = (lambda h=h, qb=qb, fi=fi: fin2(h, qb, fi))
                    fi += 1
            for k_ in sorted(deferred):
                deferred[k_]()
            S.op("pe", lambda e: e.matmul(bank[7][:, 0:2], ones_f[0:1, :], lq3[0:1, 6:8], start=True, stop=True),
                 ["ones_f", "v_sb"] + [f"kT{h}" for h in range(4)] + [f"qT{h}" for h in range(4)], [PSN[7], "kvdead", "qdead"])

            if stage == "C":
                return
            mod_tile(G1, "G1", b, 2, 1, gtmp2, "gtmp2", False, guard=("qdead",))
            mod_tile(A2, "A2", b, 4, 2, gtmp2, "gtmp2", True, guard=("qdead",))
            mod_tile(B2, "B2", b, 3, None, None, None, False, guard=("qdead",))
            mixin = [attnT[:, h, :] for h in range(4)] + [convT[:, j, :] for j in range(4)]

            def d1(tt):
                s = tt % 2
                S.dma("sp", lambda e: e.dma_start(out=xt2[s], in_=x[b, tt * 128:(tt + 1) * 128, :]), ["kvdead"], [f"xt2{s}"], f"d_xt2{s}")
                for hf in range(2):
                    def mm(e, hf=hf):
                        for k in range(8):
                            ins = e.matmul(bank[hf], mixin[k][:, tt * 128:(tt + 1) * 128], w_out_sb[:, k, hf * 512:(hf + 1) * 512], start=(k == 0), stop=(k == 7))
                        return ins
                    S.op("pe", mm, ["attnT", "convT", "w_out_sb"], [PSN[hf]])
                rs, rsn = rstd_of2(bank[0], bank[1], [PSN[0], PSN[1]], junk, 1.0 / D)
                for hf in range(2):
                    S.op("dve", lambda e, hf=hf: e.scalar_tensor_tensor(tD[:, hf * 512:(hf + 1) * 512], bank[hf], rs, G1[:, hf * 512:(hf + 1) * 512], op0=ALU.mult, op1=ALU.mult),
                         [PSN[hf], rsn, "G1", "kvdead"], ["tD"])
                S.op("dve", lambda e: e.tensor_tensor(h_sb[s], tD, xt2[s], op=ALU.add), ["tD", f"xt2{s}", "kvdead"], [f"h{s}"])
                S.dma("sp", lambda e: e.dma_start(out=h_d[b, tt * 128:(tt + 1) * 128, :], in_=h_sb[s]), [f"h{s}"], [f"h_d{b}_{tt}"], f"d_h{s}")
                rs2, rsn2 = rstd_of(h_sb[s], D, [f"h{s}"], junk, 1.0 / D)
                S.op("dve", lambda e: e.scalar_tensor_tensor(t2D, h_sb[s], rs2, A2, op0=ALU.mult, op1=ALU.mult),
                     [f"h{s}", rsn2, "A2", "kvdead"], ["t2D"])
                S.op("dve", lambda e: e.tensor_tensor(xn2[s], t2D, B2, op=ALU.add), ["t2D", "B2"], [f"xn2{s}"])
                S.dma("pool", lambda e: e.dma_start(out=xn2_d[b][tt * 128:(tt + 1) * 128, :], in_=xn2[s]), [f"xn2{s}"], [f"xn2d{b}"], f"d_x2{s}")

            def d2(tt):
                s = tt % 2

                def tp(e):
                    for k in range(8):
                        ins = e.transpose(bank[2 + k // 4][:, (k % 4) * 128:(k % 4 + 1) * 128], xn2[s][:, k * 128:(k + 1) * 128], idf[:])
                    return ins
                S.op("pe", tp, [f"xn2{s}", "idf"], [PSN[2], PSN[3]])
                for hb in range(2):
                    S.op("act", lambda e, hb=hb: e.activation(out=xn2T[:, hb * 4:(hb + 1) * 4, :], in_=bank[2 + hb].rearrange("p (k t) -> p k t", k=4), func=ACT.Copy),
                         [PSN[2 + hb], "kvdead"], ["xn2T"])

            def d3(tt):
                s = tt % 2

                def mm(e):
                    for k in range(8):
                        ins = e.matmul(bank[4][:, 0:16], xn2T[:, k, :], wr_sb[:, k, :], start=(k == 0), stop=(k == 7))
                    return ins
                S.op("pe", mm, ["xn2T", "wr_sb"], [PSN[4]])
                c = stat_slot(4)
                S.op("dve", lambda e: e.reduce_max(stat[:, c:c + 1], bank[4][:, 0:16], axis=AX.X), [PSN[4]], [f"st{c}"])
                S.op("dve", lambda e: e.tensor_scalar(stat[:, c + 1:c + 2], stat[:, c:c + 1], -1.0, None, op0=ALU.mult), [f"st{c}"], [f"st{c+1}"])
                S.op("act", lambda e: e.activation(out=ex_sb, in_=bank[4][:, 0:16], func=ACT.Exp, bias=stat[:, c + 1:c + 2], accum_out=stat[:, c + 2:c + 3]),
                     [PSN[4], f"st{c+1}"], ["ex_sb", f"st{c+2}"])
                S.op("dve", lambda e: e.reciprocal(stat[:, c + 3:c + 4], stat[:, c + 2:c + 3]), [f"st{c+2}"], [f"st{c+3}"])
                S.op("dve", lambda e: e.tensor_scalar(aff_sb[s], ex_sb, stat[:, c + 3:c + 4], None, op0=ALU.mult), ["ex_sb", f"st{c+3}"], [f"aff{s}"])

            def d4(tt):
                s = tt % 2
                S.op("pe", lambda e: e.transpose(bank[5][0:16, 0:128], aff_sb[s], idf[:]), [f"aff{s}", "idf"], [PSN[5]])
                S.op("act", lambda e: e.activation(out=affT_sb[s][0:16, :], in_=bank[5][0:16, 0:128], func=ACT.Copy), [PSN[5]], [f"affT{s}"])
                S.dma("sp", lambda e: e.dma_start(out=aff_d[32 * b:32 * b + 16, tt * 128:(tt + 1) * 128], in_=affT_sb[s][0:16, :]),
                      [f"affT{s}"], ["aff_d"], f"d_af{s}")

            for i in range(16 + 3):
                if 0 <= i - 3 < 16:
                    d4(i - 3)
                if 0 <= i - 2 < 16:
                    d3(i - 2)
                if 0 <= i - 1 < 16:
                    d2(i - 1)
                if i < 16:
                    d1(i)
            S.barrier()

        for b_ in range(NB if stage != "phase0" else 0):
            do_batch(b_)
        if stage in ("A", "B", "C", "B1", "B2", "B3", "B0a", "B0b", "B0c", "B0d"):
            S.barrier()

        if stage in ("mixer", "phase0", "A", "B", "C", "B1", "B2", "B3", "B0a", "B0b", "B0c", "B0d"):
            AR.reset()
            cp = [AR.take([D], F32) for _ in range(2)]
            for b in range(NB):
                for tt in range(16):
                    s = tt % 2
                    S.dma("sp", lambda e, b=b, tt=tt, s=s: e.dma_start(out=cp[s], in_=h_d[b, tt * 128:(tt + 1) * 128, :]), [], [f"cp{s}"], f"d_cp{s}")
                    S.dma("sp", lambda e, b=b, tt=tt, s=s: e.dma_start(out=out[b, tt * 128:(tt + 1) * 128, :], in_=cp[s]), [f"cp{s}"], ["out"], f"d_co{s}")
            S.barrier(["sp"])
            S.emit()
            return nc

        AR.reset()
        wexp = [[AR.take([8, D], BF16) for _ in range(3)] for _ in range(2)]
        xs = [AR.take([D], BF16) for _ in range(4)]
        xsT = [AR.take([8, 512], BF16) for _ in range(2)]
        actT = [AR.take([8, 512], BF16) for _ in range(2)]
        sgt = [AR.take([512], F32) for _ in range(2)]
        y_sb = [AR.take([D], F32) for _ in range(3)]
        moe_end = AR.off
        work = AR.take([T], F32)
        vals = AR.take([CAP], F32)
        idx = AR.take([CAP], U32)
        idxf = AR.take([CAP], F32)
        idxT = AR.take([2, 128], U32)
        gT = AR.take([2, 128], F32)
        rt_end = AR.off
        AR.off = 0
        ffl = [AR.take([D], F32) for _ in range(2)]
        hl = [AR.take([D], F32) for _ in range(2)]
        G2 = AR.take([D], F32)
        gtmp3 = AR.take([D], F32)
        tF = AR.take([D], F32)
        ob_t = [AR.take([D], F32) for _ in range(2)]
        junk2 = AR.take([D], BF16)
        AR.off = rt_end
        print("moe arena bytes", AR.off)

        def load_expert(e_):
            s = e_ % 2
            for wi, wsrc in enumerate((w_gate, w_up, w_down)):
                for k in range(8):
                    S.dma("pool", lambda e, s=s, wi=wi, wsrc=wsrc, k=k: e.dma_start(out=wexp[s][wi][:, k, :], in_=wsrc[e_, k * 128:(k + 1) * 128, :]),
                          [], [f"we{s}_{wi}"], f"d_we{s}_{wi}")

        load_expert(0)
        S.dma("sp", lambda e: e.dma_start(out=work, in_=aff_d), ["aff_d"], ["work"], "d_work")
        for r in range(CAP // 8):
            S.op("dve", lambda e, r=r: e.max(out=vals[:, r * 8:(r + 1) * 8], in_=work), ["work"], ["vals"])
            S.op("dve", lambda e, r=r: e.max_index(out=idx[:, r * 8:(r + 1) * 8], in_max=vals[:, r * 8:(r + 1) * 8], in_values=work), ["work", "vals"], ["idx"])
            S.op("dve", lambda e, r=r: e.match_replace(out=work, in_to_replace=vals[:, r * 8:(r + 1) * 8], in_values=work, imm_value=-1.0), ["vals", "idx"], ["work"])
        S.op("dve", lambda e: e.tensor_copy(idxf, idx), ["idx"], ["idxf"])
        for ct in range(2):
            S.op("pe", lambda e, ct=ct: e.transpose(bank[0][:, 0:128], idxf[:, ct * 128:(ct + 1) * 128], idf[:]), ["idxf", "idf"], [PSN[0]])
            S.op("dve", lambda e, ct=ct: e.tensor_copy(idxT[:, ct, :], bank[0][:, 0:128]), [PSN[0]], ["idxT"])
            S.op("pe", lambda e, ct=ct: e.transpose(bank[1][:, 0:128], vals[:, ct * 128:(ct + 1) * 128], idf[:]), ["vals", "idf"], [PSN[1]])
            S.op("dve", lambda e, ct=ct: e.tensor_copy(gT[:, ct, :], bank[1][:, 0:128]), [PSN[1]], ["gT"])

        gctr = [0]
        yctr = [0]
        NP = (NB + 1) // 2

        def do_pair(e_, pr):
            ws = e_ % 2
            wg_, wu_, wd_ = wexp[ws]
            bl = [bb for bb in (2 * pr, 2 * pr + 1) if bb < NB]
            ncol = len(bl) * 256
            ps_ = (e_ * NP + pr) % 2
            for bi, bb in enumerate(bl):
                row = 32 * bb + e_
                for ct in range(2):
                    gs = gctr[0] % 4
                    gctr[0] += 1
                    S.dma("pool", lambda e, gs=gs, bb=bb, ct=ct, row=row: e.indirect_dma_start(
                        out=xs[gs], out_offset=None, in_=xn2_d[bb],
                        in_offset=bass.IndirectOffsetOnAxis(ap=idxT[:, ct, row:row + 1], axis=0)),
                        [f"xn2d{bb}", "idxT"], [f"xs{gs}"], f"d_xs{gs}")
                    pbk = 6 + gs % 2
                    pv = bank[pbk].bitcast(BF16)

                    def tpx(e, gs=gs, pv=pv):
                        for k in range(8):
                            ins = e.transpose(pv[:, k * 128:(k + 1) * 128], xs[gs][:, k * 128:(k + 1) * 128], idb[:])
                        return ins
                    S.op("pe", tpx, [f"xs{gs}", "idb"], [PSN[pbk]])
                    c0 = (bi * 2 + ct) * 128
                    S.op("act", lambda e, ps_=ps_, c0=c0, pv=pv: e.activation(out=xsT[ps_][:, :, c0:c0 + 128], in_=pv.rearrange("p (k t) -> p k t", k=8), func=ACT.Copy),
                         [PSN[pbk]], [f"xsT{ps_}"])
            for fc in range(8):
                bg = 0 + 2 * (fc % 2)
                bu = 1 + 2 * (fc % 2)
                si_ = fc % 2

                def mmg(e, fc=fc, bg=bg):
                    for k in range(8):
                        ins = e.matmul(bank[bg][:, 0:ncol], wg_[:, k, fc * 128:(fc + 1) * 128], xsT[ps_][:, k, 0:ncol], start=(k == 0), stop=(k == 7))
                    return ins

                def mmu(e, fc=fc, bu=bu):
                    for k in range(8):
                        ins = e.matmul(bank[bu][:, 0:ncol], wu_[:, k, fc * 128:(fc + 1) * 128], xsT[ps_][:, k, 0:ncol], start=(k == 0), stop=(k == 7))
                    return ins
                S.op("pe", mmg, [f"we{ws}_0", f"xsT{ps_}"], [PSN[bg]])
                S.op("pe", mmu, [f"we{ws}_1", f"xsT{ps_}"], [PSN[bu]])
                S.op("act", lambda e, bg=bg, si_=si_: e.activation(out=sgt[si_][:, 0:ncol], in_=bank[bg][:, 0:ncol], func=ACT.Silu), [PSN[bg]], [f"sgt{si_}"])
                S.op("dve", lambda e, fc=fc, bu=bu, si_=si_: e.tensor_tensor(actT[ps_][:, fc, 0:ncol], sgt[si_][:, 0:ncol], bank[bu][:, 0:ncol], op=ALU.mult),
                     [PSN[bu], f"sgt{si_}"], [f"actT{ps_}"])
            for bi, bb in enumerate(bl):
                row = 32 * bb + e_
                for ct in range(2):
                    c0 = (bi * 2 + ct) * 128
                    ys = yctr[0] % 3
                    yctr[0] += 1
                    for hf in range(2):
                        yb = 4 + hf

                        def mmd(e, c0=c0, hf=hf, yb=yb):
                            for k in range(8):
                                ins = e.matmul(bank[yb], actT[ps_][:, k, c0:c0 + 128], wd_[:, k, hf * 512:(hf + 1) * 512], start=(k == 0), stop=(k == 7))
                            return ins
                        S.op("pe", mmd, [f"actT{ps_}", f"we{ws}_2"], [PSN[yb]])
                        if hf == 0:
                            S.op("act", lambda e, ys=ys, yb=yb, ct=ct, row=row: e.activation(out=y_sb[ys][:, 0:512], in_=bank[yb], func=ACT.Identity, scale=gT[:, ct, row:row + 1]),
                                 [PSN[yb], "gT"], [f"y{ys}a"])
                        else:
                            S.op("dve", lambda e, ys=ys, yb=yb, ct=ct, row=row: e.tensor_scalar(y_sb[ys][:, 512:1024], bank[yb], gT[:, ct, row:row + 1], None, op0=ALU.mult),
                                 [PSN[yb], "gT"], [f"y{ys}b"])
                    S.dma("pool", lambda e, ys=ys, bb=bb, ct=ct, row=row: e.indirect_dma_start(
                        out=ff_d[bb], out_offset=bass.IndirectOffsetOnAxis(ap=idxT[:, ct, row:row + 1], axis=0),
                        in_=y_sb[ys], in_offset=None, compute_op=ALU.add),
                        [f"y{ys}a", f"y{ys}b", "idxT"], [f"ff{bb}"], f"d_y{ys}")

        for e_ in range(NE):
            if e_ + 1 < NE:
                load_expert(e_ + 1)
            for pr in range(NP):
                do_pair(e_, pr)
        S.barrier()

        for b in range(NB):
            bc_load(G2, mod_d[b:b + 1, 5 * D:6 * D], "G2", "d_bc_G2")
            bc_load(gtmp3, gains[3:4, :], "gtmp3", "d_bc_gtmp3")
            S.op("dve", lambda e: e.tensor_tensor(G2, G2, gtmp3, op=ALU.mult), ["G2", "gtmp3"], ["G2"])
            for tt in range(16):
                s = tt % 2
                S.dma("sp", lambda e, b=b, tt=tt, s=s: e.dma_start(out=ffl[s], in_=ff_d[b][tt * 128:(tt + 1) * 128, :]), [f"ff{b}"], [f"ffl{s}"], f"d_ffl{s}")
                S.dma("sp", lambda e, b=b, tt=tt, s=s: e.dma_start(out=hl[s], in_=h_d[b, tt * 128:(tt + 1) * 128, :]), [f"h_d{b}_{tt}"], [f"hl{s}"], f"d_hl{s}")
                rs, rsn = rstd_of(ffl[s], D, [f"ffl{s}"], junk2, 1.0 / D)
                S.op("dve", lambda e, s=s, rs=rs: e.scalar_tensor_tensor(tF, ffl[s], rs, G2, op0=ALU.mult, op1=ALU.mult), [f"ffl{s}", rsn, "G2"], ["tF"])
                S.op("dve", lambda e, s=s: e.tensor_tensor(ob_t[s], tF, hl[s], op=ALU.add), ["tF", f"hl{s}"], [f"ob{s}"])
                S.dma("sp", lambda e, b=b, tt=tt, s=s: e.dma_start(out=out[b, tt * 128:(tt + 1) * 128, :], in_=ob_t[s]), [f"ob{s}"], ["out"], f"d_out{s}")
        S.barrier(["sp"])
        S.emit()
    return nc


def _rope_tables():
    t = np.arange(T)
    row = (t // 64).astype(np.float32)
    col = (t % 64).astype(np.float32)
    inv = (10000.0 ** (-np.arange(0, 32, 2, dtype=np.float32) / 32)).astype(np.float32)
    ang_r = row[:, None] * inv[None, :]
    ang_c = col[:, None] * inv[None, :]
    ang = np.concatenate([ang_r, ang_r, ang_c, ang_c], axis=-1)
    cos = np.cos(ang).astype(np.float32).T
    sin = np.sin(ang).astype(np.float32).T
    sgn = np.concatenate([-np.ones(16), np.ones(16), -np.ones(16), np.ones(16)]).astype(np.float32)[:, None]
    sin = sin * sgn
    cosT = np.ascontiguousarray(np.concatenate([cos, cos], 0))
    sinT = np.ascontiguousarray(np.concatenate([sin, sin], 0))
    perm = np.zeros((128, 128), np.float32)
    for i in range(128):
        perm[i ^ 16, i] = 1.0
    return cosT, sinT, perm


def make_in_maps(inputs, NB=4, ncores=NCORES, ne=NE):
    f = lambda a: np.ascontiguousarray(np.asarray(a, dtype=np.float32))
    x = f(inputs["x"]); c = f(inputs["c"]); ctx = f(inputs["ctx"]); c_ctx = f(inputs["c_ctx"])
    w_in = f(inputs["w_in"])[0]
    cosT, sinT, perm = _rope_tables()
    w_in_r = np.ascontiguousarray(w_in.reshape(8, 128, 24, 128).transpose(2, 1, 0, 3))
    w_v_r = np.ascontiguousarray(w_in[:, 1024:1536].reshape(8, 128, 512).transpose(1, 0, 2))
    shared = dict(
        w_ada=f(inputs["w_ada"])[0], b_ada=f(inputs["b_ada"]),
        gains=np.ascontiguousarray(np.concatenate([f(inputs["norm_pre_mix"]), f(inputs["norm_post_mix"]),
                                                   f(inputs["norm_pre_ffn"]), f(inputs["norm_post_ffn"])], 0)),
        w_in_r=w_in_r, w_v_r=w_v_r,
        convw=np.ascontiguousarray(f(inputs["conv_w"])[0].T.reshape(4, 128, 3).transpose(1, 0, 2)),
        lqk=np.ascontiguousarray(np.concatenate([f(inputs["lambda_q1"]), f(inputs["lambda_k1"]),
                                                 f(inputs["lambda_q2"]), f(inputs["lambda_k2"])], 1)),
        subln=np.ascontiguousarray(f(inputs["subln_g"]).reshape(128, 1)),
        w_out=f(inputs["w_out"])[0],
        w_router=np.ascontiguousarray(f(inputs["w_router"])[0].reshape(8, 128, 16).transpose(1, 0, 2)),
        w_gate=f(inputs["w_gate"])[0][:ne], w_up=f(inputs["w_up"])[0][:ne], w_down=f(inputs["w_down"])[0][:ne],
        ident=np.eye(128, dtype=np.float32), perm=perm, cosT=cosT, sinT=sinT,
    )
    maps = []
    for i in range(ncores):
        sl = slice(i * NB, (i + 1) * NB)
        cc = np.concatenate([c[sl], c_ctx[None, :]], 0)
        if cc.shape[0] < 5:
            cc = np.concatenate([cc[:-1], np.zeros((5 - cc.shape[0], D), np.float32), cc[-1:]], 0)
        ccT = np.ascontiguousarray(cc.T.reshape(8, 128, 5).transpose(1, 0, 2))
        m = dict(shared)
        m.update(x=np.ascontiguousarray(x[sl]), ctx=np.ascontiguousarray(ctx[sl]), ccT=ccT)
        maps.append(m)
    return maps


def kernel(**inputs):
    NB = 4
    nc = build(NB=NB, stage="full")
    maps = make_in_maps(inputs, NB=NB, ncores=NCORES)
    res = run_bass_kernel_spmd(nc, maps, core_ids=list(range(NCORES)))
    return np.concatenate([np.asarray(r["out"]) for r in res.results], axis=0).astype(np.float32)
```

```python
import math
from contextlib import ExitStack
import numpy as np
import concourse.bass as bass
import concourse.mybir as mybir
from concourse.bass_utils import run_bass_kernel_spmd

F32 = mybir.dt.float32
BF16 = mybir.dt.bfloat16
U32 = mybir.dt.uint32
ACT = mybir.ActivationFunctionType
ALU = mybir.AluOpType
AX = mybir.AxisListType

T = 2048
C = 256
D = 1024
TK = T + C
NE = 16
CAP = 256
EPS = 1e-6
NCORES = 8


class Sched:
    ENGS = ("pe", "act", "dve", "pool", "sp")

    def __init__(self, nc, stack):
        self.nc = nc
        self.stack = stack
        self.sems = {}
        self.eng = {}
        for n in self.ENGS:
            self.sems["s_" + n] = stack.enter_context(nc.semaphore("s_" + n))
            self.eng[n] = dict(ops=[], cnt=0, waited={})
        self.lastw = {}
        self.readers = {}
        self.dcum = {}

    def _deps(self, reads, writes):
        d = []
        for r in reads:
            if r in self.lastw:
                d.append(self.lastw[r])
        for w in writes:
            if w in self.lastw:
                d.append(self.lastw[w])
            d.extend(self.readers.get(w, ()))
        return d

    def _waits(self, en, deps):
        E = self.eng[en]
        need = {}
        for (s, v) in deps:
            if en == "pe" and s == "s_pe":
                continue
            if E["waited"].get(s, 0) >= v:
                continue
            if need.get(s, 0) < v:
                need[s] = v
        for s, v in need.items():
            E["waited"][s] = v
        return list(need.items())

    def _record(self, ev, reads, writes):
        for r in reads:
            self.readers.setdefault(r, []).append(ev)
        for w in writes:
            self.lastw[w] = ev
            self.readers[w] = []

    def op(self, en, fn, reads=(), writes=()):
        excl = [r for r in reads if r.startswith("ps") and r not in writes]
        if excl:
            reads = [r for r in reads if r not in excl]
            writes = list(writes) + excl
        deps = self._deps(reads, writes)
        waits = self._waits(en, deps)
        E = self.eng[en]
        E["cnt"] += 1
        ev = ("s_" + en, E["cnt"])
        self._record(ev, reads, writes)
        sems = self.sems

        def run(e, fn=fn, waits=waits, sem=sems["s_" + en]):
            for (s, v) in waits:
                e.wait_ge(sems[s], v)
            fn(e).then_inc(sem, 1)

        E["ops"].append(run)

    def dma(self, q, mk, reads, writes, key):
        deps = self._deps(reads, writes)
        waits = self._waits(q, deps)
        if key not in self.sems:
            self.sems[key] = self.stack.enter_context(self.nc.semaphore(key))
            self.dcum[key] = 0
        self.dcum[key] += 16
        ev = (key, self.dcum[key])
        self._record(ev, reads, writes)
        sems = self.sems

        def run(e, mk=mk, waits=waits, sem=sems[key]):
            for (s, v) in waits:
                e.wait_ge(sems[s], v)
            mk(e).then_inc(sem, 16)

        self.eng[q]["ops"].append(run)

    def _all_events(self):
        ev = [("s_" + n, self.eng[n]["cnt"]) for n in self.ENGS if self.eng[n]["cnt"] > 0]
        ev += [(k, v) for k, v in self.dcum.items() if v > 0]
        return ev

    def barrier(self, engines=None):
        allev = self._all_events()
        for n in (engines or self.ENGS):
            waits = self._waits(n, [ev for ev in allev if ev[0] != "s_" + n])
            sems = self.sems

            def run(e, waits=waits):
                for (s, v) in waits:
                    e.wait_ge(sems[s], v)

            self.eng[n]["ops"].append(run)

    def emit(self):
        nc = self.nc
        with nc.Block() as block:
            @block.tensor
            def _(e):
                for f in self.eng["pe"]["ops"]:
                    f(e)

            @block.scalar
            def _(e):
                for f in self.eng["act"]["ops"]:
                    f(e)

            @block.vector
            def _(e):
                for f in self.eng["dve"]["ops"]:
                    f(e)

            @block.gpsimd
            def _(e):
                for f in self.eng["pool"]["ops"]:
                    f(e)

            @block.sync
            def _(e):
                for f in self.eng["sp"]["ops"]:
                    f(e)


DT_BYTES = {F32: 4, BF16: 2, U32: 4}


class Arena:
    def __init__(self, t, nbytes):
        self.t = t
        self.nbytes = nbytes
        self.off = 0

    def reset(self):
        self.off = 0

    def take(self, free, dtype):
        n = 1
        for s in free:
            n *= s
        sz = n * DT_BYTES[dtype]
        a = self.off
        self.off += (sz + 63) // 64 * 64
        assert self.off <= self.nbytes, (self.off, self.nbytes)
        ap = self.t[:, a // 4:(a + sz) // 4]
        if dtype != F32:
            ap = ap.bitcast(dtype)
        if len(free) == 2:
            ap = ap.rearrange("p (a b) -> p a b", a=free[0])
        elif len(free) == 3:
            ap = ap.rearrange("p (a b c) -> p a b c", a=free[0], b=free[1])
        return ap


def build(NB=4, stage="full"):
    nc = bass.Bass("TRN2", target_bir_lowering=False)

    def din(n, s, d=F32):
        return nc.dram_tensor(n, s, d, kind="ExternalInput").ap()

    x = din("x", [NB, T, D])
    ctx = din("ctx", [NB, C, D])
    ccT = din("ccT", [128, 8, 5])
    w_ada = din("w_ada", [D, 6 * D])
    b_ada = din("b_ada", [1, 6 * D])
    gains = din("gains", [4, D])
    w_in_r = din("w_in_r", [24, 128, 8, 128])
    w_v_r = din("w_v_r", [128, 8, 512])
    convw = din("convw", [128, 4, 3])
    lqk = din("lqk", [1, 256])
    subln = din("subln", [128, 1])
    w_out = din("w_out", [D, D])
    w_router = din("w_router", [128, 8, 16])
    NE_decl = NE if stage == "full" else 1
    w_gate = din("w_gate", [NE_decl, D, D])
    w_up = din("w_up", [NE_decl, D, D])
    w_down = din("w_down", [NE_decl, D, D])
    ident = din("ident", [128, 128])
    perm = din("perm", [128, 128])
    cosT_d = din("cosT", [128, T])
    sinT_d = din("sinT", [128, T])
    out = nc.dram_tensor("out", [NB, T, D], F32, kind="ExternalOutput").ap()
    mod_d = nc.dram_tensor("mod_d", [5, 6 * D], F32).ap()
    h_d = nc.dram_tensor("h_d", [NB, T, D], F32).ap()
    aff_d = nc.dram_tensor("aff_d", [128, T], F32).ap()
    xn2_d = [nc.dram_tensor(f"xn2_d{b}", [T, D], BF16).ap() for b in range(NB)]
    ff_d = [nc.dram_tensor(f"ff_d{b}", [T, D], F32).ap() for b in range(NB)]

    with ExitStack() as st:
        S = Sched(nc, st)

        def sb(n, s, d):
            return st.enter_context(nc.sbuf_tensor(n, s, d))

        idf = sb("idf", [128, 128], F32)
        idb = sb("idb", [128, 128], BF16)
        permf = sb("permf", [128, 128], F32)
        permb = sb("permb", [128, 128], BF16)
        ones_b = sb("ones_b", [128, 128], BF16)
        ones_f = sb("ones_f", [128, 128], F32)
        neghalf = sb("neghalf", [128, 256], F32)
        eps_t = sb("eps_t", [128, 1], F32)
        cw = sb("cw", [128, 4, 3], F32)
        sgs = sb("sgs", [128, 1], F32)
        neglam = sb("neglam", [128, 2], F32)
        wr_sb = sb("wr_sb", [128, 8, 16], F32)
        stat = sb("stat", [128, 96], F32)
        lq_sb = sb("lq_sb", [1, 256], F32)
        lq2 = sb("lq2", [1, 128], F32)
        lq3 = sb("lq3", [1, 8], F32)
        ARENA_BYTES = 200 * 1024
        arena_t = sb("arena", [128, ARENA_BYTES // 4], F32)
        AR = Arena(arena_t, ARENA_BYTES)
        psS = [st.enter_context(nc.psum_tensor(f"psS{i}", [128, 512], F32)) for i in range(8)]
        bank = [p[:] for p in psS]
        PSN = [f"ps{i}" for i in range(8)]

        stat_ctr = [0]

        def stat_slot(n=1):
            c = stat_ctr[0]
            if c + n > 96:
                c = 0
            stat_ctr[0] = c + n
            return c

        def rstd_of(src, nfree, src_reads, junk, inv_n):
            c = stat_slot(3)
            r0, r1, r2 = f"st{c}", f"st{c+1}", f"st{c+2}"
            S.op("act", lambda e: e.activation(out=junk, in_=src, func=ACT.Square, accum_out=stat[:, c:c + 1]),
                 src_reads, [r0, "junk"])
            S.op("dve", lambda e: e.tensor_scalar(stat[:, c + 1:c + 2], stat[:, c:c + 1], inv_n, EPS, op0=ALU.mult, op1=ALU.add),
                 [r0], [r1])
            S.op("pool", lambda e: e.tensor_tensor(stat[:, c + 2:c + 3], stat[:, c + 1:c + 2], neghalf[:, 0:1], op=ALU.pow),
                 [r1, "neghalf"], [r2])
            return stat[:, c + 2:c + 3], r2

        def rstd_of2(src0, src1, src_reads, junk, inv_n):
            c = stat_slot(5)
            ra, rb, r0, r1, r2 = (f"st{c + i}" for i in range(5))
            S.op("act", lambda e: e.activation(out=junk[:, 0:512], in_=src0, func=ACT.Square, accum_out=stat[:, c:c + 1]),
                 src_reads, [ra, "junk"])
            S.op("act", lambda e: e.activation(out=junk[:, 512:1024], in_=src1, func=ACT.Square, accum_out=stat[:, c + 1:c + 2]),
                 src_reads, [rb, "junk"])
            S.op("dve", lambda e: e.tensor_tensor(stat[:, c + 2:c + 3], stat[:, c:c + 1], stat[:, c + 1:c + 2], op=ALU.add), [ra, rb], [r0])
            S.op("dve", lambda e: e.tensor_scalar(stat[:, c + 3:c + 4], stat[:, c + 2:c + 3], inv_n, EPS, op0=ALU.mult, op1=ALU.add), [r0], [r1])
            S.op("pool", lambda e: e.tensor_tensor(stat[:, c + 4:c + 5], stat[:, c + 3:c + 4], neghalf[:, 0:1], op=ALU.pow), [r1, "neghalf"], [r2])
            return stat[:, c + 4:c + 5], r2

        S.dma("sp", lambda e: e.dma_start(out=idf[:], in_=ident), [], ["idf"], "d_c1")
        S.dma("sp", lambda e: e.dma_start(out=permf[:], in_=perm), [], ["permf"], "d_c2")
        S.dma("sp", lambda e: e.dma_start(out=cw[:], in_=convw), [], ["cw"], "d_c3")
        S.dma("sp", lambda e: e.dma_start(out=sgs[:], in_=subln), [], ["sgs"], "d_c4")
        S.dma("sp", lambda e: e.dma_start(out=wr_sb[:], in_=w_router), [], ["wr_sb"], "d_c5")
        S.dma("sp", lambda e: e.dma_start(out=lq_sb[:], in_=lqk), [], ["lq_sb"], "d_c6")
        S.op("dve", lambda e: e.tensor_copy(idb[:], idf[:]), ["idf"], ["idb"])
        S.op("dve", lambda e: e.tensor_copy(permb[:], permf[:]), ["permf"], ["permb"])
        S.op("dve", lambda e: e.memset(ones_b[:], 1.0), [], ["ones_b"])
        S.op("dve", lambda e: e.memset(ones_f[:], 1.0), [], ["ones_f"])
        S.op("dve", lambda e: e.memset(neghalf[:], -0.5), [], ["neghalf"])
        S.op("dve", lambda e: e.memset(eps_t[:], EPS), [], ["eps_t"])
        S.op("dve", lambda e: e.tensor_scalar(sgs[:], sgs[:], 0.8, None, op0=ALU.mult), ["sgs"], ["sgs"])
        lqv = lq_sb[0:1, :].rearrange("p (a b c) -> p a b c", a=2, b=2)
        S.op("dve", lambda e: e.tensor_tensor(lq2[0:1, :].rearrange("p (a c) -> p a c", a=2), lqv[:, :, 0, :], lqv[:, :, 1, :], op=ALU.mult),
             ["lq_sb"], ["lq2"])
        S.op("dve", lambda e: e.reduce_sum(lq3[0:1, 0:2], lq2[0:1, :].rearrange("p (a c) -> p a c", a=2), axis=AX.X), ["lq2"], ["lq3a"])
        S.op("act", lambda e: e.activation(out=lq3[0:1, 2:4], in_=lq3[0:1, 0:2], func=ACT.Exp), ["lq3a"], ["lq3b"])
        S.op("dve", lambda e: e.tensor_tensor(lq3[0:1, 4:5], lq3[0:1, 3:4], lq3[0:1, 2:3], op=ALU.subtract), ["lq3b"], ["lq3c"])
        S.op("dve", lambda e: e.tensor_scalar(lq3[0:1, 6:7], lq3[0:1, 4:5], -0.2, None, op0=ALU.add), ["lq3c"], ["lq3d"])
        S.op("dve", lambda e: e.tensor_copy(lq3[0:1, 7:8], lq3[0:1, 6:7]), ["lq3d"], ["lq3e"])
        S.op("pe", lambda e: e.matmul(bank[7][:, 0:2], ones_f[0:1, :], lq3[0:1, 6:8], start=True, stop=True),
             ["ones_f", "lq3d", "lq3e"], [PSN[7]])
        S.op("dve", lambda e: e.tensor_copy(neglam[:], bank[7][:, 0:2]), [PSN[7]], ["neglam"])

        AR.reset()
        ccT_sb = AR.take([8, 5], F32)
        siluT = AR.take([8, 5], F32)
        bada_sb = AR.take([6 * D], F32)
        wa = [AR.take([8, 512], F32) for _ in range(2)]
        mstage = [AR.take([512], F32) for _ in range(2)]
        zero_t = AR.take([2048], F32)
        S.dma("sp", lambda e: e.dma_start(out=ccT_sb, in_=ccT), [], ["ccT"], "d_c7")
        S.dma("sp", lambda e: e.dma_start(out=bada_sb[0:1, :], in_=b_ada), [], ["bada"], "d_c8")
        S.op("act", lambda e: e.activation(out=siluT, in_=ccT_sb, func=ACT.Silu), ["ccT"], ["siluT"])
        S.op("dve", lambda e: e.memset(zero_t, 0.0), [], ["zero_t"])
        for b in range(NB):
            ffv = ff_d[b].rearrange("(p r) d -> p (r d)", p=128)
            for j in range(8):
                S.dma("sp", lambda e, ffv=ffv, j=j: e.dma_start(out=ffv[:, j * 2048:(j + 1) * 2048], in_=zero_t),
                      ["zero_t"], [f"ff{b}"], "d_z")
        S.dma("sp", lambda e: e.dma_start(out=aff_d, in_=zero_t), ["zero_t"], ["aff_d"], "d_z")
        for j in range(12):
            s = j % 2
            S.dma("sp", lambda e, j=j, s=s: e.dma_start(out=wa[s], in_=w_ada[:, j * 512:(j + 1) * 512].rearrange("(k p) n -> p k n", p=128)),
                  [], [f"wa{s}"], f"d_wa{s}")

            def mm0(e, j=j, s=s):
                for k in range(8):
                    e.matmul(bank[4][0:5, :], siluT[:, k, :], wa[s][:, k, :], start=(k == 0), stop=False)
                return e.matmul(bank[4][0:5, :], ones_f[0:1, 0:5], bada_sb[0:1, j * 512:(j + 1) * 512], start=False, stop=True)
            S.op("pe", mm0, ["siluT", f"wa{s}", "bada", "ones_f"], [PSN[4]])
            S.op("act", lambda e, s=s: e.activation(out=mstage[s][0:5, :], in_=bank[4][0:5, :], func=ACT.Copy), [PSN[4]], [f"ms{s}"])
            S.dma("sp", lambda e, j=j, s=s: e.dma_start(out=mod_d[:, j * 512:(j + 1) * 512], in_=mstage[s][0:5, :]),
                  [f"ms{s}"], ["mod_d"], f"d_ms{s}")
        S.barrier()

        AR.reset()
        cosT = AR.take([T], F32)
        sinT = AR.take([T], F32)
        xnT = AR.take([8, TK], BF16)
        R1 = xnT.rearrange("p a b -> p (a b)")
        attnT = R1[:, 0:4 * T].rearrange("p (a b) -> p a b", a=4)
        w_out_sb = R1[:, 4 * T:4 * T + 8 * D].rearrange("p (a b) -> p a b", a=8)
        kT = AR.take([4, TK], BF16)
        v_sb = AR.take([18, 512], BF16)
        KVf = arena_t[:, 0:0]
        qc_off = AR.off
        qT = AR.take([4, T], BF16)
        AR.off = qc_off
        u_t = AR.take([2064], F32)
        gb_t = AR.take([T], F32)
        y_t = AR.take([1024], F32)
        qc_end = max(AR.off, qc_off + 4 * T * 2)
        AR.off = qc_off
        A2 = AR.take([D], F32)
        B2 = AR.take([D], F32)
        G1 = AR.take([D], F32)
        gtmp2 = AR.take([D], F32)
        AR.off = qc_end
        convT = AR.take([4, T], BF16)
        A1 = AR.take([D], F32)
        B1 = AR.take([D], F32)
        A1c = AR.take([D], F32)
        B1c = AR.take([D], F32)
        ta_off = AR.off
        XT = [AR.take([D], F32) for _ in range(2)]
        xb = [AR.take([D], BF16) for _ in range(2)]
        tmpA = AR.take([D], F32)
        junk = AR.take([D], BF16)
        ta_end = AR.off
        AR.off = ta_off
        at_t = [AR.take([512], BF16) for _ in range(3)]
        tO = AR.take([512], F32)
        o_t = [AR.take([256], F32) for _ in range(2)]
        osq = [AR.take([256], F32) for _ in range(2)]
        ln_t = [AR.take([256], F32) for _ in range(2)]
        rs_t = [AR.take([256], F32) for _ in range(2)]
        Ocp = AR.take([512], F32)
        dcp = AR.take([512], F32)
        assert AR.off <= ta_end
        AR.off = ta_end
        pb = [AR.take([512], BF16) for _ in range(2)]
        t1 = [AR.take([512], F32) for _ in range(2)]
        t2 = [AR.take([512], F32) for _ in range(2)]
        xc_sb = [AR.take([512], F32) for _ in range(2)]
        wslot = [AR.take([8, 128], BF16) for _ in range(6)]
        wv_sb = AR.take([8, 512], BF16)
        mix_end = AR.off
        kv_off = (kT.offset if hasattr(kT, "offset") else None)
        AR.off = 2 * T * 4 + 8 * TK * 2
        xt2 = [AR.take([D], F32) for _ in range(2)]
        tD = AR.take([D], F32)
        h_sb = [AR.take([D], F32) for _ in range(2)]
        t2D = AR.take([D], F32)
        xn2 = [AR.take([D], F32) for _ in range(2)]
        xn2T = AR.take([8, 128], F32)
        assert AR.off <= 2 * T * 4 + 8 * TK * 2 + 4 * TK * 2 + 18 * 512 * 2 + 64
        AR.off = mix_end
        lg_sb = AR.take([16], F32)
        ex_sb = AR.take([16], F32)
        aff_sb = [AR.take([16], F32) for _ in range(2)]
        affT_sb = [AR.take([128], F32) for _ in range(2)]
        print("mixer arena bytes", AR.off)

        S.dma("sp", lambda e: e.dma_start(out=cosT, in_=cosT_d), [], ["cosT"], "d_c9")
        S.dma("sp", lambda e: e.dma_start(out=sinT, in_=sinT_d), [], ["sinT"], "d_c10")
        S.op("dve", lambda e: e.memset(u_t, 0.0), [], ["u_t"])

        def bc_load(dst, row_ap, region, key, guard=()):
            S.dma("sp", lambda e: e.dma_start(out=dst, in_=row_ap.partition_broadcast(128)), list(guard), [region], key)

        def mod_tile(dst, region, b, seg, gain_idx, tmp, tmp_region, plus_one, guard=()):
            bc_load(dst, mod_d[b:b + 1, seg * D:(seg + 1) * D], region, "d_bc_" + region, guard)
            if gain_idx is not None:
                bc_load(tmp, gains[gain_idx:gain_idx + 1, :], tmp_region, "d_bc_" + tmp_region, guard)
                if plus_one:
                    S.op("dve", lambda e: e.scalar_tensor_tensor(dst, dst, 1.0, tmp, op0=ALU.add, op1=ALU.mult),
                         [region, tmp_region], [region])
                else:
                    S.op("dve", lambda e: e.tensor_tensor(dst, dst, tmp, op=ALU.mult), [region, tmp_region], [region])

        mod_tile(A1c, "A1c", 4, 1, 0, tmpA, "tmpA", True)
        mod_tile(B1c, "B1c", 4, 0, None, None, None, False)

        grp_ctr = [0]

        def load_group(g):
            s = grp_ctr[0] % 6
            grp_ctr[0] += 1
            S.dma("pool", lambda e: e.dma_start(out=wslot[s], in_=w_in_r[g]), [], [f"ws{s}"], f"d_ws{s}")
            return s

        pbank_ctr = [0]

        def next_pbank():
            i = pbank_ctr[0] % 4
            pbank_ctr[0] += 1
            return i

        rope_ctr = [0]

        def do_batch(b):
            mod_tile(A1, "A1", b, 1, 0, tmpA, "tmpA", True)
            mod_tile(B1, "B1", b, 0, None, None, None, False)
            S.dma("pool", lambda e: e.dma_start(out=wv_sb, in_=w_v_r), [], ["wv"], "d_wv")
            order = [("k", h, 4 + h) for h in range(4)]
            for j in range(4):
                order += [("gb", j, 12 + j), ("gc", j, 16 + j), ("xc", j, 20 + j)]
            order += [("q", h, h) for h in range(4)]
            slots = {}
            nload = [0]

            def ensure_loaded(upto):
                while nload[0] <= min(upto, len(order) - 1):
                    slots[nload[0]] = load_group(order[nload[0]][2])
                    nload[0] += 1
            ensure_loaded(4)

            for tt in range(18):
                s = tt % 2
                src = ctx[b, tt * 128:(tt + 1) * 128, :] if tt < 2 else x[b, (tt - 2) * 128:(tt - 1) * 128, :]
                Ab, Bb, An, Bn = (A1c, B1c, "A1c", "B1c") if tt < 2 else (A1, B1, "A1", "B1")
                S.dma("sp", lambda e, s=s, src=src: e.dma_start(out=XT[s], in_=src), [], [f"XT{s}"], f"d_XT{s}")
                rs, rsn = rstd_of(XT[s], D, [f"XT{s}"], junk, 1.0 / D)
                S.op("dve", lambda e, s=s, rs=rs, Ab=Ab: e.scalar_tensor_tensor(tmpA, XT[s], rs, Ab, op0=ALU.mult, op1=ALU.mult),
                     [f"XT{s}", rsn, An], ["tmpA"])
                S.op("dve", lambda e, s=s, Bb=Bb: e.tensor_tensor(xb[s], tmpA, Bb, op=ALU.add), ["tmpA", Bn], [f"xb{s}"])
                pbk = 6 + (tt % 2)
                pv = bank[pbk].bitcast(BF16)

                def tpA(e, s=s, pv=pv):
                    for k in range(8):
                        ins = e.transpose(pv[:, k * 128:(k + 1) * 128], xb[s][:, k * 128:(k + 1) * 128], idb[:])
                    return ins
                S.op("pe", tpA, [f"xb{s}", "idb"], [PSN[pbk]])
                S.op("act", lambda e, tt=tt, pv=pv: e.activation(out=xnT[:, :, tt * 128:(tt + 1) * 128],
                                                                   in_=pv.rearrange("p (k t) -> p k t", k=8), func=ACT.Copy),
                     [PSN[pbk]], [f"xn{tt}"])

            if stage == "A":
                return
            pending = []

            def flush(keep=0):
                while len(pending) > keep:
                    pending.pop(0)()

            def proj(slot, tok0, ntok):
                pbk = next_pbank()
                tiles = sorted(set(range(tok0 // 128, (tok0 + ntok + 127) // 128)))

                def mm(e):
                    for k in range(8):
                        ins = e.matmul(bank[pbk][:, 0:ntok], wslot[slot][:, k, :], xnT[:, k, tok0:tok0 + ntok], start=(k == 0), stop=(k == 7))
                    return ins
                S.op("pe", mm, [f"ws{slot}"] + [f"xn{t}" for t in tiles], [PSN[pbk]])
                return pbk

            def rope(pbk, dst, dst_region, tb, extra_reads=()):
                i = rope_ctr[0] % 2
                rope_ctr[0] += 1
                qb_ = 4 + i
                S.op("act", lambda e: e.activation(out=pb[i], in_=bank[pbk], func=ACT.Copy), [PSN[pbk]], [f"pb{i}"])
                S.op("dve", lambda e: e.tensor_tensor(t2[i], bank[pbk], cosT[:, tb * 512:(tb + 1) * 512], op=ALU.mult),
                     [PSN[pbk], "cosT"], [f"t2{i}"])

                def part2():
                    S.op("pe", lambda e: e.matmul(bank[qb_], permb[:], pb[i], start=True, stop=True), ["permb", f"pb{i}"], [PSN[qb_]])
                    S.op("dve", lambda e: e.tensor_tensor(t1[i], bank[qb_], sinT[:, tb * 512:(tb + 1) * 512], op=ALU.mult),
                         [PSN[qb_], "sinT"], [f"t1{i}"])
                    S.op("dve", lambda e: e.tensor_tensor(dst, t1[i], t2[i], op=ALU.add),
                         [f"t1{i}", f"t2{i}"] + list(extra_reads), [dst_region])
                pending.append(part2)

            gi = 0
            for h in range(4):
                ensure_loaded(gi + 4)
                sl = slots[gi]
                gi += 1
                pbk = proj(sl, 0, C)
                flush(0)
                S.op("act", lambda e, h=h, pbk=pbk: e.activation(out=kT[:, h, 0:C], in_=bank[pbk][:, 0:C], func=ACT.Copy),
                     [PSN[pbk]], [f"kT{h}"])
                if stage == "B0a":
                    return
                for tb in range(4):
                    pbk = proj(sl, C + tb * 512, 512)
                    flush(0)
                    if stage == "B0c":
                        S.op("act", lambda e, h=h, pbk=pbk, tb=tb: e.activation(out=kT[:, h, C + tb * 512:C + (tb + 1) * 512], in_=bank[pbk], func=ACT.Copy),
                             [PSN[pbk]], [f"kT{h}"])
                        return
                    if stage == "B0d":
                        S.op("dve", lambda e, pbk=pbk, tb=tb: e.tensor_tensor(t2[0], bank[pbk], cosT[:, tb * 512:(tb + 1) * 512], op=ALU.mult),
                             [PSN[pbk], "cosT"], ["t20"])
                        return
                    rope(pbk, kT[:, h, C + tb * 512:C + (tb + 1) * 512], f"kT{h}", tb)
                    if stage == "B0b":
                        flush(0)
                        return
            flush(0)
            if stage == "B1":
                return
            for tt in range(18):
                pbk = next_pbank()

                def mmv(e, tt=tt, pbk=pbk):
                    for k in range(8):
                        ins = e.matmul(bank[pbk], xnT[:, k, tt * 128:(tt + 1) * 128], wv_sb[:, k, :], start=(k == 0), stop=(k == 7))
                    return ins
                S.op("pe", mmv, ["wv", f"xn{tt}"], [PSN[pbk]])
                S.op("act", lambda e, tt=tt, pbk=pbk: e.activation(out=v_sb[:, tt, :], in_=bank[pbk], func=ACT.Copy), [PSN[pbk]], ["v_sb"])
            if stage == "B2":
                return
            S.op("dve", lambda e: e.memset(u_t[:, 0:1], 0.0), ["qdead", "u_t"], ["u_t"])
            S.op("dve", lambda e: e.memset(u_t[:, 2049:2050], 0.0), ["qdead", "u_t"], ["u_t"])
            for j in range(4):
                ensure_loaded(gi + 5)
                sgb, sgc, sxc = slots[gi], slots[gi + 1], slots[gi + 2]
                gi += 3
                for tb in range(4):
                    i = (j * 4 + tb) % 2
                    p_xc = proj(sxc, C + tb * 512, 512)
                    p_gc = proj(sgc, C + tb * 512, 512)
                    p_gb = proj(sgb, C + tb * 512, 512)
                    S.op("act", lambda e, i=i, p_xc=p_xc: e.activation(out=xc_sb[i], in_=bank[p_xc], func=ACT.Copy), [PSN[p_xc]], [f"xc{i}"])
                    S.op("dve", lambda e, i=i, p_gc=p_gc, tb=tb: e.tensor_tensor(u_t[:, 1 + tb * 512:1 + (tb + 1) * 512], bank[p_gc], xc_sb[i], op=ALU.mult),
                         [PSN[p_gc], f"xc{i}", "qdead"], ["u_t"])
                    S.op("act", lambda e, p_gb=p_gb, tb=tb: e.activation(out=gb_t[:, tb * 512:(tb + 1) * 512], in_=bank[p_gb], func=ACT.Copy),
                         [PSN[p_gb], "qdead"], ["gb_t"])
                for hf in range(2):
                    o0 = hf * 1024
                    S.op("act", lambda e, j=j, o0=o0: e.activation(out=y_t, in_=u_t[:, 1 + o0:1 + o0 + 1024], func=ACT.Identity, scale=cw[:, j, 1:2]),
                         ["u_t", "cw", "qdead"], ["y_t"])
                    S.op("dve", lambda e, j=j, o0=o0: e.scalar_tensor_tensor(y_t, u_t[:, o0:o0 + 1024], cw[:, j, 0:1], y_t, op0=ALU.mult, op1=ALU.add),
                         ["u_t", "cw", "y_t"], ["y_t"])
                    S.op("dve", lambda e, j=j, o0=o0: e.scalar_tensor_tensor(y_t, u_t[:, 2 + o0:2 + o0 + 1024], cw[:, j, 2:3], y_t, op0=ALU.mult, op1=ALU.add),
                         ["u_t", "cw", "y_t"], ["y_t"])
                    S.op("dve", lambda e, j=j, o0=o0: e.tensor_tensor(convT[:, j, o0:o0 + 1024], y_t, gb_t[:, o0:o0 + 1024], op=ALU.mult),
                         ["y_t", "gb_t"], ["convT", "convdead"])
            if stage == "B3":
                return
            for h in range(4):
                ensure_loaded(gi + 4)
                sl = slots[gi]
                gi += 1
                for tb in range(4):
                    pbk = proj(sl, C + tb * 512, 512)
                    flush(0)
                    rope(pbk, qT[:, h, tb * 512:(tb + 1) * 512], f"qT{h}", tb, extra_reads=("convdead",))
            flush(0)
            S.op("pe", lambda e: e.matmul(bank[7][:, 0:2], ones_f[0:1, :], lq3[0:1, 6:8], start=True, stop=True),
                 [f"xn{t}" for t in range(18)] + ["ones_f"], [PSN[7], "xndead"])
            for k in range(8):
                S.dma("pool", lambda e, k=k: e.dma_start(out=w_out_sb[:, k, :], in_=w_out[k * 128:(k + 1) * 128, :]),
                      ["xndead"], ["w_out_sb"], "d_wout")

            if stage == "B":
                return
            steps = [(h, qb, kt) for h in range(4) for qb in range(8) for kt in range(18)]
            deferred = {}

            def qk(si):
                h, qb, kt = steps[si]
                sb0 = (si % 2) * 2

                def mm(e):
                    for m in range(2):
                        ins = e.matmul(bank[sb0 + m][:, 0:256], kT[64 * m:64 * (m + 1), h, kt * 128:(kt + 1) * 128],
                                       qT[64 * m:64 * (m + 1), h, qb * 256:(qb + 1) * 256], start=True, stop=True)
                    return ins
                S.op("pe", mm, [f"kT{h}", f"qT{h}"], [PSN[sb0], PSN[sb0 + 1]])

            def fin1(h, qb, fi):
                ob = 4
                db = 5
                i = fi % 2
                S.op("act", lambda e: e.activation(out=dcp, in_=bank[db], func=ACT.Copy), [PSN[db], "xndead"], ["dcp"])
                S.op("act", lambda e: e.activation(out=Ocp, in_=bank[ob], func=ACT.Copy), [PSN[ob], "xndead"], ["Ocp"])
                S.op("dve", lambda e: e.reciprocal(dcp, dcp), ["dcp", "xndead"], ["dcp"])
                S.op("dve", lambda e: e.tensor_tensor(tO, Ocp, dcp, op=ALU.mult), ["Ocp", "dcp"], ["tO"])
                S.op("dve", lambda e: e.scalar_tensor_tensor(o_t[i], tO[:, 256:512], neglam[:, 0:1], tO[:, 0:256], op0=ALU.mult, op1=ALU.add),
                     ["tO", "neglam"], [f"o{i}"])
                S.op("dve", lambda e: e.tensor_tensor(osq[i], o_t[i], o_t[i], op=ALU.mult), [f"o{i}"], [f"osq{i}"])

            def fin2(h, qb, fi):
                i = fi % 2
                S.op("pe", lambda e: e.matmul(bank[6][:, 0:256], ones_f[:], osq[i], start=True, stop=True), ["ones_f", f"osq{i}"], [PSN[6]])
                S.op("act", lambda e: e.activation(out=ln_t[i], in_=bank[6][:, 0:256], func=ACT.Ln, scale=1.0 / 128, bias=eps_t[:, 0:1]),
                     [PSN[6], "eps_t"], [f"ln{i}"])
                S.op("act", lambda e: e.activation(out=rs_t[i], in_=ln_t[i], func=ACT.Exp, scale=-0.5), [f"ln{i}"], [f"rs{i}"])
                S.op("dve", lambda e: e.scalar_tensor_tensor(attnT[:, h, qb * 256:(qb + 1) * 256], o_t[i], sgs[:, 0:1], rs_t[i], op0=ALU.mult, op1=ALU.mult),
                     [f"o{i}", f"rs{i}", "sgs", "xndead"], ["attnT"])

            qk(0)
            fi = 0
            for si, (h, qb, kt) in enumerate(steps):
                if si + 1 < len(steps):
                    qk(si + 1)
                sb0 = (si % 2) * 2
                ai = si % 3
                for m in range(2):
                    S.op("act", lambda e, sb0=sb0, ai=ai, m=m: e.activation(out=at_t[ai][:, m * 256:(m + 1) * 256], in_=bank[sb0 + m][:, 0:256], func=ACT.Exp, scale=0.125),
                         [PSN[sb0 + m], "xndead"], [f"at{ai}"])
                ob = 4
                db = 5

                def av(e, h=h, kt=kt, ai=ai, ob=ob, db=db):
                    e.matmul(bank[ob], v_sb[:, kt, h * 128:(h + 1) * 128], at_t[ai], start=(kt == 0), stop=(kt == 17))
                    return e.matmul(bank[db], ones_b[:], at_t[ai], start=(kt == 0), stop=(kt == 17))
                S.op("pe", av, ["v_sb", f"at{ai}", "ones_b"], [PSN[ob], PSN[db]])
                if si in deferred:
                    deferred.pop(si)()
                if kt == 17:
                    fin1(h, qb, fi)
                    deferred[min(si + 4, len(steps) - 1) if si + 4 < len(steps) else -1] = (lambda h=h, qb=qb, fi=fi: fin2(h, qb, fi))
                    fi += 1
            for k_ in sorted(deferred):
                deferred[k_]()
            S.op("pe", lambda e: e.matmul(bank[7][:, 0:2], ones_f[0:1, :], lq3[0:1, 6:8], start=True, stop=True),
                 ["ones_f", "v_sb"] + [f"kT{h}" for h in range(4)] + [f"qT{h}" for h in range(4)], [PSN[7], "kvdead", "qdead"])

            if stage == "C":
                return
            mod_tile(G1, "G1", b, 2, 1, gtmp2, "gtmp2", False, guard=("qdead",))
            mod_tile(A2, "A2", b, 4, 2, gtmp2, "gtmp2", True, guard=("qdead",))
            mod_tile(B2, "B2", b, 3, None, None, None, False, guard=("qdead",))
            mixin = [attnT[:, h, :] for h in range(4)] + [convT[:, j, :] for j in range(4)]

            def d1(tt):
                s = tt % 2
                S.dma("sp", lambda e: e.dma_start(out=xt2[s], in_=x[b, tt * 128:(tt + 1) * 128, :]), ["kvdead"], [f"xt2{s}"], f"d_xt2{s}")
                for hf in range(2):
                    def mm(e, hf=hf):
                        for k in range(8):
                            ins = e.matmul(bank[hf], mixin[k][:, tt * 128:(tt + 1) * 128], w_out_sb[:, k, hf * 512:(hf + 1) * 512], start=(k == 0), stop=(k == 7))
                        return ins
                    S.op("pe", mm, ["attnT", "convT", "w_out_sb"], [PSN[hf]])
                rs, rsn = rstd_of2(bank[0], bank[1], [PSN[0], PSN[1]], junk, 1.0 / D)
                for hf in range(2):
                    S.op("dve", lambda e, hf=hf: e.scalar_tensor_tensor(tD[:, hf * 512:(hf + 1) * 512], bank[hf], rs, G1[:, hf * 512:(hf + 1) * 512], op0=ALU.mult, op1=ALU.mult),
                         [PSN[hf], rsn, "G1", "kvdead"], ["tD"])
                S.op("dve", lambda e: e.tensor_tensor(h_sb[s], tD, xt2[s], op=ALU.add), ["tD", f"xt2{s}", "kvdead"], [f"h{s}"])
                S.dma("sp", lambda e: e.dma_start(out=h_d[b, tt * 128:(tt + 1) * 128, :], in_=h_sb[s]), [f"h{s}"], [f"h_d{b}_{tt}"], f"d_h{s}")
                rs2, rsn2 = rstd_of(h_sb[s], D, [f"h{s}"], junk, 1.0 / D)
                S.op("dve", lambda e: e.scalar_tensor_tensor(t2D, h_sb[s], rs2, A2, op0=ALU.mult, op1=ALU.mult),
                     [f"h{s}", rsn2, "A2", "kvdead"], ["t2D"])
                S.op("dve", lambda e: e.tensor_tensor(xn2[s], t2D, B2, op=ALU.add), ["t2D", "B2"], [f"xn2{s}"])
                S.dma("pool", lambda e: e.dma_start(out=xn2_d[b][tt * 128:(tt + 1) * 128, :], in_=xn2[s]), [f"xn2{s}"], [f"xn2d{b}"], f"d_x2{s}")

            def d2(tt):
                s = tt % 2

                def tp(e):
                    for k in range(8):
                        ins = e.transpose(bank[2 + k // 4][:, (k % 4) * 128:(k % 4 + 1) * 128], xn2[s][:, k * 128:(k + 1) * 128], idf[:])
                    return ins
                S.op("pe", tp, [f"xn2{s}", "idf"], [PSN[2], PSN[3]])
                for hb in range(2):
                    S.op("act", lambda e, hb=hb: e.activation(out=xn2T[:, hb * 4:(hb + 1) * 4, :], in_=bank[2 + hb].rearrange("p (k t) -> p k t", k=4), func=ACT.Copy),
                         [PSN[2 + hb], "kvdead"], ["xn2T"])

            def d3(tt):
                s = tt % 2

                def mm(e):
                    for k in range(8):
                        ins = e.matmul(bank[4][:, 0:16], xn2T[:, k, :], wr_sb[:, k, :], start=(k == 0), stop=(k == 7))
                    return ins
                S.op("pe", mm, ["xn2T", "wr_sb"], [PSN[4]])
                c = stat_slot(4)
                S.op("dve", lambda e: e.reduce_max(stat[:, c:c + 1], bank[4][:, 0:16], axis=AX.X), [PSN[4]], [f"st{c}"])
                S.op("dve", lambda e: e.tensor_scalar(stat[:, c + 1:c + 2], stat[:, c:c + 1], -1.0, None, op0=ALU.mult), [f"st{c}"], [f"st{c+1}"])
                S.op("act", lambda e: e.activation(out=ex_sb, in_=bank[4][:, 0:16], func=ACT.Exp, bias=stat[:, c + 1:c + 2], accum_out=stat[:, c + 2:c + 3]),
                     [PSN[4], f"st{c+1}"], ["ex_sb", f"st{c+2}"])
                S.op("dve", lambda e: e.reciprocal(stat[:, c + 3:c + 4], stat[:, c + 2:c + 3]), [f"st{c+2}"], [f"st{c+3}"])
                S.op("dve", lambda e: e.tensor_scalar(aff_sb[s], ex_sb, stat[:, c + 3:c + 4], None, op0=ALU.mult), ["ex_sb", f"st{c+3}"], [f"aff{s}"])

            def d4(tt):
                s = tt % 2
                S.op("pe", lambda e: e.transpose(bank[5][0:16, 0:128], aff_sb[s], idf[:]), [f"aff{s}", "idf"], [PSN[5]])
                S.op("act", lambda e: e.activation(out=affT_sb[s][0:16, :], in_=bank[5][0:16, 0:128], func=ACT.Copy), [PSN[5]], [f"affT{s}"])
                S.dma("sp", lambda e: e.dma_start(out=aff_d[32 * b:32 * b + 16, tt * 128:(tt + 1) * 128], in_=affT_sb[s][0:16, :]),
                      [f"affT{s}"], ["aff_d"], f"d_af{s}")

            for i in range(16 + 3):
                if 0 <= i - 3 < 16:
                    d4(i - 3)
                if 0 <= i - 2 < 16:
                    d3(i - 2)
                if 0 <= i - 1 < 16:
                    d2(i - 1)
                if i < 16:
                    d1(i)
            S.barrier()

        for b_ in range(NB if stage != "phase0" else 0):
            do_batch(b_)
        if stage in ("A", "B", "C", "B1", "B2", "B3", "B0a", "B0b", "B0c", "B0d"):
            S.barrier()

        if stage in ("mixer", "phase0", "A", "B", "C", "B1", "B2", "B3", "B0a", "B0b", "B0c", "B0d"):
            AR.reset()
            cp = [AR.take([D], F32) for _ in range(2)]
            for b in range(NB):
                for tt in range(16):
                    s = tt % 2
                    S.dma("sp", lambda e, b=b, tt=tt, s=s: e.dma_start(out=cp[s], in_=h_d[b, tt * 128:(tt + 1) * 128, :]), [], [f"cp{s}"], f"d_cp{s}")
                    S.dma("sp", lambda e, b=b, tt=tt, s=s: e.dma_start(out=out[b, tt * 128:(tt + 1) * 128, :], in_=cp[s]), [f"cp{s}"], ["out"], f"d_co{s}")
            S.barrier(["sp"])
            S.emit()
            return nc

        AR.reset()
        wexp = [[AR.take([8, D], BF16) for _ in range(3)] for _ in range(2)]
        xs = [AR.take([D], BF16) for _ in range(8)]
        xsT = [AR.take([8, 512], BF16) for _ in range(2)]
        actT = [AR.take([8, 512], BF16) for _ in range(2)]
        sgt = [AR.take([512], F32) for _ in range(2)]
        y_sb = [AR.take([D], F32) for _ in range(8)]
        moe_end = AR.off
        work = AR.take([T], F32)
        vals = AR.take([CAP], F32)
        idx = AR.take([CAP], U32)
        idxf = AR.take([CAP], F32)
        idxT = AR.take([2, 128], U32)
        gT = AR.take([2, 128], F32)
        rt_end = AR.off
        AR.off = 0
        ffl = [AR.take([D], F32) for _ in range(2)]
        hl = [AR.take([D], F32) for _ in range(2)]
        G2 = AR.take([D], F32)
        gtmp3 = AR.take([D], F32)
        tF = AR.take([D], F32)
        ob_t = [AR.take([D], F32) for _ in range(2)]
        junk2 = AR.take([D], BF16)
        AR.off = rt_end
        print("moe arena bytes", AR.off)

        def load_expert(e_):
            s = e_ % 2
            for wi, wsrc in enumerate((w_gate, w_up, w_down)):
                for k in range(8):
                    S.dma("pool", lambda e, s=s, wi=wi, wsrc=wsrc, k=k: e.dma_start(out=wexp[s][wi][:, k, :], in_=wsrc[e_, k * 128:(k + 1) * 128, :]),
                          [], [f"we{s}_{wi}"], f"d_we{s}_{wi}")

        load_expert(0)
        S.dma("sp", lambda e: e.dma_start(out=work, in_=aff_d), ["aff_d"], ["work"], "d_work")
        for r in range(CAP // 8):
            S.op("dve", lambda e, r=r: e.max(out=vals[:, r * 8:(r + 1) * 8], in_=work), ["work"], ["vals"])
            S.op("dve", lambda e, r=r: e.max_index(out=idx[:, r * 8:(r + 1) * 8], in_max=vals[:, r * 8:(r + 1) * 8], in_values=work), ["work", "vals"], ["idx"])
            S.op("dve", lambda e, r=r: e.match_replace(out=work, in_to_replace=vals[:, r * 8:(r + 1) * 8], in_values=work, imm_value=-1.0), ["vals", "idx"], ["work"])
        S.op("dve", lambda e: e.tensor_copy(idxf, idx), ["idx"], ["idxf"])
        for ct in range(2):
            S.op("pe", lambda e, ct=ct: e.transpose(bank[0][:, 0:128], idxf[:, ct * 128:(ct + 1) * 128], idf[:]), ["idxf", "idf"], [PSN[0]])
            S.op("dve", lambda e, ct=ct: e.tensor_copy(idxT[:, ct, :], bank[0][:, 0:128]), [PSN[0]], ["idxT"])
            S.op("pe", lambda e, ct=ct: e.transpose(bank[1][:, 0:128], vals[:, ct * 128:(ct + 1) * 128], idf[:]), ["vals", "idf"], [PSN[1]])
            S.op("dve", lambda e, ct=ct: e.tensor_copy(gT[:, ct, :], bank[1][:, 0:128]), [PSN[1]], ["gT"])

        NP = (NB + 1) // 2
        tiles = [(pr, bi, bb, ct) for pr in range(NP) for bi, bb in enumerate([q for q in (2 * pr, 2 * pr + 1) if q < NB]) for ct in range(2)]

        def gathers(e_):
            for j, (pr, bi, bb, ct) in enumerate(tiles):
                row = 32 * bb + e_
                S.dma("pool", lambda e, j=j, bb=bb, ct=ct, row=row: e.indirect_dma_start(
                    out=xs[j], out_offset=None, in_=xn2_d[bb],
                    in_offset=bass.IndirectOffsetOnAxis(ap=idxT[:, ct, row:row + 1], axis=0)),
                    [f"xn2d{bb}", "idxT"], [f"xs{j}"], f"d_xs{j}")

        def transposes(e_):
            for j, (pr, bi, bb, ct) in enumerate(tiles):
                pbk = 6 + j % 2
                pv = bank[pbk].bitcast(BF16)

                def tpx(e, j=j, pv=pv):
                    for k in range(8):
                        ins = e.transpose(pv[:, k * 128:(k + 1) * 128], xs[j][:, k * 128:(k + 1) * 128], idb[:])
                    return ins
                S.op("pe", tpx, [f"xs{j}", "idb"], [PSN[pbk]])
                c0 = (bi * 2 + ct) * 128
                S.op("act", lambda e, pr=pr, c0=c0, pv=pv: e.activation(out=xsT[pr][:, :, c0:c0 + 128], in_=pv.rearrange("p (k t) -> p k t", k=8), func=ACT.Copy),
                     [PSN[pbk]], [f"xsT{pr}"])

        def do_pair(e_, pr):
            ws = e_ % 2
            wg_, wu_, wd_ = wexp[ws]
            mine = [(j, t) for j, t in enumerate(tiles) if t[0] == pr]
            ncol = len(mine) * 128
            for fc in range(8):
                bg = 0 + 2 * (fc % 2)
                bu = 1 + 2 * (fc % 2)
                si_ = fc % 2

                def mmg(e, fc=fc, bg=bg):
                    for k in range(8):
                        ins = e.matmul(bank[bg][:, 0:ncol], wg_[:, k, fc * 128:(fc + 1) * 128], xsT[pr][:, k, 0:ncol], start=(k == 0), stop=(k == 7))
                    return ins

                def mmu(e, fc=fc, bu=bu):
                    for k in range(8):
                        ins = e.matmul(bank[bu][:, 0:ncol], wu_[:, k, fc * 128:(fc + 1) * 128], xsT[pr][:, k, 0:ncol], start=(k == 0), stop=(k == 7))
                    return ins
                S.op("pe", mmg, [f"we{ws}_0", f"xsT{pr}"], [PSN[bg]])
                S.op("pe", mmu, [f"we{ws}_1", f"xsT{pr}"], [PSN[bu]])
                S.op("act", lambda e, bg=bg, si_=si_: e.activation(out=sgt[si_][:, 0:ncol], in_=bank[bg][:, 0:ncol], func=ACT.Silu), [PSN[bg]], [f"sgt{si_}"])
                S.op("dve", lambda e, fc=fc, bu=bu, si_=si_: e.tensor_tensor(actT[pr][:, fc, 0:ncol], sgt[si_][:, 0:ncol], bank[bu][:, 0:ncol], op=ALU.mult),
                     [PSN[bu], f"sgt{si_}"], [f"actT{pr}"])
            for j, (pr_, bi, bb, ct) in mine:
                row = 32 * bb + e_
                c0 = (bi * 2 + ct) * 128
                for hf in range(2):
                    yb = 4 + hf

                    def mmd(e, c0=c0, hf=hf, yb=yb):
                        for k in range(8):
                            ins = e.matmul(bank[yb], actT[pr][:, k, c0:c0 + 128], wd_[:, k, hf * 512:(hf + 1) * 512], start=(k == 0), stop=(k == 7))
                        return ins
                    S.op("pe", mmd, [f"actT{pr}", f"we{ws}_2"], [PSN[yb]])
                    if hf == 0:
                        S.op("act", lambda e, j=j, yb=yb, ct=ct, row=row: e.activation(out=y_sb[j][:, 0:512], in_=bank[yb], func=ACT.Identity, scale=gT[:, ct, row:row + 1]),
                             [PSN[yb], "gT"], [f"y{j}a"])
                    else:
                        S.op("dve", lambda e, j=j, yb=yb, ct=ct, row=row: e.tensor_scalar(y_sb[j][:, 512:1024], bank[yb], gT[:, ct, row:row + 1], None, op0=ALU.mult),
                             [PSN[yb], "gT"], [f"y{j}b"])
                par, ppar = e_ % 2, (e_ - 1) % 2
                S.dma("pool", lambda e, j=j, bb=bb, ct=ct, row=row: e.indirect_dma_start(
                    out=ff_d[bb], out_offset=bass.IndirectOffsetOnAxis(ap=idxT[:, ct, row:row + 1], axis=0),
                    in_=y_sb[j], in_offset=None, compute_op=ALU.add),
                    [f"y{j}a", f"y{j}b", "idxT", f"ff{bb}", f"ffs{bb}_0_{ppar}", f"ffs{bb}_1_{ppar}"], [f"ffs{bb}_{ct}_{par}"], f"d_y{j}")

        gathers(0)
        for e_ in range(NE):
            transposes(e_)
            if e_ + 1 < NE:
                load_expert(e_ + 1)
                gathers(e_ + 1)
            for pr in range(NP):
                do_pair(e_, pr)
        S.barrier()

        for b in range(NB):
            bc_load(G2, mod_d[b:b + 1, 5 * D:6 * D], "G2", "d_bc_G2")
            bc_load(gtmp3, gains[3:4, :], "gtmp3", "d_bc_gtmp3")
            S.op("dve", lambda e: e.tensor_tensor(G2, G2, gtmp3, op=ALU.mult), ["G2", "gtmp3"], ["G2"])
            for tt in range(16):
                s = tt % 2
                S.dma("sp", lambda e, b=b, tt=tt, s=s: e.dma_start(out=ffl[s], in_=ff_d[b][tt * 128:(tt + 1) * 128, :]), [f"ff{b}"], [f"ffl{s}"], f"d_ffl{s}")
                S.dma("sp", lambda e, b=b, tt=tt, s=s: e.dma_start(out=hl[s], in_=h_d[b, tt * 128:(tt + 1) * 128, :]), [f"h_d{b}_{tt}"], [f"hl{s}"], f"d_hl{s}")
                rs, rsn = rstd_of(ffl[s], D, [f"ffl{s}"], junk2, 1.0 / D)
                S.op("dve", lambda e, s=s, rs=rs: e.scalar_tensor_tensor(tF, ffl[s], rs, G2, op0=ALU.mult, op1=ALU.mult), [f"ffl{s}", rsn, "G2"], ["tF"])
                S.op("dve", lambda e, s=s: e.tensor_tensor(ob_t[s], tF, hl[s], op=ALU.add), ["tF", f"hl{s}"], [f"ob{s}"])
                S.dma("sp", lambda e, b=b, tt=tt, s=s: e.dma_start(out=out[b, tt * 128:(tt + 1) * 128, :], in_=ob_t[s]), [f"ob{s}"], ["out"], f"d_out{s}")
        S.barrier(["sp"])
        S.emit()
    return nc


def _rope_tables():
    t = np.arange(T)
    row = (t // 64).astype(np.float32)
    col = (t % 64).astype(np.float32)
    inv = (10000.0 ** (-np.arange(0, 32, 2, dtype=np.float32) / 32)).astype(np.float32)
    ang_r = row[:, None] * inv[None, :]
    ang_c = col[:, None] * inv[None, :]
    ang = np.concatenate([ang_r, ang_r, ang_c, ang_c], axis=-1)
    cos = np.cos(ang).astype(np.float32).T
    sin = np.sin(ang).astype(np.float32).T
    sgn = np.concatenate([-np.ones(16), np.ones(16), -np.ones(16), np.ones(16)]).astype(np.float32)[:, None]
    sin = sin * sgn
    cosT = np.ascontiguousarray(np.concatenate([cos, cos], 0))
    sinT = np.ascontiguousarray(np.concatenate([sin, sin], 0))
    perm = np.zeros((128, 128), np.float32)
    for i in range(128):
        perm[i ^ 16, i] = 1.0
    return cosT, sinT, perm


def make_in_maps(inputs, NB=4, ncores=NCORES, ne=NE):
    f = lambda a: np.ascontiguousarray(np.asarray(a, dtype=np.float32))
    x = f(inputs["x"]); c = f(inputs["c"]); ctx = f(inputs["ctx"]); c_ctx = f(inputs["c_ctx"])
    w_in = f(inputs["w_in"])[0]
    cosT, sinT, perm = _rope_tables()
    w_in_r = np.ascontiguousarray(w_in.reshape(8, 128, 24, 128).transpose(2, 1, 0, 3))
    w_v_r = np.ascontiguousarray(w_in[:, 1024:1536].reshape(8, 128, 512).transpose(1, 0, 2))
    shared = dict(
        w_ada=f(inputs["w_ada"])[0], b_ada=f(inputs["b_ada"]),
        gains=np.ascontiguousarray(np.concatenate([f(inputs["norm_pre_mix"]), f(inputs["norm_post_mix"]),
                                                   f(inputs["norm_pre_ffn"]), f(inputs["norm_post_ffn"])], 0)),
        w_in_r=w_in_r, w_v_r=w_v_r,
        convw=np.ascontiguousarray(f(inputs["conv_w"])[0].T.reshape(4, 128, 3).transpose(1, 0, 2)),
        lqk=np.ascontiguousarray(np.concatenate([f(inputs["lambda_q1"]), f(inputs["lambda_k1"]),
                                                 f(inputs["lambda_q2"]), f(inputs["lambda_k2"])], 1)),
        subln=np.ascontiguousarray(f(inputs["subln_g"]).reshape(128, 1)),
        w_out=f(inputs["w_out"])[0],
        w_router=np.ascontiguousarray(f(inputs["w_router"])[0].reshape(8, 128, 16).transpose(1, 0, 2)),
        w_gate=f(inputs["w_gate"])[0][:ne], w_up=f(inputs["w_up"])[0][:ne], w_down=f(inputs["w_down"])[0][:ne],
        ident=np.eye(128, dtype=np.float32), perm=perm, cosT=cosT, sinT=sinT,
    )
    maps = []
    for i in range(ncores):
        sl = slice(i * NB, (i + 1) * NB)
        cc = np.concatenate([c[sl], c_ctx[None, :]], 0)
        if cc.shape[0] < 5:
            cc = np.concatenate([cc[:-1], np.zeros((5 - cc.shape[0], D), np.float32), cc[-1:]], 0)
        ccT = np.ascontiguousarray(cc.T.reshape(8, 128, 5).transpose(1, 0, 2))
        m = dict(shared)
        m.update(x=np.ascontiguousarray(x[sl]), ctx=np.ascontiguousarray(ctx[sl]), ccT=ccT)
        maps.append(m)
    return maps


def kernel(**inputs):
    NB = 4
    nc = build(NB=NB, stage="full")
    maps = make_in_maps(inputs, NB=NB, ncores=NCORES)
    res = run_bass_kernel_spmd(nc, maps, core_ids=list(range(NCORES)))
    return np.concatenate([np.asarray(r["out"]) for r in res.results], axis=0).astype(np.float32)
```

```python
import math
from contextlib import ExitStack
import numpy as np
import concourse.bass as bass
import concourse.mybir as mybir
from concourse.bass_utils import run_bass_kernel_spmd

F32 = mybir.dt.float32
BF16 = mybir.dt.bfloat16
U32 = mybir.dt.uint32
ACT = mybir.ActivationFunctionType
ALU = mybir.AluOpType
AX = mybir.AxisListType

T = 2048
C = 256
D = 1024
TK = T + C
NE = 16
CAP = 256
EPS = 1e-6
NCORES = 8


class Sched:
    ENGS = ("pe", "act", "dve", "pool", "sp")

    def __init__(self, nc, stack):
        self.nc = nc
        self.stack = stack
        self.sems = {}
        self.eng = {}
        for n in self.ENGS:
            self.sems["s_" + n] = stack.enter_context(nc.semaphore("s_" + n))
            self.eng[n] = dict(ops=[], cnt=0, waited={})
        self.lastw = {}
        self.readers = {}
        self.dcum = {}

    def _deps(self, reads, writes):
        d = []
        for r in reads:
            if r in self.lastw:
                d.append(self.lastw[r])
        for w in writes:
            if w in self.lastw:
                d.append(self.lastw[w])
            d.extend(self.readers.get(w, ()))
        return d

    def _waits(self, en, deps):
        E = self.eng[en]
        need = {}
        for (s, v) in deps:
            if en == "pe" and s == "s_pe":
                continue
            if E["waited"].get(s, 0) >= v:
                continue
            if need.get(s, 0) < v:
                need[s] = v
        for s, v in need.items():
            E["waited"][s] = v
        return list(need.items())

    def _record(self, ev, reads, writes):
        for r in reads:
            self.readers.setdefault(r, []).append(ev)
        for w in writes:
            self.lastw[w] = ev
            self.readers[w] = []

    def op(self, en, fn, reads=(), writes=()):
        excl = [r for r in reads if r.startswith("ps") and r not in writes]
        if excl:
            reads = [r for r in reads if r not in excl]
            writes = list(writes) + excl
        deps = self._deps(reads, writes)
        waits = self._waits(en, deps)
        E = self.eng[en]
        E["cnt"] += 1
        ev = ("s_" + en, E["cnt"])
        self._record(ev, reads, writes)
        sems = self.sems

        def run(e, fn=fn, waits=waits, sem=sems["s_" + en]):
            for (s, v) in waits:
                e.wait_ge(sems[s], v)
            fn(e).then_inc(sem, 1)

        E["ops"].append(run)

    def dma(self, q, mk, reads, writes, key):
        deps = self._deps(reads, writes)
        waits = self._waits(q, deps)
        if key not in self.sems:
            self.sems[key] = self.stack.enter_context(self.nc.semaphore(key))
            self.dcum[key] = 0
        self.dcum[key] += 16
        ev = (key, self.dcum[key])
        self._record(ev, reads, writes)
        sems = self.sems

        def run(e, mk=mk, waits=waits, sem=sems[key]):
            for (s, v) in waits:
                e.wait_ge(sems[s], v)
            mk(e).then_inc(sem, 16)

        self.eng[q]["ops"].append(run)

    def _all_events(self):
        ev = [("s_" + n, self.eng[n]["cnt"]) for n in self.ENGS if self.eng[n]["cnt"] > 0]
        ev += [(k, v) for k, v in self.dcum.items() if v > 0]
        return ev

    def barrier(self, engines=None):
        allev = self._all_events()
        for n in (engines or self.ENGS):
            waits = self._waits(n, [ev for ev in allev if ev[0] != "s_" + n])
            sems = self.sems

            def run(e, waits=waits):
                for (s, v) in waits:
                    e.wait_ge(sems[s], v)

            self.eng[n]["ops"].append(run)

    def emit(self):
        nc = self.nc
        with nc.Block() as block:
            @block.tensor
            def _(e):
                for f in self.eng["pe"]["ops"]:
                    f(e)

            @block.scalar
            def _(e):
                for f in self.eng["act"]["ops"]:
                    f(e)

            @block.vector
            def _(e):
                for f in self.eng["dve"]["ops"]:
                    f(e)

            @block.gpsimd
            def _(e):
                for f in self.eng["pool"]["ops"]:
                    f(e)

            @block.sync
            def _(e):
                for f in self.eng["sp"]["ops"]:
                    f(e)


DT_BYTES = {F32: 4, BF16: 2, U32: 4}


class Arena:
    def __init__(self, t, nbytes):
        self.t = t
        self.nbytes = nbytes
        self.off = 0

    def reset(self):
        self.off = 0

    def take(self, free, dtype):
        n = 1
        for s in free:
            n *= s
        sz = n * DT_BYTES[dtype]
        a = self.off
        self.off += (sz + 63) // 64 * 64
        assert self.off <= self.nbytes, (self.off, self.nbytes)
        ap = self.t[:, a // 4:(a + sz) // 4]
        if dtype != F32:
            ap = ap.bitcast(dtype)
        if len(free) == 2:
            ap = ap.rearrange("p (a b) -> p a b", a=free[0])
        elif len(free) == 3:
            ap = ap.rearrange("p (a b c) -> p a b c", a=free[0], b=free[1])
        return ap


def build(NB=4, stage="full"):
    nc = bass.Bass("TRN2", target_bir_lowering=False)

    def din(n, s, d=F32):
        return nc.dram_tensor(n, s, d, kind="ExternalInput").ap()

    x = din("x", [NB, T, D])
    ctx = din("ctx", [NB, C, D])
    ccT = din("ccT", [128, 8, 5])
    w_ada = din("w_ada", [D, 6 * D])
    b_ada = din("b_ada", [1, 6 * D])
    gains = din("gains", [4, D])
    w_in_r = din("w_in_r", [24, 128, 8, 128])
    w_v_r = din("w_v_r", [128, 8, 512])
    convw = din("convw", [128, 4, 3])
    lqk = din("lqk", [1, 256])
    subln = din("subln", [128, 1])
    w_out = din("w_out", [D, D])
    w_router = din("w_router", [128, 8, 16])
    NE_decl = NE if stage == "full" else 1
    w_gate = din("w_gate", [NE_decl, D, D])
    w_up = din("w_up", [NE_decl, D, D])
    w_down = din("w_down", [NE_decl, D, D])
    ident = din("ident", [128, 128])
    perm = din("perm", [128, 128])
    cosT_d = din("cosT", [128, T])
    sinT_d = din("sinT", [128, T])
    out = nc.dram_tensor("out", [NB, T, D], F32, kind="ExternalOutput").ap()
    mod_d = nc.dram_tensor("mod_d", [5, 6 * D], F32).ap()
    h_d = nc.dram_tensor("h_d", [NB, T, D], F32).ap()
    aff_d = nc.dram_tensor("aff_d", [128, T], F32).ap()
    xn2_d = [nc.dram_tensor(f"xn2_d{b}", [T, D], BF16).ap() for b in range(NB)]
    ff_d = [nc.dram_tensor(f"ff_d{b}", [T, D], F32).ap() for b in range(NB)]

    with ExitStack() as st:
        S = Sched(nc, st)

        def sb(n, s, d):
            return st.enter_context(nc.sbuf_tensor(n, s, d))

        idf = sb("idf", [128, 128], F32)
        idb = sb("idb", [128, 128], BF16)
        permf = sb("permf", [128, 128], F32)
        permb = sb("permb", [128, 128], BF16)
        ones_b = sb("ones_b", [128, 128], BF16)
        ones_f = sb("ones_f", [128, 128], F32)
        neghalf = sb("neghalf", [128, 256], F32)
        eps_t = sb("eps_t", [128, 1], F32)
        cw = sb("cw", [128, 4, 3], F32)
        sgs = sb("sgs", [128, 1], F32)
        neglam = sb("neglam", [128, 2], F32)
        wr_sb = sb("wr_sb", [128, 8, 16], F32)
        stat = sb("stat", [128, 96], F32)
        lq_sb = sb("lq_sb", [1, 256], F32)
        lq2 = sb("lq2", [1, 128], F32)
        lq3 = sb("lq3", [1, 8], F32)
        ARENA_BYTES = 200 * 1024
        arena_t = sb("arena", [128, ARENA_BYTES // 4], F32)
        AR = Arena(arena_t, ARENA_BYTES)
        psS = [st.enter_context(nc.psum_tensor(f"psS{i}", [128, 512], F32)) for i in range(8)]
        bank = [p[:] for p in psS]
        PSN = [f"ps{i}" for i in range(8)]

        stat_ctr = [0]

        def stat_slot(n=1):
            c = stat_ctr[0]
            if c + n > 96:
                c = 0
            stat_ctr[0] = c + n
            return c

        def rstd_of(src, nfree, src_reads, junk, inv_n):
            c = stat_slot(3)
            r0, r1, r2 = f"st{c}", f"st{c+1}", f"st{c+2}"
            S.op("act", lambda e: e.activation(out=junk, in_=src, func=ACT.Square, accum_out=stat[:, c:c + 1]),
                 src_reads, [r0, "junk"])
            S.op("dve", lambda e: e.tensor_scalar(stat[:, c + 1:c + 2], stat[:, c:c + 1], inv_n, EPS, op0=ALU.mult, op1=ALU.add),
                 [r0], [r1])
            S.op("pool", lambda e: e.tensor_tensor(stat[:, c + 2:c + 3], stat[:, c + 1:c + 2], neghalf[:, 0:1], op=ALU.pow),
                 [r1, "neghalf"], [r2])
            return stat[:, c + 2:c + 3], r2

        def rstd_of2(src0, src1, src_reads, junk, inv_n):
            c = stat_slot(5)
            ra, rb, r0, r1, r2 = (f"st{c + i}" for i in range(5))
            S.op("act", lambda e: e.activation(out=junk[:, 0:512], in_=src0, func=ACT.Square, accum_out=stat[:, c:c + 1]),
                 src_reads, [ra, "junk"])
            S.op("act", lambda e: e.activation(out=junk[:, 512:1024], in_=src1, func=ACT.Square, accum_out=stat[:, c + 1:c + 2]),
                 src_reads, [rb, "junk"])
            S.op("dve", lambda e: e.tensor_tensor(stat[:, c + 2:c + 3], stat[:, c:c + 1], stat[:, c + 1:c + 2], op=ALU.add), [ra, rb], [r0])
            S.op("dve", lambda e: e.tensor_scalar(stat[:, c + 3:c + 4], stat[:, c + 2:c + 3], inv_n, EPS, op0=ALU.mult, op1=ALU.add), [r0], [r1])
            S.op("pool", lambda e: e.tensor_tensor(stat[:, c + 4:c + 5], stat[:, c + 3:c + 4], neghalf[:, 0:1], op=ALU.pow), [r1, "neghalf"], [r2])
            return stat[:, c + 4:c + 5], r2

        S.dma("sp", lambda e: e.dma_start(out=idf[:], in_=ident), [], ["idf"], "d_c1")
        S.dma("sp", lambda e: e.dma_start(out=permf[:], in_=perm), [], ["permf"], "d_c2")
        S.dma("sp", lambda e: e.dma_start(out=cw[:], in_=convw), [], ["cw"], "d_c3")
        S.dma("sp", lambda e: e.dma_start(out=sgs[:], in_=subln), [], ["sgs"], "d_c4")
        S.dma("sp", lambda e: e.dma_start(out=wr_sb[:], in_=w_router), [], ["wr_sb"], "d_c5")
        S.dma("sp", lambda e: e.dma_start(out=lq_sb[:], in_=lqk), [], ["lq_sb"], "d_c6")
        S.op("dve", lambda e: e.tensor_copy(idb[:], idf[:]), ["idf"], ["idb"])
        S.op("dve", lambda e: e.tensor_copy(permb[:], permf[:]), ["permf"], ["permb"])
        S.op("dve", lambda e: e.memset(ones_b[:], 1.0), [], ["ones_b"])
        S.op("dve", lambda e: e.memset(ones_f[:], 1.0), [], ["ones_f"])
        S.op("dve", lambda e: e.memset(neghalf[:], -0.5), [], ["neghalf"])
        S.op("dve", lambda e: e.memset(eps_t[:], EPS), [], ["eps_t"])
        S.op("dve", lambda e: e.tensor_scalar(sgs[:], sgs[:], 0.8, None, op0=ALU.mult), ["sgs"], ["sgs"])
        lqv = lq_sb[0:1, :].rearrange("p (a b c) -> p a b c", a=2, b=2)
        S.op("dve", lambda e: e.tensor_tensor(lq2[0:1, :].rearrange("p (a c) -> p a c", a=2), lqv[:, :, 0, :], lqv[:, :, 1, :], op=ALU.mult),
             ["lq_sb"], ["lq2"])
        S.op("dve", lambda e: e.reduce_sum(lq3[0:1, 0:2], lq2[0:1, :].rearrange("p (a c) -> p a c", a=2), axis=AX.X), ["lq2"], ["lq3a"])
        S.op("act", lambda e: e.activation(out=lq3[0:1, 2:4], in_=lq3[0:1, 0:2], func=ACT.Exp), ["lq3a"], ["lq3b"])
        S.op("dve", lambda e: e.tensor_tensor(lq3[0:1, 4:5], lq3[0:1, 3:4], lq3[0:1, 2:3], op=ALU.subtract), ["lq3b"], ["lq3c"])
        S.op("dve", lambda e: e.tensor_scalar(lq3[0:1, 6:7], lq3[0:1, 4:5], -0.2, None, op0=ALU.add), ["lq3c"], ["lq3d"])
        S.op("dve", lambda e: e.tensor_copy(lq3[0:1, 7:8], lq3[0:1, 6:7]), ["lq3d"], ["lq3e"])
        S.op("pe", lambda e: e.matmul(bank[7][:, 0:2], ones_f[0:1, :], lq3[0:1, 6:8], start=True, stop=True),
             ["ones_f", "lq3d", "lq3e"], [PSN[7]])
        S.op("dve", lambda e: e.tensor_copy(neglam[:], bank[7][:, 0:2]), [PSN[7]], ["neglam"])

        AR.reset()
        ccT_sb = AR.take([8, 5], F32)
        siluT = AR.take([8, 5], F32)
        bada_sb = AR.take([6 * D], F32)
        wa = [AR.take([8, 512], F32) for _ in range(2)]
        mstage = [AR.take([512], F32) for _ in range(2)]
        zero_t = AR.take([2048], F32)
        S.dma("sp", lambda e: e.dma_start(out=ccT_sb, in_=ccT), [], ["ccT"], "d_c7")
        S.dma("sp", lambda e: e.dma_start(out=bada_sb[0:1, :], in_=b_ada), [], ["bada"], "d_c8")
        S.op("act", lambda e: e.activation(out=siluT, in_=ccT_sb, func=ACT.Silu), ["ccT"], ["siluT"])
        S.op("dve", lambda e: e.memset(zero_t, 0.0), [], ["zero_t"])
        for b in range(NB):
            ffv = ff_d[b].rearrange("(p r) d -> p (r d)", p=128)
            for j in range(8):
                S.dma("sp", lambda e, ffv=ffv, j=j: e.dma_start(out=ffv[:, j * 2048:(j + 1) * 2048], in_=zero_t),
                      ["zero_t"], [f"ff{b}"], "d_z")
        S.dma("sp", lambda e: e.dma_start(out=aff_d, in_=zero_t), ["zero_t"], ["aff_d"], "d_z")
        for j in range(12):
            s = j % 2
            S.dma("sp", lambda e, j=j, s=s: e.dma_start(out=wa[s], in_=w_ada[:, j * 512:(j + 1) * 512].rearrange("(k p) n -> p k n", p=128)),
                  [], [f"wa{s}"], f"d_wa{s}")

            def mm0(e, j=j, s=s):
                for k in range(8):
                    e.matmul(bank[4][0:5, :], siluT[:, k, :], wa[s][:, k, :], start=(k == 0), stop=False)
                return e.matmul(bank[4][0:5, :], ones_f[0:1, 0:5], bada_sb[0:1, j * 512:(j + 1) * 512], start=False, stop=True)
            S.op("pe", mm0, ["siluT", f"wa{s}", "bada", "ones_f"], [PSN[4]])
            S.op("act", lambda e, s=s: e.activation(out=mstage[s][0:5, :], in_=bank[4][0:5, :], func=ACT.Copy), [PSN[4]], [f"ms{s}"])
            S.dma("sp", lambda e, j=j, s=s: e.dma_start(out=mod_d[:, j * 512:(j + 1) * 512], in_=mstage[s][0:5, :]),
                  [f"ms{s}"], ["mod_d"], f"d_ms{s}")
        S.barrier()

        AR.reset()
        cosT = AR.take([T], F32)
        sinT = AR.take([T], F32)
        xnT = AR.take([8, TK], BF16)
        R1 = xnT.rearrange("p a b -> p (a b)")
        attnT = R1[:, 0:4 * T].rearrange("p (a b) -> p a b", a=4)
        w_out_sb = R1[:, 4 * T:4 * T + 8 * D].rearrange("p (a b) -> p a b", a=8)
        kT = AR.take([4, TK], BF16)
        v_sb = AR.take([18, 512], BF16)
        KVf = arena_t[:, 0:0]
        qc_off = AR.off
        qT = AR.take([4, T], BF16)
        AR.off = qc_off
        u_t = AR.take([2064], F32)
        gb_t = AR.take([T], F32)
        y_t = AR.take([1024], F32)
        qc_end = max(AR.off, qc_off + 4 * T * 2)
        AR.off = qc_off
        A2 = AR.take([D], F32)
        B2 = AR.take([D], F32)
        G1 = AR.take([D], F32)
        gtmp2 = AR.take([D], F32)
        AR.off = qc_end
        convT = AR.take([4, T], BF16)
        A1 = AR.take([D], F32)
        B1 = AR.take([D], F32)
        A1c = AR.take([D], F32)
        B1c = AR.take([D], F32)
        ta_off = AR.off
        XT = [AR.take([D], F32) for _ in range(2)]
        xb = [AR.take([D], BF16) for _ in range(2)]
        tmpA = AR.take([D], F32)
        junk = AR.take([D], BF16)
        ta_end = AR.off
        AR.off = ta_off
        at_t = [AR.take([512], BF16) for _ in range(3)]
        tO = AR.take([512], F32)
        o_t = [AR.take([256], F32) for _ in range(2)]
        osq = [AR.take([256], F32) for _ in range(2)]
        ln_t = [AR.take([256], F32) for _ in range(2)]
        rs_t = [AR.take([256], F32) for _ in range(2)]
        Ocp = AR.take([512], F32)
        dcp = AR.take([512], F32)
        assert AR.off <= ta_end
        AR.off = ta_end
        pb = [AR.take([512], BF16) for _ in range(2)]
        t1 = [AR.take([512], F32) for _ in range(2)]
        t2 = [AR.take([512], F32) for _ in range(2)]
        xc_sb = [AR.take([512], F32) for _ in range(2)]
        wslot = [AR.take([8, 128], BF16) for _ in range(6)]
        wv_sb = AR.take([8, 512], BF16)
        mix_end = AR.off
        kv_off = (kT.offset if hasattr(kT, "offset") else None)
        AR.off = 2 * T * 4 + 8 * TK * 2
        xt2 = [AR.take([D], F32) for _ in range(2)]
        tD = AR.take([D], F32)
        h_sb = [AR.take([D], F32) for _ in range(2)]
        t2D = AR.take([D], F32)
        xn2 = [AR.take([D], F32) for _ in range(2)]
        xn2T = AR.take([8, 128], F32)
        assert AR.off <= 2 * T * 4 + 8 * TK * 2 + 4 * TK * 2 + 18 * 512 * 2 + 64
        AR.off = mix_end
        lg_sb = AR.take([16], F32)
        ex_sb = AR.take([16], F32)
        aff_sb = [AR.take([16], F32) for _ in range(2)]
        affT_sb = [AR.take([128], F32) for _ in range(2)]
        print("mixer arena bytes", AR.off)

        S.dma("sp", lambda e: e.dma_start(out=cosT, in_=cosT_d), [], ["cosT"], "d_c9")
        S.dma("sp", lambda e: e.dma_start(out=sinT, in_=sinT_d), [], ["sinT"], "d_c10")
        S.op("dve", lambda e: e.memset(u_t, 0.0), [], ["u_t"])

        def bc_load(dst, row_ap, region, key, guard=()):
            S.dma("sp", lambda e: e.dma_start(out=dst, in_=row_ap.partition_broadcast(128)), list(guard), [region], key)

        def mod_tile(dst, region, b, seg, gain_idx, tmp, tmp_region, plus_one, guard=()):
            bc_load(dst, mod_d[b:b + 1, seg * D:(seg + 1) * D], region, "d_bc_" + region, guard)
            if gain_idx is not None:
                bc_load(tmp, gains[gain_idx:gain_idx + 1, :], tmp_region, "d_bc_" + tmp_region, guard)
                if plus_one:
                    S.op("dve", lambda e: e.scalar_tensor_tensor(dst, dst, 1.0, tmp, op0=ALU.add, op1=ALU.mult),
                         [region, tmp_region], [region])
                else:
                    S.op("dve", lambda e: e.tensor_tensor(dst, dst, tmp, op=ALU.mult), [region, tmp_region], [region])

        mod_tile(A1c, "A1c", 4, 1, 0, tmpA, "tmpA", True)
        mod_tile(B1c, "B1c", 4, 0, None, None, None, False)

        grp_ctr = [0]

        def load_group(g):
            s = grp_ctr[0] % 6
            grp_ctr[0] += 1
            S.dma("pool", lambda e: e.dma_start(out=wslot[s], in_=w_in_r[g]), [], [f"ws{s}"], f"d_ws{s}")
            return s

        pbank_ctr = [0]

        def next_pbank():
            i = pbank_ctr[0] % 4
            pbank_ctr[0] += 1
            return i

        rope_ctr = [0]

        def do_batch(b):
            mod_tile(A1, "A1", b, 1, 0, tmpA, "tmpA", True)
            mod_tile(B1, "B1", b, 0, None, None, None, False)
            S.dma("pool", lambda e: e.dma_start(out=wv_sb, in_=w_v_r), [], ["wv"], "d_wv")
            order = [("k", h, 4 + h) for h in range(4)]
            for j in range(4):
                order += [("gb", j, 12 + j), ("gc", j, 16 + j), ("xc", j, 20 + j)]
            order += [("q", h, h) for h in range(4)]
            slots = {}
            nload = [0]

            def ensure_loaded(upto):
                while nload[0] <= min(upto, len(order) - 1):
                    slots[nload[0]] = load_group(order[nload[0]][2])
                    nload[0] += 1
            ensure_loaded(4)

            for tt in range(18):
                s = tt % 2
                src = ctx[b, tt * 128:(tt + 1) * 128, :] if tt < 2 else x[b, (tt - 2) * 128:(tt - 1) * 128, :]
                Ab, Bb, An, Bn = (A1c, B1c, "A1c", "B1c") if tt < 2 else (A1, B1, "A1", "B1")
                S.dma("sp", lambda e, s=s, src=src: e.dma_start(out=XT[s], in_=src), [], [f"XT{s}"], f"d_XT{s}")
                rs, rsn = rstd_of(XT[s], D, [f"XT{s}"], junk, 1.0 / D)
                S.op("dve", lambda e, s=s, rs=rs, Ab=Ab: e.scalar_tensor_tensor(tmpA, XT[s], rs, Ab, op0=ALU.mult, op1=ALU.mult),
                     [f"XT{s}", rsn, An], ["tmpA"])
                S.op("dve", lambda e, s=s, Bb=Bb: e.tensor_tensor(xb[s], tmpA, Bb, op=ALU.add), ["tmpA", Bn], [f"xb{s}"])
                pbk = 6 + (tt % 2)
                pv = bank[pbk].bitcast(BF16)

                def tpA(e, s=s, pv=pv):
                    for k in range(8):
                        ins = e.transpose(pv[:, k * 128:(k + 1) * 128], xb[s][:, k * 128:(k + 1) * 128], idb[:])
                    return ins
                S.op("pe", tpA, [f"xb{s}", "idb"], [PSN[pbk]])
                S.op("act", lambda e, tt=tt, pv=pv: e.activation(out=xnT[:, :, tt * 128:(tt + 1) * 128],
                                                                   in_=pv.rearrange("p (k t) -> p k t", k=8), func=ACT.Copy),
                     [PSN[pbk]], [f"xn{tt}"])

            if stage == "A":
                return
            pending = []

            def flush(keep=0):
                while len(pending) > keep:
                    pending.pop(0)()

            def proj(slot, tok0, ntok):
                pbk = next_pbank()
                tiles = sorted(set(range(tok0 // 128, (tok0 + ntok + 127) // 128)))

                def mm(e):
                    for k in range(8):
                        ins = e.matmul(bank[pbk][:, 0:ntok], wslot[slot][:, k, :], xnT[:, k, tok0:tok0 + ntok], start=(k == 0), stop=(k == 7))
                    return ins
                S.op("pe", mm, [f"ws{slot}"] + [f"xn{t}" for t in tiles], [PSN[pbk]])
                return pbk

            def rope(pbk, dst, dst_region, tb, extra_reads=()):
                i = rope_ctr[0] % 2
                rope_ctr[0] += 1
                qb_ = 4 + i
                S.op("act", lambda e: e.activation(out=pb[i], in_=bank[pbk], func=ACT.Copy), [PSN[pbk]], [f"pb{i}"])
                S.op("dve", lambda e: e.tensor_tensor(t2[i], bank[pbk], cosT[:, tb * 512:(tb + 1) * 512], op=ALU.mult),
                     [PSN[pbk], "cosT"], [f"t2{i}"])

                def part2():
                    S.op("pe", lambda e: e.matmul(bank[qb_], permb[:], pb[i], start=True, stop=True), ["permb", f"pb{i}"], [PSN[qb_]])
                    S.op("dve", lambda e: e.tensor_tensor(t1[i], bank[qb_], sinT[:, tb * 512:(tb + 1) * 512], op=ALU.mult),
                         [PSN[qb_], "sinT"], [f"t1{i}"])
                    S.op("dve", lambda e: e.tensor_tensor(dst, t1[i], t2[i], op=ALU.add),
                         [f"t1{i}", f"t2{i}"] + list(extra_reads), [dst_region])
                pending.append(part2)

            gi = 0
            for h in range(4):
                ensure_loaded(gi + 4)
                sl = slots[gi]
                gi += 1
                pbk = proj(sl, 0, C)
                flush(0)
                S.op("act", lambda e, h=h, pbk=pbk: e.activation(out=kT[:, h, 0:C], in_=bank[pbk][:, 0:C], func=ACT.Copy),
                     [PSN[pbk]], [f"kT{h}"])
                if stage == "B0a":
                    return
                for tb in range(4):
                    pbk = proj(sl, C + tb * 512, 512)
                    flush(0)
                    if stage == "B0c":
                        S.op("act", lambda e, h=h, pbk=pbk, tb=tb: e.activation(out=kT[:, h, C + tb * 512:C + (tb + 1) * 512], in_=bank[pbk], func=ACT.Copy),
                             [PSN[pbk]], [f"kT{h}"])
                        return
                    if stage == "B0d":
                        S.op("dve", lambda e, pbk=pbk, tb=tb: e.tensor_tensor(t2[0], bank[pbk], cosT[:, tb * 512:(tb + 1) * 512], op=ALU.mult),
                             [PSN[pbk], "cosT"], ["t20"])
                        return
                    rope(pbk, kT[:, h, C + tb * 512:C + (tb + 1) * 512], f"kT{h}", tb)
                    if stage == "B0b":
                        flush(0)
                        return
            flush(0)
            if stage == "B1":
                return
            for tt in range(18):
                pbk = next_pbank()

                def mmv(e, tt=tt, pbk=pbk):
                    for k in range(8):
                        ins = e.matmul(bank[pbk], xnT[:, k, tt * 128:(tt + 1) * 128], wv_sb[:, k, :], start=(k == 0), stop=(k == 7))
                    return ins
                S.op("pe", mmv, ["wv", f"xn{tt}"], [PSN[pbk]])
                S.op("act", lambda e, tt=tt, pbk=pbk: e.activation(out=v_sb[:, tt, :], in_=bank[pbk], func=ACT.Copy), [PSN[pbk]], ["v_sb"])
            if stage == "B2":
                return
            S.op("dve", lambda e: e.memset(u_t[:, 0:1], 0.0), ["qdead", "u_t"], ["u_t"])
            S.op("dve", lambda e: e.memset(u_t[:, 2049:2050], 0.0), ["qdead", "u_t"], ["u_t"])
            for j in range(4):
                ensure_loaded(gi + 5)
                sgb, sgc, sxc = slots[gi], slots[gi + 1], slots[gi + 2]
                gi += 3
                for tb in range(4):
                    i = (j * 4 + tb) % 2
                    p_xc = proj(sxc, C + tb * 512, 512)
                    p_gc = proj(sgc, C + tb * 512, 512)
                    p_gb = proj(sgb, C + tb * 512, 512)
                    S.op("act", lambda e, i=i, p_xc=p_xc: e.activation(out=xc_sb[i], in_=bank[p_xc], func=ACT.Copy), [PSN[p_xc]], [f"xc{i}"])
                    S.op("dve", lambda e, i=i, p_gc=p_gc, tb=tb: e.tensor_tensor(u_t[:, 1 + tb * 512:1 + (tb + 1) * 512], bank[p_gc], xc_sb[i], op=ALU.mult),
                         [PSN[p_gc], f"xc{i}", "qdead"], ["u_t"])
                    S.op("act", lambda e, p_gb=p_gb, tb=tb: e.activation(out=gb_t[:, tb * 512:(tb + 1) * 512], in_=bank[p_gb], func=ACT.Copy),
                         [PSN[p_gb], "qdead"], ["gb_t"])
                for hf in range(2):
                    o0 = hf * 1024
                    S.op("act", lambda e, j=j, o0=o0: e.activation(out=y_t, in_=u_t[:, 1 + o0:1 + o0 + 1024], func=ACT.Identity, scale=cw[:, j, 1:2]),
                         ["u_t", "cw", "qdead"], ["y_t"])
                    S.op("dve", lambda e, j=j, o0=o0: e.scalar_tensor_tensor(y_t, u_t[:, o0:o0 + 1024], cw[:, j, 0:1], y_t, op0=ALU.mult, op1=ALU.add),
                         ["u_t", "cw", "y_t"], ["y_t"])
                    S.op("dve", lambda e, j=j, o0=o0: e.scalar_tensor_tensor(y_t, u_t[:, 2 + o0:2 + o0 + 1024], cw[:, j, 2:3], y_t, op0=ALU.mult, op1=ALU.add),
                         ["u_t", "cw", "y_t"], ["y_t"])
                    S.op("dve", lambda e, j=j, o0=o0: e.tensor_tensor(convT[:, j, o0:o0 + 1024], y_t, gb_t[:, o0:o0 + 1024], op=ALU.mult),
                         ["y_t", "gb_t"], ["convT", "convdead"])
            if stage == "B3":
                return
            for h in range(4):
                ensure_loaded(gi + 4)
                sl = slots[gi]
                gi += 1
                for tb in range(4):
                    pbk = proj(sl, C + tb * 512, 512)
                    flush(0)
                    rope(pbk, qT[:, h, tb * 512:(tb + 1) * 512], f"qT{h}", tb, extra_reads=("convdead",))
            flush(0)
            S.op("pe", lambda e: e.matmul(bank[7][:, 0:2], ones_f[0:1, :], lq3[0:1, 6:8], start=True, stop=True),
                 [f"xn{t}" for t in range(18)] + ["ones_f"], [PSN[7], "xndead"])
            for k in range(8):
                S.dma("pool", lambda e, k=k: e.dma_start(out=w_out_sb[:, k, :], in_=w_out[k * 128:(k + 1) * 128, :]),
                      ["xndead"], ["w_out_sb"], "d_wout")

            if stage == "B":
                return
            steps = [(h, qb, kt) for h in range(4) for qb in range(8) for kt in range(18)]
            deferred = {}

            def qk(si):
                h, qb, kt = steps[si]
                sb0 = (si % 2) * 2

                def mm(e):
                    for m in range(2):
                        ins = e.matmul(bank[sb0 + m][:, 0:256], kT[64 * m:64 * (m + 1), h, kt * 128:(kt + 1) * 128],
                                       qT[64 * m:64 * (m + 1), h, qb * 256:(qb + 1) * 256], start=True, stop=True)
                    return ins
                S.op("pe", mm, [f"kT{h}", f"qT{h}"], [PSN[sb0], PSN[sb0 + 1]])

            def fin1(h, qb, fi):
                ob = 4
                db = 5
                i = fi % 2
                S.op("act", lambda e: e.activation(out=dcp, in_=bank[db], func=ACT.Copy), [PSN[db], "xndead"], ["dcp"])
                S.op("act", lambda e: e.activation(out=Ocp, in_=bank[ob], func=ACT.Copy), [PSN[ob], "xndead"], ["Ocp"])
                S.op("dve", lambda e: e.reciprocal(dcp, dcp), ["dcp", "xndead"], ["dcp"])
                S.op("dve", lambda e: e.tensor_tensor(tO, Ocp, dcp, op=ALU.mult), ["Ocp", "dcp"], ["tO"])
                S.op("dve", lambda e: e.scalar_tensor_tensor(o_t[i], tO[:, 256:512], neglam[:, 0:1], tO[:, 0:256], op0=ALU.mult, op1=ALU.add),
                     ["tO", "neglam"], [f"o{i}"])
                S.op("dve", lambda e: e.tensor_tensor(osq[i], o_t[i], o_t[i], op=ALU.mult), [f"o{i}"], [f"osq{i}"])

            def fin2(h, qb, fi):
                i = fi % 2
                S.op("pe", lambda e: e.matmul(bank[6][:, 0:256], ones_f[:], osq[i], start=True, stop=True), ["ones_f", f"osq{i}"], [PSN[6]])
                S.op("act", lambda e: e.activation(out=ln_t[i], in_=bank[6][:, 0:256], func=ACT.Ln, scale=1.0 / 128, bias=eps_t[:, 0:1]),
                     [PSN[6], "eps_t"], [f"ln{i}"])
                S.op("act", lambda e: e.activation(out=rs_t[i], in_=ln_t[i], func=ACT.Exp, scale=-0.5), [f"ln{i}"], [f"rs{i}"])
                S.op("dve", lambda e: e.scalar_tensor_tensor(attnT[:, h, qb * 256:(qb + 1) * 256], o_t[i], sgs[:, 0:1], rs_t[i], op0=ALU.mult, op1=ALU.mult),
                     [f"o{i}", f"rs{i}", "sgs", "xndead"], ["attnT"])

            qk(0)
            fi = 0
            for si, (h, qb, kt) in enumerate(steps):
                if si + 1 < len(steps):
                    qk(si + 1)
                sb0 = (si % 2) * 2
                ai = si % 3
                for m in range(2):
                    S.op("act", lambda e, sb0=sb0, ai=ai, m=m: e.activation(out=at_t[ai][:, m * 256:(m + 1) * 256], in_=bank[sb0 + m][:, 0:256], func=ACT.Exp, scale=0.125),
                         [PSN[sb0 + m], "xndead"], [f"at{ai}"])
                ob = 4
                db = 5

                def av(e, h=h, kt=kt, ai=ai, ob=ob, db=db):
                    e.matmul(bank[ob], v_sb[:, kt, h * 128:(h + 1) * 128], at_t[ai], start=(kt == 0), stop=(kt == 17))
                    return e.matmul(bank[db], ones_b[:], at_t[ai], start=(kt == 0), stop=(kt == 17))
                S.op("pe", av, ["v_sb", f"at{ai}", "ones_b"], [PSN[ob], PSN[db]])
                if si in deferred:
                    deferred.pop(si)()
                if kt == 17:
                    fin1(h, qb, fi)
                    deferred[min(si + 4, len(steps) - 1) if si + 4 < len(steps) else -1] = (lambda h=h, qb=qb, fi=fi: fin2(h, qb, fi))
                    fi += 1
            for k_ in sorted(deferred):
                deferred[k_]()
            S.op("pe", lambda e: e.matmul(bank[7][:, 0:2], ones_f[0:1, :], lq3[0:1, 6:8], start=True, stop=True),
                 ["ones_f", "v_sb"] + [f"kT{h}" for h in range(4)] + [f"qT{h}" for h in range(4)], [PSN[7], "kvdead", "qdead"])

            if stage == "C":
                return
            mod_tile(G1, "G1", b, 2, 1, gtmp2, "gtmp2", False, guard=("qdead",))
            mod_tile(A2, "A2", b, 4, 2, gtmp2, "gtmp2", True, guard=("qdead",))
            mod_tile(B2, "B2", b, 3, None, None, None, False, guard=("qdead",))
            mixin = [attnT[:, h, :] for h in range(4)] + [convT[:, j, :] for j in range(4)]

            def d1(tt):
                s = tt % 2
                S.dma("sp", lambda e: e.dma_start(out=xt2[s], in_=x[b, tt * 128:(tt + 1) * 128, :]), ["kvdead"], [f"xt2{s}"], f"d_xt2{s}")
                for hf in range(2):
                    def mm(e, hf=hf):
                        for k in range(8):
                            ins = e.matmul(bank[hf], mixin[k][:, tt * 128:(tt + 1) * 128], w_out_sb[:, k, hf * 512:(hf + 1) * 512], start=(k == 0), stop=(k == 7))
                        return ins
                    S.op("pe", mm, ["attnT", "convT", "w_out_sb"], [PSN[hf]])
                rs, rsn = rstd_of2(bank[0], bank[1], [PSN[0], PSN[1]], junk, 1.0 / D)
                for hf in range(2):
                    S.op("dve", lambda e, hf=hf: e.scalar_tensor_tensor(tD[:, hf * 512:(hf + 1) * 512], bank[hf], rs, G1[:, hf * 512:(hf + 1) * 512], op0=ALU.mult, op1=ALU.mult),
                         [PSN[hf], rsn, "G1", "kvdead"], ["tD"])
                S.op("dve", lambda e: e.tensor_tensor(h_sb[s], tD, xt2[s], op=ALU.add), ["tD", f"xt2{s}", "kvdead"], [f"h{s}"])
                S.dma("sp", lambda e: e.dma_start(out=h_d[b, tt * 128:(tt + 1) * 128, :], in_=h_sb[s]), [f"h{s}"], [f"h_d{b}_{tt}"], f"d_h{s}")
                rs2, rsn2 = rstd_of(h_sb[s], D, [f"h{s}"], junk, 1.0 / D)
                S.op("dve", lambda e: e.scalar_tensor_tensor(t2D, h_sb[s], rs2, A2, op0=ALU.mult, op1=ALU.mult),
                     [f"h{s}", rsn2, "A2", "kvdead"], ["t2D"])
                S.op("dve", lambda e: e.tensor_tensor(xn2[s], t2D, B2, op=ALU.add), ["t2D", "B2"], [f"xn2{s}"])
                S.dma("pool", lambda e: e.dma_start(out=xn2_d[b][tt * 128:(tt + 1) * 128, :], in_=xn2[s]), [f"xn2{s}"], [f"xn2d{b}"], f"d_x2{s}")

            def d2(tt):
                s = tt % 2

                def tp(e):
                    for k in range(8):
                        ins = e.transpose(bank[2 + k // 4][:, (k % 4) * 128:(k % 4 + 1) * 128], xn2[s][:, k * 128:(k + 1) * 128], idf[:])
                    return ins
                S.op("pe", tp, [f"xn2{s}", "idf"], [PSN[2], PSN[3]])
                for hb in range(2):
                    S.op("act", lambda e, hb=hb: e.activation(out=xn2T[:, hb * 4:(hb + 1) * 4, :], in_=bank[2 + hb].rearrange("p (k t) -> p k t", k=4), func=ACT.Copy),
                         [PSN[2 + hb], "kvdead"], ["xn2T"])

            def d3(tt):
                s = tt % 2

                def mm(e):
                    for k in range(8):
                        ins = e.matmul(bank[4][:, 0:16], xn2T[:, k, :], wr_sb[:, k, :], start=(k == 0), stop=(k == 7))
                    return ins
                S.op("pe", mm, ["xn2T", "wr_sb"], [PSN[4]])
                c = stat_slot(4)
                S.op("dve", lambda e: e.reduce_max(stat[:, c:c + 1], bank[4][:, 0:16], axis=AX.X), [PSN[4]], [f"st{c}"])
                S.op("dve", lambda e: e.tensor_scalar(stat[:, c + 1:c + 2], stat[:, c:c + 1], -1.0, None, op0=ALU.mult), [f"st{c}"], [f"st{c+1}"])
                S.op("act", lambda e: e.activation(out=ex_sb, in_=bank[4][:, 0:16], func=ACT.Exp, bias=stat[:, c + 1:c + 2], accum_out=stat[:, c + 2:c + 3]),
                     [PSN[4], f"st{c+1}"], ["ex_sb", f"st{c+2}"])
                S.op("dve", lambda e: e.reciprocal(stat[:, c + 3:c + 4], stat[:, c + 2:c + 3]), [f"st{c+2}"], [f"st{c+3}"])
                S.op("dve", lambda e: e.tensor_scalar(aff_sb[s], ex_sb, stat[:, c + 3:c + 4], None, op0=ALU.mult), ["ex_sb", f"st{c+3}"], [f"aff{s}"])

            def d4(tt):
                s = tt % 2
                S.op("pe", lambda e: e.transpose(bank[5][0:16, 0:128], aff_sb[s], idf[:]), [f"aff{s}", "idf"], [PSN[5]])
                S.op("act", lambda e: e.activation(out=affT_sb[s][0:16, :], in_=bank[5][0:16, 0:128], func=ACT.Copy), [PSN[5]], [f"affT{s}"])
                S.dma("sp", lambda e: e.dma_start(out=aff_d[32 * b:32 * b + 16, tt * 128:(tt + 1) * 128], in_=affT_sb[s][0:16, :]),
                      [f"affT{s}"], ["aff_d"], f"d_af{s}")

            for i in range(16 + 3):
                if 0 <= i - 3 < 16:
                    d4(i - 3)
                if 0 <= i - 2 < 16:
                    d3(i - 2)
                if 0 <= i - 1 < 16:
                    d2(i - 1)
                if i < 16:
                    d1(i)
            S.barrier()

        for b_ in range(NB if stage != "phase0" else 0):
            do_batch(b_)
        if stage in ("A", "B", "C", "B1", "B2", "B3", "B0a", "B0b", "B0c", "B0d"):
            S.barrier()

        if stage in ("mixer", "phase0", "A", "B", "C", "B1", "B2", "B3", "B0a", "B0b", "B0c", "B0d"):
            AR.reset()
            cp = [AR.take([D], F32) for _ in range(2)]
            for b in range(NB):
                for tt in range(16):
                    s = tt % 2
                    S.dma("sp", lambda e, b=b, tt=tt, s=s: e.dma_start(out=cp[s], in_=h_d[b, tt * 128:(tt + 1) * 128, :]), [], [f"cp{s}"], f"d_cp{s}")
                    S.dma("sp", lambda e, b=b, tt=tt, s=s: e.dma_start(out=out[b, tt * 128:(tt + 1) * 128, :], in_=cp[s]), [f"cp{s}"], ["out"], f"d_co{s}")
            S.barrier(["sp"])
            S.emit()
            return nc

        AR.reset()
        wexp = [[AR.take([8, D], BF16) for _ in range(3)] for _ in range(2)]
        xs = [AR.take([D], BF16) for _ in range(8)]
        xsT = [AR.take([8, 512], BF16) for _ in range(2)]
        actT = [AR.take([8, 512], BF16) for _ in range(2)]
        sgt = [AR.take([512], F32) for _ in range(2)]
        y_sb = [AR.take([D], F32) for _ in range(8)]
        moe_end = AR.off
        work = AR.take([T], F32)
        vals = AR.take([CAP], F32)
        idx = AR.take([CAP], U32)
        idxf = AR.take([CAP], F32)
        idxT = AR.take([2, 128], U32)
        gT = AR.take([2, 128], F32)
        rt_end = AR.off
        AR.off = 0
        NFB = 4
        ffl = [AR.take([D], F32) for _ in range(NFB)]
        hl = [AR.take([D], F32) for _ in range(NFB)]
        G2 = [AR.take([D], F32) for _ in range(2)]
        gtmp3 = AR.take([D], F32)
        tF = [AR.take([D], F32) for _ in range(NFB)]
        ob_t = [AR.take([D], F32) for _ in range(NFB)]
        junk2 = AR.take([D], BF16)
        AR.off = rt_end
        print("moe arena bytes", AR.off)

        def load_expert(e_):
            s = e_ % 2
            for wi, wsrc in enumerate((w_gate, w_up, w_down)):
                for k in range(8):
                    S.dma("pool", lambda e, s=s, wi=wi, wsrc=wsrc, k=k: e.dma_start(out=wexp[s][wi][:, k, :], in_=wsrc[e_, k * 128:(k + 1) * 128, :]),
                          [], [f"we{s}_{wi}"], f"d_we{s}_{wi}")

        load_expert(0)
        S.dma("sp", lambda e: e.dma_start(out=work, in_=aff_d), ["aff_d"], ["work"], "d_work")
        for r in range(CAP // 8):
            S.op("dve", lambda e, r=r: e.max(out=vals[:, r * 8:(r + 1) * 8], in_=work), ["work"], ["vals"])
            S.op("dve", lambda e, r=r: e.max_index(out=idx[:, r * 8:(r + 1) * 8], in_max=vals[:, r * 8:(r + 1) * 8], in_values=work), ["work", "vals"], ["idx"])
            S.op("dve", lambda e, r=r: e.match_replace(out=work, in_to_replace=vals[:, r * 8:(r + 1) * 8], in_values=work, imm_value=-1.0), ["vals", "idx"], ["work"])
        S.op("dve", lambda e: e.tensor_copy(idxf, idx), ["idx"], ["idxf"])
        for ct in range(2):
            S.op("pe", lambda e, ct=ct: e.transpose(bank[0][:, 0:128], idxf[:, ct * 128:(ct + 1) * 128], idf[:]), ["idxf", "idf"], [PSN[0]])
            S.op("dve", lambda e, ct=ct: e.tensor_copy(idxT[:, ct, :], bank[0][:, 0:128]), [PSN[0]], ["idxT"])
            S.op("pe", lambda e, ct=ct: e.transpose(bank[1][:, 0:128], vals[:, ct * 128:(ct + 1) * 128], idf[:]), ["vals", "idf"], [PSN[1]])
            S.op("dve", lambda e, ct=ct: e.tensor_copy(gT[:, ct, :], bank[1][:, 0:128]), [PSN[1]], ["gT"])

        NP = (NB + 1) // 2
        tiles = [(pr, bi, bb, ct) for pr in range(NP) for bi, bb in enumerate([q for q in (2 * pr, 2 * pr + 1) if q < NB]) for ct in range(2)]

        def gathers(e_):
            for j, (pr, bi, bb, ct) in enumerate(tiles):
                row = 32 * bb + e_
                S.dma("pool", lambda e, j=j, bb=bb, ct=ct, row=row: e.indirect_dma_start(
                    out=xs[j], out_offset=None, in_=xn2_d[bb],
                    in_offset=bass.IndirectOffsetOnAxis(ap=idxT[:, ct, row:row + 1], axis=0)),
                    [f"xn2d{bb}", "idxT"], [f"xs{j}"], f"d_xs{j}")

        def transposes(e_):
            for j, (pr, bi, bb, ct) in enumerate(tiles):
                pbk = 6 + j % 2
                pv = bank[pbk].bitcast(BF16)

                def tpx(e, j=j, pv=pv):
                    for k in range(8):
                        ins = e.transpose(pv[:, k * 128:(k + 1) * 128], xs[j][:, k * 128:(k + 1) * 128], idb[:])
                    return ins
                S.op("pe", tpx, [f"xs{j}", "idb"], [PSN[pbk]])
                c0 = (bi * 2 + ct) * 128
                S.op("act", lambda e, pr=pr, c0=c0, pv=pv: e.activation(out=xsT[pr][:, :, c0:c0 + 128], in_=pv.rearrange("p (k t) -> p k t", k=8), func=ACT.Copy),
                     [PSN[pbk]], [f"xsT{pr}"])

        def do_pair(e_, pr):
            ws = e_ % 2
            wg_, wu_, wd_ = wexp[ws]
            mine = [(j, t) for j, t in enumerate(tiles) if t[0] == pr]
            ncol = len(mine) * 128
            for fc in range(8):
                bg = 0 + 2 * (fc % 2)
                bu = 1 + 2 * (fc % 2)
                si_ = fc % 2

                def mmg(e, fc=fc, bg=bg):
                    for k in range(8):
                        ins = e.matmul(bank[bg][:, 0:ncol], wg_[:, k, fc * 128:(fc + 1) * 128], xsT[pr][:, k, 0:ncol], start=(k == 0), stop=(k == 7))
                    return ins

                def mmu(e, fc=fc, bu=bu):
                    for k in range(8):
                        ins = e.matmul(bank[bu][:, 0:ncol], wu_[:, k, fc * 128:(fc + 1) * 128], xsT[pr][:, k, 0:ncol], start=(k == 0), stop=(k == 7))
                    return ins
                S.op("pe", mmg, [f"we{ws}_0", f"xsT{pr}"], [PSN[bg]])
                S.op("pe", mmu, [f"we{ws}_1", f"xsT{pr}"], [PSN[bu]])
                S.op("act", lambda e, bg=bg, si_=si_: e.activation(out=sgt[si_][:, 0:ncol], in_=bank[bg][:, 0:ncol], func=ACT.Silu), [PSN[bg]], [f"sgt{si_}"])
                S.op("dve", lambda e, fc=fc, bu=bu, si_=si_: e.tensor_tensor(actT[pr][:, fc, 0:ncol], sgt[si_][:, 0:ncol], bank[bu][:, 0:ncol], op=ALU.mult),
                     [PSN[bu], f"sgt{si_}"], [f"actT{pr}"])
            for j, (pr_, bi, bb, ct) in mine:
                row = 32 * bb + e_
                c0 = (bi * 2 + ct) * 128
                for hf in range(2):
                    yb = 4 + hf

                    def mmd(e, c0=c0, hf=hf, yb=yb):
                        for k in range(8):
                            ins = e.matmul(bank[yb], actT[pr][:, k, c0:c0 + 128], wd_[:, k, hf * 512:(hf + 1) * 512], start=(k == 0), stop=(k == 7))
                        return ins
                    S.op("pe", mmd, [f"actT{pr}", f"we{ws}_2"], [PSN[yb]])
                    if hf == 0:
                        S.op("act", lambda e, j=j, yb=yb, ct=ct, row=row: e.activation(out=y_sb[j][:, 0:512], in_=bank[yb], func=ACT.Identity, scale=gT[:, ct, row:row + 1]),
                             [PSN[yb], "gT"], [f"y{j}a"])
                    else:
                        S.op("dve", lambda e, j=j, yb=yb, ct=ct, row=row: e.tensor_scalar(y_sb[j][:, 512:1024], bank[yb], gT[:, ct, row:row + 1], None, op0=ALU.mult),
                             [PSN[yb], "gT"], [f"y{j}b"])
                par, ppar = e_ % 2, (e_ - 1) % 2
                S.dma("pool", lambda e, j=j, bb=bb, ct=ct, row=row: e.indirect_dma_start(
                    out=ff_d[bb], out_offset=bass.IndirectOffsetOnAxis(ap=idxT[:, ct, row:row + 1], axis=0),
                    in_=y_sb[j], in_offset=None, compute_op=ALU.add),
                    [f"y{j}a", f"y{j}b", "idxT", f"ff{bb}", f"ffs{bb}_0_{ppar}", f"ffs{bb}_1_{ppar}"], [f"ffs{bb}_{ct}_{par}"], f"d_y{j}")

        gathers(0)
        for e_ in range(NE):
            transposes(e_)
            if e_ + 1 < NE:
                load_expert(e_ + 1)
                gathers(e_ + 1)
            for pr in range(NP):
                do_pair(e_, pr)
        S.barrier()

        bc_load(gtmp3, gains[3:4, :], "gtmp3", "d_bc_gtmp3")
        for b in range(NB):
            g = b % 2
            bc_load(G2[g], mod_d[b:b + 1, 5 * D:6 * D], f"G2{g}", f"d_bc_G2{g}")
            S.op("dve", lambda e, g=g: e.tensor_tensor(G2[g], G2[g], gtmp3, op=ALU.mult), [f"G2{g}", "gtmp3"], [f"G2{g}"])
            for tt in range(16):
                s = (b * 16 + tt) % NFB
                S.dma("sp", lambda e, b=b, tt=tt, s=s: e.dma_start(out=ffl[s], in_=ff_d[b][tt * 128:(tt + 1) * 128, :]), [f"ff{b}"], [f"ffl{s}"], f"d_ffl{s}")
                S.dma("sp", lambda e, b=b, tt=tt, s=s: e.dma_start(out=hl[s], in_=h_d[b, tt * 128:(tt + 1) * 128, :]), [f"h_d{b}_{tt}"], [f"hl{s}"], f"d_hl{s}")
                rs, rsn = rstd_of(ffl[s], D, [f"ffl{s}"], junk2, 1.0 / D)
                S.op("dve", lambda e, s=s, rs=rs, g=g: e.scalar_tensor_tensor(tF[s], ffl[s], rs, G2[g], op0=ALU.mult, op1=ALU.mult), [f"ffl{s}", rsn, f"G2{g}"], [f"tF{s}"])
                S.op("dve", lambda e, s=s: e.tensor_tensor(ob_t[s], tF[s], hl[s], op=ALU.add), [f"tF{s}", f"hl{s}"], [f"ob{s}"])
                S.dma("sp", lambda e, b=b, tt=tt, s=s: e.dma_start(out=out[b, tt * 128:(tt + 1) * 128, :], in_=ob_t[s]), [f"ob{s}"], ["out"], f"d_out{s}")
        S.barrier(["sp"])
        S.emit()
    return nc


def _rope_tables():
    t = np.arange(T)
    row = (t // 64).astype(np.float32)
    col = (t % 64).astype(np.float32)
    inv = (10000.0 ** (-np.arange(0, 32, 2, dtype=np.float32) / 32)).astype(np.float32)
    ang_r = row[:, None] * inv[None, :]
    ang_c = col[:, None] * inv[None, :]
    ang = np.concatenate([ang_r, ang_r, ang_c, ang_c], axis=-1)
    cos = np.cos(ang).astype(np.float32).T
    sin = np.sin(ang).astype(np.float32).T
    sgn = np.concatenate([-np.ones(16), np.ones(16), -np.ones(16), np.ones(16)]).astype(np.float32)[:, None]
    sin = sin * sgn
    cosT = np.ascontiguousarray(np.concatenate([cos, cos], 0))
    sinT = np.ascontiguousarray(np.concatenate([sin, sin], 0))
    perm = np.zeros((128, 128), np.float32)
    for i in range(128):
        perm[i ^ 16, i] = 1.0
    return cosT, sinT, perm


def make_in_maps(inputs, NB=4, ncores=NCORES, ne=NE):
    f = lambda a: np.ascontiguousarray(np.asarray(a, dtype=np.float32))
    x = f(inputs["x"]); c = f(inputs["c"]); ctx = f(inputs["ctx"]); c_ctx = f(inputs["c_ctx"])
    w_in = f(inputs["w_in"])[0]
    cosT, sinT, perm = _rope_tables()
    w_in_r = np.ascontiguousarray(w_in.reshape(8, 128, 24, 128).transpose(2, 1, 0, 3))
    w_v_r = np.ascontiguousarray(w_in[:, 1024:1536].reshape(8, 128, 512).transpose(1, 0, 2))
    shared = dict(
        w_ada=f(inputs["w_ada"])[0], b_ada=f(inputs["b_ada"]),
        gains=np.ascontiguousarray(np.concatenate([f(inputs["norm_pre_mix"]), f(inputs["norm_post_mix"]),
                                                   f(inputs["norm_pre_ffn"]), f(inputs["norm_post_ffn"])], 0)),
        w_in_r=w_in_r, w_v_r=w_v_r,
        convw=np.ascontiguousarray(f(inputs["conv_w"])[0].T.reshape(4, 128, 3).transpose(1, 0, 2)),
        lqk=np.ascontiguousarray(np.concatenate([f(inputs["lambda_q1"]), f(inputs["lambda_k1"]),
                                                 f(inputs["lambda_q2"]), f(inputs["lambda_k2"])], 1)),
        subln=np.ascontiguousarray(f(inputs["subln_g"]).reshape(128, 1)),
        w_out=f(inputs["w_out"])[0],
        w_router=np.ascontiguousarray(f(inputs["w_router"])[0].reshape(8, 128, 16).transpose(1, 0, 2)),
        w_gate=f(inputs["w_gate"])[0][:ne], w_up=f(inputs["w_up"])[0][:ne], w_down=f(inputs["w_down"])[0][:ne],
        ident=np.eye(128, dtype=np.float32), perm=perm, cosT=cosT, sinT=sinT,
    )
    maps = []
    for i in range(ncores):
        sl = slice(i * NB, (i + 1) * NB)
        cc = np.concatenate([c[sl], c_ctx[None, :]], 0)
        if cc.shape[0] < 5:
            cc = np.concatenate([cc[:-1], np.zeros((5 - cc.shape[0], D), np.float32), cc[-1:]], 0)
        ccT = np.ascontiguousarray(cc.T.reshape(8, 128, 5).transpose(1, 0, 2))
        m = dict(shared)
        m.update(x=np.ascontiguousarray(x[sl]), ctx=np.ascontiguousarray(ctx[sl]), ccT=ccT)
        maps.append(m)
    return maps


def kernel(**inputs):
    NB = 4
    nc = build(NB=NB, stage="full")
    maps = make_in_maps(inputs, NB=NB, ncores=NCORES)
    res = run_bass_kernel_spmd(nc, maps, core_ids=list(range(NCORES)))
    return np.concatenate([np.asarray(r["out"]) for r in res.results], axis=0).astype(np.float32)
```

```python
import math
from contextlib import ExitStack
import numpy as np
import concourse.bass as bass
import concourse.mybir as mybir
from concourse.bass_utils import run_bass_kernel_spmd

F32 = mybir.dt.float32
BF16 = mybir.dt.bfloat16
U32 = mybir.dt.uint32
ACT = mybir.ActivationFunctionType
ALU = mybir.AluOpType
AX = mybir.AxisListType

T = 2048
C = 256
D = 1024
TK = T + C
NE = 16
CAP = 256
EPS = 1e-6
NCORES = 8


class Sched:
    ENGS = ("pe", "act", "dve", "pool", "sp")

    def __init__(self, nc, stack):
        self.nc = nc
        self.stack = stack
        self.sems = {}
        self.eng = {}
        for n in self.ENGS:
            self.sems["s_" + n] = stack.enter_context(nc.semaphore("s_" + n))
            self.eng[n] = dict(ops=[], cnt=0, waited={})
        self.lastw = {}
        self.readers = {}
        self.dcum = {}

    def _deps(self, reads, writes):
        d = []
        for r in reads:
            if r in self.lastw:
                d.append(self.lastw[r])
        for w in writes:
            if w in self.lastw:
                d.append(self.lastw[w])
            d.extend(self.readers.get(w, ()))
        return d

    def _waits(self, en, deps):
        E = self.eng[en]
        need = {}
        for (s, v) in deps:
            if en == "pe" and s == "s_pe":
                continue
            if E["waited"].get(s, 0) >= v:
                continue
            if need.get(s, 0) < v:
                need[s] = v
        for s, v in need.items():
            E["waited"][s] = v
        return list(need.items())

    def _record(self, ev, reads, writes):
        for r in reads:
            self.readers.setdefault(r, []).append(ev)
        for w in writes:
            self.lastw[w] = ev
            self.readers[w] = []

    def op(self, en, fn, reads=(), writes=()):
        excl = [r for r in reads if r.startswith("ps") and r not in writes]
        if excl:
            reads = [r for r in reads if r not in excl]
            writes = list(writes) + excl
        deps = self._deps(reads, writes)
        waits = self._waits(en, deps)
        E = self.eng[en]
        E["cnt"] += 1
        ev = ("s_" + en, E["cnt"])
        self._record(ev, reads, writes)
        sems = self.sems

        def run(e, fn=fn, waits=waits, sem=sems["s_" + en]):
            for (s, v) in waits:
                e.wait_ge(sems[s], v)
            fn(e).then_inc(sem, 1)

        E["ops"].append(run)

    def dma(self, q, mk, reads, writes, key):
        deps = self._deps(reads, writes)
        waits = self._waits(q, deps)
        if key not in self.sems:
            self.sems[key] = self.stack.enter_context(self.nc.semaphore(key))
            self.dcum[key] = 0
        self.dcum[key] += 16
        ev = (key, self.dcum[key])
        self._record(ev, reads, writes)
        sems = self.sems

        def run(e, mk=mk, waits=waits, sem=sems[key]):
            for (s, v) in waits:
                e.wait_ge(sems[s], v)
            mk(e).then_inc(sem, 16)

        self.eng[q]["ops"].append(run)

    def _all_events(self):
        ev = [("s_" + n, self.eng[n]["cnt"]) for n in self.ENGS if self.eng[n]["cnt"] > 0]
        ev += [(k, v) for k, v in self.dcum.items() if v > 0]
        return ev

    def barrier(self, engines=None):
        allev = self._all_events()
        for n in (engines or self.ENGS):
            waits = self._waits(n, [ev for ev in allev if ev[0] != "s_" + n])
            sems = self.sems

            def run(e, waits=waits):
                for (s, v) in waits:
                    e.wait_ge(sems[s], v)

            self.eng[n]["ops"].append(run)

    def emit(self):
        nc = self.nc
        with nc.Block() as block:
            @block.tensor
            def _(e):
                for f in self.eng["pe"]["ops"]:
                    f(e)

            @block.scalar
            def _(e):
                for f in self.eng["act"]["ops"]:
                    f(e)

            @block.vector
            def _(e):
                for f in self.eng["dve"]["ops"]:
                    f(e)

            @block.gpsimd
            def _(e):
                for f in self.eng["pool"]["ops"]:
                    f(e)

            @block.sync
            def _(e):
                for f in self.eng["sp"]["ops"]:
                    f(e)


DT_BYTES = {F32: 4, BF16: 2, U32: 4}


class Arena:
    def __init__(self, t, nbytes):
        self.t = t
        self.nbytes = nbytes
        self.off = 0

    def reset(self):
        self.off = 0

    def take(self, free, dtype):
        n = 1
        for s in free:
            n *= s
        sz = n * DT_BYTES[dtype]
        a = self.off
        self.off += (sz + 63) // 64 * 64
        assert self.off <= self.nbytes, (self.off, self.nbytes)
        ap = self.t[:, a // 4:(a + sz) // 4]
        if dtype != F32:
            ap = ap.bitcast(dtype)
        if len(free) == 2:
            ap = ap.rearrange("p (a b) -> p a b", a=free[0])
        elif len(free) == 3:
            ap = ap.rearrange("p (a b c) -> p a b c", a=free[0], b=free[1])
        return ap


def build(NB=4, stage="full"):
    nc = bass.Bass("TRN2", target_bir_lowering=False)

    def din(n, s, d=F32):
        return nc.dram_tensor(n, s, d, kind="ExternalInput").ap()

    x = din("x", [NB, T, D])
    ctx = din("ctx", [NB, C, D])
    ccT = din("ccT", [128, 8, 5])
    w_ada = din("w_ada", [D, 6 * D])
    b_ada = din("b_ada", [1, 6 * D])
    gains = din("gains", [4, D])
    w_in_r = din("w_in_r", [24, 128, 8, 128])
    w_v_r = din("w_v_r", [128, 8, 512])
    convw = din("convw", [128, 4, 3])
    lqk = din("lqk", [1, 256])
    subln = din("subln", [128, 1])
    w_out = din("w_out", [D, D])
    w_router = din("w_router", [128, 8, 16])
    NE_decl = NE if stage == "full" else 1
    w_gate = din("w_gate", [NE_decl, D, D])
    w_up = din("w_up", [NE_decl, D, D])
    w_down = din("w_down", [NE_decl, D, D])
    ident = din("ident", [128, 128])
    perm = din("perm", [128, 128])
    cosT_d = din("cosT", [128, T])
    sinT_d = din("sinT", [128, T])
    out = nc.dram_tensor("out", [NB, T, D], F32, kind="ExternalOutput").ap()
    mod_d = nc.dram_tensor("mod_d", [5, 6 * D], F32).ap()
    h_d = nc.dram_tensor("h_d", [NB, T, D], F32).ap()
    aff_d = nc.dram_tensor("aff_d", [128, T], F32).ap()
    xn2_d = [nc.dram_tensor(f"xn2_d{b}", [T, D], BF16).ap() for b in range(NB)]
    ff_d = [nc.dram_tensor(f"ff_d{b}", [T, D], F32).ap() for b in range(NB)]

    with ExitStack() as st:
        S = Sched(nc, st)

        def sb(n, s, d):
            return st.enter_context(nc.sbuf_tensor(n, s, d))

        idf = sb("idf", [128, 128], F32)
        idb = sb("idb", [128, 128], BF16)
        permf = sb("permf", [128, 128], F32)
        permb = sb("permb", [128, 128], BF16)
        ones_b = sb("ones_b", [128, 128], BF16)
        ones_f = sb("ones_f", [128, 128], F32)
        neghalf = sb("neghalf", [128, 256], F32)
        eps_t = sb("eps_t", [128, 1], F32)
        cw = sb("cw", [128, 4, 3], F32)
        sgs = sb("sgs", [128, 1], F32)
        neglam = sb("neglam", [128, 2], F32)
        wr_sb = sb("wr_sb", [128, 8, 16], F32)
        stat = sb("stat", [128, 96], F32)
        lq_sb = sb("lq_sb", [1, 256], F32)
        lq2 = sb("lq2", [1, 128], F32)
        lq3 = sb("lq3", [1, 8], F32)
        ARENA_BYTES = 200 * 1024
        arena_t = sb("arena", [128, ARENA_BYTES // 4], F32)
        AR = Arena(arena_t, ARENA_BYTES)
        psS = [st.enter_context(nc.psum_tensor(f"psS{i}", [128, 512], F32)) for i in range(8)]
        bank = [p[:] for p in psS]
        PSN = [f"ps{i}" for i in range(8)]

        stat_ctr = [0]

        def stat_slot(n=1):
            c = stat_ctr[0]
            if c + n > 96:
                c = 0
            stat_ctr[0] = c + n
            return c

        def rstd_of(src, nfree, src_reads, junk, inv_n):
            c = stat_slot(3)
            r0, r1, r2 = f"st{c}", f"st{c+1}", f"st{c+2}"
            S.op("act", lambda e: e.activation(out=junk, in_=src, func=ACT.Square, accum_out=stat[:, c:c + 1]),
                 src_reads, [r0, "junk"])
            S.op("dve", lambda e: e.tensor_scalar(stat[:, c + 1:c + 2], stat[:, c:c + 1], inv_n, EPS, op0=ALU.mult, op1=ALU.add),
                 [r0], [r1])
            S.op("pool", lambda e: e.tensor_tensor(stat[:, c + 2:c + 3], stat[:, c + 1:c + 2], neghalf[:, 0:1], op=ALU.pow),
                 [r1, "neghalf"], [r2])
            return stat[:, c + 2:c + 3], r2

        def rstd_of2(src0, src1, src_reads, junk, inv_n):
            c = stat_slot(5)
            ra, rb, r0, r1, r2 = (f"st{c + i}" for i in range(5))
            S.op("act", lambda e: e.activation(out=junk[:, 0:512], in_=src0, func=ACT.Square, accum_out=stat[:, c:c + 1]),
                 src_reads, [ra, "junk"])
            S.op("act", lambda e: e.activation(out=junk[:, 512:1024], in_=src1, func=ACT.Square, accum_out=stat[:, c + 1:c + 2]),
                 src_reads, [rb, "junk"])
            S.op("dve", lambda e: e.tensor_tensor(stat[:, c + 2:c + 3], stat[:, c:c + 1], stat[:, c + 1:c + 2], op=ALU.add), [ra, rb], [r0])
            S.op("dve", lambda e: e.tensor_scalar(stat[:, c + 3:c + 4], stat[:, c + 2:c + 3], inv_n, EPS, op0=ALU.mult, op1=ALU.add), [r0], [r1])
            S.op("pool", lambda e: e.tensor_tensor(stat[:, c + 4:c + 5], stat[:, c + 3:c + 4], neghalf[:, 0:1], op=ALU.pow), [r1, "neghalf"], [r2])
            return stat[:, c + 4:c + 5], r2

        S.dma("sp", lambda e: e.dma_start(out=idf[:], in_=ident), [], ["idf"], "d_c1")
        S.dma("sp", lambda e: e.dma_start(out=permf[:], in_=perm), [], ["permf"], "d_c2")
        S.dma("sp", lambda e: e.dma_start(out=cw[:], in_=convw), [], ["cw"], "d_c3")
        S.dma("sp", lambda e: e.dma_start(out=sgs[:], in_=subln), [], ["sgs"], "d_c4")
        S.dma("sp", lambda e: e.dma_start(out=wr_sb[:], in_=w_router), [], ["wr_sb"], "d_c5")
        S.dma("sp", lambda e: e.dma_start(out=lq_sb[:], in_=lqk), [], ["lq_sb"], "d_c6")
        S.op("dve", lambda e: e.tensor_copy(idb[:], idf[:]), ["idf"], ["idb"])
        S.op("dve", lambda e: e.tensor_copy(permb[:], permf[:]), ["permf"], ["permb"])
        S.op("dve", lambda e: e.memset(ones_b[:], 1.0), [], ["ones_b"])
        S.op("dve", lambda e: e.memset(ones_f[:], 1.0), [], ["ones_f"])
        S.op("dve", lambda e: e.memset(neghalf[:], -0.5), [], ["neghalf"])
        S.op("dve", lambda e: e.memset(eps_t[:], EPS), [], ["eps_t"])
        S.op("dve", lambda e: e.tensor_scalar(sgs[:], sgs[:], 0.8, None, op0=ALU.mult), ["sgs"], ["sgs"])
        lqv = lq_sb[0:1, :].rearrange("p (a b c) -> p a b c", a=2, b=2)
        S.op("dve", lambda e: e.tensor_tensor(lq2[0:1, :].rearrange("p (a c) -> p a c", a=2), lqv[:, :, 0, :], lqv[:, :, 1, :], op=ALU.mult),
             ["lq_sb"], ["lq2"])
        S.op("dve", lambda e: e.reduce_sum(lq3[0:1, 0:2], lq2[0:1, :].rearrange("p (a c) -> p a c", a=2), axis=AX.X), ["lq2"], ["lq3a"])
        S.op("act", lambda e: e.activation(out=lq3[0:1, 2:4], in_=lq3[0:1, 0:2], func=ACT.Exp), ["lq3a"], ["lq3b"])
        S.op("dve", lambda e: e.tensor_tensor(lq3[0:1, 4:5], lq3[0:1, 3:4], lq3[0:1, 2:3], op=ALU.subtract), ["lq3b"], ["lq3c"])
        S.op("dve", lambda e: e.tensor_scalar(lq3[0:1, 6:7], lq3[0:1, 4:5], -0.2, None, op0=ALU.add), ["lq3c"], ["lq3d"])
        S.op("dve", lambda e: e.tensor_copy(lq3[0:1, 7:8], lq3[0:1, 6:7]), ["lq3d"], ["lq3e"])
        S.op("pe", lambda e: e.matmul(bank[7][:, 0:2], ones_f[0:1, :], lq3[0:1, 6:8], start=True, stop=True),
             ["ones_f", "lq3d", "lq3e"], [PSN[7]])
        S.op("dve", lambda e: e.tensor_copy(neglam[:], bank[7][:, 0:2]), [PSN[7]], ["neglam"])

        AR.reset()
        ccT_sb = AR.take([8, 5], F32)
        siluT = AR.take([8, 5], F32)
        bada_sb = AR.take([6 * D], F32)
        wa = [AR.take([8, 512], F32) for _ in range(2)]
        mstage = [AR.take([512], F32) for _ in range(2)]
        zero_t = AR.take([2048], F32)
        S.dma("sp", lambda e: e.dma_start(out=ccT_sb, in_=ccT), [], ["ccT"], "d_c7")
        S.dma("sp", lambda e: e.dma_start(out=bada_sb[0:1, :], in_=b_ada), [], ["bada"], "d_c8")
        S.op("act", lambda e: e.activation(out=siluT, in_=ccT_sb, func=ACT.Silu), ["ccT"], ["siluT"])
        S.op("dve", lambda e: e.memset(zero_t, 0.0), [], ["zero_t"])
        for b in range(NB):
            ffv = ff_d[b].rearrange("(p r) d -> p (r d)", p=128)
            for j in range(8):
                S.dma("sp", lambda e, ffv=ffv, j=j: e.dma_start(out=ffv[:, j * 2048:(j + 1) * 2048], in_=zero_t),
                      ["zero_t"], [f"ff{b}"], "d_z")
        S.dma("sp", lambda e: e.dma_start(out=aff_d, in_=zero_t), ["zero_t"], ["aff_d"], "d_z")
        for j in range(12):
            s = j % 2
            S.dma("sp", lambda e, j=j, s=s: e.dma_start(out=wa[s], in_=w_ada[:, j * 512:(j + 1) * 512].rearrange("(k p) n -> p k n", p=128)),
                  [], [f"wa{s}"], f"d_wa{s}")

            def mm0(e, j=j, s=s):
                for k in range(8):
                    e.matmul(bank[4][0:5, :], siluT[:, k, :], wa[s][:, k, :], start=(k == 0), stop=False)
                return e.matmul(bank[4][0:5, :], ones_f[0:1, 0:5], bada_sb[0:1, j * 512:(j + 1) * 512], start=False, stop=True)
            S.op("pe", mm0, ["siluT", f"wa{s}", "bada", "ones_f"], [PSN[4]])
            S.op("act", lambda e, s=s: e.activation(out=mstage[s][0:5, :], in_=bank[4][0:5, :], func=ACT.Copy), [PSN[4]], [f"ms{s}"])
            S.dma("sp", lambda e, j=j, s=s: e.dma_start(out=mod_d[:, j * 512:(j + 1) * 512], in_=mstage[s][0:5, :]),
                  [f"ms{s}"], ["mod_d"], f"d_ms{s}")
        S.barrier()

        AR.reset()
        cosT = AR.take([T], F32)
        sinT = AR.take([T], F32)
        xnT = AR.take([8, TK], BF16)
        R1 = xnT.rearrange("p a b -> p (a b)")
        attnT = R1[:, 0:4 * T].rearrange("p (a b) -> p a b", a=4)
        w_out_sb = R1[:, 4 * T:4 * T + 8 * D].rearrange("p (a b) -> p a b", a=8)
        kT = AR.take([4, TK], BF16)
        v_sb = AR.take([18, 512], BF16)
        KVf = arena_t[:, 0:0]
        qc_off = AR.off
        qT = AR.take([4, T], BF16)
        AR.off = qc_off
        u_t = AR.take([2064], F32)
        gb_t = AR.take([T], F32)
        y_t = AR.take([1024], F32)
        qc_end = max(AR.off, qc_off + 4 * T * 2)
        AR.off = qc_off
        A2 = AR.take([D], F32)
        B2 = AR.take([D], F32)
        G1 = AR.take([D], F32)
        gtmp2 = AR.take([D], F32)
        AR.off = qc_end
        convT = AR.take([4, T], BF16)
        A1 = AR.take([D], F32)
        B1 = AR.take([D], F32)
        A1c = AR.take([D], F32)
        B1c = AR.take([D], F32)
        ta_off = AR.off
        XT = [AR.take([D], F32) for _ in range(2)]
        xb = [AR.take([D], BF16) for _ in range(2)]
        tmpA = AR.take([D], F32)
        junk = AR.take([D], BF16)
        ta_end = AR.off
        AR.off = ta_off
        at_t = [AR.take([512], BF16) for _ in range(3)]
        tO = AR.take([512], F32)
        o_t = [AR.take([256], F32) for _ in range(2)]
        osq = [AR.take([256], F32) for _ in range(2)]
        ln_t = [AR.take([256], F32) for _ in range(2)]
        rs_t = [AR.take([256], F32) for _ in range(2)]
        Ocp = AR.take([512], F32)
        dcp = AR.take([512], F32)
        assert AR.off <= ta_end
        AR.off = ta_end
        pb = [AR.take([512], BF16) for _ in range(2)]
        t1 = [AR.take([512], F32) for _ in range(2)]
        t2 = [AR.take([512], F32) for _ in range(2)]
        xc_sb = [AR.take([512], F32) for _ in range(2)]
        wslot = [AR.take([8, 128], BF16) for _ in range(6)]
        wv_sb = AR.take([8, 512], BF16)
        mix_end = AR.off
        kv_off = (kT.offset if hasattr(kT, "offset") else None)
        AR.off = 2 * T * 4 + 8 * TK * 2
        xt2 = [AR.take([D], F32) for _ in range(2)]
        tD = AR.take([D], F32)
        h_sb = [AR.take([D], F32) for _ in range(2)]
        t2D = AR.take([D], F32)
        xn2 = [AR.take([D], F32) for _ in range(2)]
        xn2T = AR.take([8, 128], F32)
        assert AR.off <= 2 * T * 4 + 8 * TK * 2 + 4 * TK * 2 + 18 * 512 * 2 + 64
        AR.off = mix_end
        lg_sb = AR.take([16], F32)
        ex_sb = AR.take([16], F32)
        aff_sb = [AR.take([16], F32) for _ in range(2)]
        affT_sb = [AR.take([128], F32) for _ in range(2)]
        print("mixer arena bytes", AR.off)

        S.dma("sp", lambda e: e.dma_start(out=cosT, in_=cosT_d), [], ["cosT"], "d_c9")
        S.dma("sp", lambda e: e.dma_start(out=sinT, in_=sinT_d), [], ["sinT"], "d_c10")
        S.op("dve", lambda e: e.memset(u_t, 0.0), [], ["u_t"])

        def bc_load(dst, row_ap, region, key, guard=()):
            S.dma("sp", lambda e: e.dma_start(out=dst, in_=row_ap.partition_broadcast(128)), list(guard), [region], key)

        def mod_tile(dst, region, b, seg, gain_idx, tmp, tmp_region, plus_one, guard=()):
            bc_load(dst, mod_d[b:b + 1, seg * D:(seg + 1) * D], region, "d_bc_" + region, guard)
            if gain_idx is not None:
                bc_load(tmp, gains[gain_idx:gain_idx + 1, :], tmp_region, "d_bc_" + tmp_region, guard)
                if plus_one:
                    S.op("dve", lambda e: e.scalar_tensor_tensor(dst, dst, 1.0, tmp, op0=ALU.add, op1=ALU.mult),
                         [region, tmp_region], [region])
                else:
                    S.op("dve", lambda e: e.tensor_tensor(dst, dst, tmp, op=ALU.mult), [region, tmp_region], [region])

        mod_tile(A1c, "A1c", 4, 1, 0, tmpA, "tmpA", True)
        mod_tile(B1c, "B1c", 4, 0, None, None, None, False)

        grp_ctr = [0]

        def load_group(g):
            s = grp_ctr[0] % 6
            grp_ctr[0] += 1
            S.dma("pool", lambda e: e.dma_start(out=wslot[s], in_=w_in_r[g]), [], [f"ws{s}"], f"d_ws{s}")
            return s

        pbank_ctr = [0]

        def next_pbank():
            i = pbank_ctr[0] % 4
            pbank_ctr[0] += 1
            return i

        rope_ctr = [0]

        def do_batch(b):
            mod_tile(A1, "A1", b, 1, 0, tmpA, "tmpA", True)
            mod_tile(B1, "B1", b, 0, None, None, None, False)
            S.dma("pool", lambda e: e.dma_start(out=wv_sb, in_=w_v_r), [], ["wv"], "d_wv")
            order = [("k", h, 4 + h) for h in range(4)]
            for j in range(4):
                order += [("gb", j, 12 + j), ("gc", j, 16 + j), ("xc", j, 20 + j)]
            order += [("q", h, h) for h in range(4)]
            slots = {}
            nload = [0]

            def ensure_loaded(upto):
                while nload[0] <= min(upto, len(order) - 1):
                    slots[nload[0]] = load_group(order[nload[0]][2])
                    nload[0] += 1
            ensure_loaded(4)

            for tt in range(18):
                s = tt % 2
                src = ctx[b, tt * 128:(tt + 1) * 128, :] if tt < 2 else x[b, (tt - 2) * 128:(tt - 1) * 128, :]
                Ab, Bb, An, Bn = (A1c, B1c, "A1c", "B1c") if tt < 2 else (A1, B1, "A1", "B1")
                S.dma("sp", lambda e, s=s, src=src: e.dma_start(out=XT[s], in_=src), [], [f"XT{s}"], f"d_XT{s}")
                rs, rsn = rstd_of(XT[s], D, [f"XT{s}"], junk, 1.0 / D)
                S.op("dve", lambda e, s=s, rs=rs, Ab=Ab: e.scalar_tensor_tensor(tmpA, XT[s], rs, Ab, op0=ALU.mult, op1=ALU.mult),
                     [f"XT{s}", rsn, An], ["tmpA"])
                S.op("dve", lambda e, s=s, Bb=Bb: e.tensor_tensor(xb[s], tmpA, Bb, op=ALU.add), ["tmpA", Bn], [f"xb{s}"])
                pbk = 6 + (tt % 2)
                pv = bank[pbk].bitcast(BF16)

                def tpA(e, s=s, pv=pv):
                    for k in range(8):
                        ins = e.transpose(pv[:, k * 128:(k + 1) * 128], xb[s][:, k * 128:(k + 1) * 128], idb[:])
                    return ins
                S.op("pe", tpA, [f"xb{s}", "idb"], [PSN[pbk]])
                S.op("act", lambda e, tt=tt, pv=pv: e.activation(out=xnT[:, :, tt * 128:(tt + 1) * 128],
                                                                   in_=pv.rearrange("p (k t) -> p k t", k=8), func=ACT.Copy),
                     [PSN[pbk]], [f"xn{tt}"])

            if stage == "A":
                return
            pending = []

            def flush(keep=0):
                while len(pending) > keep:
                    pending.pop(0)()

            def proj(slot, tok0, ntok):
                pbk = next_pbank()
                tiles = sorted(set(range(tok0 // 128, (tok0 + ntok + 127) // 128)))

                def mm(e):
                    for k in range(8):
                        ins = e.matmul(bank[pbk][:, 0:ntok], wslot[slot][:, k, :], xnT[:, k, tok0:tok0 + ntok], start=(k == 0), stop=(k == 7))
                    return ins
                S.op("pe", mm, [f"ws{slot}"] + [f"xn{t}" for t in tiles], [PSN[pbk]])
                return pbk

            def rope(pbk, dst, dst_region, tb, extra_reads=()):
                i = rope_ctr[0] % 2
                rope_ctr[0] += 1
                qb_ = 4 + i
                S.op("act", lambda e: e.activation(out=pb[i], in_=bank[pbk], func=ACT.Copy), [PSN[pbk]], [f"pb{i}"])
                S.op("dve", lambda e: e.tensor_tensor(t2[i], bank[pbk], cosT[:, tb * 512:(tb + 1) * 512], op=ALU.mult),
                     [PSN[pbk], "cosT"], [f"t2{i}"])

                def part2():
                    S.op("pe", lambda e: e.matmul(bank[qb_], permb[:], pb[i], start=True, stop=True), ["permb", f"pb{i}"], [PSN[qb_]])
                    S.op("dve", lambda e: e.tensor_tensor(t1[i], bank[qb_], sinT[:, tb * 512:(tb + 1) * 512], op=ALU.mult),
                         [PSN[qb_], "sinT"], [f"t1{i}"])
                    S.op("dve", lambda e: e.tensor_tensor(dst, t1[i], t2[i], op=ALU.add),
                         [f"t1{i}", f"t2{i}"] + list(extra_reads), [dst_region])
                pending.append(part2)

            gi = 0
            for h in range(4):
                ensure_loaded(gi + 4)
                sl = slots[gi]
                gi += 1
                pbk = proj(sl, 0, C)
                flush(0)
                S.op("act", lambda e, h=h, pbk=pbk: e.activation(out=kT[:, h, 0:C], in_=bank[pbk][:, 0:C], func=ACT.Copy),
                     [PSN[pbk]], [f"kT{h}"])
                if stage == "B0a":
                    return
                for tb in range(4):
                    pbk = proj(sl, C + tb * 512, 512)
                    flush(0)
                    if stage == "B0c":
                        S.op("act", lambda e, h=h, pbk=pbk, tb=tb: e.activation(out=kT[:, h, C + tb * 512:C + (tb + 1) * 512], in_=bank[pbk], func=ACT.Copy),
                             [PSN[pbk]], [f"kT{h}"])
                        return
                    if stage == "B0d":
                        S.op("dve", lambda e, pbk=pbk, tb=tb: e.tensor_tensor(t2[0], bank[pbk], cosT[:, tb * 512:(tb + 1) * 512], op=ALU.mult),
                             [PSN[pbk], "cosT"], ["t20"])
                        return
                    rope(pbk, kT[:, h, C + tb * 512:C + (tb + 1) * 512], f"kT{h}", tb)
                    if stage == "B0b":
                        flush(0)
                        return
            flush(0)
            if stage == "B1":
                return
            for tt in range(18):
                pbk = next_pbank()

                def mmv(e, tt=tt, pbk=pbk):
                    for k in range(8):
                        ins = e.matmul(bank[pbk], xnT[:, k, tt * 128:(tt + 1) * 128], wv_sb[:, k, :], start=(k == 0), stop=(k == 7))
                    return ins
                S.op("pe", mmv, ["wv", f"xn{tt}"], [PSN[pbk]])
                S.op("act", lambda e, tt=tt, pbk=pbk: e.activation(out=v_sb[:, tt, :], in_=bank[pbk], func=ACT.Copy), [PSN[pbk]], ["v_sb"])
            if stage == "B2":
                return
            S.op("dve", lambda e: e.memset(u_t[:, 0:1], 0.0), ["qdead", "u_t"], ["u_t"])
            S.op("dve", lambda e: e.memset(u_t[:, 2049:2050], 0.0), ["qdead", "u_t"], ["u_t"])
            for j in range(4):
                ensure_loaded(gi + 5)
                sgb, sgc, sxc = slots[gi], slots[gi + 1], slots[gi + 2]
                gi += 3
                for tb in range(4):
                    i = (j * 4 + tb) % 2
                    p_xc = proj(sxc, C + tb * 512, 512)
                    p_gc = proj(sgc, C + tb * 512, 512)
                    p_gb = proj(sgb, C + tb * 512, 512)
                    S.op("act", lambda e, i=i, p_xc=p_xc: e.activation(out=xc_sb[i], in_=bank[p_xc], func=ACT.Copy), [PSN[p_xc]], [f"xc{i}"])
                    S.op("dve", lambda e, i=i, p_gc=p_gc, tb=tb: e.tensor_tensor(u_t[:, 1 + tb * 512:1 + (tb + 1) * 512], bank[p_gc], xc_sb[i], op=ALU.mult),
                         [PSN[p_gc], f"xc{i}", "qdead"], ["u_t"])
                    S.op("act", lambda e, p_gb=p_gb, tb=tb: e.activation(out=gb_t[:, tb * 512:(tb + 1) * 512], in_=bank[p_gb], func=ACT.Copy),
                         [PSN[p_gb], "qdead"], ["gb_t"])
                for hf in range(2):
                    o0 = hf * 1024
                    S.op("act", lambda e, j=j, o0=o0: e.activation(out=y_t, in_=u_t[:, 1 + o0:1 + o0 + 1024], func=ACT.Identity, scale=cw[:, j, 1:2]),
                         ["u_t", "cw", "qdead"], ["y_t"])
                    S.op("dve", lambda e, j=j, o0=o0: e.scalar_tensor_tensor(y_t, u_t[:, o0:o0 + 1024], cw[:, j, 0:1], y_t, op0=ALU.mult, op1=ALU.add),
                         ["u_t", "cw", "y_t"], ["y_t"])
                    S.op("dve", lambda e, j=j, o0=o0: e.scalar_tensor_tensor(y_t, u_t[:, 2 + o0:2 + o0 + 1024], cw[:, j, 2:3], y_t, op0=ALU.mult, op1=ALU.add),
                         ["u_t", "cw", "y_t"], ["y_t"])
                    S.op("dve", lambda e, j=j, o0=o0: e.tensor_tensor(convT[:, j, o0:o0 + 1024], y_t, gb_t[:, o0:o0 + 1024], op=ALU.mult),
                         ["y_t", "gb_t"], ["convT", "convdead"])
            if stage == "B3":
                return
            for h in range(4):
                ensure_loaded(gi + 4)
                sl = slots[gi]
                gi += 1
                for tb in range(4):
                    pbk = proj(sl, C + tb * 512, 512)
                    flush(0)
                    rope(pbk, qT[:, h, tb * 512:(tb + 1) * 512], f"qT{h}", tb, extra_reads=("convdead",))
            flush(0)
            S.op("pe", lambda e: e.matmul(bank[7][:, 0:2], ones_f[0:1, :], lq3[0:1, 6:8], start=True, stop=True),
                 [f"xn{t}" for t in range(18)] + ["ones_f"], [PSN[7], "xndead"])
            for k in range(8):
                S.dma("pool", lambda e, k=k: e.dma_start(out=w_out_sb[:, k, :], in_=w_out[k * 128:(k + 1) * 128, :]),
                      ["xndead"], ["w_out_sb"], "d_wout")

            if stage == "B":
                return
            steps = [(h, qb, kt) for h in range(4) for qb in range(8) for kt in range(18)]
            deferred = {}

            def qk(si):
                h, qb, kt = steps[si]
                sb0 = (si % 2) * 2

                def mm(e):
                    for m in range(2):
                        ins = e.matmul(bank[sb0 + m][:, 0:256], kT[64 * m:64 * (m + 1), h, kt * 128:(kt + 1) * 128],
                                       qT[64 * m:64 * (m + 1), h, qb * 256:(qb + 1) * 256], start=True, stop=True)
                    return ins
                S.op("pe", mm, [f"kT{h}", f"qT{h}"], [PSN[sb0], PSN[sb0 + 1]])

            def fin1(h, qb, fi):
                ob = 4
                db = 5
                i = fi % 2
                S.op("act", lambda e: e.activation(out=dcp, in_=bank[db], func=ACT.Copy), [PSN[db], "xndead"], ["dcp"])
                S.op("act", lambda e: e.activation(out=Ocp, in_=bank[ob], func=ACT.Copy), [PSN[ob], "xndead"], ["Ocp"])
                S.op("dve", lambda e: e.reciprocal(dcp, dcp), ["dcp", "xndead"], ["dcp"])
                S.op("dve", lambda e: e.tensor_tensor(tO, Ocp, dcp, op=ALU.mult), ["Ocp", "dcp"], ["tO"])
                S.op("dve", lambda e: e.scalar_tensor_tensor(o_t[i], tO[:, 256:512], neglam[:, 0:1], tO[:, 0:256], op0=ALU.mult, op1=ALU.add),
                     ["tO", "neglam"], [f"o{i}"])
                S.op("dve", lambda e: e.tensor_tensor(osq[i], o_t[i], o_t[i], op=ALU.mult), [f"o{i}"], [f"osq{i}"])

            def fin2(h, qb, fi):
                i = fi % 2
                S.op("pe", lambda e: e.matmul(bank[6][:, 0:256], ones_f[:], osq[i], start=True, stop=True), ["ones_f", f"osq{i}"], [PSN[6]])
                S.op("act", lambda e: e.activation(out=ln_t[i], in_=bank[6][:, 0:256], func=ACT.Ln, scale=1.0 / 128, bias=eps_t[:, 0:1]),
                     [PSN[6], "eps_t"], [f"ln{i}"])
                S.op("act", lambda e: e.activation(out=rs_t[i], in_=ln_t[i], func=ACT.Exp, scale=-0.5), [f"ln{i}"], [f"rs{i}"])
                S.op("dve", lambda e: e.scalar_tensor_tensor(attnT[:, h, qb * 256:(qb + 1) * 256], o_t[i], sgs[:, 0:1], rs_t[i], op0=ALU.mult, op1=ALU.mult),
                     [f"o{i}", f"rs{i}", "sgs", "xndead"], ["attnT"])

            qk(0)
            fi = 0
            for si, (h, qb, kt) in enumerate(steps):
                if si + 1 < len(steps):
                    qk(si + 1)
                sb0 = (si % 2) * 2
                ai = si % 3
                for m in range(2):
                    S.op("act", lambda e, sb0=sb0, ai=ai, m=m: e.activation(out=at_t[ai][:, m * 256:(m + 1) * 256], in_=bank[sb0 + m][:, 0:256], func=ACT.Exp, scale=0.125),
                         [PSN[sb0 + m], "xndead"], [f"at{ai}"])
                ob = 4
                db = 5

                def av(e, h=h, kt=kt, ai=ai, ob=ob, db=db):
                    e.matmul(bank[ob], v_sb[:, kt, h * 128:(h + 1) * 128], at_t[ai], start=(kt == 0), stop=(kt == 17))
                    return e.matmul(bank[db], ones_b[:], at_t[ai], start=(kt == 0), stop=(kt == 17))
                S.op("pe", av, ["v_sb", f"at{ai}", "ones_b"], [PSN[ob], PSN[db]])
                if si in deferred:
                    deferred.pop(si)()
                if kt == 17:
                    fin1(h, qb, fi)
                    deferred[min(si + 4, len(steps) - 1) if si + 4 < len(steps) else -1] = (lambda h=h, qb=qb, fi=fi: fin2(h, qb, fi))
                    fi += 1
            for k_ in sorted(deferred):
                deferred[k_]()
            S.op("pe", lambda e: e.matmul(bank[7][:, 0:2], ones_f[0:1, :], lq3[0:1, 6:8], start=True, stop=True),
                 ["ones_f", "v_sb"] + [f"kT{h}" for h in range(4)] + [f"qT{h}" for h in range(4)], [PSN[7], "kvdead", "qdead"])

            if stage == "C":
                return
            mod_tile(G1, "G1", b, 2, 1, gtmp2, "gtmp2", False, guard=("qdead",))
            mod_tile(A2, "A2", b, 4, 2, gtmp2, "gtmp2", True, guard=("qdead",))
            mod_tile(B2, "B2", b, 3, None, None, None, False, guard=("qdead",))
            mixin = [attnT[:, h, :] for h in range(4)] + [convT[:, j, :] for j in range(4)]

            def d0(tt):
                s = tt % 2
                S.dma("sp", lambda e: e.dma_start(out=xt2[s], in_=x[b, tt * 128:(tt + 1) * 128, :]), ["kvdead"], [f"xt2{s}"], f"d_xt2{s}")

            def d1(tt):
                s = tt % 2
                if tt + 1 < 16:
                    d0(tt + 1)
                for hf in range(2):
                    def mm(e, hf=hf):
                        for k in range(8):
                            ins = e.matmul(bank[hf], mixin[k][:, tt * 128:(tt + 1) * 128], w_out_sb[:, k, hf * 512:(hf + 1) * 512], start=(k == 0), stop=(k == 7))
                        return ins
                    S.op("pe", mm, ["attnT", "convT", "w_out_sb"], [PSN[hf]])
                rs, rsn = rstd_of2(bank[0], bank[1], [PSN[0], PSN[1]], junk, 1.0 / D)
                for hf in range(2):
                    S.op("dve", lambda e, hf=hf: e.scalar_tensor_tensor(tD[:, hf * 512:(hf + 1) * 512], bank[hf], rs, G1[:, hf * 512:(hf + 1) * 512], op0=ALU.mult, op1=ALU.mult),
                         [PSN[hf], rsn, "G1", "kvdead"], ["tD"])
                S.op("dve", lambda e: e.tensor_tensor(h_sb[s], tD, xt2[s], op=ALU.add), ["tD", f"xt2{s}", "kvdead"], [f"h{s}"])
                S.dma("sp", lambda e: e.dma_start(out=h_d[b, tt * 128:(tt + 1) * 128, :], in_=h_sb[s]), [f"h{s}"], [f"h_d{b}_{tt}"], f"d_h{s}")
                rs2, rsn2 = rstd_of(h_sb[s], D, [f"h{s}"], junk, 1.0 / D)
                S.op("dve", lambda e: e.scalar_tensor_tensor(t2D, h_sb[s], rs2, A2, op0=ALU.mult, op1=ALU.mult),
                     [f"h{s}", rsn2, "A2", "kvdead"], ["t2D"])
                S.op("dve", lambda e: e.tensor_tensor(xn2[s], t2D, B2, op=ALU.add), ["t2D", "B2"], [f"xn2{s}"])
                S.dma("pool", lambda e: e.dma_start(out=xn2_d[b][tt * 128:(tt + 1) * 128, :], in_=xn2[s]), [f"xn2{s}"], [f"xn2d{b}"], f"d_x2{s}")

            def d2(tt):
                s = tt % 2

                def tp(e):
                    for k in range(8):
                        ins = e.transpose(bank[2 + k // 4][:, (k % 4) * 128:(k % 4 + 1) * 128], xn2[s][:, k * 128:(k + 1) * 128], idf[:])
                    return ins
                S.op("pe", tp, [f"xn2{s}", "idf"], [PSN[2], PSN[3]])
                for hb in range(2):
                    S.op("act", lambda e, hb=hb: e.activation(out=xn2T[:, hb * 4:(hb + 1) * 4, :], in_=bank[2 + hb].rearrange("p (k t) -> p k t", k=4), func=ACT.Copy),
                         [PSN[2 + hb], "kvdead"], ["xn2T"])

            def d3(tt):
                s = tt % 2

                def mm(e):
                    for k in range(8):
                        ins = e.matmul(bank[4][:, 0:16], xn2T[:, k, :], wr_sb[:, k, :], start=(k == 0), stop=(k == 7))
                    return ins
                S.op("pe", mm, ["xn2T", "wr_sb"], [PSN[4]])
                c = stat_slot(4)
                S.op("dve", lambda e: e.reduce_max(stat[:, c:c + 1], bank[4][:, 0:16], axis=AX.X), [PSN[4]], [f"st{c}"])
                S.op("dve", lambda e: e.tensor_scalar(stat[:, c + 1:c + 2], stat[:, c:c + 1], -1.0, None, op0=ALU.mult), [f"st{c}"], [f"st{c+1}"])
                S.op("act", lambda e: e.activation(out=ex_sb, in_=bank[4][:, 0:16], func=ACT.Exp, bias=stat[:, c + 1:c + 2], accum_out=stat[:, c + 2:c + 3]),
                     [PSN[4], f"st{c+1}"], ["ex_sb", f"st{c+2}"])
                S.op("dve", lambda e: e.reciprocal(stat[:, c + 3:c + 4], stat[:, c + 2:c + 3]), [f"st{c+2}"], [f"st{c+3}"])
                S.op("dve", lambda e: e.tensor_scalar(aff_sb[s], ex_sb, stat[:, c + 3:c + 4], None, op0=ALU.mult), ["ex_sb", f"st{c+3}"], [f"aff{s}"])

            def d4(tt):
                s = tt % 2
                S.op("pe", lambda e: e.transpose(bank[5][0:16, 0:128], aff_sb[s], idf[:]), [f"aff{s}", "idf"], [PSN[5]])
                S.op("act", lambda e: e.activation(out=affT_sb[s][0:16, :], in_=bank[5][0:16, 0:128], func=ACT.Copy), [PSN[5]], [f"affT{s}"])
                S.dma("sp", lambda e: e.dma_start(out=aff_d[32 * b:32 * b + 16, tt * 128:(tt + 1) * 128], in_=affT_sb[s][0:16, :]),
                      [f"affT{s}"], ["aff_d"], f"d_af{s}")

            d0(0)
            for i in range(16 + 3):
                if 0 <= i - 3 < 16:
                    d4(i - 3)
                if 0 <= i - 2 < 16:
                    d3(i - 2)
                if 0 <= i - 1 < 16:
                    d2(i - 1)
                if i < 16:
                    d1(i)
            S.barrier()

        for b_ in range(NB if stage != "phase0" else 0):
            do_batch(b_)
        if stage in ("A", "B", "C", "B1", "B2", "B3", "B0a", "B0b", "B0c", "B0d"):
            S.barrier()

        if stage in ("mixer", "phase0", "A", "B", "C", "B1", "B2", "B3", "B0a", "B0b", "B0c", "B0d"):
            AR.reset()
            cp = [AR.take([D], F32) for _ in range(2)]
            for b in range(NB):
                for tt in range(16):
                    s = tt % 2
                    S.dma("sp", lambda e, b=b, tt=tt, s=s: e.dma_start(out=cp[s], in_=h_d[b, tt * 128:(tt + 1) * 128, :]), [], [f"cp{s}"], f"d_cp{s}")
                    S.dma("sp", lambda e, b=b, tt=tt, s=s: e.dma_start(out=out[b, tt * 128:(tt + 1) * 128, :], in_=cp[s]), [f"cp{s}"], ["out"], f"d_co{s}")
            S.barrier(["sp"])
            S.emit()
            return nc

        AR.reset()
        wexp = [[AR.take([8, D], BF16) for _ in range(3)] for _ in range(2)]
        xs = [AR.take([D], BF16) for _ in range(8)]
        xsT = [AR.take([8, 512], BF16) for _ in range(2)]
        actT = [AR.take([8, 512], BF16) for _ in range(2)]
        sgt = [AR.take([512], F32) for _ in range(2)]
        y_sb = [AR.take([D], F32) for _ in range(8)]
        moe_end = AR.off
        work = AR.take([T], F32)
        vals = AR.take([CAP], F32)
        idx = AR.take([CAP], U32)
        idxf = AR.take([CAP], F32)
        idxT = AR.take([2, 128], U32)
        gT = AR.take([2, 128], F32)
        rt_end = AR.off
        AR.off = 0
        NFB = 4
        ffl = [AR.take([D], F32) for _ in range(NFB)]
        hl = [AR.take([D], F32) for _ in range(NFB)]
        G2 = [AR.take([D], F32) for _ in range(2)]
        gtmp3 = AR.take([D], F32)
        tF = [AR.take([D], F32) for _ in range(NFB)]
        ob_t = [AR.take([D], F32) for _ in range(NFB)]
        junk2 = AR.take([D], BF16)
        AR.off = rt_end
        print("moe arena bytes", AR.off)

        def load_expert(e_):
            s = e_ % 2
            for wi, wsrc in enumerate((w_gate, w_up, w_down)):
                for k in range(8):
                    S.dma("pool", lambda e, s=s, wi=wi, wsrc=wsrc, k=k: e.dma_start(out=wexp[s][wi][:, k, :], in_=wsrc[e_, k * 128:(k + 1) * 128, :]),
                          [], [f"we{s}_{wi}"], f"d_we{s}_{wi}")

        load_expert(0)
        S.dma("sp", lambda e: e.dma_start(out=work, in_=aff_d), ["aff_d"], ["work"], "d_work")
        for r in range(CAP // 8):
            S.op("dve", lambda e, r=r: e.max(out=vals[:, r * 8:(r + 1) * 8], in_=work), ["work"], ["vals"])
            S.op("dve", lambda e, r=r: e.max_index(out=idx[:, r * 8:(r + 1) * 8], in_max=vals[:, r * 8:(r + 1) * 8], in_values=work), ["work", "vals"], ["idx"])
            S.op("dve", lambda e, r=r: e.match_replace(out=work, in_to_replace=vals[:, r * 8:(r + 1) * 8], in_values=work, imm_value=-1.0), ["vals", "idx"], ["work"])
        S.op("dve", lambda e: e.tensor_copy(idxf, idx), ["idx"], ["idxf"])
        for ct in range(2):
            S.op("pe", lambda e, ct=ct: e.transpose(bank[0][:, 0:128], idxf[:, ct * 128:(ct + 1) * 128], idf[:]), ["idxf", "idf"], [PSN[0]])
            S.op("dve", lambda e, ct=ct: e.tensor_copy(idxT[:, ct, :], bank[0][:, 0:128]), [PSN[0]], ["idxT"])
            S.op("pe", lambda e, ct=ct: e.transpose(bank[1][:, 0:128], vals[:, ct * 128:(ct + 1) * 128], idf[:]), ["vals", "idf"], [PSN[1]])
            S.op("dve", lambda e, ct=ct: e.tensor_copy(gT[:, ct, :], bank[1][:, 0:128]), [PSN[1]], ["gT"])

        NP = (NB + 1) // 2
        tiles = [(pr, bi, bb, ct) for pr in range(NP) for bi, bb in enumerate([q for q in (2 * pr, 2 * pr + 1) if q < NB]) for ct in range(2)]

        def gathers(e_):
            for j, (pr, bi, bb, ct) in enumerate(tiles):
                row = 32 * bb + e_
                S.dma("pool", lambda e, j=j, bb=bb, ct=ct, row=row: e.indirect_dma_start(
                    out=xs[j], out_offset=None, in_=xn2_d[bb],
                    in_offset=bass.IndirectOffsetOnAxis(ap=idxT[:, ct, row:row + 1], axis=0)),
                    [f"xn2d{bb}", "idxT"], [f"xs{j}"], f"d_xs{j}")

        def transposes(e_):
            for j, (pr, bi, bb, ct) in enumerate(tiles):
                pbk = 6 + j % 2
                pv = bank[pbk].bitcast(BF16)

                def tpx(e, j=j, pv=pv):
                    for k in range(8):
                        ins = e.transpose(pv[:, k * 128:(k + 1) * 128], xs[j][:, k * 128:(k + 1) * 128], idb[:])
                    return ins
                S.op("pe", tpx, [f"xs{j}", "idb"], [PSN[pbk]])
                c0 = (bi * 2 + ct) * 128
                S.op("act", lambda e, pr=pr, c0=c0, pv=pv: e.activation(out=xsT[pr][:, :, c0:c0 + 128], in_=pv.rearrange("p (k t) -> p k t", k=8), func=ACT.Copy),
                     [PSN[pbk]], [f"xsT{pr}"])

        def do_pair(e_, pr):
            ws = e_ % 2
            wg_, wu_, wd_ = wexp[ws]
            mine = [(j, t) for j, t in enumerate(tiles) if t[0] == pr]
            ncol = len(mine) * 128
            for fc in range(8):
                bg = 0 + 2 * (fc % 2)
                bu = 1 + 2 * (fc % 2)
                si_ = fc % 2

                def mmg(e, fc=fc, bg=bg):
                    for k in range(8):
                        ins = e.matmul(bank[bg][:, 0:ncol], wg_[:, k, fc * 128:(fc + 1) * 128], xsT[pr][:, k, 0:ncol], start=(k == 0), stop=(k == 7))
                    return ins

                def mmu(e, fc=fc, bu=bu):
                    for k in range(8):
                        ins = e.matmul(bank[bu][:, 0:ncol], wu_[:, k, fc * 128:(fc + 1) * 128], xsT[pr][:, k, 0:ncol], start=(k == 0), stop=(k == 7))
                    return ins
                S.op("pe", mmg, [f"we{ws}_0", f"xsT{pr}"], [PSN[bg]])
                S.op("pe", mmu, [f"we{ws}_1", f"xsT{pr}"], [PSN[bu]])
                S.op("act", lambda e, bg=bg, si_=si_: e.activation(out=sgt[si_][:, 0:ncol], in_=bank[bg][:, 0:ncol], func=ACT.Silu), [PSN[bg]], [f"sgt{si_}"])
                S.op("dve", lambda e, fc=fc, bu=bu, si_=si_: e.tensor_tensor(actT[pr][:, fc, 0:ncol], sgt[si_][:, 0:ncol], bank[bu][:, 0:ncol], op=ALU.mult),
                     [PSN[bu], f"sgt{si_}"], [f"actT{pr}"])
            for j, (pr_, bi, bb, ct) in mine:
                row = 32 * bb + e_
                c0 = (bi * 2 + ct) * 128
                for hf in range(2):
                    yb = 4 + hf

                    def mmd(e, c0=c0, hf=hf, yb=yb):
                        for k in range(8):
                            ins = e.matmul(bank[yb], actT[pr][:, k, c0:c0 + 128], wd_[:, k, hf * 512:(hf + 1) * 512], start=(k == 0), stop=(k == 7))
                        return ins
                    S.op("pe", mmd, [f"actT{pr}", f"we{ws}_2"], [PSN[yb]])
                    if hf == 0:
                        S.op("act", lambda e, j=j, yb=yb, ct=ct, row=row: e.activation(out=y_sb[j][:, 0:512], in_=bank[yb], func=ACT.Identity, scale=gT[:, ct, row:row + 1]),
                             [PSN[yb], "gT"], [f"y{j}a"])
                    else:
                        S.op("dve", lambda e, j=j, yb=yb, ct=ct, row=row: e.tensor_scalar(y_sb[j][:, 512:1024], bank[yb], gT[:, ct, row:row + 1], None, op0=ALU.mult),
                             [PSN[yb], "gT"], [f"y{j}b"])
                par, ppar = e_ % 2, (e_ - 1) % 2
                S.dma("pool", lambda e, j=j, bb=bb, ct=ct, row=row: e.indirect_dma_start(
                    out=ff_d[bb], out_offset=bass.IndirectOffsetOnAxis(ap=idxT[:, ct, row:row + 1], axis=0),
                    in_=y_sb[j], in_offset=None, compute_op=ALU.add),
                    [f"y{j}a", f"y{j}b", "idxT", f"ff{bb}", f"ffs{bb}_0_{ppar}", f"ffs{bb}_1_{ppar}"], [f"ffs{bb}_{ct}_{par}"], f"d_y{j}")

        gathers(0)
        for e_ in range(NE):
            transposes(e_)
            if e_ + 1 < NE:
                load_expert(e_ + 1)
                gathers(e_ + 1)
            for pr in range(NP):
                do_pair(e_, pr)
        S.barrier()

        bc_load(gtmp3, gains[3:4, :], "gtmp3", "d_bc_gtmp3")
        ftiles = [(b, tt) for b in range(NB) for tt in range(16)]

        def f_load(i):
            b, tt = ftiles[i]
            s = i % NFB
            if tt == 0:
                g = b % 2
                bc_load(G2[g], mod_d[b:b + 1, 5 * D:6 * D], f"G2{g}", f"d_bc_G2{g}")
                S.op("dve", lambda e, g=g: e.tensor_tensor(G2[g], G2[g], gtmp3, op=ALU.mult), [f"G2{g}", "gtmp3"], [f"G2{g}"])
            S.dma("sp", lambda e: e.dma_start(out=ffl[s], in_=ff_d[b][tt * 128:(tt + 1) * 128, :]), [f"ff{b}"], [f"ffl{s}"], f"d_ffl{s}")
            S.dma("sp", lambda e: e.dma_start(out=hl[s], in_=h_d[b, tt * 128:(tt + 1) * 128, :]), [f"h_d{b}_{tt}"], [f"hl{s}"], f"d_hl{s}")

        def f_compute(i):
            b, tt = ftiles[i]
            s = i % NFB
            g = b % 2
            rs, rsn = rstd_of(ffl[s], D, [f"ffl{s}"], junk2, 1.0 / D)
            S.op("dve", lambda e: e.scalar_tensor_tensor(tF[s], ffl[s], rs, G2[g], op0=ALU.mult, op1=ALU.mult), [f"ffl{s}", rsn, f"G2{g}"], [f"tF{s}"])
            S.op("dve", lambda e: e.tensor_tensor(ob_t[s], tF[s], hl[s], op=ALU.add), [f"tF{s}", f"hl{s}"], [f"ob{s}"])
            S.dma("sp", lambda e: e.dma_start(out=out[b, tt * 128:(tt + 1) * 128, :], in_=ob_t[s]), [f"ob{s}"], ["out"], f"d_out{s}")

        AHEAD = NFB - 1
        for i in range(min(AHEAD, len(ftiles))):
            f_load(i)
        for i in range(len(ftiles)):
            if i + AHEAD < len(ftiles):
                f_load(i + AHEAD)
            f_compute(i)
        S.barrier(["sp"])
        S.emit()
    return nc


def _rope_tables():
    t = np.arange(T)
    row = (t // 64).astype(np.float32)
    col = (t % 64).astype(np.float32)
    inv = (10000.0 ** (-np.arange(0, 32, 2, dtype=np.float32) / 32)).astype(np.float32)
    ang_r = row[:, None] * inv[None, :]
    ang_c = col[:, None] * inv[None, :]
    ang = np.concatenate([ang_r, ang_r, ang_c, ang_c], axis=-1)
    cos = np.cos(ang).astype(np.float32).T
    sin = np.sin(ang).astype(np.float32).T
    sgn = np.concatenate([-np.ones(16), np.ones(16), -np.ones(16), np.ones(16)]).astype(np.float32)[:, None]
    sin = sin * sgn
    cosT = np.ascontiguousarray(np.concatenate([cos, cos], 0))
    sinT = np.ascontiguousarray(np.concatenate([sin, sin], 0))
    perm = np.zeros((128, 128), np.float32)
    for i in range(128):
        perm[i ^ 16, i] = 1.0
    return cosT, sinT, perm


def make_in_maps(inputs, NB=4, ncores=NCORES, ne=NE):
    f = lambda a: np.ascontiguousarray(np.asarray(a, dtype=np.float32))
    x = f(inputs["x"]); c = f(inputs["c"]); ctx = f(inputs["ctx"]); c_ctx = f(inputs["c_ctx"])
    w_in = f(inputs["w_in"])[0]
    cosT, sinT, perm = _rope_tables()
    w_in_r = np.ascontiguousarray(w_in.reshape(8, 128, 24, 128).transpose(2, 1, 0, 3))
    w_v_r = np.ascontiguousarray(w_in[:, 1024:1536].reshape(8, 128, 512).transpose(1, 0, 2))
    shared = dict(
        w_ada=f(inputs["w_ada"])[0], b_ada=f(inputs["b_ada"]),
        gains=np.ascontiguousarray(np.concatenate([f(inputs["norm_pre_mix"]), f(inputs["norm_post_mix"]),
                                                   f(inputs["norm_pre_ffn"]), f(inputs["norm_post_ffn"])], 0)),
        w_in_r=w_in_r, w_v_r=w_v_r,
        convw=np.ascontiguousarray(f(inputs["conv_w"])[0].T.reshape(4, 128, 3).transpose(1, 0, 2)),
        lqk=np.ascontiguousarray(np.concatenate([f(inputs["lambda_q1"]), f(inputs["lambda_k1"]),
                                                 f(inputs["lambda_q2"]), f(inputs["lambda_k2"])], 1)),
        subln=np.ascontiguousarray(f(inputs["subln_g"]).reshape(128, 1)),
        w_out=f(inputs["w_out"])[0],
        w_router=np.ascontiguousarray(f(inputs["w_router"])[0].reshape(8, 128, 16).transpose(1, 0, 2)),
        w_gate=f(inputs["w_gate"])[0][:ne], w_up=f(inputs["w_up"])[0][:ne], w_down=f(inputs["w_down"])[0][:ne],
        ident=np.eye(128, dtype=np.float32), perm=perm, cosT=cosT, sinT=sinT,
    )
    maps = []
    for i in range(ncores):
        sl = slice(i * NB, (i + 1) * NB)
        cc = np.concatenate([c[sl], c_ctx[None, :]], 0)
        if cc.shape[0] < 5:
            cc = np.concatenate([cc[:-1], np.zeros((5 - cc.shape[0], D), np.float32), cc[-1:]], 0)
        ccT = np.ascontiguousarray(cc.T.reshape(8, 128, 5).transpose(1, 0, 2))
        m = dict(shared)
        m.update(x=np.ascontiguousarray(x[sl]), ctx=np.ascontiguousarray(ctx[sl]), ccT=ccT)
        maps.append(m)
    return maps


def kernel(**inputs):
    NB = 4
    nc = build(NB=NB, stage="full")
    maps = make_in_maps(inputs, NB=NB, ncores=NCORES)
    res = run_bass_kernel_spmd(nc, maps, core_ids=list(range(NCORES)))
    return np.concatenate([np.asarray(r["out"]) for r in res.results], axis=0).astype(np.float32)
```

```python
import math
from contextlib import ExitStack
import numpy as np
import concourse.bass as bass
import concourse.mybir as mybir
from concourse.bass_utils import run_bass_kernel_spmd

F32 = mybir.dt.float32
BF16 = mybir.dt.bfloat16
U32 = mybir.dt.uint32
ACT = mybir.ActivationFunctionType
ALU = mybir.AluOpType
AX = mybir.AxisListType

T = 2048
C = 256
D = 1024
TK = T + C
NE = 16
CAP = 256
EPS = 1e-6
NCORES = 8


class Sched:
    ENGS = ("pe", "act", "dve", "pool", "sp")

    def __init__(self, nc, stack):
        self.nc = nc
        self.stack = stack
        self.sems = {}
        self.eng = {}
        for n in self.ENGS:
            self.sems["s_" + n] = stack.enter_context(nc.semaphore("s_" + n))
            self.eng[n] = dict(ops=[], cnt=0, waited={})
        self.lastw = {}
        self.readers = {}
        self.dcum = {}

    def _deps(self, reads, writes):
        d = []
        for r in reads:
            if r in self.lastw:
                d.append(self.lastw[r])
        for w in writes:
            if w in self.lastw:
                d.append(self.lastw[w])
            d.extend(self.readers.get(w, ()))
        return d

    def _waits(self, en, deps):
        E = self.eng[en]
        need = {}
        for (s, v) in deps:
            if en == "pe" and s == "s_pe":
                continue
            if E["waited"].get(s, 0) >= v:
                continue
            if need.get(s, 0) < v:
                need[s] = v
        for s, v in need.items():
            E["waited"][s] = v
        return list(need.items())

    def _record(self, ev, reads, writes):
        for r in reads:
            self.readers.setdefault(r, []).append(ev)
        for w in writes:
            self.lastw[w] = ev
            self.readers[w] = []

    def op(self, en, fn, reads=(), writes=()):
        excl = [r for r in reads if r.startswith("ps") and r not in writes]
        if excl:
            reads = [r for r in reads if r not in excl]
            writes = list(writes) + excl
        deps = self._deps(reads, writes)
        waits = self._waits(en, deps)
        E = self.eng[en]
        E["cnt"] += 1
        ev = ("s_" + en, E["cnt"])
        self._record(ev, reads, writes)
        sems = self.sems

        def run(e, fn=fn, waits=waits, sem=sems["s_" + en]):
            for (s, v) in waits:
                e.wait_ge(sems[s], v)
            fn(e).then_inc(sem, 1)

        E["ops"].append(run)

    def dma(self, q, mk, reads, writes, key):
        deps = self._deps(reads, writes)
        waits = self._waits(q, deps)
        if key not in self.sems:
            self.sems[key] = self.stack.enter_context(self.nc.semaphore(key))
            self.dcum[key] = 0
        self.dcum[key] += 16
        ev = (key, self.dcum[key])
        self._record(ev, reads, writes)
        sems = self.sems

        def run(e, mk=mk, waits=waits, sem=sems[key]):
            for (s, v) in waits:
                e.wait_ge(sems[s], v)
            mk(e).then_inc(sem, 16)

        self.eng[q]["ops"].append(run)

    def _all_events(self):
        ev = [("s_" + n, self.eng[n]["cnt"]) for n in self.ENGS if self.eng[n]["cnt"] > 0]
        ev += [(k, v) for k, v in self.dcum.items() if v > 0]
        return ev

    def barrier(self, engines=None, skip_keys=()):
        allev = [ev for ev in self._all_events() if ev[0] not in skip_keys]
        for n in (engines or self.ENGS):
            waits = self._waits(n, [ev for ev in allev if ev[0] != "s_" + n])
            sems = self.sems

            def run(e, waits=waits):
                for (s, v) in waits:
                    e.wait_ge(sems[s], v)

            self.eng[n]["ops"].append(run)

    def emit(self):
        nc = self.nc
        with nc.Block() as block:
            @block.tensor
            def _(e):
                for f in self.eng["pe"]["ops"]:
                    f(e)

            @block.scalar
            def _(e):
                for f in self.eng["act"]["ops"]:
                    f(e)

            @block.vector
            def _(e):
                for f in self.eng["dve"]["ops"]:
                    f(e)

            @block.gpsimd
            def _(e):
                for f in self.eng["pool"]["ops"]:
                    f(e)

            @block.sync
            def _(e):
                for f in self.eng["sp"]["ops"]:
                    f(e)


DT_BYTES = {F32: 4, BF16: 2, U32: 4}


class Arena:
    def __init__(self, t, nbytes):
        self.t = t
        self.nbytes = nbytes
        self.off = 0

    def reset(self):
        self.off = 0

    def take(self, free, dtype):
        n = 1
        for s in free:
            n *= s
        sz = n * DT_BYTES[dtype]
        a = self.off
        self.off += (sz + 63) // 64 * 64
        assert self.off <= self.nbytes, (self.off, self.nbytes)
        ap = self.t[:, a // 4:(a + sz) // 4]
        if dtype != F32:
            ap = ap.bitcast(dtype)
        if len(free) == 2:
            ap = ap.rearrange("p (a b) -> p a b", a=free[0])
        elif len(free) == 3:
            ap = ap.rearrange("p (a b c) -> p a b c", a=free[0], b=free[1])
        return ap


def build(NB=4, stage="full"):
    nc = bass.Bass("TRN2", target_bir_lowering=False)

    def din(n, s, d=F32):
        return nc.dram_tensor(n, s, d, kind="ExternalInput").ap()

    x = din("x", [NB, T, D])
    ctx = din("ctx", [NB, C, D])
    ccT = din("ccT", [128, 8, 5])
    w_ada = din("w_ada", [D, 6 * D])
    b_ada = din("b_ada", [1, 6 * D])
    gains = din("gains", [4, D])
    w_in_r = din("w_in_r", [24, 128, 8, 128])
    w_v_r = din("w_v_r", [128, 8, 512])
    convw = din("convw", [128, 4, 3])
    lqk = din("lqk", [1, 256])
    subln = din("subln", [128, 1])
    w_out = din("w_out", [D, D])
    w_router = din("w_router", [128, 8, 16])
    NE_decl = NE if stage == "full" else 1
    w_gate = din("w_gate", [NE_decl, D, D])
    w_up = din("w_up", [NE_decl, D, D])
    w_down = din("w_down", [NE_decl, D, D])
    ident = din("ident", [128, 128])
    perm = din("perm", [128, 128])
    cosT_d = din("cosT", [128, T])
    sinT_d = din("sinT", [128, T])
    out = nc.dram_tensor("out", [NB, T, D], F32, kind="ExternalOutput").ap()
    mod_d = nc.dram_tensor("mod_d", [5, 6 * D], F32).ap()
    h_d = nc.dram_tensor("h_d", [NB, T, D], F32).ap()
    aff_d = nc.dram_tensor("aff_d", [128, T], F32).ap()
    xn2_d = [nc.dram_tensor(f"xn2_d{b}", [T, D], BF16).ap() for b in range(NB)]
    ff_d = [nc.dram_tensor(f"ff_d{b}", [T, D], F32).ap() for b in range(NB)]

    with ExitStack() as st:
        S = Sched(nc, st)

        def sb(n, s, d):
            return st.enter_context(nc.sbuf_tensor(n, s, d))

        idf = sb("idf", [128, 128], F32)
        idb = sb("idb", [128, 128], BF16)
        permf = sb("permf", [128, 128], F32)
        permb = sb("permb", [128, 128], BF16)
        ones_b = sb("ones_b", [128, 128], BF16)
        ones_f = sb("ones_f", [128, 128], F32)
        neghalf = sb("neghalf", [128, 256], F32)
        eps_t = sb("eps_t", [128, 1], F32)
        cw = sb("cw", [128, 4, 3], F32)
        sgs = sb("sgs", [128, 1], F32)
        neglam = sb("neglam", [128, 2], F32)
        wr_sb = sb("wr_sb", [128, 8, 16], F32)
        stat = sb("stat", [128, 96], F32)
        lq_sb = sb("lq_sb", [1, 256], F32)
        lq2 = sb("lq2", [1, 128], F32)
        lq3 = sb("lq3", [1, 8], F32)
        ARENA_BYTES = 200 * 1024
        arena_t = sb("arena", [128, ARENA_BYTES // 4], F32)
        AR = Arena(arena_t, ARENA_BYTES)
        psS = [st.enter_context(nc.psum_tensor(f"psS{i}", [128, 512], F32)) for i in range(8)]
        bank = [p[:] for p in psS]
        PSN = [f"ps{i}" for i in range(8)]

        stat_ctr = [0]

        def stat_slot(n=1):
            c = stat_ctr[0]
            if c + n > 96:
                c = 0
            stat_ctr[0] = c + n
            return c

        def rstd_of(src, nfree, src_reads, junk, inv_n):
            c = stat_slot(3)
            r0, r1, r2 = f"st{c}", f"st{c+1}", f"st{c+2}"
            S.op("act", lambda e: e.activation(out=junk, in_=src, func=ACT.Square, accum_out=stat[:, c:c + 1]),
                 src_reads, [r0, "junk"])
            S.op("dve", lambda e: e.tensor_scalar(stat[:, c + 1:c + 2], stat[:, c:c + 1], inv_n, EPS, op0=ALU.mult, op1=ALU.add),
                 [r0], [r1])
            S.op("pool", lambda e: e.tensor_tensor(stat[:, c + 2:c + 3], stat[:, c + 1:c + 2], neghalf[:, 0:1], op=ALU.pow),
                 [r1, "neghalf"], [r2])
            return stat[:, c + 2:c + 3], r2

        def rstd_of2(src0, src1, src_reads, junk, inv_n):
            c = stat_slot(5)
            ra, rb, r0, r1, r2 = (f"st{c + i}" for i in range(5))
            S.op("act", lambda e: e.activation(out=junk[:, 0:512], in_=src0, func=ACT.Square, accum_out=stat[:, c:c + 1]),
                 src_reads, [ra, "junk"])
            S.op("act", lambda e: e.activation(out=junk[:, 512:1024], in_=src1, func=ACT.Square, accum_out=stat[:, c + 1:c + 2]),
                 src_reads, [rb, "junk"])
            S.op("dve", lambda e: e.tensor_tensor(stat[:, c + 2:c + 3], stat[:, c:c + 1], stat[:, c + 1:c + 2], op=ALU.add), [ra, rb], [r0])
            S.op("dve", lambda e: e.tensor_scalar(stat[:, c + 3:c + 4], stat[:, c + 2:c + 3], inv_n, EPS, op0=ALU.mult, op1=ALU.add), [r0], [r1])
            S.op("pool", lambda e: e.tensor_tensor(stat[:, c + 4:c + 5], stat[:, c + 3:c + 4], neghalf[:, 0:1], op=ALU.pow), [r1, "neghalf"], [r2])
            return stat[:, c + 4:c + 5], r2

        S.dma("sp", lambda e: e.dma_start(out=idf[:], in_=ident), [], ["idf"], "d_c1")
        S.dma("sp", lambda e: e.dma_start(out=permf[:], in_=perm), [], ["permf"], "d_c2")
        S.dma("sp", lambda e: e.dma_start(out=cw[:], in_=convw), [], ["cw"], "d_c3")
        S.dma("sp", lambda e: e.dma_start(out=sgs[:], in_=subln), [], ["sgs"], "d_c4")
        S.dma("sp", lambda e: e.dma_start(out=wr_sb[:], in_=w_router), [], ["wr_sb"], "d_c5")
        S.dma("sp", lambda e: e.dma_start(out=lq_sb[:], in_=lqk), [], ["lq_sb"], "d_c6")
        S.op("dve", lambda e: e.tensor_copy(idb[:], idf[:]), ["idf"], ["idb"])
        S.op("dve", lambda e: e.tensor_copy(permb[:], permf[:]), ["permf"], ["permb"])
        S.op("dve", lambda e: e.memset(ones_b[:], 1.0), [], ["ones_b"])
        S.op("dve", lambda e: e.memset(ones_f[:], 1.0), [], ["ones_f"])
        S.op("dve", lambda e: e.memset(neghalf[:], -0.5), [], ["neghalf"])
        S.op("dve", lambda e: e.memset(eps_t[:], EPS), [], ["eps_t"])
        S.op("dve", lambda e: e.tensor_scalar(sgs[:], sgs[:], 0.8, None, op0=ALU.mult), ["sgs"], ["sgs"])
        lqv = lq_sb[0:1, :].rearrange("p (a b c) -> p a b c", a=2, b=2)
        S.op("dve", lambda e: e.tensor_tensor(lq2[0:1, :].rearrange("p (a c) -> p a c", a=2), lqv[:, :, 0, :], lqv[:, :, 1, :], op=ALU.mult),
             ["lq_sb"], ["lq2"])
        S.op("dve", lambda e: e.reduce_sum(lq3[0:1, 0:2], lq2[0:1, :].rearrange("p (a c) -> p a c", a=2), axis=AX.X), ["lq2"], ["lq3a"])
        S.op("act", lambda e: e.activation(out=lq3[0:1, 2:4], in_=lq3[0:1, 0:2], func=ACT.Exp), ["lq3a"], ["lq3b"])
        S.op("dve", lambda e: e.tensor_tensor(lq3[0:1, 4:5], lq3[0:1, 3:4], lq3[0:1, 2:3], op=ALU.subtract), ["lq3b"], ["lq3c"])
        S.op("dve", lambda e: e.tensor_scalar(lq3[0:1, 6:7], lq3[0:1, 4:5], -0.2, None, op0=ALU.add), ["lq3c"], ["lq3d"])
        S.op("dve", lambda e: e.tensor_copy(lq3[0:1, 7:8], lq3[0:1, 6:7]), ["lq3d"], ["lq3e"])
        S.op("pe", lambda e: e.matmul(bank[7][:, 0:2], ones_f[0:1, :], lq3[0:1, 6:8], start=True, stop=True),
             ["ones_f", "lq3d", "lq3e"], [PSN[7]])
        S.op("dve", lambda e: e.tensor_copy(neglam[:], bank[7][:, 0:2]), [PSN[7]], ["neglam"])

        AR.reset()
        ccT_sb = AR.take([8, 5], F32)
        siluT = AR.take([8, 5], F32)
        bada_sb = AR.take([6 * D], F32)
        wa = [AR.take([8, 512], F32) for _ in range(2)]
        mstage = [AR.take([512], F32) for _ in range(2)]
        zero_t = AR.take([2048], F32)
        S.dma("sp", lambda e: e.dma_start(out=ccT_sb, in_=ccT), [], ["ccT"], "d_c7")
        S.dma("sp", lambda e: e.dma_start(out=bada_sb[0:1, :], in_=b_ada), [], ["bada"], "d_c8")
        S.op("act", lambda e: e.activation(out=siluT, in_=ccT_sb, func=ACT.Silu), ["ccT"], ["siluT"])
        S.op("dve", lambda e: e.memset(zero_t, 0.0), [], ["zero_t"])
        for j in range(12):
            s = j % 2
            S.dma("sp", lambda e, j=j, s=s: e.dma_start(out=wa[s], in_=w_ada[:, j * 512:(j + 1) * 512].rearrange("(k p) n -> p k n", p=128)),
                  [], [f"wa{s}"], f"d_wa{s}")

            def mm0(e, j=j, s=s):
                for k in range(8):
                    e.matmul(bank[4][0:5, :], siluT[:, k, :], wa[s][:, k, :], start=(k == 0), stop=False)
                return e.matmul(bank[4][0:5, :], ones_f[0:1, 0:5], bada_sb[0:1, j * 512:(j + 1) * 512], start=False, stop=True)
            S.op("pe", mm0, ["siluT", f"wa{s}", "bada", "ones_f"], [PSN[4]])
            S.op("act", lambda e, s=s: e.activation(out=mstage[s][0:5, :], in_=bank[4][0:5, :], func=ACT.Copy), [PSN[4]], [f"ms{s}"])
            S.dma("sp", lambda e, j=j, s=s: e.dma_start(out=mod_d[:, j * 512:(j + 1) * 512], in_=mstage[s][0:5, :]),
                  [f"ms{s}"], ["mod_d"], f"d_ms{s}")
        for b in range(NB):
            ffv = ff_d[b].rearrange("(p r) d -> p (r d)", p=128)
            for j in range(8):
                S.dma("sp", lambda e, ffv=ffv, j=j: e.dma_start(out=ffv[:, j * 2048:(j + 1) * 2048], in_=zero_t),
                      ["zero_t"], [f"ff{b}"], "d_z")
        S.dma("sp", lambda e: e.dma_start(out=aff_d, in_=zero_t), ["zero_t"], ["aff_d"], "d_z")
        S.barrier(skip_keys=("d_z",))

        AR.reset()
        cosT = AR.take([T], F32)
        sinT = AR.take([T], F32)
        xnT = AR.take([8, TK], BF16)
        R1 = xnT.rearrange("p a b -> p (a b)")
        attnT = R1[:, 0:4 * T].rearrange("p (a b) -> p a b", a=4)
        w_out_sb = R1[:, 4 * T:4 * T + 8 * D].rearrange("p (a b) -> p a b", a=8)
        kT = AR.take([4, TK], BF16)
        v_sb = AR.take([18, 512], BF16)
        KVf = arena_t[:, 0:0]
        qc_off = AR.off
        qT = AR.take([4, T], BF16)
        AR.off = qc_off
        u_t = AR.take([2064], F32)
        gb_t = AR.take([T], F32)
        y_t = AR.take([1024], F32)
        qc_end = max(AR.off, qc_off + 4 * T * 2)
        AR.off = qc_off
        A2 = AR.take([D], F32)
        B2 = AR.take([D], F32)
        G1 = AR.take([D], F32)
        gtmp2 = AR.take([D], F32)
        AR.off = qc_end
        convT = AR.take([4, T], BF16)
        A1 = AR.take([D], F32)
        B1 = AR.take([D], F32)
        A1c = AR.take([D], F32)
        B1c = AR.take([D], F32)
        ta_off = AR.off
        XT = [AR.take([D], F32) for _ in range(2)]
        xb = [AR.take([D], BF16) for _ in range(2)]
        tmpA = AR.take([D], F32)
        junk = AR.take([D], BF16)
        ta_end = AR.off
        AR.off = ta_off
        at_t = [AR.take([512], BF16) for _ in range(3)]
        tO = AR.take([512], F32)
        o_t = [AR.take([256], F32) for _ in range(2)]
        osq = [AR.take([256], F32) for _ in range(2)]
        ln_t = [AR.take([256], F32) for _ in range(2)]
        rs_t = [AR.take([256], F32) for _ in range(2)]
        Ocp = AR.take([512], F32)
        dcp = AR.take([512], F32)
        assert AR.off <= ta_end
        AR.off = ta_end
        pb = [AR.take([512], BF16) for _ in range(2)]
        t1 = [AR.take([512], F32) for _ in range(2)]
        t2 = [AR.take([512], F32) for _ in range(2)]
        xc_sb = [AR.take([512], F32) for _ in range(2)]
        wslot = [AR.take([8, 128], BF16) for _ in range(6)]
        wv_sb = AR.take([8, 512], BF16)
        mix_end = AR.off
        kv_off = (kT.offset if hasattr(kT, "offset") else None)
        AR.off = 2 * T * 4 + 8 * TK * 2
        xt2 = [AR.take([D], F32) for _ in range(2)]
        tD = AR.take([D], F32)
        h_sb = [AR.take([D], F32) for _ in range(2)]
        t2D = AR.take([D], F32)
        xn2 = [AR.take([D], F32) for _ in range(2)]
        xn2T = AR.take([8, 128], F32)
        assert AR.off <= 2 * T * 4 + 8 * TK * 2 + 4 * TK * 2 + 18 * 512 * 2 + 64
        AR.off = mix_end
        lg_sb = AR.take([16], F32)
        ex_sb = AR.take([16], F32)
        aff_sb = [AR.take([16], F32) for _ in range(2)]
        affT_sb = [AR.take([128], F32) for _ in range(2)]
        print("mixer arena bytes", AR.off)

        S.dma("sp", lambda e: e.dma_start(out=cosT, in_=cosT_d), [], ["cosT"], "d_c9")
        S.dma("sp", lambda e: e.dma_start(out=sinT, in_=sinT_d), [], ["sinT"], "d_c10")
        S.op("dve", lambda e: e.memset(u_t, 0.0), [], ["u_t"])

        def bc_load(dst, row_ap, region, key, guard=()):
            S.dma("sp", lambda e: e.dma_start(out=dst, in_=row_ap.partition_broadcast(128)), list(guard), [region], key)

        def mod_tile(dst, region, b, seg, gain_idx, tmp, tmp_region, plus_one, guard=()):
            bc_load(dst, mod_d[b:b + 1, seg * D:(seg + 1) * D], region, "d_bc_" + region, guard)
            if gain_idx is not None:
                bc_load(tmp, gains[gain_idx:gain_idx + 1, :], tmp_region, "d_bc_" + tmp_region, guard)
                if plus_one:
                    S.op("dve", lambda e: e.scalar_tensor_tensor(dst, dst, 1.0, tmp, op0=ALU.add, op1=ALU.mult),
                         [region, tmp_region], [region])
                else:
                    S.op("dve", lambda e: e.tensor_tensor(dst, dst, tmp, op=ALU.mult), [region, tmp_region], [region])

        mod_tile(A1c, "A1c", 4, 1, 0, tmpA, "tmpA", True)
        mod_tile(B1c, "B1c", 4, 0, None, None, None, False)

        grp_ctr = [0]

        def load_group(g):
            s = grp_ctr[0] % 6
            grp_ctr[0] += 1
            S.dma("pool", lambda e: e.dma_start(out=wslot[s], in_=w_in_r[g]), [], [f"ws{s}"], f"d_ws{s}")
            return s

        pbank_ctr = [0]

        def next_pbank():
            i = pbank_ctr[0] % 4
            pbank_ctr[0] += 1
            return i

        rope_ctr = [0]

        def do_batch(b):
            mod_tile(A1, "A1", b, 1, 0, tmpA, "tmpA", True)
            mod_tile(B1, "B1", b, 0, None, None, None, False)
            S.dma("pool", lambda e: e.dma_start(out=wv_sb, in_=w_v_r), [], ["wv"], "d_wv")
            order = [("k", h, 4 + h) for h in range(4)]
            for j in range(4):
                order += [("gb", j, 12 + j), ("gc", j, 16 + j), ("xc", j, 20 + j)]
            order += [("q", h, h) for h in range(4)]
            slots = {}
            nload = [0]

            def ensure_loaded(upto):
                while nload[0] <= min(upto, len(order) - 1):
                    slots[nload[0]] = load_group(order[nload[0]][2])
                    nload[0] += 1
            ensure_loaded(4)

            for tt in range(18):
                s = tt % 2
                src = ctx[b, tt * 128:(tt + 1) * 128, :] if tt < 2 else x[b, (tt - 2) * 128:(tt - 1) * 128, :]
                Ab, Bb, An, Bn = (A1c, B1c, "A1c", "B1c") if tt < 2 else (A1, B1, "A1", "B1")
                S.dma("sp", lambda e, s=s, src=src: e.dma_start(out=XT[s], in_=src), [], [f"XT{s}"], f"d_XT{s}")
                rs, rsn = rstd_of(XT[s], D, [f"XT{s}"], junk, 1.0 / D)
                S.op("dve", lambda e, s=s, rs=rs, Ab=Ab: e.scalar_tensor_tensor(tmpA, XT[s], rs, Ab, op0=ALU.mult, op1=ALU.mult),
                     [f"XT{s}", rsn, An], ["tmpA"])
                S.op("dve", lambda e, s=s, Bb=Bb: e.tensor_tensor(xb[s], tmpA, Bb, op=ALU.add), ["tmpA", Bn], [f"xb{s}"])
                pbk = 6 + (tt % 2)
                pv = bank[pbk].bitcast(BF16)

                def tpA(e, s=s, pv=pv):
                    for k in range(8):
                        ins = e.transpose(pv[:, k * 128:(k + 1) * 128], xb[s][:, k * 128:(k + 1) * 128], idb[:])
                    return ins
                S.op("pe", tpA, [f"xb{s}", "idb"], [PSN[pbk]])
                S.op("act", lambda e, tt=tt, pv=pv: e.activation(out=xnT[:, :, tt * 128:(tt + 1) * 128],
                                                                   in_=pv.rearrange("p (k t) -> p k t", k=8), func=ACT.Copy),
                     [PSN[pbk]], [f"xn{tt}"])

            if stage == "A":
                return
            pending = []

            def flush(keep=0):
                while len(pending) > keep:
                    pending.pop(0)()

            def proj(slot, tok0, ntok):
                pbk = next_pbank()
                tiles = sorted(set(range(tok0 // 128, (tok0 + ntok + 127) // 128)))

                def mm(e):
                    for k in range(8):
                        ins = e.matmul(bank[pbk][:, 0:ntok], wslot[slot][:, k, :], xnT[:, k, tok0:tok0 + ntok], start=(k == 0), stop=(k == 7))
                    return ins
                S.op("pe", mm, [f"ws{slot}"] + [f"xn{t}" for t in tiles], [PSN[pbk]])
                return pbk

            def rope(pbk, dst, dst_region, tb, extra_reads=(), extra_writes=()):
                i = rope_ctr[0] % 2
                rope_ctr[0] += 1
                qb_ = 4 + i
                S.op("act", lambda e: e.activation(out=pb[i], in_=bank[pbk], func=ACT.Copy), [PSN[pbk]], [f"pb{i}"])
                S.op("dve", lambda e: e.tensor_tensor(t2[i], bank[pbk], cosT[:, tb * 512:(tb + 1) * 512], op=ALU.mult),
                     [PSN[pbk], "cosT"], [f"t2{i}"])

                def part2():
                    S.op("pe", lambda e: e.matmul(bank[qb_], permb[:], pb[i], start=True, stop=True), ["permb", f"pb{i}"], [PSN[qb_]])
                    S.op("dve", lambda e: e.tensor_tensor(t1[i], bank[qb_], sinT[:, tb * 512:(tb + 1) * 512], op=ALU.mult),
                         [PSN[qb_], "sinT"], [f"t1{i}"])
                    S.op("dve", lambda e: e.tensor_tensor(dst, t1[i], t2[i], op=ALU.add),
                         [f"t1{i}", f"t2{i}"] + list(extra_reads), [dst_region] + list(extra_writes))
                pending.append(part2)

            gi = 0
            for h in range(4):
                ensure_loaded(gi + 4)
                sl = slots[gi]
                gi += 1
                pbk = proj(sl, 0, C)
                flush(0)
                S.op("act", lambda e, h=h, pbk=pbk: e.activation(out=kT[:, h, 0:C], in_=bank[pbk][:, 0:C], func=ACT.Copy),
                     [PSN[pbk]], [f"kT{h}"] + (["zero_t"] if b == 0 else []))
                if stage == "B0a":
                    return
                for tb in range(4):
                    pbk = proj(sl, C + tb * 512, 512)
                    flush(0)
                    if stage == "B0c":
                        S.op("act", lambda e, h=h, pbk=pbk, tb=tb: e.activation(out=kT[:, h, C + tb * 512:C + (tb + 1) * 512], in_=bank[pbk], func=ACT.Copy),
                             [PSN[pbk]], [f"kT{h}"])
                        return
                    if stage == "B0d":
                        S.op("dve", lambda e, pbk=pbk, tb=tb: e.tensor_tensor(t2[0], bank[pbk], cosT[:, tb * 512:(tb + 1) * 512], op=ALU.mult),
                             [PSN[pbk], "cosT"], ["t20"])
                        return
                    rope(pbk, kT[:, h, C + tb * 512:C + (tb + 1) * 512], f"kT{h}", tb, extra_writes=(("zero_t",) if b == 0 else ()))
                    if stage == "B0b":
                        flush(0)
                        return
            flush(0)
            if stage == "B1":
                return
            for tt in range(18):
                pbk = next_pbank()

                def mmv(e, tt=tt, pbk=pbk):
                    for k in range(8):
                        ins = e.matmul(bank[pbk], xnT[:, k, tt * 128:(tt + 1) * 128], wv_sb[:, k, :], start=(k == 0), stop=(k == 7))
                    return ins
                S.op("pe", mmv, ["wv", f"xn{tt}"], [PSN[pbk]])
                S.op("act", lambda e, tt=tt, pbk=pbk: e.activation(out=v_sb[:, tt, :], in_=bank[pbk], func=ACT.Copy), [PSN[pbk]], ["v_sb"])
            if stage == "B2":
                return
            S.op("dve", lambda e: e.memset(u_t[:, 0:1], 0.0), ["qdead", "u_t"], ["u_t"])
            S.op("dve", lambda e: e.memset(u_t[:, 2049:2050], 0.0), ["qdead", "u_t"], ["u_t"])
            for j in range(4):
                ensure_loaded(gi + 5)
                sgb, sgc, sxc = slots[gi], slots[gi + 1], slots[gi + 2]
                gi += 3
                for tb in range(4):
                    i = (j * 4 + tb) % 2
                    p_xc = proj(sxc, C + tb * 512, 512)
                    p_gc = proj(sgc, C + tb * 512, 512)
                    p_gb = proj(sgb, C + tb * 512, 512)
                    S.op("act", lambda e, i=i, p_xc=p_xc: e.activation(out=xc_sb[i], in_=bank[p_xc], func=ACT.Copy), [PSN[p_xc]], [f"xc{i}"])
                    S.op("dve", lambda e, i=i, p_gc=p_gc, tb=tb: e.tensor_tensor(u_t[:, 1 + tb * 512:1 + (tb + 1) * 512], bank[p_gc], xc_sb[i], op=ALU.mult),
                         [PSN[p_gc], f"xc{i}", "qdead"], ["u_t"])
                    S.op("act", lambda e, p_gb=p_gb, tb=tb: e.activation(out=gb_t[:, tb * 512:(tb + 1) * 512], in_=bank[p_gb], func=ACT.Copy),
                         [PSN[p_gb], "qdead"], ["gb_t"])
                for hf in range(2):
                    o0 = hf * 1024
                    S.op("act", lambda e, j=j, o0=o0: e.activation(out=y_t, in_=u_t[:, 1 + o0:1 + o0 + 1024], func=ACT.Identity, scale=cw[:, j, 1:2]),
                         ["u_t", "cw", "qdead"], ["y_t"])
                    S.op("dve", lambda e, j=j, o0=o0: e.scalar_tensor_tensor(y_t, u_t[:, o0:o0 + 1024], cw[:, j, 0:1], y_t, op0=ALU.mult, op1=ALU.add),
                         ["u_t", "cw", "y_t"], ["y_t"])
                    S.op("dve", lambda e, j=j, o0=o0: e.scalar_tensor_tensor(y_t, u_t[:, 2 + o0:2 + o0 + 1024], cw[:, j, 2:3], y_t, op0=ALU.mult, op1=ALU.add),
                         ["u_t", "cw", "y_t"], ["y_t"])
                    S.op("dve", lambda e, j=j, o0=o0: e.tensor_tensor(convT[:, j, o0:o0 + 1024], y_t, gb_t[:, o0:o0 + 1024], op=ALU.mult),
                         ["y_t", "gb_t"], ["convT", "convdead"])
            if stage == "B3":
                return
            for h in range(4):
                ensure_loaded(gi + 4)
                sl = slots[gi]
                gi += 1
                for tb in range(4):
                    pbk = proj(sl, C + tb * 512, 512)
                    flush(0)
                    rope(pbk, qT[:, h, tb * 512:(tb + 1) * 512], f"qT{h}", tb, extra_reads=("convdead",))
            flush(0)
            S.op("pe", lambda e: e.matmul(bank[7][:, 0:2], ones_f[0:1, :], lq3[0:1, 6:8], start=True, stop=True),
                 [f"xn{t}" for t in range(18)] + ["ones_f"], [PSN[7], "xndead"])
            for k in range(8):
                S.dma("pool", lambda e, k=k: e.dma_start(out=w_out_sb[:, k, :], in_=w_out[k * 128:(k + 1) * 128, :]),
                      ["xndead"], ["w_out_sb"], "d_wout")

            if stage == "B":
                return
            steps = [(h, qb, kt) for h in range(4) for qb in range(8) for kt in range(18)]
            deferred = {}

            def qk(si):
                h, qb, kt = steps[si]
                sb0 = (si % 2) * 2

                def mm(e):
                    for m in range(2):
                        ins = e.matmul(bank[sb0 + m][:, 0:256], kT[64 * m:64 * (m + 1), h, kt * 128:(kt + 1) * 128],
                                       qT[64 * m:64 * (m + 1), h, qb * 256:(qb + 1) * 256], start=True, stop=True)
                    return ins
                S.op("pe", mm, [f"kT{h}", f"qT{h}"], [PSN[sb0], PSN[sb0 + 1]])

            def fin1(h, qb, fi):
                ob = 4
                db = 5
                i = fi % 2
                S.op("act", lambda e: e.activation(out=dcp, in_=bank[db], func=ACT.Copy), [PSN[db], "xndead"], ["dcp"])
                S.op("act", lambda e: e.activation(out=Ocp, in_=bank[ob], func=ACT.Copy), [PSN[ob], "xndead"], ["Ocp"])
                S.op("dve", lambda e: e.reciprocal(dcp, dcp), ["dcp", "xndead"], ["dcp"])
                S.op("dve", lambda e: e.tensor_tensor(tO, Ocp, dcp, op=ALU.mult), ["Ocp", "dcp"], ["tO"])
                S.op("dve", lambda e: e.scalar_tensor_tensor(o_t[i], tO[:, 256:512], neglam[:, 0:1], tO[:, 0:256], op0=ALU.mult, op1=ALU.add),
                     ["tO", "neglam"], [f"o{i}"])
                S.op("dve", lambda e: e.tensor_tensor(osq[i], o_t[i], o_t[i], op=ALU.mult), [f"o{i}"], [f"osq{i}"])

            def fin2(h, qb, fi):
                i = fi % 2
                S.op("pe", lambda e: e.matmul(bank[6][:, 0:256], ones_f[:], osq[i], start=True, stop=True), ["ones_f", f"osq{i}"], [PSN[6]])
                S.op("act", lambda e: e.activation(out=ln_t[i], in_=bank[6][:, 0:256], func=ACT.Ln, scale=1.0 / 128, bias=eps_t[:, 0:1]),
                     [PSN[6], "eps_t"], [f"ln{i}"])
                S.op("act", lambda e: e.activation(out=rs_t[i], in_=ln_t[i], func=ACT.Exp, scale=-0.5), [f"ln{i}"], [f"rs{i}"])
                S.op("dve", lambda e: e.scalar_tensor_tensor(attnT[:, h, qb * 256:(qb + 1) * 256], o_t[i], sgs[:, 0:1], rs_t[i], op0=ALU.mult, op1=ALU.mult),
                     [f"o{i}", f"rs{i}", "sgs", "xndead"], ["attnT"])

            qk(0)
            fi = 0
            for si, (h, qb, kt) in enumerate(steps):
                if si + 1 < len(steps):
                    qk(si + 1)
                sb0 = (si % 2) * 2
                ai = si % 3
                for m in range(2):
                    S.op("act", lambda e, sb0=sb0, ai=ai, m=m: e.activation(out=at_t[ai][:, m * 256:(m + 1) * 256], in_=bank[sb0 + m][:, 0:256], func=ACT.Exp, scale=0.125),
                         [PSN[sb0 + m], "xndead"], [f"at{ai}"])
                ob = 4
                db = 5

                def av(e, h=h, kt=kt, ai=ai, ob=ob, db=db):
                    e.matmul(bank[ob], v_sb[:, kt, h * 128:(h + 1) * 128], at_t[ai], start=(kt == 0), stop=(kt == 17))
                    return e.matmul(bank[db], ones_b[:], at_t[ai], start=(kt == 0), stop=(kt == 17))
                S.op("pe", av, ["v_sb", f"at{ai}", "ones_b"], [PSN[ob], PSN[db]])
                if si in deferred:
                    deferred.pop(si)()
                if kt == 17:
                    fin1(h, qb, fi)
                    deferred[min(si + 4, len(steps) - 1) if si + 4 < len(steps) else -1] = (lambda h=h, qb=qb, fi=fi: fin2(h, qb, fi))
                    fi += 1
            for k_ in sorted(deferred):
                deferred[k_]()
            S.op("pe", lambda e: e.matmul(bank[7][:, 0:2], ones_f[0:1, :], lq3[0:1, 6:8], start=True, stop=True),
                 ["ones_f", "v_sb"] + [f"kT{h}" for h in range(4)] + [f"qT{h}" for h in range(4)], [PSN[7], "kvdead", "qdead"])

            if stage == "C":
                return
            mod_tile(G1, "G1", b, 2, 1, gtmp2, "gtmp2", False, guard=("qdead",))
            mod_tile(A2, "A2", b, 4, 2, gtmp2, "gtmp2", True, guard=("qdead",))
            mod_tile(B2, "B2", b, 3, None, None, None, False, guard=("qdead",))
            mixin = [attnT[:, h, :] for h in range(4)] + [convT[:, j, :] for j in range(4)]

            def d0(tt):
                s = tt % 2
                S.dma("sp", lambda e: e.dma_start(out=xt2[s], in_=x[b, tt * 128:(tt + 1) * 128, :]), ["kvdead"], [f"xt2{s}"], f"d_xt2{s}")

            def d1(tt):
                s = tt % 2
                if tt + 1 < 16:
                    d0(tt + 1)
                for hf in range(2):
                    def mm(e, hf=hf):
                        for k in range(8):
                            ins = e.matmul(bank[hf], mixin[k][:, tt * 128:(tt + 1) * 128], w_out_sb[:, k, hf * 512:(hf + 1) * 512], start=(k == 0), stop=(k == 7))
                        return ins
                    S.op("pe", mm, ["attnT", "convT", "w_out_sb"], [PSN[hf]])
                rs, rsn = rstd_of2(bank[0], bank[1], [PSN[0], PSN[1]], junk, 1.0 / D)
                for hf in range(2):
                    S.op("dve", lambda e, hf=hf: e.scalar_tensor_tensor(tD[:, hf * 512:(hf + 1) * 512], bank[hf], rs, G1[:, hf * 512:(hf + 1) * 512], op0=ALU.mult, op1=ALU.mult),
                         [PSN[hf], rsn, "G1", "kvdead"], ["tD"])
                S.op("dve", lambda e: e.tensor_tensor(h_sb[s], tD, xt2[s], op=ALU.add), ["tD", f"xt2{s}", "kvdead"], [f"h{s}"])
                S.dma("sp", lambda e: e.dma_start(out=h_d[b, tt * 128:(tt + 1) * 128, :], in_=h_sb[s]), [f"h{s}"], [f"h_d{b}_{tt}"], f"d_h{s}")
                rs2, rsn2 = rstd_of(h_sb[s], D, [f"h{s}"], junk, 1.0 / D)
                S.op("dve", lambda e: e.scalar_tensor_tensor(t2D, h_sb[s], rs2, A2, op0=ALU.mult, op1=ALU.mult),
                     [f"h{s}", rsn2, "A2", "kvdead"], ["t2D"])
                S.op("dve", lambda e: e.tensor_tensor(xn2[s], t2D, B2, op=ALU.add), ["t2D", "B2"], [f"xn2{s}"])
                S.dma("pool", lambda e: e.dma_start(out=xn2_d[b][tt * 128:(tt + 1) * 128, :], in_=xn2[s]), [f"xn2{s}"], [f"xn2d{b}"], f"d_x2{s}")

            def d2(tt):
                s = tt % 2

                def tp(e):
                    for k in range(8):
                        ins = e.transpose(bank[2 + k // 4][:, (k % 4) * 128:(k % 4 + 1) * 128], xn2[s][:, k * 128:(k + 1) * 128], idf[:])
                    return ins
                S.op("pe", tp, [f"xn2{s}", "idf"], [PSN[2], PSN[3]])
                for hb in range(2):
                    S.op("act", lambda e, hb=hb: e.activation(out=xn2T[:, hb * 4:(hb + 1) * 4, :], in_=bank[2 + hb].rearrange("p (k t) -> p k t", k=4), func=ACT.Copy),
                         [PSN[2 + hb], "kvdead"], ["xn2T"])

            def d3(tt):
                s = tt % 2

                def mm(e):
                    for k in range(8):
                        ins = e.matmul(bank[4][:, 0:16], xn2T[:, k, :], wr_sb[:, k, :], start=(k == 0), stop=(k == 7))
                    return ins
                S.op("pe", mm, ["xn2T", "wr_sb"], [PSN[4]])
                c = stat_slot(4)
                S.op("dve", lambda e: e.reduce_max(stat[:, c:c + 1], bank[4][:, 0:16], axis=AX.X), [PSN[4]], [f"st{c}"])
                S.op("dve", lambda e: e.tensor_scalar(stat[:, c + 1:c + 2], stat[:, c:c + 1], -1.0, None, op0=ALU.mult), [f"st{c}"], [f"st{c+1}"])
                S.op("act", lambda e: e.activation(out=ex_sb, in_=bank[4][:, 0:16], func=ACT.Exp, bias=stat[:, c + 1:c + 2], accum_out=stat[:, c + 2:c + 3]),
                     [PSN[4], f"st{c+1}"], ["ex_sb", f"st{c+2}"])
                S.op("dve", lambda e: e.reciprocal(stat[:, c + 3:c + 4], stat[:, c + 2:c + 3]), [f"st{c+2}"], [f"st{c+3}"])
                S.op("dve", lambda e: e.tensor_scalar(aff_sb[s], ex_sb, stat[:, c + 3:c + 4], None, op0=ALU.mult), ["ex_sb", f"st{c+3}"], [f"aff{s}"])

            def d4(tt):
                s = tt % 2
                S.op("pe", lambda e: e.transpose(bank[5][0:16, 0:128], aff_sb[s], idf[:]), [f"aff{s}", "idf"], [PSN[5]])
                S.op("act", lambda e: e.activation(out=affT_sb[s][0:16, :], in_=bank[5][0:16, 0:128], func=ACT.Copy), [PSN[5]], [f"affT{s}"])
                S.dma("sp", lambda e: e.dma_start(out=aff_d[32 * b:32 * b + 16, tt * 128:(tt + 1) * 128], in_=affT_sb[s][0:16, :]),
                      [f"affT{s}"], ["aff_d"], f"d_af{s}")

            d0(0)
            for i in range(16 + 3):
                if 0 <= i - 3 < 16:
                    d4(i - 3)
                if 0 <= i - 2 < 16:
                    d3(i - 2)
                if 0 <= i - 1 < 16:
                    d2(i - 1)
                if i < 16:
                    d1(i)
            S.barrier()

        for b_ in range(NB if stage != "phase0" else 0):
            do_batch(b_)
        if stage in ("A", "B", "C", "B1", "B2", "B3", "B0a", "B0b", "B0c", "B0d"):
            S.barrier()

        if stage in ("mixer", "phase0", "A", "B", "C", "B1", "B2", "B3", "B0a", "B0b", "B0c", "B0d"):
            AR.reset()
            cp = [AR.take([D], F32) for _ in range(2)]
            for b in range(NB):
                for tt in range(16):
                    s = tt % 2
                    S.dma("sp", lambda e, b=b, tt=tt, s=s: e.dma_start(out=cp[s], in_=h_d[b, tt * 128:(tt + 1) * 128, :]), [], [f"cp{s}"], f"d_cp{s}")
                    S.dma("sp", lambda e, b=b, tt=tt, s=s: e.dma_start(out=out[b, tt * 128:(tt + 1) * 128, :], in_=cp[s]), [f"cp{s}"], ["out"], f"d_co{s}")
            S.barrier(["sp"])
            S.emit()
            return nc

        AR.reset()
        wexp = [[AR.take([8, D], BF16) for _ in range(3)] for _ in range(2)]
        xs = [AR.take([D], BF16) for _ in range(8)]
        xsT = [AR.take([8, 512], BF16) for _ in range(2)]
        actT = [AR.take([8, 512], BF16) for _ in range(2)]
        sgt = [AR.take([512], F32) for _ in range(2)]
        y_sb = [AR.take([D], F32) for _ in range(8)]
        moe_end = AR.off
        work = AR.take([T], F32)
        vals = AR.take([CAP], F32)
        idx = AR.take([CAP], U32)
        idxf = AR.take([CAP], F32)
        idxT = AR.take([2, 128], U32)
        gT = AR.take([2, 128], F32)
        rt_end = AR.off
        AR.off = 0
        NFB = 4
        ffl = [AR.take([D], F32) for _ in range(NFB)]
        hl = [AR.take([D], F32) for _ in range(NFB)]
        G2 = [AR.take([D], F32) for _ in range(2)]
        gtmp3 = AR.take([D], F32)
        tF = [AR.take([D], F32) for _ in range(NFB)]
        ob_t = [AR.take([D], F32) for _ in range(NFB)]
        junk2 = AR.take([D], BF16)
        AR.off = rt_end
        print("moe arena bytes", AR.off)

        def load_expert(e_):
            s = e_ % 2
            for wi, wsrc in enumerate((w_gate, w_up, w_down)):
                for k in range(8):
                    S.dma("pool", lambda e, s=s, wi=wi, wsrc=wsrc, k=k: e.dma_start(out=wexp[s][wi][:, k, :], in_=wsrc[e_, k * 128:(k + 1) * 128, :]),
                          [], [f"we{s}_{wi}"], f"d_we{s}_{wi}")

        load_expert(0)
        S.dma("sp", lambda e: e.dma_start(out=work, in_=aff_d), ["aff_d"], ["work"], "d_work")
        for r in range(CAP // 8):
            S.op("dve", lambda e, r=r: e.max(out=vals[:, r * 8:(r + 1) * 8], in_=work), ["work"], ["vals"])
            S.op("dve", lambda e, r=r: e.max_index(out=idx[:, r * 8:(r + 1) * 8], in_max=vals[:, r * 8:(r + 1) * 8], in_values=work), ["work", "vals"], ["idx"])
            S.op("dve", lambda e, r=r: e.match_replace(out=work, in_to_replace=vals[:, r * 8:(r + 1) * 8], in_values=work, imm_value=-1.0), ["vals", "idx"], ["work"])
        S.op("dve", lambda e: e.tensor_copy(idxf, idx), ["idx"], ["idxf"])
        for ct in range(2):
            S.op("pe", lambda e, ct=ct: e.transpose(bank[0][:, 0:128], idxf[:, ct * 128:(ct + 1) * 128], idf[:]), ["idxf", "idf"], [PSN[0]])
            S.op("dve", lambda e, ct=ct: e.tensor_copy(idxT[:, ct, :], bank[0][:, 0:128]), [PSN[0]], ["idxT"])
            S.op("pe", lambda e, ct=ct: e.transpose(bank[1][:, 0:128], vals[:, ct * 128:(ct + 1) * 128], idf[:]), ["vals", "idf"], [PSN[1]])
            S.op("dve", lambda e, ct=ct: e.tensor_copy(gT[:, ct, :], bank[1][:, 0:128]), [PSN[1]], ["gT"])

        NP = (NB + 1) // 2
        tiles = [(pr, bi, bb, ct) for pr in range(NP) for bi, bb in enumerate([q for q in (2 * pr, 2 * pr + 1) if q < NB]) for ct in range(2)]

        def gathers(e_):
            for j, (pr, bi, bb, ct) in enumerate(tiles):
                row = 32 * bb + e_
                S.dma("pool", lambda e, j=j, bb=bb, ct=ct, row=row: e.indirect_dma_start(
                    out=xs[j], out_offset=None, in_=xn2_d[bb],
                    in_offset=bass.IndirectOffsetOnAxis(ap=idxT[:, ct, row:row + 1], axis=0)),
                    [f"xn2d{bb}", "idxT"], [f"xs{j}"], f"d_xs{j}")

        def transposes(e_):
            for j, (pr, bi, bb, ct) in enumerate(tiles):
                pbk = 6 + j % 2
                pv = bank[pbk].bitcast(BF16)

                def tpx(e, j=j, pv=pv):
                    for k in range(8):
                        ins = e.transpose(pv[:, k * 128:(k + 1) * 128], xs[j][:, k * 128:(k + 1) * 128], idb[:])
                    return ins
                S.op("pe", tpx, [f"xs{j}", "idb"], [PSN[pbk]])
                c0 = (bi * 2 + ct) * 128
                S.op("act", lambda e, pr=pr, c0=c0, pv=pv: e.activation(out=xsT[pr][:, :, c0:c0 + 128], in_=pv.rearrange("p (k t) -> p k t", k=8), func=ACT.Copy),
                     [PSN[pbk]], [f"xsT{pr}"])

        def do_pair(e_, pr):
            ws = e_ % 2
            wg_, wu_, wd_ = wexp[ws]
            mine = [(j, t) for j, t in enumerate(tiles) if t[0] == pr]
            ncol = len(mine) * 128
            for fc in range(8):
                bg = 0 + 2 * (fc % 2)
                bu = 1 + 2 * (fc % 2)
                si_ = fc % 2

                def mmg(e, fc=fc, bg=bg):
                    for k in range(8):
                        ins = e.matmul(bank[bg][:, 0:ncol], wg_[:, k, fc * 128:(fc + 1) * 128], xsT[pr][:, k, 0:ncol], start=(k == 0), stop=(k == 7))
                    return ins

                def mmu(e, fc=fc, bu=bu):
                    for k in range(8):
                        ins = e.matmul(bank[bu][:, 0:ncol], wu_[:, k, fc * 128:(fc + 1) * 128], xsT[pr][:, k, 0:ncol], start=(k == 0), stop=(k == 7))
                    return ins
                S.op("pe", mmg, [f"we{ws}_0", f"xsT{pr}"], [PSN[bg]])
                S.op("pe", mmu, [f"we{ws}_1", f"xsT{pr}"], [PSN[bu]])
                S.op("act", lambda e, bg=bg, si_=si_: e.activation(out=sgt[si_][:, 0:ncol], in_=bank[bg][:, 0:ncol], func=ACT.Silu), [PSN[bg]], [f"sgt{si_}"])
                S.op("dve", lambda e, fc=fc, bu=bu, si_=si_: e.tensor_tensor(actT[pr][:, fc, 0:ncol], sgt[si_][:, 0:ncol], bank[bu][:, 0:ncol], op=ALU.mult),
                     [PSN[bu], f"sgt{si_}"], [f"actT{pr}"])
            for j, (pr_, bi, bb, ct) in mine:
                row = 32 * bb + e_
                c0 = (bi * 2 + ct) * 128
                for hf in range(2):
                    yb = 4 + hf

                    def mmd(e, c0=c0, hf=hf, yb=yb):
                        for k in range(8):
                            ins = e.matmul(bank[yb], actT[pr][:, k, c0:c0 + 128], wd_[:, k, hf * 512:(hf + 1) * 512], start=(k == 0), stop=(k == 7))
                        return ins
                    S.op("pe", mmd, [f"actT{pr}", f"we{ws}_2"], [PSN[yb]])
                    if hf == 0:
                        S.op("act", lambda e, j=j, yb=yb, ct=ct, row=row: e.activation(out=y_sb[j][:, 0:512], in_=bank[yb], func=ACT.Identity, scale=gT[:, ct, row:row + 1]),
                             [PSN[yb], "gT"], [f"y{j}a"])
                    else:
                        S.op("dve", lambda e, j=j, yb=yb, ct=ct, row=row: e.tensor_scalar(y_sb[j][:, 512:1024], bank[yb], gT[:, ct, row:row + 1], None, op0=ALU.mult),
                             [PSN[yb], "gT"], [f"y{j}b"])
                par, ppar = e_ % 2, (e_ - 1) % 2
                S.dma("pool", lambda e, j=j, bb=bb, ct=ct, row=row: e.indirect_dma_start(
                    out=ff_d[bb], out_offset=bass.IndirectOffsetOnAxis(ap=idxT[:, ct, row:row + 1], axis=0),
                    in_=y_sb[j], in_offset=None, compute_op=ALU.add),
                    [f"y{j}a", f"y{j}b", "idxT", f"ff{bb}", f"ffs{bb}_0_{ppar}", f"ffs{bb}_1_{ppar}"], [f"ffs{bb}_{ct}_{par}"], f"d_y{j}")

        gathers(0)
        for e_ in range(NE):
            transposes(e_)
            if e_ + 1 < NE:
                load_expert(e_ + 1)
                gathers(e_ + 1)
            for pr in range(NP):
                do_pair(e_, pr)
        S.barrier()

        bc_load(gtmp3, gains[3:4, :], "gtmp3", "d_bc_gtmp3")
        ftiles = [(b, tt) for b in range(NB) for tt in range(16)]

        def f_load(i):
            b, tt = ftiles[i]
            s = i % NFB
            if tt == 0:
                g = b % 2
                bc_load(G2[g], mod_d[b:b + 1, 5 * D:6 * D], f"G2{g}", f"d_bc_G2{g}")
                S.op("dve", lambda e, g=g: e.tensor_tensor(G2[g], G2[g], gtmp3, op=ALU.mult), [f"G2{g}", "gtmp3"], [f"G2{g}"])
            S.dma("sp", lambda e: e.dma_start(out=ffl[s], in_=ff_d[b][tt * 128:(tt + 1) * 128, :]), [f"ff{b}"], [f"ffl{s}"], f"d_ffl{s}")
            S.dma("sp", lambda e: e.dma_start(out=hl[s], in_=h_d[b, tt * 128:(tt + 1) * 128, :]), [f"h_d{b}_{tt}"], [f"hl{s}"], f"d_hl{s}")

        def f_compute(i):
            b, tt = ftiles[i]
            s = i % NFB
            g = b % 2
            rs, rsn = rstd_of(ffl[s], D, [f"ffl{s}"], junk2, 1.0 / D)
            S.op("dve", lambda e: e.scalar_tensor_tensor(tF[s], ffl[s], rs, G2[g], op0=ALU.mult, op1=ALU.mult), [f"ffl{s}", rsn, f"G2{g}"], [f"tF{s}"])
            S.op("dve", lambda e: e.tensor_tensor(ob_t[s], tF[s], hl[s], op=ALU.add), [f"tF{s}", f"hl{s}"], [f"ob{s}"])
            S.dma("sp", lambda e: e.dma_start(out=out[b, tt * 128:(tt + 1) * 128, :], in_=ob_t[s]), [f"ob{s}"], ["out"], f"d_out{s}")

        AHEAD = NFB - 1
        for i in range(min(AHEAD, len(ftiles))):
            f_load(i)
        for i in range(len(ftiles)):
            if i + AHEAD < len(ftiles):
                f_load(i + AHEAD)
            f_compute(i)
        S.barrier(["sp"])
        S.emit()
    return nc


def _rope_tables():
    t = np.arange(T)
    row = (t // 64).astype(np.float32)
    col = (t % 64).astype(np.float32)
    inv = (10000.0 ** (-np.arange(0, 32, 2, dtype=np.float32) / 32)).astype(np.float32)
    ang_r = row[:, None] * inv[None, :]
    ang_c = col[:, None] * inv[None, :]
    ang = np.concatenate([ang_r, ang_r, ang_c, ang_c], axis=-1)
    cos = np.cos(ang).astype(np.float32).T
    sin = np.sin(ang).astype(np.float32).T
    sgn = np.concatenate([-np.ones(16), np.ones(16), -np.ones(16), np.ones(16)]).astype(np.float32)[:, None]
    sin = sin * sgn
    cosT = np.ascontiguousarray(np.concatenate([cos, cos], 0))
    sinT = np.ascontiguousarray(np.concatenate([sin, sin], 0))
    perm = np.zeros((128, 128), np.float32)
    for i in range(128):
        perm[i ^ 16, i] = 1.0
    return cosT, sinT, perm


def make_in_maps(inputs, NB=4, ncores=NCORES, ne=NE):
    f = lambda a: np.ascontiguousarray(np.asarray(a, dtype=np.float32))
    x = f(inputs["x"]); c = f(inputs["c"]); ctx = f(inputs["ctx"]); c_ctx = f(inputs["c_ctx"])
    w_in = f(inputs["w_in"])[0]
    cosT, sinT, perm = _rope_tables()
    w_in_r = np.ascontiguousarray(w_in.reshape(8, 128, 24, 128).transpose(2, 1, 0, 3))
    w_v_r = np.ascontiguousarray(w_in[:, 1024:1536].reshape(8, 128, 512).transpose(1, 0, 2))
    shared = dict(
        w_ada=f(inputs["w_ada"])[0], b_ada=f(inputs["b_ada"]),
        gains=np.ascontiguousarray(np.concatenate([f(inputs["norm_pre_mix"]), f(inputs["norm_post_mix"]),
                                                   f(inputs["norm_pre_ffn"]), f(inputs["norm_post_ffn"])], 0)),
        w_in_r=w_in_r, w_v_r=w_v_r,
        convw=np.ascontiguousarray(f(inputs["conv_w"])[0].T.reshape(4, 128, 3).transpose(1, 0, 2)),
        lqk=np.ascontiguousarray(np.concatenate([f(inputs["lambda_q1"]), f(inputs["lambda_k1"]),
                                                 f(inputs["lambda_q2"]), f(inputs["lambda_k2"])], 1)),
        subln=np.ascontiguousarray(f(inputs["subln_g"]).reshape(128, 1)),
        w_out=f(inputs["w_out"])[0],
        w_router=np.ascontiguousarray(f(inputs["w_router"])[0].reshape(8, 128, 16).transpose(1, 0, 2)),
        w_gate=f(inputs["w_gate"])[0][:ne], w_up=f(inputs["w_up"])[0][:ne], w_down=f(inputs["w_down"])[0][:ne],
        ident=np.eye(128, dtype=np.float32), perm=perm, cosT=cosT, sinT=sinT,
    )
    maps = []
    for i in range(ncores):
        sl = slice(i * NB, (i + 1) * NB)
        cc = np.concatenate([c[sl], c_ctx[None, :]], 0)
        if cc.shape[0] < 5:
            cc = np.concatenate([cc[:-1], np.zeros((5 - cc.shape[0], D), np.float32), cc[-1:]], 0)
        ccT = np.ascontiguousarray(cc.T.reshape(8, 128, 5).transpose(1, 0, 2))
        m = dict(shared)
        m.update(x=np.ascontiguousarray(x[sl]), ctx=np.ascontiguousarray(ctx[sl]), ccT=ccT)
        maps.append(m)
    return maps


def kernel(**inputs):
    NB = 4
    nc = build(NB=NB, stage="full")
    maps = make_in_maps(inputs, NB=NB, ncores=NCORES)
    res = run_bass_kernel_spmd(nc, maps, core_ids=list(range(NCORES)))
    return np.concatenate([np.asarray(r["out"]) for r in res.results], axis=0).astype(np.float32)
```

```python
import math
from contextlib import ExitStack
import numpy as np
import concourse.bass as bass
import concourse.mybir as mybir
from concourse.bass_utils import run_bass_kernel_spmd

F32 = mybir.dt.float32
BF16 = mybir.dt.bfloat16
U32 = mybir.dt.uint32
ACT = mybir.ActivationFunctionType
ALU = mybir.AluOpType
AX = mybir.AxisListType

T = 2048
C = 256
D = 1024
TK = T + C
NE = 16
CAP = 256
EPS = 1e-6
NCORES = 8


class Sched:
    ENGS = ("pe", "act", "dve", "pool", "sp")

    def __init__(self, nc, stack):
        self.nc = nc
        self.stack = stack
        self.sems = {}
        self.eng = {}
        for n in self.ENGS:
            self.sems["s_" + n] = stack.enter_context(nc.semaphore("s_" + n))
            self.eng[n] = dict(ops=[], cnt=0, waited={})
        self.lastw = {}
        self.readers = {}
        self.dcum = {}

    def _deps(self, reads, writes):
        d = []
        for r in reads:
            if r in self.lastw:
                d.append(self.lastw[r])
        for w in writes:
            if w in self.lastw:
                d.append(self.lastw[w])
            d.extend(self.readers.get(w, ()))
        return d

    def _waits(self, en, deps):
        E = self.eng[en]
        need = {}
        for (s, v) in deps:
            if en == "pe" and s == "s_pe":
                continue
            if E["waited"].get(s, 0) >= v:
                continue
            if need.get(s, 0) < v:
                need[s] = v
        for s, v in need.items():
            E["waited"][s] = v
        return list(need.items())

    def _record(self, ev, reads, writes):
        for r in reads:
            self.readers.setdefault(r, []).append(ev)
        for w in writes:
            self.lastw[w] = ev
            self.readers[w] = []

    def op(self, en, fn, reads=(), writes=()):
        excl = [r for r in reads if r.startswith("ps") and r not in writes]
        if excl:
            reads = [r for r in reads if r not in excl]
            writes = list(writes) + excl
        deps = self._deps(reads, writes)
        waits = self._waits(en, deps)
        E = self.eng[en]
        E["cnt"] += 1
        ev = ("s_" + en, E["cnt"])
        self._record(ev, reads, writes)
        sems = self.sems

        def run(e, fn=fn, waits=waits, sem=sems["s_" + en]):
            for (s, v) in waits:
                e.wait_ge(sems[s], v)
            fn(e).then_inc(sem, 1)

        E["ops"].append(run)

    def dma(self, q, mk, reads, writes, key):
        deps = self._deps(reads, writes)
        waits = self._waits(q, deps)
        if key not in self.sems:
            self.sems[key] = self.stack.enter_context(self.nc.semaphore(key))
            self.dcum[key] = 0
        self.dcum[key] += 16
        ev = (key, self.dcum[key])
        self._record(ev, reads, writes)
        sems = self.sems

        def run(e, mk=mk, waits=waits, sem=sems[key]):
            for (s, v) in waits:
                e.wait_ge(sems[s], v)
            mk(e).then_inc(sem, 16)

        self.eng[q]["ops"].append(run)

    def _all_events(self):
        ev = [("s_" + n, self.eng[n]["cnt"]) for n in self.ENGS if self.eng[n]["cnt"] > 0]
        ev += [(k, v) for k, v in self.dcum.items() if v > 0]
        return ev

    def barrier(self, engines=None):
        allev = self._all_events()
        for n in (engines or self.ENGS):
            waits = self._waits(n, [ev for ev in allev if ev[0] != "s_" + n])
            sems = self.sems

            def run(e, waits=waits):
                for (s, v) in waits:
                    e.wait_ge(sems[s], v)

            self.eng[n]["ops"].append(run)

    def emit(self):
        nc = self.nc
        with nc.Block() as block:
            @block.tensor
            def _(e):
                for f in self.eng["pe"]["ops"]:
                    f(e)

            @block.scalar
            def _(e):
                for f in self.eng["act"]["ops"]:
                    f(e)

            @block.vector
            def _(e):
                for f in self.eng["dve"]["ops"]:
                    f(e)

            @block.gpsimd
            def _(e):
                for f in self.eng["pool"]["ops"]:
                    f(e)

            @block.sync
            def _(e):
                for f in self.eng["sp"]["ops"]:
                    f(e)


DT_BYTES = {F32: 4, BF16: 2, U32: 4}


class Arena:
    def __init__(self, t, nbytes):
        self.t = t
        self.nbytes = nbytes
        self.off = 0

    def reset(self):
        self.off = 0

    def take(self, free, dtype):
        n = 1
        for s in free:
            n *= s
        sz = n * DT_BYTES[dtype]
        a = self.off
        self.off += (sz + 63) // 64 * 64
        assert self.off <= self.nbytes, (self.off, self.nbytes)
        ap = self.t[:, a // 4:(a + sz) // 4]
        if dtype != F32:
            ap = ap.bitcast(dtype)
        if len(free) == 2:
            ap = ap.rearrange("p (a b) -> p a b", a=free[0])
        elif len(free) == 3:
            ap = ap.rearrange("p (a b c) -> p a b c", a=free[0], b=free[1])
        return ap


def build(NB=4, stage="full"):
    nc = bass.Bass("TRN2", target_bir_lowering=False)

    def din(n, s, d=F32):
        return nc.dram_tensor(n, s, d, kind="ExternalInput").ap()

    x = din("x", [NB, T, D])
    ctx = din("ctx", [NB, C, D])
    ccT = din("ccT", [128, 8, 5])
    w_ada = din("w_ada", [D, 6 * D])
    b_ada = din("b_ada", [1, 6 * D])
    gains = din("gains", [4, D])
    w_in_r = din("w_in_r", [24, 128, 8, 128])
    w_v_r = din("w_v_r", [128, 8, 512])
    convw = din("convw", [128, 4, 3])
    lqk = din("lqk", [1, 256])
    subln = din("subln", [128, 1])
    w_out = din("w_out", [D, D])
    w_router = din("w_router", [128, 8, 16])
    NE_decl = NE if stage == "full" else 1
    w_gate = din("w_gate", [NE_decl, D, D])
    w_up = din("w_up", [NE_decl, D, D])
    w_down = din("w_down", [NE_decl, D, D])
    ident = din("ident", [128, 128])
    perm = din("perm", [128, 128])
    cosT_d = din("cosT", [128, T])
    sinT_d = din("sinT", [128, T])
    out = nc.dram_tensor("out", [NB, T, D], F32, kind="ExternalOutput").ap()
    mod_d = nc.dram_tensor("mod_d", [5, 6 * D], F32).ap()
    h_d = nc.dram_tensor("h_d", [NB, T, D], F32).ap()
    aff_d = nc.dram_tensor("aff_d", [128, T], F32).ap()
    xn2_d = [nc.dram_tensor(f"xn2_d{b}", [T, D], BF16).ap() for b in range(NB)]
    ff_d = [nc.dram_tensor(f"ff_d{b}", [T, D], F32).ap() for b in range(NB)]

    with ExitStack() as st:
        S = Sched(nc, st)

        def sb(n, s, d):
            return st.enter_context(nc.sbuf_tensor(n, s, d))

        idf = sb("idf", [128, 128], F32)
        idb = sb("idb", [128, 128], BF16)
        permf = sb("permf", [128, 128], F32)
        permb = sb("permb", [128, 128], BF16)
        ones_b = sb("ones_b", [128, 128], BF16)
        ones_f = sb("ones_f", [128, 128], F32)
        neghalf = sb("neghalf", [128, 256], F32)
        eps_t = sb("eps_t", [128, 1], F32)
        cw = sb("cw", [128, 4, 3], F32)
        sgs = sb("sgs", [128, 1], F32)
        neglam = sb("neglam", [128, 2], F32)
        wr_sb = sb("wr_sb", [128, 8, 16], F32)
        stat = sb("stat", [128, 96], F32)
        lq_sb = sb("lq_sb", [1, 256], F32)
        lq2 = sb("lq2", [1, 128], F32)
        lq3 = sb("lq3", [1, 8], F32)
        ARENA_BYTES = 200 * 1024
        arena_t = sb("arena", [128, ARENA_BYTES // 4], F32)
        AR = Arena(arena_t, ARENA_BYTES)
        stAB = [st.enter_context(nc.psum_tensor(f"psST{i}", [128, 1024], F32)) for i in range(2)]
        psS = [st.enter_context(nc.psum_tensor(f"psS{i}", [128, 512], F32)) for i in range(4)]
        bank = [stAB[0][:, 0:512], stAB[0][:, 512:1024], stAB[1][:, 0:512], stAB[1][:, 512:1024]] + [p[:] for p in psS]
        PSN = [f"ps{i}" for i in range(8)]

        stat_ctr = [0]

        def stat_slot(n=1):
            c = stat_ctr[0]
            if c + n > 96:
                c = 0
            stat_ctr[0] = c + n
            return c

        def rstd_of(src, nfree, src_reads, junk, inv_n):
            c = stat_slot(3)
            r0, r1, r2 = f"st{c}", f"st{c+1}", f"st{c+2}"
            S.op("act", lambda e: e.activation(out=junk, in_=src, func=ACT.Square, accum_out=stat[:, c:c + 1]),
                 src_reads, [r0, "junk"])
            S.op("dve", lambda e: e.tensor_scalar(stat[:, c + 1:c + 2], stat[:, c:c + 1], inv_n, EPS, op0=ALU.mult, op1=ALU.add),
                 [r0], [r1])
            S.op("pool", lambda e: e.tensor_tensor(stat[:, c + 2:c + 3], stat[:, c + 1:c + 2], neghalf[:, 0:1], op=ALU.pow),
                 [r1, "neghalf"], [r2])
            return stat[:, c + 2:c + 3], r2

        def rstd_of2(src0, src1, src_reads, junk, inv_n):
            c = stat_slot(5)
            ra, rb, r0, r1, r2 = (f"st{c + i}" for i in range(5))
            S.op("act", lambda e: e.activation(out=junk[:, 0:512], in_=src0, func=ACT.Square, accum_out=stat[:, c:c + 1]),
                 src_reads, [ra, "junk"])
            S.op("act", lambda e: e.activation(out=junk[:, 512:1024], in_=src1, func=ACT.Square, accum_out=stat[:, c + 1:c + 2]),
                 src_reads, [rb, "junk"])
            S.op("dve", lambda e: e.tensor_tensor(stat[:, c + 2:c + 3], stat[:, c:c + 1], stat[:, c + 1:c + 2], op=ALU.add), [ra, rb], [r0])
            S.op("dve", lambda e: e.tensor_scalar(stat[:, c + 3:c + 4], stat[:, c + 2:c + 3], inv_n, EPS, op0=ALU.mult, op1=ALU.add), [r0], [r1])
            S.op("pool", lambda e: e.tensor_tensor(stat[:, c + 4:c + 5], stat[:, c + 3:c + 4], neghalf[:, 0:1], op=ALU.pow), [r1, "neghalf"], [r2])
            return stat[:, c + 4:c + 5], r2

        S.dma("sp", lambda e: e.dma_start(out=idf[:], in_=ident), [], ["idf"], "d_c1")
        S.dma("sp", lambda e: e.dma_start(out=permf[:], in_=perm), [], ["permf"], "d_c2")
        S.dma("sp", lambda e: e.dma_start(out=cw[:], in_=convw), [], ["cw"], "d_c3")
        S.dma("sp", lambda e: e.dma_start(out=sgs[:], in_=subln), [], ["sgs"], "d_c4")
        S.dma("sp", lambda e: e.dma_start(out=wr_sb[:], in_=w_router), [], ["wr_sb"], "d_c5")
        S.dma("sp", lambda e: e.dma_start(out=lq_sb[:], in_=lqk), [], ["lq_sb"], "d_c6")
        S.op("dve", lambda e: e.tensor_copy(idb[:], idf[:]), ["idf"], ["idb"])
        S.op("dve", lambda e: e.tensor_copy(permb[:], permf[:]), ["permf"], ["permb"])
        S.op("dve", lambda e: e.memset(ones_b[:], 1.0), [], ["ones_b"])
        S.op("dve", lambda e: e.memset(ones_f[:], 1.0), [], ["ones_f"])
        S.op("dve", lambda e: e.memset(neghalf[:], -0.5), [], ["neghalf"])
        S.op("dve", lambda e: e.memset(eps_t[:], EPS), [], ["eps_t"])
        S.op("dve", lambda e: e.tensor_scalar(sgs[:], sgs[:], 0.8, None, op0=ALU.mult), ["sgs"], ["sgs"])
        lqv = lq_sb[0:1, :].rearrange("p (a b c) -> p a b c", a=2, b=2)
        S.op("dve", lambda e: e.tensor_tensor(lq2[0:1, :].rearrange("p (a c) -> p a c", a=2), lqv[:, :, 0, :], lqv[:, :, 1, :], op=ALU.mult),
             ["lq_sb"], ["lq2"])
        S.op("dve", lambda e: e.reduce_sum(lq3[0:1, 0:2], lq2[0:1, :].rearrange("p (a c) -> p a c", a=2), axis=AX.X), ["lq2"], ["lq3a"])
        S.op("act", lambda e: e.activation(out=lq3[0:1, 2:4], in_=lq3[0:1, 0:2], func=ACT.Exp), ["lq3a"], ["lq3b"])
        S.op("dve", lambda e: e.tensor_tensor(lq3[0:1, 4:5], lq3[0:1, 3:4], lq3[0:1, 2:3], op=ALU.subtract), ["lq3b"], ["lq3c"])
        S.op("dve", lambda e: e.tensor_scalar(lq3[0:1, 6:7], lq3[0:1, 4:5], -0.2, None, op0=ALU.add), ["lq3c"], ["lq3d"])
        S.op("dve", lambda e: e.tensor_copy(lq3[0:1, 7:8], lq3[0:1, 6:7]), ["lq3d"], ["lq3e"])
        S.op("pe", lambda e: e.matmul(bank[7][:, 0:2], ones_f[0:1, :], lq3[0:1, 6:8], start=True, stop=True),
             ["ones_f", "lq3d", "lq3e"], [PSN[7]])
        S.op("dve", lambda e: e.tensor_copy(neglam[:], bank[7][:, 0:2]), [PSN[7]], ["neglam"])

        AR.reset()
        ccT_sb = AR.take([8, 5], F32)
        siluT = AR.take([8, 5], F32)
        bada_sb = AR.take([6 * D], F32)
        wa = [AR.take([8, 512], F32) for _ in range(2)]
        mstage = [AR.take([512], F32) for _ in range(2)]
        zero_t = AR.take([2048], F32)
        S.dma("sp", lambda e: e.dma_start(out=ccT_sb, in_=ccT), [], ["ccT"], "d_c7")
        S.dma("sp", lambda e: e.dma_start(out=bada_sb[0:1, :], in_=b_ada), [], ["bada"], "d_c8")
        S.op("act", lambda e: e.activation(out=siluT, in_=ccT_sb, func=ACT.Silu), ["ccT"], ["siluT"])
        S.op("dve", lambda e: e.memset(zero_t, 0.0), [], ["zero_t"])
        for b in range(NB):
            ffv = ff_d[b].rearrange("(p r) d -> p (r d)", p=128)
            for j in range(8):
                S.dma("sp", lambda e, ffv=ffv, j=j: e.dma_start(out=ffv[:, j * 2048:(j + 1) * 2048], in_=zero_t),
                      ["zero_t"], [f"ff{b}"], "d_z")
        S.dma("sp", lambda e: e.dma_start(out=aff_d, in_=zero_t), ["zero_t"], ["aff_d"], "d_z")
        for j in range(12):
            s = j % 2
            S.dma("sp", lambda e, j=j, s=s: e.dma_start(out=wa[s], in_=w_ada[:, j * 512:(j + 1) * 512].rearrange("(k p) n -> p k n", p=128)),
                  [], [f"wa{s}"], f"d_wa{s}")

            def mm0(e, j=j, s=s):
                for k in range(8):
                    e.matmul(bank[4][0:5, :], siluT[:, k, :], wa[s][:, k, :], start=(k == 0), stop=False)
                return e.matmul(bank[4][0:5, :], ones_f[0:1, 0:5], bada_sb[0:1, j * 512:(j + 1) * 512], start=False, stop=True)
            S.op("pe", mm0, ["siluT", f"wa{s}", "bada", "ones_f"], [PSN[4]])
            S.op("act", lambda e, s=s: e.activation(out=mstage[s][0:5, :], in_=bank[4][0:5, :], func=ACT.Copy), [PSN[4]], [f"ms{s}"])
            S.dma("sp", lambda e, j=j, s=s: e.dma_start(out=mod_d[:, j * 512:(j + 1) * 512], in_=mstage[s][0:5, :]),
                  [f"ms{s}"], ["mod_d"], f"d_ms{s}")
        S.barrier()

        AR.reset()
        cosT = AR.take([T], F32)
        sinT = AR.take([T], F32)
        xnT = AR.take([8, TK], BF16)
        R1 = xnT.rearrange("p a b -> p (a b)")
        attnT = R1[:, 0:4 * T].rearrange("p (a b) -> p a b", a=4)
        w_out_sb = R1[:, 4 * T:4 * T + 8 * D].rearrange("p (a b) -> p a b", a=8)
        kT = AR.take([4, TK], BF16)
        v_sb = AR.take([18, 512], BF16)
        KVf = arena_t[:, 0:0]
        qc_off = AR.off
        qT = AR.take([4, T], BF16)
        AR.off = qc_off
        u_t = AR.take([2064], F32)
        gb_t = AR.take([T], F32)
        y_t = AR.take([1024], F32)
        qc_end = max(AR.off, qc_off + 4 * T * 2)
        AR.off = qc_off
        A2 = AR.take([D], F32)
        B2 = AR.take([D], F32)
        G1 = AR.take([D], F32)
        gtmp2 = AR.take([D], F32)
        AR.off = qc_end
        convT = AR.take([4, T], BF16)
        A1 = AR.take([D], F32)
        B1 = AR.take([D], F32)
        A1c = AR.take([D], F32)
        B1c = AR.take([D], F32)
        ta_off = AR.off
        XT = [AR.take([D], F32) for _ in range(2)]
        xb = [AR.take([D], BF16) for _ in range(2)]
        tmpA = AR.take([D], F32)
        junk = AR.take([D], BF16)
        ta_end = AR.off
        AR.off = ta_off
        at_t = [AR.take([512], BF16) for _ in range(3)]
        tO = AR.take([512], F32)
        o_t = [AR.take([256], F32) for _ in range(2)]
        osq = [AR.take([256], F32) for _ in range(2)]
        ln_t = [AR.take([256], F32) for _ in range(2)]
        rs_t = [AR.take([256], F32) for _ in range(2)]
        Ocp = AR.take([512], F32)
        dcp = AR.take([512], F32)
        assert AR.off <= ta_end
        AR.off = ta_end
        pb = [AR.take([512], BF16) for _ in range(2)]
        t1 = [AR.take([512], F32) for _ in range(2)]
        t2 = [AR.take([512], F32) for _ in range(2)]
        xc_sb = [AR.take([512], F32) for _ in range(2)]
        wslot = [AR.take([8, 128], BF16) for _ in range(6)]
        wv_sb = AR.take([8, 512], BF16)
        mix_end = AR.off
        kv_off = (kT.offset if hasattr(kT, "offset") else None)
        AR.off = 2 * T * 4 + 8 * TK * 2
        xt2 = [AR.take([D], F32) for _ in range(2)]
        tD = AR.take([D], F32)
        h_sb = [AR.take([D], F32) for _ in range(2)]
        t2D = AR.take([D], F32)
        xn2 = [AR.take([D], F32) for _ in range(2)]
        xn2T = AR.take([8, 128], F32)
        assert AR.off <= 2 * T * 4 + 8 * TK * 2 + 4 * TK * 2 + 18 * 512 * 2 + 64
        AR.off = mix_end
        lg_sb = AR.take([16], F32)
        ex_sb = AR.take([16], F32)
        aff_sb = [AR.take([16], F32) for _ in range(2)]
        affT_sb = [AR.take([128], F32) for _ in range(2)]
        print("mixer arena bytes", AR.off)

        S.dma("sp", lambda e: e.dma_start(out=cosT, in_=cosT_d), [], ["cosT"], "d_c9")
        S.dma("sp", lambda e: e.dma_start(out=sinT, in_=sinT_d), [], ["sinT"], "d_c10")
        S.op("dve", lambda e: e.memset(u_t, 0.0), [], ["u_t"])

        def bc_load(dst, row_ap, region, key, guard=()):
            S.dma("sp", lambda e: e.dma_start(out=dst, in_=row_ap.partition_broadcast(128)), list(guard), [region], key)

        def mod_tile(dst, region, b, seg, gain_idx, tmp, tmp_region, plus_one, guard=()):
            bc_load(dst, mod_d[b:b + 1, seg * D:(seg + 1) * D], region, "d_bc_" + region, guard)
            if gain_idx is not None:
                bc_load(tmp, gains[gain_idx:gain_idx + 1, :], tmp_region, "d_bc_" + tmp_region, guard)
                if plus_one:
                    S.op("dve", lambda e: e.scalar_tensor_tensor(dst, dst, 1.0, tmp, op0=ALU.add, op1=ALU.mult),
                         [region, tmp_region], [region])
                else:
                    S.op("dve", lambda e: e.tensor_tensor(dst, dst, tmp, op=ALU.mult), [region, tmp_region], [region])

        mod_tile(A1c, "A1c", 4, 1, 0, tmpA, "tmpA", True)
        mod_tile(B1c, "B1c", 4, 0, None, None, None, False)

        grp_ctr = [0]

        def load_group(g):
            s = grp_ctr[0] % 6
            grp_ctr[0] += 1
            S.dma("pool", lambda e: e.dma_start(out=wslot[s], in_=w_in_r[g]), [], [f"ws{s}"], f"d_ws{s}")
            return s

        pbank_ctr = [0]

        def next_pbank():
            i = pbank_ctr[0] % 4
            pbank_ctr[0] += 1
            return i

        rope_ctr = [0]

        def do_batch(b):
            mod_tile(A1, "A1", b, 1, 0, tmpA, "tmpA", True)
            mod_tile(B1, "B1", b, 0, None, None, None, False)
            S.dma("pool", lambda e: e.dma_start(out=wv_sb, in_=w_v_r), [], ["wv"], "d_wv")
            order = [("k", h, 4 + h) for h in range(4)]
            for j in range(4):
                order += [("gb", j, 12 + j), ("gc", j, 16 + j), ("xc", j, 20 + j)]
            order += [("q", h, h) for h in range(4)]
            slots = {}
            nload = [0]

            def ensure_loaded(upto):
                while nload[0] <= min(upto, len(order) - 1):
                    slots[nload[0]] = load_group(order[nload[0]][2])
                    nload[0] += 1
            ensure_loaded(4)

            for tt in range(18):
                s = tt % 2
                src = ctx[b, tt * 128:(tt + 1) * 128, :] if tt < 2 else x[b, (tt - 2) * 128:(tt - 1) * 128, :]
                Ab, Bb, An, Bn = (A1c, B1c, "A1c", "B1c") if tt < 2 else (A1, B1, "A1", "B1")
                S.dma("sp", lambda e, s=s, src=src: e.dma_start(out=XT[s], in_=src), [], [f"XT{s}"], f"d_XT{s}")
                rs, rsn = rstd_of(XT[s], D, [f"XT{s}"], junk, 1.0 / D)
                S.op("dve", lambda e, s=s, rs=rs, Ab=Ab: e.scalar_tensor_tensor(tmpA, XT[s], rs, Ab, op0=ALU.mult, op1=ALU.mult),
                     [f"XT{s}", rsn, An], ["tmpA"])
                S.op("dve", lambda e, s=s, Bb=Bb: e.tensor_tensor(xb[s], tmpA, Bb, op=ALU.add), ["tmpA", Bn], [f"xb{s}"])
                pbk = 6 + (tt % 2)
                pv = bank[pbk].bitcast(BF16)

                def tpA(e, s=s, pv=pv):
                    for k in range(8):
                        ins = e.transpose(pv[:, k * 128:(k + 1) * 128], xb[s][:, k * 128:(k + 1) * 128], idb[:])
                    return ins
                S.op("pe", tpA, [f"xb{s}", "idb"], [PSN[pbk]])
                S.op("act", lambda e, tt=tt, pv=pv: e.activation(out=xnT[:, :, tt * 128:(tt + 1) * 128],
                                                                   in_=pv.rearrange("p (k t) -> p k t", k=8), func=ACT.Copy),
                     [PSN[pbk]], [f"xn{tt}"])

            if stage == "A":
                return
            pending = []

            def flush(keep=0):
                while len(pending) > keep:
                    pending.pop(0)()

            def proj(slot, tok0, ntok):
                pbk = next_pbank()
                tiles = sorted(set(range(tok0 // 128, (tok0 + ntok + 127) // 128)))

                def mm(e):
                    for k in range(8):
                        ins = e.matmul(bank[pbk][:, 0:ntok], wslot[slot][:, k, :], xnT[:, k, tok0:tok0 + ntok], start=(k == 0), stop=(k == 7))
                    return ins
                S.op("pe", mm, [f"ws{slot}"] + [f"xn{t}" for t in tiles], [PSN[pbk]])
                return pbk

            def rope(pbk, dst, dst_region, tb, extra_reads=()):
                i = rope_ctr[0] % 2
                rope_ctr[0] += 1
                qb_ = 4 + i
                S.op("act", lambda e: e.activation(out=pb[i], in_=bank[pbk], func=ACT.Copy), [PSN[pbk]], [f"pb{i}"])
                S.op("dve", lambda e: e.tensor_tensor(t2[i], bank[pbk], cosT[:, tb * 512:(tb + 1) * 512], op=ALU.mult),
                     [PSN[pbk], "cosT"], [f"t2{i}"])

                def part2():
                    S.op("pe", lambda e: e.matmul(bank[qb_], permb[:], pb[i], start=True, stop=True), ["permb", f"pb{i}"], [PSN[qb_]])
                    S.op("dve", lambda e: e.tensor_tensor(t1[i], bank[qb_], sinT[:, tb * 512:(tb + 1) * 512], op=ALU.mult),
                         [PSN[qb_], "sinT"], [f"t1{i}"])
                    S.op("dve", lambda e: e.tensor_tensor(dst, t1[i], t2[i], op=ALU.add),
                         [f"t1{i}", f"t2{i}"] + list(extra_reads), [dst_region])
                pending.append(part2)

            gi = 0
            for h in range(4):
                ensure_loaded(gi + 4)
                sl = slots[gi]
                gi += 1
                pbk = proj(sl, 0, C)
                flush(0)
                S.op("act", lambda e, h=h, pbk=pbk: e.activation(out=kT[:, h, 0:C], in_=bank[pbk][:, 0:C], func=ACT.Copy),
                     [PSN[pbk]], [f"kT{h}"])
                if stage == "B0a":
                    return
                for tb in range(4):
                    pbk = proj(sl, C + tb * 512, 512)
                    flush(0)
                    if stage == "B0c":
                        S.op("act", lambda e, h=h, pbk=pbk, tb=tb: e.activation(out=kT[:, h, C + tb * 512:C + (tb + 1) * 512], in_=bank[pbk], func=ACT.Copy),
                             [PSN[pbk]], [f"kT{h}"])
                        return
                    if stage == "B0d":
                        S.op("dve", lambda e, pbk=pbk, tb=tb: e.tensor_tensor(t2[0], bank[pbk], cosT[:, tb * 512:(tb + 1) * 512], op=ALU.mult),
                             [PSN[pbk], "cosT"], ["t20"])
                        return
                    rope(pbk, kT[:, h, C + tb * 512:C + (tb + 1) * 512], f"kT{h}", tb)
                    if stage == "B0b":
                        flush(0)
                        return
            flush(0)
            if stage == "B1":
                return
            for tt in range(18):
                pbk = next_pbank()

                def mmv(e, tt=tt, pbk=pbk):
                    for k in range(8):
                        ins = e.matmul(bank[pbk], xnT[:, k, tt * 128:(tt + 1) * 128], wv_sb[:, k, :], start=(k == 0), stop=(k == 7))
                    return ins
                S.op("pe", mmv, ["wv", f"xn{tt}"], [PSN[pbk]])
                S.op("act", lambda e, tt=tt, pbk=pbk: e.activation(out=v_sb[:, tt, :], in_=bank[pbk], func=ACT.Copy), [PSN[pbk]], ["v_sb"])
            if stage == "B2":
                return
            S.op("dve", lambda e: e.memset(u_t[:, 0:1], 0.0), ["qdead", "u_t"], ["u_t"])
            S.op("dve", lambda e: e.memset(u_t[:, 2049:2050], 0.0), ["qdead", "u_t"], ["u_t"])
            for j in range(4):
                ensure_loaded(gi + 5)
                sgb, sgc, sxc = slots[gi], slots[gi + 1], slots[gi + 2]
                gi += 3
                for tb in range(4):
                    i = (j * 4 + tb) % 2
                    p_xc = proj(sxc, C + tb * 512, 512)
                    p_gc = proj(sgc, C + tb * 512, 512)
                    p_gb = proj(sgb, C + tb * 512, 512)
                    S.op("act", lambda e, i=i, p_xc=p_xc: e.activation(out=xc_sb[i], in_=bank[p_xc], func=ACT.Copy), [PSN[p_xc]], [f"xc{i}"])
                    S.op("dve", lambda e, i=i, p_gc=p_gc, tb=tb: e.tensor_tensor(u_t[:, 1 + tb * 512:1 + (tb + 1) * 512], bank[p_gc], xc_sb[i], op=ALU.mult),
                         [PSN[p_gc], f"xc{i}", "qdead"], ["u_t"])
                    S.op("act", lambda e, p_gb=p_gb, tb=tb: e.activation(out=gb_t[:, tb * 512:(tb + 1) * 512], in_=bank[p_gb], func=ACT.Copy),
                         [PSN[p_gb], "qdead"], ["gb_t"])
                for hf in range(2):
                    o0 = hf * 1024
                    S.op("act", lambda e, j=j, o0=o0: e.activation(out=y_t, in_=u_t[:, 1 + o0:1 + o0 + 1024], func=ACT.Identity, scale=cw[:, j, 1:2]),
                         ["u_t", "cw", "qdead"], ["y_t"])
                    S.op("dve", lambda e, j=j, o0=o0: e.scalar_tensor_tensor(y_t, u_t[:, o0:o0 + 1024], cw[:, j, 0:1], y_t, op0=ALU.mult, op1=ALU.add),
                         ["u_t", "cw", "y_t"], ["y_t"])
                    S.op("dve", lambda e, j=j, o0=o0: e.scalar_tensor_tensor(y_t, u_t[:, 2 + o0:2 + o0 + 1024], cw[:, j, 2:3], y_t, op0=ALU.mult, op1=ALU.add),
                         ["u_t", "cw", "y_t"], ["y_t"])
                    S.op("dve", lambda e, j=j, o0=o0: e.tensor_tensor(convT[:, j, o0:o0 + 1024], y_t, gb_t[:, o0:o0 + 1024], op=ALU.mult),
                         ["y_t", "gb_t"], ["convT", "convdead"])
            if stage == "B3":
                return
            for h in range(4):
                ensure_loaded(gi + 4)
                sl = slots[gi]
                gi += 1
                for tb in range(4):
                    pbk = proj(sl, C + tb * 512, 512)
                    flush(0)
                    rope(pbk, qT[:, h, tb * 512:(tb + 1) * 512], f"qT{h}", tb, extra_reads=("convdead",))
            flush(0)
            S.op("pe", lambda e: e.matmul(bank[7][:, 0:2], ones_f[0:1, :], lq3[0:1, 6:8], start=True, stop=True),
                 [f"xn{t}" for t in range(18)] + ["ones_f"], [PSN[7], "xndead"])
            for k in range(8):
                S.dma("pool", lambda e, k=k: e.dma_start(out=w_out_sb[:, k, :], in_=w_out[k * 128:(k + 1) * 128, :]),
                      ["xndead"], ["w_out_sb"], "d_wout")

            if stage == "B":
                return
            steps = [(h, qb, kt) for h in range(4) for qb in range(8) for kt in range(18)]
            deferred = {}

            def qk(si):
                h, qb, kt = steps[si]
                sb0 = (si % 2) * 2

                def mm(e):
                    for m in range(2):
                        ins = e.matmul(bank[sb0 + m][:, 0:256], kT[64 * m:64 * (m + 1), h, kt * 128:(kt + 1) * 128],
                                       qT[64 * m:64 * (m + 1), h, qb * 256:(qb + 1) * 256], start=True, stop=True)
                    return ins
                S.op("pe", mm, [f"kT{h}", f"qT{h}"], [PSN[sb0], PSN[sb0 + 1]])

            def fin1(h, qb, fi):
                ob = 4
                db = 5
                i = fi % 2
                S.op("act", lambda e: e.activation(out=dcp, in_=bank[db], func=ACT.Copy), [PSN[db], "xndead"], ["dcp"])
                S.op("act", lambda e: e.activation(out=Ocp, in_=bank[ob], func=ACT.Copy), [PSN[ob], "xndead"], ["Ocp"])
                S.op("dve", lambda e: e.reciprocal(dcp, dcp), ["dcp", "xndead"], ["dcp"])
                S.op("dve", lambda e: e.tensor_tensor(tO, Ocp, dcp, op=ALU.mult), ["Ocp", "dcp"], ["tO"])
                S.op("dve", lambda e: e.scalar_tensor_tensor(o_t[i], tO[:, 256:512], neglam[:, 0:1], tO[:, 0:256], op0=ALU.mult, op1=ALU.add),
                     ["tO", "neglam"], [f"o{i}"])
                S.op("dve", lambda e: e.tensor_tensor(osq[i], o_t[i], o_t[i], op=ALU.mult), [f"o{i}"], [f"osq{i}"])

            def fin2(h, qb, fi):
                i = fi % 2
                S.op("pe", lambda e: e.matmul(bank[6][:, 0:256], ones_f[:], osq[i], start=True, stop=True), ["ones_f", f"osq{i}"], [PSN[6]])
                S.op("act", lambda e: e.activation(out=ln_t[i], in_=bank[6][:, 0:256], func=ACT.Ln, scale=1.0 / 128, bias=eps_t[:, 0:1]),
                     [PSN[6], "eps_t"], [f"ln{i}"])
                S.op("act", lambda e: e.activation(out=rs_t[i], in_=ln_t[i], func=ACT.Exp, scale=-0.5), [f"ln{i}"], [f"rs{i}"])
                S.op("dve", lambda e: e.scalar_tensor_tensor(attnT[:, h, qb * 256:(qb + 1) * 256], o_t[i], sgs[:, 0:1], rs_t[i], op0=ALU.mult, op1=ALU.mult),
                     [f"o{i}", f"rs{i}", "sgs", "xndead"], ["attnT"])

            qk(0)
            fi = 0
            for si, (h, qb, kt) in enumerate(steps):
                if si + 1 < len(steps):
                    qk(si + 1)
                sb0 = (si % 2) * 2
                ai = si % 3
                stx = stAB[si % 2]
                S.op("act", lambda e, stx=stx, ai=ai: e.activation(out=at_t[ai].rearrange("p (m c) -> p m c", m=2),
                                                                    in_=stx[:, :].rearrange("p (m c) -> p m c", m=2)[:, :, 0:256], func=ACT.Exp, scale=0.125),
                     [PSN[sb0], PSN[sb0 + 1], "xndead"], [f"at{ai}"])
                ob = 4
                db = 5

                def av(e, h=h, kt=kt, ai=ai, ob=ob, db=db):
                    e.matmul(bank[ob], v_sb[:, kt, h * 128:(h + 1) * 128], at_t[ai], start=(kt == 0), stop=(kt == 17))
                    return e.matmul(bank[db], ones_b[:], at_t[ai], start=(kt == 0), stop=(kt == 17))
                S.op("pe", av, ["v_sb", f"at{ai}", "ones_b"], [PSN[ob], PSN[db]])
                if si in deferred:
                    deferred.pop(si)()
                if kt == 17:
                    fin1(h, qb, fi)
                    deferred[min(si + 4, len(steps) - 1) if si + 4 < len(steps) else -1] = (lambda h=h, qb=qb, fi=fi: fin2(h, qb, fi))
                    fi += 1
            for k_ in sorted(deferred):
                deferred[k_]()
            S.op("pe", lambda e: e.matmul(bank[7][:, 0:2], ones_f[0:1, :], lq3[0:1, 6:8], start=True, stop=True),
                 ["ones_f", "v_sb"] + [f"kT{h}" for h in range(4)] + [f"qT{h}" for h in range(4)], [PSN[7], "kvdead", "qdead"])

            if stage == "C":
                return
            mod_tile(G1, "G1", b, 2, 1, gtmp2, "gtmp2", False, guard=("qdead",))
            mod_tile(A2, "A2", b, 4, 2, gtmp2, "gtmp2", True, guard=("qdead",))
            mod_tile(B2, "B2", b, 3, None, None, None, False, guard=("qdead",))
            mixin = [attnT[:, h, :] for h in range(4)] + [convT[:, j, :] for j in range(4)]

            def d0(tt):
                s = tt % 2
                S.dma("sp", lambda e: e.dma_start(out=xt2[s], in_=x[b, tt * 128:(tt + 1) * 128, :]), ["kvdead"], [f"xt2{s}"], f"d_xt2{s}")

            def d1(tt):
                s = tt % 2
                if tt + 1 < 16:
                    d0(tt + 1)
                for hf in range(2):
                    def mm(e, hf=hf):
                        for k in range(8):
                            ins = e.matmul(bank[hf], mixin[k][:, tt * 128:(tt + 1) * 128], w_out_sb[:, k, hf * 512:(hf + 1) * 512], start=(k == 0), stop=(k == 7))
                        return ins
                    S.op("pe", mm, ["attnT", "convT", "w_out_sb"], [PSN[hf]])
                rs, rsn = rstd_of2(bank[0], bank[1], [PSN[0], PSN[1]], junk, 1.0 / D)
                for hf in range(2):
                    S.op("dve", lambda e, hf=hf: e.scalar_tensor_tensor(tD[:, hf * 512:(hf + 1) * 512], bank[hf], rs, G1[:, hf * 512:(hf + 1) * 512], op0=ALU.mult, op1=ALU.mult),
                         [PSN[hf], rsn, "G1", "kvdead"], ["tD"])
                S.op("dve", lambda e: e.tensor_tensor(h_sb[s], tD, xt2[s], op=ALU.add), ["tD", f"xt2{s}", "kvdead"], [f"h{s}"])
                S.dma("sp", lambda e: e.dma_start(out=h_d[b, tt * 128:(tt + 1) * 128, :], in_=h_sb[s]), [f"h{s}"], [f"h_d{b}_{tt}"], f"d_h{s}")
                rs2, rsn2 = rstd_of(h_sb[s], D, [f"h{s}"], junk, 1.0 / D)
                S.op("dve", lambda e: e.scalar_tensor_tensor(t2D, h_sb[s], rs2, A2, op0=ALU.mult, op1=ALU.mult),
                     [f"h{s}", rsn2, "A2", "kvdead"], ["t2D"])
                S.op("dve", lambda e: e.tensor_tensor(xn2[s], t2D, B2, op=ALU.add), ["t2D", "B2"], [f"xn2{s}"])
                S.dma("pool", lambda e: e.dma_start(out=xn2_d[b][tt * 128:(tt + 1) * 128, :], in_=xn2[s]), [f"xn2{s}"], [f"xn2d{b}"], f"d_x2{s}")

            def d2(tt):
                s = tt % 2

                def tp(e):
                    for k in range(8):
                        ins = e.transpose(bank[2 + k // 4][:, (k % 4) * 128:(k % 4 + 1) * 128], xn2[s][:, k * 128:(k + 1) * 128], idf[:])
                    return ins
                S.op("pe", tp, [f"xn2{s}", "idf"], [PSN[2], PSN[3]])
                for hb in range(2):
                    S.op("act", lambda e, hb=hb: e.activation(out=xn2T[:, hb * 4:(hb + 1) * 4, :], in_=bank[2 + hb].rearrange("p (k t) -> p k t", k=4), func=ACT.Copy),
                         [PSN[2 + hb], "kvdead"], ["xn2T"])

            def d3(tt):
                s = tt % 2

                def mm(e):
                    for k in range(8):
                        ins = e.matmul(bank[4][:, 0:16], xn2T[:, k, :], wr_sb[:, k, :], start=(k == 0), stop=(k == 7))
                    return ins
                S.op("pe", mm, ["xn2T", "wr_sb"], [PSN[4]])
                c = stat_slot(4)
                S.op("dve", lambda e: e.reduce_max(stat[:, c:c + 1], bank[4][:, 0:16], axis=AX.X), [PSN[4]], [f"st{c}"])
                S.op("dve", lambda e: e.tensor_scalar(stat[:, c + 1:c + 2], stat[:, c:c + 1], -1.0, None, op0=ALU.mult), [f"st{c}"], [f"st{c+1}"])
                S.op("act", lambda e: e.activation(out=ex_sb, in_=bank[4][:, 0:16], func=ACT.Exp, bias=stat[:, c + 1:c + 2], accum_out=stat[:, c + 2:c + 3]),
                     [PSN[4], f"st{c+1}"], ["ex_sb", f"st{c+2}"])
                S.op("dve", lambda e: e.reciprocal(stat[:, c + 3:c + 4], stat[:, c + 2:c + 3]), [f"st{c+2}"], [f"st{c+3}"])
                S.op("dve", lambda e: e.tensor_scalar(aff_sb[s], ex_sb, stat[:, c + 3:c + 4], None, op0=ALU.mult), ["ex_sb", f"st{c+3}"], [f"aff{s}"])

            def d4(tt):
                s = tt % 2
                S.op("pe", lambda e: e.transpose(bank[5][0:16, 0:128], aff_sb[s], idf[:]), [f"aff{s}", "idf"], [PSN[5]])
                S.op("act", lambda e: e.activation(out=affT_sb[s][0:16, :], in_=bank[5][0:16, 0:128], func=ACT.Copy), [PSN[5]], [f"affT{s}"])
                S.dma("sp", lambda e: e.dma_start(out=aff_d[32 * b:32 * b + 16, tt * 128:(tt + 1) * 128], in_=affT_sb[s][0:16, :]),
                      [f"affT{s}"], ["aff_d"], f"d_af{s}")

            d0(0)
            for i in range(16 + 3):
                if 0 <= i - 3 < 16:
                    d4(i - 3)
                if 0 <= i - 2 < 16:
                    d3(i - 2)
                if 0 <= i - 1 < 16:
                    d2(i - 1)
                if i < 16:
                    d1(i)
            S.barrier()

        for b_ in range(NB if stage != "phase0" else 0):
            do_batch(b_)
        if stage in ("A", "B", "C", "B1", "B2", "B3", "B0a", "B0b", "B0c", "B0d"):
            S.barrier()

        if stage in ("mixer", "phase0", "A", "B", "C", "B1", "B2", "B3", "B0a", "B0b", "B0c", "B0d"):
            AR.reset()
            cp = [AR.take([D], F32) for _ in range(2)]
            for b in range(NB):
                for tt in range(16):
                    s = tt % 2
                    S.dma("sp", lambda e, b=b, tt=tt, s=s: e.dma_start(out=cp[s], in_=h_d[b, tt * 128:(tt + 1) * 128, :]), [], [f"cp{s}"], f"d_cp{s}")
                    S.dma("sp", lambda e, b=b, tt=tt, s=s: e.dma_start(out=out[b, tt * 128:(tt + 1) * 128, :], in_=cp[s]), [f"cp{s}"], ["out"], f"d_co{s}")
            S.barrier(["sp"])
            S.emit()
            return nc

        AR.reset()
        wexp = [[AR.take([8, D], BF16) for _ in range(3)] for _ in range(2)]
        xs = [AR.take([D], BF16) for _ in range(8)]
        xsT = [AR.take([8, 512], BF16) for _ in range(2)]
        actT = [AR.take([8, 512], BF16) for _ in range(2)]
        sgt = [AR.take([512], F32) for _ in range(2)]
        y_sb = [AR.take([D], F32) for _ in range(8)]
        moe_end = AR.off
        work = AR.take([T], F32)
        vals = AR.take([CAP], F32)
        idx = AR.take([CAP], U32)
        idxf = AR.take([CAP], F32)
        idxT = AR.take([2, 128], U32)
        gT = AR.take([2, 128], F32)
        rt_end = AR.off
        AR.off = 0
        NFB = 4
        ffl = [AR.take([D], F32) for _ in range(NFB)]
        hl = [AR.take([D], F32) for _ in range(NFB)]
        G2 = [AR.take([D], F32) for _ in range(2)]
        gtmp3 = AR.take([D], F32)
        tF = [AR.take([D], F32) for _ in range(NFB)]
        ob_t = [AR.take([D], F32) for _ in range(NFB)]
        junk2 = AR.take([D], BF16)
        AR.off = rt_end
        print("moe arena bytes", AR.off)

        def load_expert(e_):
            s = e_ % 2
            for wi, wsrc in enumerate((w_gate, w_up, w_down)):
                for k in range(8):
                    S.dma("pool", lambda e, s=s, wi=wi, wsrc=wsrc, k=k: e.dma_start(out=wexp[s][wi][:, k, :], in_=wsrc[e_, k * 128:(k + 1) * 128, :]),
                          [], [f"we{s}_{wi}"], f"d_we{s}_{wi}")

        load_expert(0)
        S.dma("sp", lambda e: e.dma_start(out=work, in_=aff_d), ["aff_d"], ["work"], "d_work")
        for r in range(CAP // 8):
            S.op("dve", lambda e, r=r: e.max(out=vals[:, r * 8:(r + 1) * 8], in_=work), ["work"], ["vals"])
            S.op("dve", lambda e, r=r: e.max_index(out=idx[:, r * 8:(r + 1) * 8], in_max=vals[:, r * 8:(r + 1) * 8], in_values=work), ["work", "vals"], ["idx"])
            S.op("dve", lambda e, r=r: e.match_replace(out=work, in_to_replace=vals[:, r * 8:(r + 1) * 8], in_values=work, imm_value=-1.0), ["vals", "idx"], ["work"])
        S.op("dve", lambda e: e.tensor_copy(idxf, idx), ["idx"], ["idxf"])
        for ct in range(2):
            S.op("pe", lambda e, ct=ct: e.transpose(bank[0][:, 0:128], idxf[:, ct * 128:(ct + 1) * 128], idf[:]), ["idxf", "idf"], [PSN[0]])
            S.op("dve", lambda e, ct=ct: e.tensor_copy(idxT[:, ct, :], bank[0][:, 0:128]), [PSN[0]], ["idxT"])
            S.op("pe", lambda e, ct=ct: e.transpose(bank[1][:, 0:128], vals[:, ct * 128:(ct + 1) * 128], idf[:]), ["vals", "idf"], [PSN[1]])
            S.op("dve", lambda e, ct=ct: e.tensor_copy(gT[:, ct, :], bank[1][:, 0:128]), [PSN[1]], ["gT"])

        NP = (NB + 1) // 2
        tiles = [(pr, bi, bb, ct) for pr in range(NP) for bi, bb in enumerate([q for q in (2 * pr, 2 * pr + 1) if q < NB]) for ct in range(2)]

        def gathers(e_):
            for j, (pr, bi, bb, ct) in enumerate(tiles):
                row = 32 * bb + e_
                S.dma("pool", lambda e, j=j, bb=bb, ct=ct, row=row: e.indirect_dma_start(
                    out=xs[j], out_offset=None, in_=xn2_d[bb],
                    in_offset=bass.IndirectOffsetOnAxis(ap=idxT[:, ct, row:row + 1], axis=0)),
                    [f"xn2d{bb}", "idxT"], [f"xs{j}"], f"d_xs{j}")

        def transposes(e_):
            for j, (pr, bi, bb, ct) in enumerate(tiles):
                pbk = 6 + j % 2
                pv = bank[pbk].bitcast(BF16)

                def tpx(e, j=j, pv=pv):
                    for k in range(8):
                        ins = e.transpose(pv[:, k * 128:(k + 1) * 128], xs[j][:, k * 128:(k + 1) * 128], idb[:])
                    return ins
                S.op("pe", tpx, [f"xs{j}", "idb"], [PSN[pbk]])
                c0 = (bi * 2 + ct) * 128
                S.op("act", lambda e, pr=pr, c0=c0, pv=pv: e.activation(out=xsT[pr][:, :, c0:c0 + 128], in_=pv.rearrange("p (k t) -> p k t", k=8), func=ACT.Copy),
                     [PSN[pbk]], [f"xsT{pr}"])

        def do_pair(e_, pr):
            ws = e_ % 2
            wg_, wu_, wd_ = wexp[ws]
            mine = [(j, t) for j, t in enumerate(tiles) if t[0] == pr]
            ncol = len(mine) * 128
            for fc in range(8):
                bg = 0 + 2 * (fc % 2)
                bu = 1 + 2 * (fc % 2)
                si_ = fc % 2

                def mmg(e, fc=fc, bg=bg):
                    for k in range(8):
                        ins = e.matmul(bank[bg][:, 0:ncol], wg_[:, k, fc * 128:(fc + 1) * 128], xsT[pr][:, k, 0:ncol], start=(k == 0), stop=(k == 7))
                    return ins

                def mmu(e, fc=fc, bu=bu):
                    for k in range(8):
                        ins = e.matmul(bank[bu][:, 0:ncol], wu_[:, k, fc * 128:(fc + 1) * 128], xsT[pr][:, k, 0:ncol], start=(k == 0), stop=(k == 7))
                    return ins
                S.op("pe", mmg, [f"we{ws}_0", f"xsT{pr}"], [PSN[bg]])
                S.op("pe", mmu, [f"we{ws}_1", f"xsT{pr}"], [PSN[bu]])
                S.op("act", lambda e, bg=bg, si_=si_: e.activation(out=sgt[si_][:, 0:ncol], in_=bank[bg][:, 0:ncol], func=ACT.Silu), [PSN[bg]], [f"sgt{si_}"])
                S.op("dve", lambda e, fc=fc, bu=bu, si_=si_: e.tensor_tensor(actT[pr][:, fc, 0:ncol], sgt[si_][:, 0:ncol], bank[bu][:, 0:ncol], op=ALU.mult),
                     [PSN[bu], f"sgt{si_}"], [f"actT{pr}"])
            for j, (pr_, bi, bb, ct) in mine:
                row = 32 * bb + e_
                c0 = (bi * 2 + ct) * 128
                for hf in range(2):
                    yb = 4 + hf

                    def mmd(e, c0=c0, hf=hf, yb=yb):
                        for k in range(8):
                            ins = e.matmul(bank[yb], actT[pr][:, k, c0:c0 + 128], wd_[:, k, hf * 512:(hf + 1) * 512], start=(k == 0), stop=(k == 7))
                        return ins
                    S.op("pe", mmd, [f"actT{pr}", f"we{ws}_2"], [PSN[yb]])
                    if hf == 0:
                        S.op("act", lambda e, j=j, yb=yb, ct=ct, row=row: e.activation(out=y_sb[j][:, 0:512], in_=bank[yb], func=ACT.Identity, scale=gT[:, ct, row:row + 1]),
                             [PSN[yb], "gT"], [f"y{j}a"])
                    else:
                        S.op("dve", lambda e, j=j, yb=yb, ct=ct, row=row: e.tensor_scalar(y_sb[j][:, 512:1024], bank[yb], gT[:, ct, row:row + 1], None, op0=ALU.mult),
                             [PSN[yb], "gT"], [f"y{j}b"])
                par, ppar = e_ % 2, (e_ - 1) % 2
                S.dma("pool", lambda e, j=j, bb=bb, ct=ct, row=row: e.indirect_dma_start(
                    out=ff_d[bb], out_offset=bass.IndirectOffsetOnAxis(ap=idxT[:, ct, row:row + 1], axis=0),
                    in_=y_sb[j], in_offset=None, compute_op=ALU.add),
                    [f"y{j}a", f"y{j}b", "idxT", f"ff{bb}", f"ffs{bb}_0_{ppar}", f"ffs{bb}_1_{ppar}"], [f"ffs{bb}_{ct}_{par}"], f"d_y{j}")

        gathers(0)
        for e_ in range(NE):
            transposes(e_)
            if e_ + 1 < NE:
                load_expert(e_ + 1)
                gathers(e_ + 1)
            for pr in range(NP):
                do_pair(e_, pr)
        S.barrier()

        bc_load(gtmp3, gains[3:4, :], "gtmp3", "d_bc_gtmp3")
        ftiles = [(b, tt) for b in range(NB) for tt in range(16)]

        def f_load(i):
            b, tt = ftiles[i]
            s = i % NFB
            if tt == 0:
                g = b % 2
                bc_load(G2[g], mod_d[b:b + 1, 5 * D:6 * D], f"G2{g}", f"d_bc_G2{g}")
                S.op("dve", lambda e, g=g: e.tensor_tensor(G2[g], G2[g], gtmp3, op=ALU.mult), [f"G2{g}", "gtmp3"], [f"G2{g}"])
            S.dma("sp", lambda e: e.dma_start(out=ffl[s], in_=ff_d[b][tt * 128:(tt + 1) * 128, :]), [f"ff{b}"], [f"ffl{s}"], f"d_ffl{s}")
            S.dma("sp", lambda e: e.dma_start(out=hl[s], in_=h_d[b, tt * 128:(tt + 1) * 128, :]), [f"h_d{b}_{tt}"], [f"hl{s}"], f"d_hl{s}")

        def f_compute(i):
            b, tt = ftiles[i]
            s = i % NFB
            g = b % 2
            rs, rsn = rstd_of(ffl[s], D, [f"ffl{s}"], junk2, 1.0 / D)
            S.op("dve", lambda e: e.scalar_tensor_tensor(tF[s], ffl[s], rs, G2[g], op0=ALU.mult, op1=ALU.mult), [f"ffl{s}", rsn, f"G2{g}"], [f"tF{s}"])
            S.op("dve", lambda e: e.tensor_tensor(ob_t[s], tF[s], hl[s], op=ALU.add), [f"tF{s}", f"hl{s}"], [f"ob{s}"])
            S.dma("sp", lambda e: e.dma_start(out=out[b, tt * 128:(tt + 1) * 128, :], in_=ob_t[s]), [f"ob{s}"], ["out"], f"d_out{s}")

        AHEAD = NFB - 1
        for i in range(min(AHEAD, len(ftiles))):
            f_load(i)
        for i in range(len(ftiles)):
            if i + AHEAD < len(ftiles):
                f_load(i + AHEAD)
            f_compute(i)
        S.barrier(["sp"])
        S.emit()
    return nc


def _rope_tables():
    t = np.arange(T)
    row = (t // 64).astype(np.float32)
    col = (t % 64).astype(np.float32)
    inv = (10000.0 ** (-np.arange(0, 32, 2, dtype=np.float32) / 32)).astype(np.float32)
    ang_r = row[:, None] * inv[None, :]
    ang_c = col[:, None] * inv[None, :]
    ang = np.concatenate([ang_r, ang_r, ang_c, ang_c], axis=-1)
    cos = np.cos(ang).astype(np.float32).T
    sin = np.sin(ang).astype(np.float32).T
    sgn = np.concatenate([-np.ones(16), np.ones(16), -np.ones(16), np.ones(16)]).astype(np.float32)[:, None]
    sin = sin * sgn
    cosT = np.ascontiguousarray(np.concatenate([cos, cos], 0))
    sinT = np.ascontiguousarray(np.concatenate([sin, sin], 0))
    perm = np.zeros((128, 128), np.float32)
    for i in range(128):
        perm[i ^ 16, i] = 1.0
    return cosT, sinT, perm


def make_in_maps(inputs, NB=4, ncores=NCORES, ne=NE):
    f = lambda a: np.ascontiguousarray(np.asarray(a, dtype=np.float32))
    x = f(inputs["x"]); c = f(inputs["c"]); ctx = f(inputs["ctx"]); c_ctx = f(inputs["c_ctx"])
    w_in = f(inputs["w_in"])[0]
    cosT, sinT, perm = _rope_tables()
    w_in_r = np.ascontiguousarray(w_in.reshape(8, 128, 24, 128).transpose(2, 1, 0, 3))
    w_v_r = np.ascontiguousarray(w_in[:, 1024:1536].reshape(8, 128, 512).transpose(1, 0, 2))
    shared = dict(
        w_ada=f(inputs["w_ada"])[0], b_ada=f(inputs["b_ada"]),
        gains=np.ascontiguousarray(np.concatenate([f(inputs["norm_pre_mix"]), f(inputs["norm_post_mix"]),
                                                   f(inputs["norm_pre_ffn"]), f(inputs["norm_post_ffn"])], 0)),
        w_in_r=w_in_r, w_v_r=w_v_r,
        convw=np.ascontiguousarray(f(inputs["conv_w"])[0].T.reshape(4, 128, 3).transpose(1, 0, 2)),
        lqk=np.ascontiguousarray(np.concatenate([f(inputs["lambda_q1"]), f(inputs["lambda_k1"]),
                                                 f(inputs["lambda_q2"]), f(inputs["lambda_k2"])], 1)),
        subln=np.ascontiguousarray(f(inputs["subln_g"]).reshape(128, 1)),
        w_out=f(inputs["w_out"])[0],
        w_router=np.ascontiguousarray(f(inputs["w_router"])[0].reshape(8, 128, 16).transpose(1, 0, 2)),
        w_gate=f(inputs["w_gate"])[0][:ne], w_up=f(inputs["w_up"])[0][:ne], w_down=f(inputs["w_down"])[0][:ne],
        ident=np.eye(128, dtype=np.float32), perm=perm, cosT=cosT, sinT=sinT,
    )
    maps = []
    for i in range(ncores):
        sl = slice(i * NB, (i + 1) * NB)
        cc = np.concatenate([c[sl], c_ctx[None, :]], 0)
        if cc.shape[0] < 5:
            cc = np.concatenate([cc[:-1], np.zeros((5 - cc.shape[0], D), np.float32), cc[-1:]], 0)
        ccT = np.ascontiguousarray(cc.T.reshape(8, 128, 5).transpose(1, 0, 2))
        m = dict(shared)
        m.update(x=np.ascontiguousarray(x[sl]), ctx=np.ascontiguousarray(ctx[sl]), ccT=ccT)
        maps.append(m)
    return maps


def kernel(**inputs):
    NB = 4
    nc = build(NB=NB, stage="full")
    maps = make_in_maps(inputs, NB=NB, ncores=NCORES)
    res = run_bass_kernel_spmd(nc, maps, core_ids=list(range(NCORES)))
    return np.concatenate([np.asarray(r["out"]) for r in res.results], axis=0).astype(np.float32)
```
